# Optimizing a Trainium2 kernel written in Bass

```python
import math
import jax, jax.numpy as jnp
from jax import lax
import numpy as np

D_MODEL = 1024
BATCH = 4
SEQ = 8192
DEPTH = 4
DEC_BATCH = 32
DEC_SEQ = 2048
PAST_LEN = 128

GRID_W = 64
N_HEADS = 8
ATTN_DIM = D_MODEL // 2
HEAD_DIM = ATTN_DIM // N_HEADS
WIN_ROWS = 8
WIN_COLS = 16
POOL_WINDOWS = (2, 4, 8, 16)
N_POOL_GROUPS = len(POOL_WINDOWS)
POOL_DIM = D_MODEL // 2
POOL_GROUP_DIM = POOL_DIM // N_POOL_GROUPS
PROJ_DIM = 3 * ATTN_DIM + POOL_DIM + 2 * D_MODEL
N_EXPERTS = 16
N_GROUPS = 4
EXPERTS_PER_GROUP = N_EXPERTS // N_GROUPS
TOP_K = 2
D_FF = 2 * D_MODEL
MOE_BLOCK = 256
ALPHA = (2 * DEPTH) ** 0.25
BETA = (8 * DEPTH) ** -0.25
LN_EPS = 1e-5

kernel_name = "hybrid_natten_pool_grouped_moe_encoder"


def layer_norm(x, g, b):
    xf = x.astype(jnp.float32)
    mu = jnp.mean(xf, axis=-1, keepdims=True)
    var = jnp.mean(jnp.square(xf - mu), axis=-1, keepdims=True)
    y = (xf - mu) * lax.rsqrt(var + LN_EPS) * g.astype(jnp.float32) + b.astype(jnp.float32)
    return y.astype(x.dtype)


def neighborhood_attention(q, k, v, rpb):
    B, L, H, Dh = q.shape
    rows = L // GRID_W
    kr = min(WIN_ROWS, rows)
    qg = q.reshape(B, rows, GRID_W, H, Dh)
    kg = k.reshape(B, rows, GRID_W, H, Dh)
    vg = v.reshape(B, rows, GRID_W, H, Dh)
    col = jnp.arange(GRID_W, dtype=jnp.int32)
    col_start = jnp.clip(col - WIN_COLS // 2, 0, GRID_W - WIN_COLS)
    col_idx = col_start[:, None] + jnp.arange(WIN_COLS, dtype=jnp.int32)[None, :]
    dc_idx = col_idx - col[:, None] + (WIN_COLS - 1)
    scale = HEAD_DIM ** -0.5

    def row_step(r):
        rs = jnp.clip(r - kr // 2, 0, rows - kr)
        k_rows = lax.dynamic_slice_in_dim(kg, rs, kr, axis=1)
        v_rows = lax.dynamic_slice_in_dim(vg, rs, kr, axis=1)
        k_win = jnp.take(k_rows, col_idx, axis=2)
        v_win = jnp.take(v_rows, col_idx, axis=2)
        q_row = lax.dynamic_index_in_dim(qg, r, axis=1, keepdims=False)
        dr_idx = rs + jnp.arange(kr, dtype=jnp.int32) - r + (WIN_ROWS - 1)
        bias = rpb[:, dr_idx[:, None, None], dc_idx[None, :, :]]
        bias = jnp.transpose(bias, (0, 2, 1, 3)).astype(jnp.float32)
        s = jnp.einsum('bqhd,biqjhd->bhqij', q_row, k_win).astype(jnp.float32) * scale + bias[None]
        p = jax.nn.softmax(s.reshape(B, H, GRID_W, kr * WIN_COLS), axis=-1)
        p = p.reshape(s.shape).astype(v.dtype)
        return jnp.einsum('bhqij,biqjhd->bqhd', p, v_win)

    out = lax.map(row_step, jnp.arange(rows, dtype=jnp.int32))
    return jnp.transpose(out, (1, 0, 2, 3, 4)).reshape(B, L, H * Dh)


def pool_mixer(p, w_pool, b_pool, pool_scale):
    B, L, _ = p.shape
    pg = p.reshape(B, L, N_POOL_GROUPS, POOL_GROUP_DIM)
    pos = jnp.arange(L, dtype=jnp.int32)
    diffs = []
    for g, w in enumerate(POOL_WINDOWS):
        xg = pg[:, :, g].astype(jnp.float32)
        cs = jnp.concatenate([jnp.zeros((B, 1, POOL_GROUP_DIM), jnp.float32),
                              jnp.cumsum(xg, axis=1)], axis=1)
        lo = jnp.clip(pos - w // 2, 0, L)
        hi = jnp.clip(pos + w // 2, 0, L)
        mean = (cs[:, hi] - cs[:, lo]) / (hi - lo).astype(jnp.float32)[None, :, None]
        diffs.append(mean - xg)
    d = jnp.stack(diffs, axis=2).astype(p.dtype)
    y = jnp.einsum('blgc,gcd->blgd', d, w_pool) + b_pool
    return y.reshape(B, L, POOL_DIM) * pool_scale


def mixer_block(x, w_in, b_in, rpb, w_pool, b_pool, pool_scale, w_oa, w_op, w_out, b_out):
    B, L, _ = x.shape
    proj = x @ w_in + b_in
    splits = [ATTN_DIM, 2 * ATTN_DIM, 3 * ATTN_DIM, 3 * ATTN_DIM + POOL_DIM,
              3 * ATTN_DIM + POOL_DIM + D_MODEL]
    q, k, v, p, ga, gp = jnp.split(proj, splits, axis=-1)
    to_heads = lambda t: t.reshape(B, L, N_HEADS, HEAD_DIM)
    a = neighborhood_attention(to_heads(q), to_heads(k), to_heads(v), rpb)
    pm = pool_mixer(p, w_pool, b_pool, pool_scale)
    mix = jax.nn.sigmoid(ga) * (a @ w_oa) + jax.nn.sigmoid(gp) * (pm @ w_op)
    return mix @ w_out + b_out


def moe_ffn(x, w_router, b_router, w1, b1, w2, b2):
    B, L, D = x.shape
    T = B * L
    xt = x.reshape(T, D)
    logits = xt.astype(jnp.float32) @ w_router.astype(jnp.float32) + b_router.astype(jnp.float32)
    probs = jax.nn.softmax(logits, axis=-1)
    probs_g = probs.reshape(T, N_GROUPS, EXPERTS_PER_GROUP)
    group_score = lax.top_k(probs_g, TOP_K)[0].sum(-1)
    chosen = jnp.argmax(group_score, axis=-1).astype(jnp.int32)
    in_group = jnp.take_along_axis(probs_g, chosen[:, None, None], axis=1)[:, 0]
    vals, idx = lax.top_k(in_group, TOP_K)
    expert = chosen[:, None] * EXPERTS_PER_GROUP + idx.astype(jnp.int32)
    gates = (vals / jnp.sum(vals, axis=-1, keepdims=True)).astype(x.dtype)

    A = T * TOP_K
    e_flat = expert.reshape(A)
    tok_flat = jnp.repeat(jnp.arange(T, dtype=jnp.int32), TOP_K)
    w_flat = gates.reshape(A)
    order = jnp.argsort(e_flat)
    e_sorted = e_flat[order]
    counts = jnp.bincount(e_flat, length=N_EXPERTS).astype(jnp.int32)
    starts = jnp.cumsum(counts) - counts
    padded = (counts + MOE_BLOCK - 1) // MOE_BLOCK * MOE_BLOCK
    pad_ends = jnp.cumsum(padded)
    pad_starts = pad_ends - padded
    dest = pad_starts[e_sorted] + jnp.arange(A, dtype=jnp.int32) - starts[e_sorted]
    n_blocks = -(-(A + N_EXPERTS * (MOE_BLOCK - 1)) // MOE_BLOCK)
    P = n_blocks * MOE_BLOCK
    buf_tok = jnp.full((P,), T, jnp.int32).at[dest].set(tok_flat[order])
    buf_w = jnp.zeros((P,), x.dtype).at[dest].set(w_flat[order])
    block_e = jnp.clip(jnp.searchsorted(pad_ends, jnp.arange(n_blocks, dtype=jnp.int32) * MOE_BLOCK,
                                        side='right'), 0, N_EXPERTS - 1).astype(jnp.int32)
    x_pad = jnp.concatenate([xt, jnp.zeros((1, D), xt.dtype)], axis=0)

    def block_ffn(args):
        tok, e = args
        xb = jnp.take(x_pad, tok, axis=0)
        hdn = jax.nn.gelu(xb @ w1[e] + b1[e], approximate=False)
        return hdn @ w2[e] + b2[e]

    y_buf = lax.map(block_ffn, (buf_tok.reshape(n_blocks, MOE_BLOCK), block_e))
    y = jnp.zeros((T + 1, D), x.dtype).at[buf_tok].add(y_buf.reshape(P, D) * buf_w[:, None])
    return y[:T].reshape(B, L, D)


def setup_inputs(seed: int = 0) -> dict:
    key = jax.random.key(seed)
    ks = jax.random.split(key, 24)
    nrm = lambda k, shape, s: jax.random.normal(k, shape, jnp.float32) * s
    col_scale = jnp.concatenate([jnp.ones((2 * ATTN_DIM,), jnp.float32),
                                 jnp.full((ATTN_DIM,), BETA, jnp.float32),
                                 jnp.ones((POOL_DIM + 2 * D_MODEL,), jnp.float32)])
    return {
        "x_prompt": nrm(ks[0], (BATCH, SEQ, D_MODEL), 1.0),
        "x_sample": nrm(ks[1], (DEC_BATCH, DEC_SEQ, D_MODEL), 1.0),
        "ln_in_g": 1.0 + nrm(ks[2], (D_MODEL,), 0.05),
        "ln_in_b": nrm(ks[3], (D_MODEL,), 0.02),
        "w_in": nrm(ks[4], (DEPTH, D_MODEL, PROJ_DIM), D_MODEL ** -0.5) * col_scale,
        "b_in": nrm(ks[5], (DEPTH, PROJ_DIM), 0.02),
        "rpb": nrm(ks[6], (DEPTH, N_HEADS, 2 * WIN_ROWS - 1, 2 * WIN_COLS - 1), 0.1),
        "w_pool": nrm(ks[7], (DEPTH, N_POOL_GROUPS, POOL_GROUP_DIM, POOL_GROUP_DIM), POOL_GROUP_DIM ** -0.5),
        "b_pool": nrm(ks[8], (DEPTH, N_POOL_GROUPS, POOL_GROUP_DIM), 0.02),
        "pool_scale": 1.0 + nrm(ks[9], (DEPTH, POOL_DIM), 0.1),
        "w_oa": nrm(ks[10], (DEPTH, ATTN_DIM, D_MODEL), BETA * ATTN_DIM ** -0.5),
        "w_op": nrm(ks[11], (DEPTH, POOL_DIM, D_MODEL), BETA * POOL_DIM ** -0.5),
        "w_out": nrm(ks[12], (DEPTH, D_MODEL, D_MODEL), BETA * D_MODEL ** -0.5),
        "b_out": nrm(ks[13], (DEPTH, D_MODEL), 0.02),
        "ln1_g": 1.0 + nrm(ks[14], (DEPTH, D_MODEL), 0.05),
        "ln1_b": nrm(ks[15], (DEPTH, D_MODEL), 0.02),
        "w_router": nrm(ks[16], (D_MODEL, N_EXPERTS), D_MODEL ** -0.5),
        "b_router": nrm(ks[17], (N_EXPERTS,), 0.01),
        "w1": nrm(ks[18], (DEPTH, N_EXPERTS, D_MODEL, D_FF), BETA * D_MODEL ** -0.5),
        "b1": nrm(ks[19], (DEPTH, N_EXPERTS, D_FF), 0.02),
        "w2": nrm(ks[20], (DEPTH, N_EXPERTS, D_FF, D_MODEL), BETA * D_FF ** -0.5),
        "b2": nrm(ks[21], (DEPTH, N_EXPERTS, D_MODEL), 0.02),
        "ln2_g": 1.0 + nrm(ks[22], (DEPTH, D_MODEL), 0.05),
        "ln2_b": nrm(ks[23], (DEPTH, D_MODEL), 0.02),
    }


def reference(x_prompt, x_sample, ln_in_g, ln_in_b, w_in, b_in, rpb, w_pool, b_pool, pool_scale,
              w_oa, w_op, w_out, b_out, ln1_g, ln1_b, w_router, b_router, w1, b1, w2, b2,
              ln2_g, ln2_b):
    def run(x):
        x = layer_norm(x, ln_in_g, ln_in_b)
        for l in range(DEPTH):
            y = mixer_block(x, w_in[l], b_in[l], rpb[l], w_pool[l], b_pool[l], pool_scale[l],
                            w_oa[l], w_op[l], w_out[l], b_out[l])
            x = layer_norm(ALPHA * x + y, ln1_g[l], ln1_b[l])
            y = moe_ffn(x, w_router, b_router, w1[l], b1[l], w2[l], b2[l])
            x = layer_norm(ALPHA * x + y, ln2_g[l], ln2_b[l])
        return x

    y_prompt = run(x_prompt)
    y_sample = run(x_sample)
    return (y_prompt, y_sample)
```

```python
from contextlib import ExitStack
import numpy as np
import ml_dtypes
import concourse.bass as bass
import concourse.mybir as mybir
from concourse.bass_utils import run_bass_kernel_spmd

F32 = mybir.dt.float32
BF16 = mybir.dt.bfloat16
I32 = mybir.dt.int32
AF = mybir.ActivationFunctionType
ALU = mybir.AluOpType
AX = mybir.AxisListType

D = 1024
DEPTH = 4
NE = 16
DFF = 2048
ALPHA = (2 * DEPTH) ** 0.25
EPS = 1e-5
BLK = 512
TRASH = 2048
NEG = -30000.0

ENGS = ("pe", "act", "dve", "pool", "sp")
EPOCH = 30000


class Res:
    __slots__ = ("name", "lw", "rd", "sem", "dcnt")

    def __init__(self, name):
        self.name = name
        self.lw = None
        self.rd = []
        self.sem = None
        self.dcnt = 0


class Op:
    __slots__ = ("eng", "fn", "deps", "marked", "seq", "dma", "dtok")

    def __init__(self, eng, fn, dma):
        self.eng = eng
        self.fn = fn
        self.deps = []
        self.marked = False
        self.seq = None
        self.dma = dma
        self.dtok = None


class Prog:
    def __init__(self, nc):
        self.nc = nc
        self.ops = []
        self.n_dma_sems = 0
        self.res_list = []
        self.last = {e: None for e in ENGS}
        self.sem_pool = []

    def res(self, name):
        r = Res(name)
        self.res_list.append(r)
        return r

    def release(self, rs):
        for r in rs:
            if r.sem is not None:
                self.sem_pool.append((r.sem, r.dcnt))
                r.sem = None

    def _add(self, eng, fn, reads, writes, dma_res=None):
        op = Op(eng, fn, dma_res is not None)
        deps = []
        for r in reads:
            if r.lw is not None:
                deps.append(r.lw)
        for w in writes:
            if w.lw is not None:
                deps.append(w.lw)
            deps.extend(w.rd)
        seen = set()
        for d in deps:
            if id(d) in seen:
                continue
            seen.add(id(d))
            if isinstance(d, Op):
                if d.eng == eng and eng == "pe" and not op.dma:
                    continue
                d.marked = True
            op.deps.append(d)
        if dma_res is not None:
            if dma_res.sem is None:
                if self.sem_pool:
                    dma_res.sem, dma_res.dcnt = self.sem_pool.pop()
                else:
                    dma_res.sem = self.n_dma_sems
                    dma_res.dcnt = 0
                    self.n_dma_sems += 1
            dma_res.dcnt += 16
            tok = ("D", dma_res.sem, dma_res.dcnt)
            op.dtok = tok
        else:
            tok = op
            if fn is not None:
                self.last[eng] = op
        for r in reads:
            if r.rd:
                p = r.rd[-1]
                if isinstance(tok, Op) and isinstance(p, Op) and p.eng == tok.eng:
                    r.rd[-1] = tok
                    continue
                if (not isinstance(tok, Op)) and (not isinstance(p, Op)) and p[1] == tok[1]:
                    r.rd[-1] = tok
                    continue
            r.rd.append(tok)
        for w in writes:
            w.lw = tok
            w.rd = []
        self.ops.append(op)
        return op

    def op(self, eng, fn, reads=(), writes=()):
        return self._add(eng, fn, reads, writes)

    def dma(self, eng, fn, reads, writes, sem_res):
        return self._add(eng, fn, reads, writes, dma_res=sem_res)

    def barrier(self):
        toks = []
        for e in ENGS:
            if self.last[e] is not None:
                toks.append(self.last[e])
        dt = {}
        for r in self.res_list:
            if r.sem is not None:
                dt[r.sem] = max(dt.get(r.sem, 0), r.dcnt)
        for s, c in self.sem_pool:
            dt[s] = max(dt.get(s, 0), c)
        for s, c in dt.items():
            toks.append(("D", s, c))
        for e in ENGS:
            op = Op(e, None, False)
            for t in toks:
                if isinstance(t, Op):
                    if t.eng == e:
                        continue
                    t.marked = True
                op.deps.append(t)
            self.ops.append(op)
        for r in self.res_list:
            r.lw = None
            r.rd = []

    def emit(self):
        nc = self.nc
        cnt = {e: 0 for e in ENGS}
        for op in self.ops:
            if op.marked and not op.dma:
                cnt[op.eng] += 1
                op.seq = cnt[op.eng]
        n_eng_sems = {e: (cnt[e] + EPOCH - 1) // EPOCH for e in ENGS}
        with ExitStack() as st:
            esems = {e: [st.enter_context(nc.semaphore(f"e_{e}_{i}")) for i in range(n_eng_sems[e])]
                     for e in ENGS}
            dsems = [st.enter_context(nc.semaphore(f"d_{i}")) for i in range(self.n_dma_sems)]
            block = st.enter_context(nc.Block())
            by_eng = {e: [op for op in self.ops if op.eng == e] for e in ENGS}

            def resolve(tok):
                if isinstance(tok, Op):
                    s = (tok.seq - 1) // EPOCH
                    return esems[tok.eng][s], tok.seq - s * EPOCH, ("E", tok.eng, s)
                return dsems[tok[1]], tok[2], ("D", tok[1])

            def run_engine(eng_name, eng):
                known = {}
                for op in by_eng[eng_name]:
                    for d in op.deps:
                        sem, val, key = resolve(d)
                        if known.get(key, 0) >= val:
                            continue
                        known[key] = val
                        eng.wait_ge(sem, val)
                    if op.fn is None:
                        continue
                    inst = op.fn(eng)
                    if op.dma:
                        inst.then_inc(dsems[op.dtok[1]], 16)
                    elif op.marked:
                        s = (op.seq - 1) // EPOCH
                        inst.then_inc(esems[op.eng][s], 1)

            @block.tensor
            def _(e):
                run_engine("pe", e)

            @block.scalar
            def _(e):
                run_engine("act", e)

            @block.vector
            def _(e):
                run_engine("dve", e)

            @block.gpsimd
            def _(e):
                run_engine("pool", e)

            @block.sync
            def _(e):
                run_engine("sp", e)
        return cnt


def build_program(T, NU, depth=DEPTH, debug=None):
    NT = T // 128
    NBLK = -(-(2 * T + NE * (BLK - 1)) // BLK)
    NSLOT = NBLK * BLK
    nc = bass.Bass("TRN2", target_bir_lowering=False)

    def din(name, shape, dt=F32):
        return nc.dram_tensor(name, list(shape), dt, kind="ExternalInput").ap()

    x_in = din("x_in", [T, D])
    ln_in_g = din("ln_in_g", [D]); ln_in_b = din("ln_in_b", [D])
    w_in = din("w_in", [DEPTH, D, 4096]); b_in = din("b_in", [DEPTH, 4096])
    rpbT = din("rpbT", [DEPTH, 128, 4 * 15 * 64])
    w_pool = din("w_pool", [DEPTH, 4, 128, 128]); b_pool = din("b_pool", [DEPTH, 4, 128])
    pool_scale = din("pool_scale", [DEPTH, 512])
    w_oa = din("w_oa", [DEPTH, 512, D]); w_op = din("w_op", [DEPTH, 512, D])
    w_out = din("w_out", [DEPTH, D, D]); b_out = din("b_out", [DEPTH, D])
    ln1_g = din("ln1_g", [DEPTH, D]); ln1_b = din("ln1_b", [DEPTH, D])
    w_router = din("w_router", [D, NE]); b_router = din("b_router", [NE])
    w1 = din("w1", [DEPTH, NE, D, DFF]); b1 = din("b1", [DEPTH, NE, DFF])
    w2 = din("w2", [DEPTH, NE, DFF, D]); b2 = din("b2", [DEPTH, NE, D])
    ln2_g = din("ln2_g", [DEPTH, D]); ln2_b = din("ln2_b", [DEPTH, D])
    kvidx_d = din("kvidx", [128, NU * 16], I32)
    outidx_d = din("outidx", [128, NU * 16], I32)
    c_ident_bf = din("c_ident_bf", [128, 128], BF16)
    c_ident_f = din("c_ident_f", [128, 128])
    c_tri = din("c_tri", [128, 128], BF16)
    c_ones = din("c_ones", [128, 128], BF16)
    c_mask = din("c_mask", [128, 64])
    c_invc = din("c_invc", [128, 4, 16])
    c_tokid = din("c_tokid", [128, NT], I32)
    c_rowc = din("c_rowc", [128, 16])
    c_bstart = din("c_bstart", [128, NBLK])
    y_out = nc.dram_tensor("y_out", [T, D], F32, kind="ExternalOutput").ap()

    sk = "ExternalOutput" if debug else "Internal"
    xa = nc.dram_tensor("xa", [T + TRASH, D], F32, kind=sk).ap()
    x1b = nc.dram_tensor("x1b", [T + TRASH, D], F32, kind=sk).ap()
    kvp = nc.dram_tensor("kvp", [NU, 128, 3, 8192], BF16, kind=sk).ap()
    ybuf = nc.dram_tensor("ybuf", [NSLOT, D], F32, kind=sk).ap()
    table = nc.dram_tensor("table", [NSLOT, 1], I32, kind=sk).ap()

    w1_rows = w1.rearrange("l e d f -> (l e d) f")
    w2_rows = w2.rearrange("l e f d -> (l e f) d")
    b1_rows = b1.rearrange("l e (c p) -> (l e c) p", p=128)
    b2_rows = b2.rearrange("l e d -> (l e) d")

    P = Prog(nc)
    top = ExitStack()

    uniq = [0]

    def sbt(st, name, shape, dt):
        uniq[0] += 1
        return st.enter_context(nc.sbuf_tensor(f"{name}_{uniq[0]}", list(shape), dt))

    def pst(st, name, shape, dt):
        uniq[0] += 1
        return st.enter_context(nc.psum_tensor(f"{name}_{uniq[0]}", list(shape), dt))

    with top:
        ident_bf = sbt(top, "ident_bf", [128, 128], BF16)
        ident_f = sbt(top, "ident_f", [128, 128], F32)
        tri = sbt(top, "tri", [128, 128], BF16)
        ones = sbt(top, "ones", [128, 128], BF16)
        tokid = sbt(top, "tokid", [128, NT], I32)
        kvidx = sbt(top, "kvidx_sb", [128, NU * 16], I32)
        outidx = sbt(top, "outidx_sb", [128, NU * 16], I32)
        gates = sbt(top, "gates", [128, NT, 2], F32)
        slot0 = sbt(top, "slot0", [128, NT], I32)
        slot1 = sbt(top, "slot1", [128, NT], I32)
        mhalf = sbt(top, "mhalf", [128, 1], F32)
        widx1 = sbt(top, "widx1", [128, NBLK, 8], I32)
        widx2 = sbt(top, "widx2", [128, NBLK, 16], I32)
        bidx1 = sbt(top, "bidx1", [128, NBLK], I32)
        bidx2 = sbt(top, "bidx2", [128, NBLK], I32)
        r_const = P.res("const")
        r_gates = P.res("gates")
        r_slots = P.res("slots")
        r_widx = P.res("widx")
        for t, s in ((ident_bf, c_ident_bf), (ident_f, c_ident_f), (tri, c_tri), (ones, c_ones),
                     (tokid, c_tokid), (kvidx, kvidx_d), (outidx, outidx_d)):
            P.dma("sp", lambda e, t=t, s=s: e.dma_start(out=t[:], in_=s), [], [r_const], r_const)
        P.op("dve", lambda e: e.memset(mhalf[:], -0.5), [], [r_const])
        P.barrier()

        def emit_ln(xt, r_x, g_t, b_t, scr, r_scr, out_t=None, r_out=None, cread=()):
            if out_t is None:
                out_t, r_out = xt, r_x
            st6, mv, rs, nm = scr
            P.op("dve", lambda e: e.bn_stats(out=st6[:, 0, :], in_=xt[:, 0:512]), [r_x], [r_scr])
            P.op("dve", lambda e: e.bn_stats(out=st6[:, 1, :], in_=xt[:, 512:1024]), [r_x], [r_scr])
            P.op("dve", lambda e: e.bn_aggr(out=mv[:], in_=st6[:].rearrange("p a b -> p (a b)")), [r_scr], [r_scr])
            P.op("dve", lambda e: e.tensor_scalar(out=rs[:], in0=mv[:, 1:2], scalar1=EPS, scalar2=None,
                                                  op0=ALU.add), [r_scr], [r_scr])
            P.op("act", lambda e: e.activation(out=rs[:], in_=rs[:], func=AF.Ln), [r_scr], [r_scr])
            P.op("act", lambda e: e.activation(out=rs[:], in_=rs[:], func=AF.Exp, scale=-0.5), [r_scr], [r_scr])
            P.op("dve", lambda e: e.tensor_scalar(out=nm[:], in0=mv[:, 0:1], scalar1=rs[:, 0:1], scalar2=-1.0,
                                                  op0=ALU.mult, op1=ALU.mult), [r_scr], [r_scr])
            P.op("act", lambda e: e.activation(out=out_t, in_=xt, func=AF.Identity, bias=nm[:, 0:1],
                                               scale=rs[:, 0:1]), [r_x, r_scr], [r_out])
            P.op("dve", lambda e: e.tensor_tensor(out=out_t, in0=out_t, in1=g_t, op=ALU.mult),
                 [r_out] + list(cread), [r_out])
            P.op("dve", lambda e: e.tensor_tensor(out=out_t, in0=out_t, in1=b_t, op=ALU.add),
                 [r_out] + list(cread), [r_out])

        def ln_scratch(st, name):
            return (sbt(st, name + "_s6", [128, 2, 6], F32), sbt(st, name + "_mv", [128, 2], F32),
                    sbt(st, name + "_rs", [128, 1], F32), sbt(st, name + "_nm", [128, 1], F32))

        def _pass(st, l=(l if "l" in dir() else 0), last=(last if "last" in dir() else False)):
            g_t = sbt(st, "p0_g", [128, D], F32)
            b_t = sbt(st, "p0_b", [128, D], F32)
            r_gb = P.res("p0_gb")
            P.dma("sp", lambda e: e.dma_start(out=g_t[:], in_=ln_in_g[None, :].to_broadcast([128, D])), [], [r_gb], r_gb)
            P.dma("sp", lambda e: e.dma_start(out=b_t[:], in_=ln_in_b[None, :].to_broadcast([128, D])), [], [r_gb], r_gb)
            NB = 3
            xts = [sbt(st, f"p0_x{i}", [128, D], F32) for i in range(NB)]
            rxs = [P.res(f"p0_x{i}") for i in range(NB)]
            scrs = [ln_scratch(st, f"p0_l{i}") for i in range(NB)]
            rss = [P.res(f"p0_s{i}") for i in range(NB)]
            for i in range(NT):
                k = i % NB
                xt, rx = xts[k], rxs[k]
                P.dma("sp", lambda e, xt=xt, i=i: e.dma_start(out=xt[:], in_=x_in[i * 128:(i + 1) * 128, :]), [], [rx], rx)
                emit_ln(xt[:], rx, g_t[:], b_t[:], scrs[k], rss[k], cread=[r_gb])
                P.dma("sp", lambda e, xt=xt, i=i: e.dma_start(out=xa[i * 128:(i + 1) * 128, :], in_=xt[:]), [rx], [], rx)
            P.barrier()
            P.release(rxs + [r_gb])

        with ExitStack() as _st:
            _pass(_st)
        for l in range(depth):
            last = (l == depth - 1)
            def _pass(st, l=(l if "l" in dir() else 0), last=(last if "last" in dir() else False)):
                wA = sbt(st, "wA", [128, 8, 1536], BF16)
                r_wA = P.res("wA")
                for kc in range(8):
                    P.dma("pool", lambda e, kc=kc: e.dma_start(out=wA[:, kc, :], in_=w_in[l, kc * 128:(kc + 1) * 128, 512:2048]),
                          [], [r_wA], r_wA)
                bcol = sbt(st, "A_bcol", [128, 32], F32)
                bv = sbt(st, "A_bv", [128, 512], F32)
                r_bA = P.res("A_b")
                P.dma("sp", lambda e: e.dma_start(out=bcol[:], in_=b_in[l].rearrange("(c p) -> p c", p=128),
                                                  allow_slow_non_contiguous=True), [], [r_bA], r_bA)
                P.dma("sp", lambda e: e.dma_start(out=bv[:], in_=b_in[l, 1024:1536][None, :].to_broadcast([128, 512])), [], [r_bA], r_bA)
                NB = 2
                xbf = [sbt(st, f"A_xbf{i}", [128, 4, D], BF16) for i in range(NB)]
                r_xbf = [P.res(f"A_xbf{i}") for i in range(NB)]
                xT = [sbt(st, f"A_xT{i}", [128, 8, 512], BF16) for i in range(NB)]
                r_xT = [P.res(f"A_xT{i}") for i in range(NB)]
                kTc = [sbt(st, f"A_kT{i}", [128, 4, 512], BF16) for i in range(NB)]
                r_kTc = [P.res(f"A_kT{i}") for i in range(NB)]
                pTc = [sbt(st, f"A_pT{i}", [128, 4, 512], BF16) for i in range(NB)]
                r_pTc = [P.res(f"A_pT{i}") for i in range(NB)]
                Vc = [sbt(st, f"A_V{i}", [128, 4, 512], BF16) for i in range(NB)]
                r_Vc = [P.res(f"A_V{i}") for i in range(NB)]
                psT = [pst(st, f"A_psT{i}", [128, 8, 128], BF16) for i in range(2)]
                r_psT = [P.res(f"A_psT{i}") for i in range(2)]
                psA = [pst(st, f"A_ps{i}", [128, 512], F32) for i in range(4)]
                r_psA = [P.res(f"A_ps{i}") for i in range(4)]
                nps = 0
                npt = 0
                for u in range(NU):
                    for c in range(4):
                        k = (u * 4 + c) % NB
                        for j in range(4):
                            col = u * 16 + c * 4 + j
                            P.dma("pool", lambda e, k=k, j=j, col=col: e.indirect_dma_start(
                                out=xbf[k][:, j, :], out_offset=None, in_=xa[:, :],
                                in_offset=bass.IndirectOffsetOnAxis(ap=kvidx[:, col:col + 1], axis=0)),
                                [r_const], [r_xbf[k]], r_xbf[k])
                        for j in range(4):
                            q = npt % 2
                            npt += 1
                            for kc in range(8):
                                P.op("pe", lambda e, q=q, k=k, j=j, kc=kc: e.transpose(
                                    out=psT[q][:, kc, :], in_=xbf[k][:, j, kc * 128:(kc + 1) * 128], identity=ident_bf[:]),
                                    [r_xbf[k], r_const], [r_psT[q]])
                            eng = "act" if j % 2 == 0 else "dve"
                            if eng == "act":
                                P.op("act", lambda e, q=q, k=k, j=j: e.copy(out=xT[k][:, :, j * 128:(j + 1) * 128], in_=psT[q][:]),
                                     [r_psT[q]], [r_xT[k]])
                            else:
                                P.op("dve", lambda e, q=q, k=k, j=j: e.tensor_copy(out=xT[k][:, :, j * 128:(j + 1) * 128], in_=psT[q][:]),
                                     [r_psT[q]], [r_xT[k]])
                        for m in range(4):
                            q = nps % 4
                            nps += 1
                            for kc in range(8):
                                P.op("pe", lambda e, q=q, k=k, m=m, kc=kc: e.matmul(
                                    psA[q][:], lhsT=wA[:, kc, m * 128:(m + 1) * 128], rhs=xT[k][:, kc, :],
                                    start=(kc == 0), stop=(kc == 7)), [r_wA, r_xT[k]], [r_psA[q]])
                            P.op("act", lambda e, q=q, k=k, m=m: e.activation(
                                out=kTc[k][:, m, :], in_=psA[q][:], func=AF.Identity, bias=bcol[:, 4 + m:5 + m], scale=1.0),
                                [r_psA[q], r_bA], [r_kTc[k]])
                        for m in range(4):
                            q = nps % 4
                            nps += 1
                            for kc in range(8):
                                P.op("pe", lambda e, q=q, k=k, m=m, kc=kc: e.matmul(
                                    psA[q][:], lhsT=wA[:, kc, 1024 + m * 128:1024 + (m + 1) * 128], rhs=xT[k][:, kc, :],
                                    start=(kc == 0), stop=(kc == 7)), [r_wA, r_xT[k]], [r_psA[q]])
                            P.op("act", lambda e, q=q, k=k, m=m: e.activation(
                                out=pTc[k][:, m, :], in_=psA[q][:], func=AF.Identity, bias=bcol[:, 12 + m:13 + m], scale=1.0),
                                [r_psA[q], r_bA], [r_pTc[k]])
                        for j in range(4):
                            q = nps % 4
                            nps += 1
                            for kc in range(8):
                                P.op("pe", lambda e, q=q, k=k, j=j, kc=kc: e.matmul(
                                    psA[q][:], lhsT=xT[k][:, kc, j * 128:(j + 1) * 128], rhs=wA[:, kc, 512:1024],
                                    start=(kc == 0), stop=(kc == 7)), [r_wA, r_xT[k]], [r_psA[q]])
                            P.op("dve", lambda e, q=q, k=k, j=j: e.tensor_tensor(
                                out=Vc[k][:, j, :], in0=psA[q][:], in1=bv[:], op=ALU.add),
                                [r_psA[q], r_bA], [r_Vc[k]])
                        P.dma("sp", lambda e, k=k, u=u, c=c: e.dma_start(
                            out=kvp[u, :, 0, :].rearrange("p (m t) -> p m t", m=4)[:, :, c * 512:(c + 1) * 512], in_=kTc[k][:]),
                            [r_kTc[k]], [], r_kTc[k])
                        P.dma("sp", lambda e, k=k, u=u, c=c: e.dma_start(
                            out=kvp[u, :, 2, :].rearrange("p (m t) -> p m t", m=4)[:, :, c * 512:(c + 1) * 512], in_=pTc[k][:]),
                            [r_pTc[k]], [], r_pTc[k])
                        P.dma("sp", lambda e, k=k, u=u, c=c: e.dma_start(
                            out=kvp[u, :, 1, c * 2048:(c + 1) * 2048], in_=Vc[k][:].rearrange("p j f -> p (j f)")),
                            [r_Vc[k]], [], r_Vc[k])
                P.barrier()
                P.release([r_wA, r_bA] + r_xbf + r_kTc + r_pTc + r_Vc)

            with ExitStack() as _st:
                _pass(_st)
            def _pass(st, l=(l if "l" in dir() else 0), last=(last if "last" in dir() else False)):
                CH = 256
                RPC = CH // 64
                TPC = CH // 128
                NCH = 2048 // CH
                EXT = CH + 16
                wB = sbt(st, "wB", [128, 8, 2560], BF16)
                woa = sbt(st, "woa", [128, 4, D], BF16)
                wop = sbt(st, "wop", [128, 4, D], BF16)
                wout = sbt(st, "wout", [128, 8, D], BF16)
                wpl = sbt(st, "wpl", [128, 4, 128], BF16)
                r_wB = P.res("wB")
                for kc in range(8):
                    P.dma("pool", lambda e, kc=kc: e.dma_start(out=wB[:, kc, 0:512], in_=w_in[l, kc * 128:(kc + 1) * 128, 0:512]), [], [r_wB], r_wB)
                    P.dma("pool", lambda e, kc=kc: e.dma_start(out=wB[:, kc, 512:2560], in_=w_in[l, kc * 128:(kc + 1) * 128, 2048:4096]), [], [r_wB], r_wB)
                    P.dma("pool", lambda e, kc=kc: e.dma_start(out=wout[:, kc, :], in_=w_out[l, kc * 128:(kc + 1) * 128, :]), [], [r_wB], r_wB)
                for kc in range(4):
                    P.dma("pool", lambda e, kc=kc: e.dma_start(out=woa[:, kc, :], in_=w_oa[l, kc * 128:(kc + 1) * 128, :]), [], [r_wB], r_wB)
                    P.dma("pool", lambda e, kc=kc: e.dma_start(out=wop[:, kc, :], in_=w_op[l, kc * 128:(kc + 1) * 128, :]), [], [r_wB], r_wB)
                    P.dma("pool", lambda e, kc=kc: e.dma_start(out=wpl[:, kc, :], in_=w_pool[l, kc, :, :]), [], [r_wB], r_wB)
                bcol = sbt(st, "B_bcol", [128, 32], F32)
                bq8 = sbt(st, "B_bq8", [128, 4], F32)
                psc = sbt(st, "B_psc", [128, 4], F32)
                bpc = sbt(st, "B_bpc", [128, 4], F32)
                bout_f = sbt(st, "B_boutf", [1, D], F32)
                bout = sbt(st, "B_bout", [1, D], BF16)
                g1 = sbt(st, "B_g1", [128, D], F32)
                b1t = sbt(st, "B_b1", [128, D], F32)
                invc = sbt(st, "B_invc", [128, 4, 16], F32)
                maskt = sbt(st, "B_mask", [128, 64], F32)
                Tb = sbt(st, "B_Tb", [128, 4, 15, 64], BF16)
                r_cB = P.res("B_c")
                P.dma("sp", lambda e: e.dma_start(out=bcol[:], in_=b_in[l].rearrange("(c p) -> p c", p=128),
                                                  allow_slow_non_contiguous=True), [], [r_cB], r_cB)
                P.dma("sp", lambda e: e.dma_start(out=psc[:], in_=pool_scale[l].rearrange("(c p) -> p c", p=128),
                                                  allow_slow_non_contiguous=True), [], [r_cB], r_cB)
                P.dma("sp", lambda e: e.dma_start(out=bpc[:], in_=b_pool[l].rearrange("c p -> p c"),
                                                  allow_slow_non_contiguous=True), [], [r_cB], r_cB)
                P.dma("sp", lambda e: e.dma_start(out=bout_f[:], in_=b_out[l][None, :]), [], [r_cB], r_cB)
                P.dma("sp", lambda e: e.dma_start(out=g1[:], in_=ln1_g[l][None, :].to_broadcast([128, D])), [], [r_cB], r_cB)
                P.dma("sp", lambda e: e.dma_start(out=b1t[:], in_=ln1_b[l][None, :].to_broadcast([128, D])), [], [r_cB], r_cB)
                P.dma("sp", lambda e: e.dma_start(out=invc[:], in_=c_invc), [], [r_cB], r_cB)
                P.dma("sp", lambda e: e.dma_start(out=maskt[:], in_=c_mask), [], [r_cB], r_cB)
                for hp in range(4):
                    P.dma("pool", lambda e, hp=hp: e.dma_start(out=Tb[:, hp, :, :].rearrange("p a b -> p (a b)"),
                                                               in_=rpbT[l, :, hp * 960:(hp + 1) * 960]), [], [r_cB], r_cB)
                P.op("dve", lambda e: e.tensor_tensor(
                    out=Tb[:].rearrange("p a b c -> p (a b) c"), in0=Tb[:].rearrange("p a b c -> p (a b) c"),
                    in1=maskt[:][:, None, :].to_broadcast([128, 60, 64]), op=ALU.add), [r_cB], [r_cB])
                P.op("dve", lambda e: e.tensor_scalar(out=bq8[:], in0=bcol[:, 0:4], scalar1=0.125, scalar2=None, op0=ALU.mult), [r_cB], [r_cB])
                P.op("dve", lambda e: e.tensor_tensor(out=bpc[:], in0=bpc[:], in1=psc[:], op=ALU.mult), [r_cB], [r_cB])
                P.op("dve", lambda e: e.tensor_copy(out=bout[:], in_=bout_f[:]), [r_cB], [r_cB])

                kT = sbt(st, "B_kT", [128, 4, 2048], BF16)
                Vt = sbt(st, "B_V", [128, 16, 512], BF16)
                r_kT, r_V = P.res("B_kT"), P.res("B_V")
                xres = [sbt(st, f"B_xres{i}", [128, D], F32) for i in range(2)]
                r_xres = [P.res(f"B_xres{i}") for i in range(2)]
                xbf = [sbt(st, f"B_xbf{i}", [128, D], BF16) for i in range(2)]
                r_xbf = [P.res(f"B_xbf{i}") for i in range(2)]
                xT = sbt(st, "B_xT", [128, 8, CH], BF16)
                r_xT = P.res("B_xT")
                qT = sbt(st, "B_qT", [128, 4, CH], BF16)
                r_qT = P.res("B_qT")
                Sb = [sbt(st, f"B_S{i}", [128, 512], F32) for i in range(2)]
                r_Sb = [P.res(f"B_S{i}") for i in range(2)]
                Eb = [sbt(st, f"B_E{i}", [128, 512], BF16) for i in range(2)]
                r_Eb = [P.res(f"B_E{i}") for i in range(2)]
                rsum = [sbt(st, f"B_rs{i}", [128, 2], F32) for i in range(2)]
                Pb = [sbt(st, f"B_P{i}", [128, 640], BF16) for i in range(2)]
                r_Pb = [P.res(f"B_P{i}") for i in range(2)]
                PT = [sbt(st, f"B_PT{i}", [128, 5, 128], BF16) for i in range(2)]
                r_PT = [P.res(f"B_PT{i}") for i in range(2)]
                aT = sbt(st, "B_aT", [128, 4, CH], BF16)
                r_aT = P.res("B_aT")
                pex = sbt(st, "B_pex", [128, 4, EXT], BF16)
                r_pex = P.res("B_pex")
                pe_ = sbt(st, "B_pe", [128, EXT], F32)
                sA = sbt(st, "B_sA", [128, EXT], F32)
                sB_ = sbt(st, "B_sB", [128, EXT], F32)
                r_pl = P.res("B_pl")
                dT = sbt(st, "B_dT", [128, 4, CH], BF16)
                r_dT = P.res("B_dT")
                pmT = sbt(st, "B_pmT", [128, 4, CH], BF16)
                r_pmT = P.res("B_pmT")
                sg = [sbt(st, f"B_sg{i}", [128, CH], BF16) for i in range(2)]
                r_sg = [P.res(f"B_sg{i}") for i in range(2)]
                tm = [sbt(st, f"B_tm{i}", [128, CH], F32) for i in range(2)]
                r_tm = [P.res(f"B_tm{i}") for i in range(2)]
                mixT = sbt(st, "B_mixT", [128, 8, CH], BF16)
                r_mixT = P.res("B_mixT")
                lsc = [ln_scratch(st, f"B_l{i}") for i in range(2)]
                r_lsc = [P.res(f"B_ls{i}") for i in range(2)]
                psT = pst(st, "B_psT", [128, 8, 128], BF16)
                r_psT = P.res("B_psT")
                psP = [pst(st, f"B_psP{i}", [128, 512], F32) for i in range(3)]
                r_psP = [P.res(f"B_psP{i}") for i in range(3)]
                psS = [pst(st, f"B_psS{i}", [128, 512], F32) for i in range(2)]
                r_psS = [P.res(f"B_psS{i}") for i in range(2)]
                psPT = pst(st, "B_psPT", [128, 8, 128], BF16)
                r_psPT = P.res("B_psPT")
                psAT = pst(st, "B_psAT", [128, 512], F32)
                r_psAT = P.res("B_psAT")
                P.op("dve", lambda e: e.memset(Pb[0][:], 0.0), [], [r_Pb[0]])
                P.op("dve", lambda e: e.memset(Pb[1][:], 0.0), [], [r_Pb[1]])
                npp = 0
                nat = 0
                nxr = 0
                nxb = 0
                for u in range(NU):
                    P.dma("sp", lambda e, u=u: e.dma_start(out=kT[:].rearrange("p m t -> p (m t)"), in_=kvp[u, :, 0, :]), [], [r_kT], r_kT)
                    P.dma("sp", lambda e, u=u: e.dma_start(out=Vt[:].rearrange("p m t -> p (m t)"), in_=kvp[u, :, 1, :]), [], [r_V], r_V)
                    for c in range(NCH):
                        for j in range(TPC):
                            col = u * 16 + c * TPC + j
                            kb = nxb % 2
                            nxb += 1
                            P.dma("pool", lambda e, kb=kb, col=col: e.indirect_dma_start(
                                out=xbf[kb][:, :], out_offset=None, in_=xa[:, :],
                                in_offset=bass.IndirectOffsetOnAxis(ap=kvidx[:, col:col + 1], axis=0)),
                                [r_const], [r_xbf[kb]], r_xbf[kb])
                            for kc in range(8):
                                P.op("pe", lambda e, kb=kb, kc=kc: e.transpose(
                                    out=psT[:, kc, :], in_=xbf[kb][:, kc * 128:(kc + 1) * 128], identity=ident_bf[:]),
                                    [r_xbf[kb], r_const], [r_psT])
                            P.op("dve", lambda e, j=j: e.tensor_copy(out=xT[:, :, j * 128:(j + 1) * 128], in_=psT[:]),
                                 [r_psT], [r_xT])
                        lo_t = max(c * CH - 8, 0)
                        hi_t = min(c * CH + CH + 8, 2048)
                        eo = lo_t - (c * CH - 8)
                        nn = hi_t - lo_t
                        P.op("pool", lambda e: e.memset(pex[:], 0.0), [], [r_pex])
                        P.dma("sp", lambda e, u=u, lo_t=lo_t, hi_t=hi_t, eo=eo, nn=nn: e.dma_start(
                            out=pex[:, :, eo:eo + nn],
                            in_=kvp[u, :, 2, :].rearrange("p (m t) -> p m t", m=4)[:, :, lo_t:hi_t]), [], [r_pex], r_pex)
                        for m in range(4):
                            q = npp % 3
                            npp += 1
                            for kc in range(8):
                                P.op("pe", lambda e, q=q, m=m, kc=kc: e.matmul(
                                    psP[q][:, 0:CH], lhsT=wB[:, kc, m * 128:(m + 1) * 128], rhs=xT[:, kc, :],
                                    start=(kc == 0), stop=(kc == 7)), [r_wB, r_xT], [r_psP[q]])
                            P.op("act", lambda e, q=q, m=m: e.activation(
                                out=qT[:, m, :], in_=psP[q][:, 0:CH], func=AF.Identity, bias=bq8[:, m:m + 1], scale=0.125),
                                [r_psP[q], r_cB], [r_qT])
                        for hp in range(4):
                            for r8 in range(RPC):
                                ql = c * RPC + r8
                                ws = min(max(ql - 4, 0), 24)
                                dr0 = ws - ql + 7
                                k = nat % 2
                                nat += 1
                                for hh in range(2):
                                    lo, hi = hh * 64, (hh + 1) * 64
                                    P.op("pe", lambda e, k=k, hp=hp, r8=r8, ws=ws, lo=lo, hi=hi: e.matmul(
                                        psS[k][lo:hi, :], lhsT=qT[lo:hi, hp, r8 * 64:(r8 + 1) * 64],
                                        rhs=kT[lo:hi, hp, ws * 64:ws * 64 + 512], start=True, stop=True),
                                        [r_qT, r_kT], [r_psS[k]])
                                P.op("dve", lambda e, k=k, hp=hp, dr0=dr0: e.tensor_tensor(
                                    out=Sb[k][:], in0=psS[k][:],
                                    in1=Tb[:, hp, dr0:dr0 + 8, :].rearrange("p a b -> p (a b)"), op=ALU.add),
                                    [r_psS[k], r_cB], [r_Sb[k]])
                                P.op("act", lambda e, k=k: e.activation(
                                    out=Eb[k][:], in_=Sb[k][:], func=AF.Exp, accum_out=rsum[k][:, 0:1]),
                                    [r_Sb[k]], [r_Eb[k]])
                                P.op("dve", lambda e, k=k: e.reciprocal(out=rsum[k][:, 1:2], in_=rsum[k][:, 0:1]),
                                     [r_Eb[k]], [r_Eb[k]])
                                P.op("dve", lambda e, k=k: e.tensor_scalar(
                                    out=Pb[k][:, 64:576], in0=Eb[k][:], scalar1=rsum[k][:, 1:2], scalar2=None, op0=ALU.mult),
                                    [r_Eb[k]], [r_Pb[k]])
                                if ws % 2 == 0:
                                    nch, off, vt0 = 4, 64, ws // 2
                                else:
                                    nch, off, vt0 = 5, 0, (ws - 1) // 2
                                for ch in range(nch):
                                    P.op("pe", lambda e, k=k, ch=ch, off=off: e.transpose(
                                        out=psPT[:, ch, :], in_=Pb[k][:, off + ch * 128:off + (ch + 1) * 128], identity=ident_bf[:]),
                                        [r_Pb[k], r_const], [r_psPT])
                                P.op("act", lambda e, k=k, nch=nch: e.copy(out=PT[k][:, 0:nch, :], in_=psPT[:, 0:nch, :]),
                                     [r_psPT], [r_PT[k]])
                                for hh in range(2):
                                    lo, hi = hh * 64, (hh + 1) * 64
                                    hcol = (2 * hp + hh) * 64
                                    for ch in range(nch):
                                        P.op("pe", lambda e, k=k, ch=ch, lo=lo, hi=hi, hcol=hcol, r8=r8, vt0=vt0, nch=nch: e.matmul(
                                            psAT[lo:hi, r8 * 64:(r8 + 1) * 64], lhsT=Vt[:, vt0 + ch, hcol:hcol + 64],
                                            rhs=PT[k][:, ch, lo:hi], start=(ch == 0), stop=(ch == nch - 1)),
                                            [r_V, r_PT[k]], [r_psAT])
                            P.op("act", lambda e, hp=hp: e.copy(out=aT[:, hp, :], in_=psAT[:, 0:CH]), [r_psAT], [r_aT])
                        for g in range(4):
                            wv = 2 ** (g + 1)
                            P.op("dve", lambda e, g=g: e.tensor_copy(out=pe_[:], in_=pex[:, g, :]), [r_pex], [r_pl])
                            P.op("dve", lambda e: e.memset(sA[:], 0.0), [], [r_pl])
                            P.op("dve", lambda e: e.tensor_tensor(out=sA[:, 1:EXT], in0=pe_[:, 0:EXT - 1], in1=pe_[:, 1:EXT], op=ALU.add), [r_pl], [r_pl])
                            cur, oth = sA, sB_
                            for sh in (1, 2, 4)[:g]:
                                P.op("dve", lambda e, oth=oth: e.memset(oth[:], 0.0), [], [r_pl])
                                P.op("dve", lambda e, cur=cur, oth=oth, sh=sh: e.tensor_tensor(
                                    out=oth[:, sh:EXT - sh], in0=cur[:, 0:EXT - 2 * sh], in1=cur[:, 2 * sh:EXT], op=ALU.add), [r_pl], [r_pl])
                                cur, oth = oth, cur
                            P.op("dve", lambda e, cur=cur, g=g, wv=wv: e.scalar_tensor_tensor(
                                out=dT[:, g, :], in0=cur[:, 8:8 + CH], scalar=1.0 / wv, in1=pe_[:, 8:8 + CH],
                                op0=ALU.mult, op1=ALU.subtract), [r_pl], [r_dT])
                            if c == 0:
                                P.op("dve", lambda e, cur=cur, oth=oth, g=g: e.tensor_tensor(
                                    out=oth[:, 0:8], in0=cur[:, 8:16], in1=invc[:, g, 0:8], op=ALU.mult), [r_pl, r_cB], [r_pl])
                                P.op("dve", lambda e, oth=oth, g=g: e.tensor_tensor(
                                    out=dT[:, g, 0:8], in0=oth[:, 0:8], in1=pe_[:, 8:16], op=ALU.subtract), [r_pl], [r_dT])
                            if c == NCH - 1:
                                P.op("dve", lambda e, cur=cur, oth=oth, g=g: e.tensor_tensor(
                                    out=oth[:, 0:8], in0=cur[:, CH:CH + 8], in1=invc[:, g, 8:16], op=ALU.mult), [r_pl, r_cB], [r_pl])
                                P.op("dve", lambda e, oth=oth, g=g: e.tensor_tensor(
                                    out=dT[:, g, CH - 8:CH], in0=oth[:, 0:8], in1=pe_[:, CH:CH + 8], op=ALU.subtract), [r_pl], [r_dT])
                        for g in range(4):
                            q = npp % 3
                            npp += 1
                            P.op("pe", lambda e, q=q, g=g: e.matmul(psP[q][:, 0:CH], lhsT=wpl[:, g, :], rhs=dT[:, g, :], start=True, stop=True),
                                 [r_wB, r_dT], [r_psP[q]])
                            P.op("act", lambda e, q=q, g=g: e.activation(
                                out=pmT[:, g, :], in_=psP[q][:, 0:CH], func=AF.Identity, bias=bpc[:, g:g + 1], scale=psc[:, g:g + 1]),
                                [r_psP[q], r_cB], [r_pmT])
                        for m in range(8):
                            for br in range(2):
                                q = npp % 3
                                npp += 1
                                wc = 512 + br * 1024 + m * 128
                                for kc in range(8):
                                    P.op("pe", lambda e, q=q, wc=wc, kc=kc: e.matmul(
                                        psP[q][:, 0:CH], lhsT=wB[:, kc, wc:wc + 128], rhs=xT[:, kc, :], start=(kc == 0), stop=(kc == 7)),
                                        [r_wB, r_xT], [r_psP[q]])
                                bc = 16 + br * 8 + m
                                P.op("act", lambda e, q=q, br=br, bc=bc: e.activation(
                                    out=sg[br][:], in_=psP[q][:, 0:CH], func=AF.Sigmoid, bias=bcol[:, bc:bc + 1], scale=1.0),
                                    [r_psP[q], r_cB], [r_sg[br]])
                                q = npp % 3
                                npp += 1
                                wsrc, asrc, r_a = (woa, aT, r_aT) if br == 0 else (wop, pmT, r_pmT)
                                for kc in range(4):
                                    P.op("pe", lambda e, q=q, wsrc=wsrc, asrc=asrc, m=m, kc=kc: e.matmul(
                                        psP[q][:, 0:CH], lhsT=wsrc[:, kc, m * 128:(m + 1) * 128], rhs=asrc[:, kc, :], start=(kc == 0), stop=(kc == 3)),
                                        [r_wB, r_a], [r_psP[q]])
                                P.op("dve", lambda e, q=q, br=br: e.tensor_tensor(out=tm[br][:], in0=psP[q][:, 0:CH], in1=sg[br][:], op=ALU.mult),
                                     [r_psP[q], r_sg[br]], [r_tm[br]])
                            P.op("pool", lambda e, m=m: e.tensor_tensor(out=mixT[:, m, :], in0=tm[0][:], in1=tm[1][:], op=ALU.add),
                                 [r_tm[0], r_tm[1]], [r_mixT])
                        for j in range(TPC):
                            col = u * 16 + c * TPC + j
                            k = nxr % 2
                            nxr += 1
                            P.dma("pool", lambda e, k=k, col=col: e.indirect_dma_start(
                                out=xres[k][:, :], out_offset=None, in_=xa[:, :],
                                in_offset=bass.IndirectOffsetOnAxis(ap=kvidx[:, col:col + 1], axis=0)),
                                [r_const], [r_xres[k]], r_xres[k])
                            for h in range(2):
                                q = npp % 3
                                npp += 1
                                for kc in range(8):
                                    P.op("pe", lambda e, q=q, j=j, h=h, kc=kc: e.matmul(
                                        psP[q][:], lhsT=mixT[:, kc, j * 128:(j + 1) * 128], rhs=wout[:, kc, h * 512:(h + 1) * 512],
                                        start=(kc == 0), stop=False), [r_wB, r_mixT], [r_psP[q]])
                                P.op("pe", lambda e, q=q, h=h: e.matmul(
                                    psP[q][:], lhsT=ones[0:1, :], rhs=bout[0:1, h * 512:(h + 1) * 512], start=False, stop=True),
                                    [r_cB, r_const], [r_psP[q]])
                                P.op("dve", lambda e, q=q, k=k, h=h: e.scalar_tensor_tensor(
                                    out=xres[k][:, h * 512:(h + 1) * 512], in0=xres[k][:, h * 512:(h + 1) * 512], scalar=ALPHA,
                                    in1=psP[q][:], op0=ALU.mult, op1=ALU.add), [r_psP[q], r_xres[k]], [r_xres[k]])
                            emit_ln(xres[k][:], r_xres[k], g1[:], b1t[:], lsc[k], r_lsc[k], cread=[r_cB])
                            P.dma("pool", lambda e, k=k, col=col: e.indirect_dma_start(
                                out=x1b[:, :], out_offset=bass.IndirectOffsetOnAxis(ap=outidx[:, col:col + 1], axis=0),
                                in_=xres[k][:, :], in_offset=None), [r_xres[k], r_const], [], r_xres[k])
                P.barrier()
                P.release([r_wB, r_cB, r_kT, r_V, r_pex] + r_xbf + r_xres)

            with ExitStack() as _st:
                _pass(_st)
            def _pass(st, l=(l if "l" in dir() else 0), last=(last if "last" in dir() else False)):
                wr = sbt(st, "R_wr", [128, 8, NE], F32)
                brt = sbt(st, "R_br", [128, NE], F32)
                bst = sbt(st, "R_bst", [128, NBLK], F32)
                rowc = sbt(st, "R_rowc", [128, 16], F32)
                r_cR = P.res("R_c")
                P.dma("sp", lambda e: e.dma_start(out=wr[:], in_=w_router.rearrange("(k p) n -> p k n", p=128)), [], [r_cR], r_cR)
                P.dma("sp", lambda e: e.dma_start(out=brt[:], in_=b_router[None, :].to_broadcast([128, NE])), [], [r_cR], r_cR)
                P.dma("sp", lambda e: e.dma_start(out=bst[:], in_=c_bstart), [], [r_cR], r_cR)
                P.dma("sp", lambda e: e.dma_start(out=rowc[:], in_=c_rowc), [], [r_cR], r_cR)
                A0 = sbt(st, "R_A0", [128, NT, NE], F32)
                A1 = sbt(st, "R_A1", [128, NT, NE], F32)
                POS = sbt(st, "R_POS", [128, NT, NE], F32)
                r_A = P.res("R_A")
                base = sbt(st, "R_base", [128, NE], F32)
                r_base = P.res("R_base")
                P.op("dve", lambda e: e.memset(base[:], 0.0), [], [r_base])
                xt = [sbt(st, f"R_x{i}", [128, D], F32) for i in range(2)]
                r_xt = [P.res(f"R_x{i}") for i in range(2)]
                xT32 = [sbt(st, f"R_xT{i}", [128, 8, 128], F32) for i in range(2)]
                r_xT32 = [P.res(f"R_xT{i}") for i in range(2)]
                zt = [sbt(st, f"R_z{i}", [128, 16 * 8], F32) for i in range(2)]
                r_zt = [P.res(f"R_z{i}") for i in range(2)]
                sm = [sbt(st, f"R_sm{i}", [128, 32], F32) for i in range(2)]
                Abf = [sbt(st, f"R_Ab{i}", [128, NE], BF16) for i in range(2)]
                psX = [pst(st, f"R_psX{i}", [128, 8, 128], F32) for i in range(1)]
                r_psX = [P.res(f"R_psX{i}") for i in range(1)]
                psL_ = [pst(st, f"R_psL{i}", [128, 512], F32) for i in range(2)]
                psL = [t[:, 0:NE] for t in psL_]
                r_psL = [P.res(f"R_psL{i}") for i in range(2)]
                psC_ = [pst(st, f"R_psC{i}", [128, 512], F32) for i in range(2)]
                psC = [t[:, 0:2 * NE].rearrange("p (a b) -> p a b", a=2) for t in psC_]
                r_psC = [P.res(f"R_psC{i}") for i in range(2)]
                for i in range(NT):
                    k = i % 2
                    P.dma("sp", lambda e, k=k, i=i: e.dma_start(out=xt[k][:], in_=x1b[i * 128:(i + 1) * 128, :]), [], [r_xt[k]], r_xt[k])
                    for kc in range(8):
                        P.op("pe", lambda e, k=k, kc=kc: e.transpose(out=psX[0][:, kc, :], in_=xt[k][:, kc * 128:(kc + 1) * 128], identity=ident_f[:]),
                             [r_xt[k], r_const], [r_psX[0]])
                    P.op("act", lambda e, k=k: e.copy(out=xT32[k][:, 0:4, :], in_=psX[0][:, 0:4, :]), [r_psX[0]], [r_xT32[k]])
                    P.op("dve", lambda e, k=k: e.tensor_copy(out=xT32[k][:, 4:8, :], in_=psX[0][:, 4:8, :]), [r_psX[0]], [r_xT32[k]])
                    for kc in range(8):
                        P.op("pe", lambda e, k=k, kc=kc: e.matmul(psL[k], lhsT=xT32[k][:, kc, :], rhs=wr[:, kc, :], start=(kc == 0), stop=(kc == 7)),
                             [r_xT32[k], r_cR], [r_psL[k]])
                    z = zt[k]
                    Z = lambda a, z=z: z[:, a * 16:(a + 1) * 16]
                    Z4 = lambda a, z=z: z[:, a * 16:(a + 1) * 16].rearrange("p (g j) -> p g j", g=4)
                    s = sm[k]
                    rz = r_zt[k]
                    P.op("dve", lambda e, k=k, Z=Z: e.tensor_tensor(out=Z(0), in0=psL[k], in1=brt[:], op=ALU.add), [r_psL[k], r_cR], [rz])
                    P.op("dve", lambda e, Z=Z, s=s: e.tensor_reduce(out=s[:, 0:1], in_=Z(0), axis=AX.X, op=ALU.max, negate=True), [rz], [rz])
                    P.op("act", lambda e, Z=Z, s=s: e.activation(out=Z(1), in_=Z(0), func=AF.Exp, bias=s[:, 0:1], scale=1.0), [rz], [rz])
                    P.op("dve", lambda e, Z4=Z4, s=s: e.tensor_reduce(out=s[:, 4:8], in_=Z4(1), axis=AX.X, op=ALU.max), [rz], [rz])
                    P.op("dve", lambda e, Z4=Z4, s=s: e.tensor_tensor(out=Z4(2), in0=Z4(1), in1=s[:, 4:8][:, :, None].to_broadcast([128, 4, 4]), op=ALU.is_equal), [rz], [rz])
                    P.op("dve", lambda e, Z=Z: e.scalar_tensor_tensor(out=Z(3), in0=Z(2), scalar=-2.0, in1=Z(1), op0=ALU.mult, op1=ALU.add), [rz], [rz])
                    P.op("dve", lambda e, Z4=Z4, s=s: e.tensor_reduce(out=s[:, 8:12], in_=Z4(3), axis=AX.X, op=ALU.max), [rz], [rz])
                    P.op("dve", lambda e, Z4=Z4, s=s: e.tensor_tensor(out=Z4(4), in0=Z4(3), in1=s[:, 8:12][:, :, None].to_broadcast([128, 4, 4]), op=ALU.is_equal), [rz], [rz])
                    P.op("dve", lambda e, s=s: e.tensor_tensor(out=s[:, 12:16], in0=s[:, 4:8], in1=s[:, 8:12], op=ALU.add), [rz], [rz])
                    P.op("dve", lambda e, s=s: e.tensor_reduce(out=s[:, 1:2], in_=s[:, 12:16], axis=AX.X, op=ALU.max), [rz], [rz])
                    P.op("dve", lambda e, s=s: e.tensor_scalar(out=s[:, 16:20], in0=s[:, 12:16], scalar1=s[:, 1:2], scalar2=None, op0=ALU.is_equal), [rz], [rz])
                    P.op("dve", lambda e, Z4=Z4, s=s, i=i: e.tensor_tensor(out=A0[:, i, :].rearrange("p (g j) -> p g j", g=4), in0=Z4(2),
                                                                             in1=s[:, 16:20][:, :, None].to_broadcast([128, 4, 4]), op=ALU.mult), [rz], [r_A])
                    P.op("dve", lambda e, Z4=Z4, s=s, i=i: e.tensor_tensor(out=A1[:, i, :].rearrange("p (g j) -> p g j", g=4), in0=Z4(4),
                                                                             in1=s[:, 16:20][:, :, None].to_broadcast([128, 4, 4]), op=ALU.mult), [rz], [r_A])
                    P.op("dve", lambda e, s=s: e.tensor_tensor(out=s[:, 20:24], in0=s[:, 16:20], in1=s[:, 4:8], op=ALU.mult), [rz], [rz])
                    P.op("dve", lambda e, s=s: e.tensor_tensor(out=s[:, 24:28], in0=s[:, 16:20], in1=s[:, 8:12], op=ALU.mult), [rz], [rz])
                    P.op("dve", lambda e, s=s: e.tensor_reduce(out=s[:, 28:30], in_=s[:, 20:28].rearrange("p (a b) -> p a b", a=2), axis=AX.X, op=ALU.add), [rz], [rz])
                    P.op("dve", lambda e, s=s: e.tensor_reduce(out=s[:, 30:31], in_=s[:, 28:30], axis=AX.X, op=ALU.add), [rz], [rz])
                    P.op("dve", lambda e, s=s: e.reciprocal(out=s[:, 31:32], in_=s[:, 30:31]), [rz], [rz])
                    P.op("dve", lambda e, s=s, i=i: e.tensor_scalar(out=gates[:, i, :], in0=s[:, 28:30], scalar1=s[:, 31:32], scalar2=None, op0=ALU.mult), [rz], [r_gates])
                    P.op("dve", lambda e, k=k, i=i: e.tensor_tensor(out=Abf[k][:], in0=A0[:, i, :], in1=A1[:, i, :], op=ALU.add), [r_A], [rz])
                    P.op("pe", lambda e, k=k: e.matmul(psC[k][:, 0, :], lhsT=tri[:], rhs=Abf[k][:], start=True, stop=True), [rz, r_const], [r_psC[k]])
                    P.op("pe", lambda e, k=k: e.matmul(psC[k][:, 1, :], lhsT=ones[:], rhs=Abf[k][:], start=True, stop=True), [rz, r_const], [r_psC[k]])
                    P.op("dve", lambda e, k=k, i=i: e.tensor_tensor(out=POS[:, i, :], in0=psC[k][:, 0, :], in1=base[:], op=ALU.add), [r_psC[k], r_base], [r_A])
                    P.op("dve", lambda e, k=k: e.tensor_tensor(out=base[:], in0=psC[k][:, 1, :], in1=base[:], op=ALU.add), [r_psC[k], r_base], [r_base])
                ci = sbt(st, "R_ci", [128, NE], I32)
                pf = sbt(st, "R_pf", [128, 4, NE], F32)
                r_pf = P.res("R_pf")
                P.op("dve", lambda e: e.tensor_scalar(out=ci[:], in0=base[:], scalar1=float(BLK - 1), scalar2=None, op0=ALU.add), [r_base], [r_pf])
                P.op("dve", lambda e: e.tensor_scalar(out=ci[:], in0=ci[:], scalar1=9, scalar2=9, op0=ALU.arith_shift_right, op1=ALU.arith_shift_left), [r_pf], [r_pf])
                P.op("dve", lambda e: e.tensor_copy(out=pf[:, 0, :], in_=ci[:]), [r_pf], [r_pf])
                P.op("dve", lambda e: e.tensor_copy(out=pf[:, 1, :], in_=pf[:, 0, :]), [r_pf], [r_pf])
                for sh in (1, 2, 4, 8):
                    P.op("dve", lambda e: e.tensor_copy(out=pf[:, 2, :], in_=pf[:, 1, :]), [r_pf], [r_pf])
                    P.op("dve", lambda e, sh=sh: e.tensor_tensor(out=pf[:, 1, sh:NE], in0=pf[:, 2, sh:NE], in1=pf[:, 2, 0:NE - sh], op=ALU.add), [r_pf], [r_pf])
                P.op("dve", lambda e: e.tensor_tensor(out=pf[:, 3, :], in0=pf[:, 1, :], in1=pf[:, 0, :], op=ALU.subtract), [r_pf], [r_pf])
                P.op("dve", lambda e: e.tensor_tensor(out=POS[:], in0=POS[:], in1=pf[:, 3:4, :].to_broadcast([128, NT, NE]), op=ALU.add), [r_pf, r_A], [r_A])
                sf = sbt(st, "R_sf", [128, NT], F32)
                for Ak, sl in ((A0, slot0), (A1, slot1)):
                    P.op("dve", lambda e, Ak=Ak: e.tensor_tensor(out=Ak[:], in0=Ak[:], in1=POS[:], op=ALU.mult), [r_A], [r_A])
                    P.op("dve", lambda e, Ak=Ak: e.tensor_reduce(out=sf[:], in_=Ak[:], axis=AX.X, op=ALU.add), [r_A], [r_pf])
                    P.op("dve", lambda e, sl=sl: e.tensor_copy(out=sl[:], in_=sf[:]), [r_pf], [r_slots])
                eb = sbt(st, "R_eb", [128, NBLK], F32)
                tmpb = sbt(st, "R_tmpb", [128, NBLK], F32)
                r_eb = P.res("R_eb")
                P.op("dve", lambda e: e.memset(eb[:], 0.0), [], [r_eb])
                for ex in range(NE):
                    P.op("dve", lambda e, ex=ex: e.tensor_scalar(out=tmpb[:], in0=bst[:], scalar1=pf[:, 1, ex:ex + 1], scalar2=None, op0=ALU.is_ge), [r_pf, r_cR], [r_eb])
                    P.op("dve", lambda e: e.tensor_tensor(out=eb[:], in0=eb[:], in1=tmpb[:], op=ALU.add), [r_eb], [r_eb])
                P.op("dve", lambda e: e.tensor_scalar(out=eb[:], in0=eb[:], scalar1=float(NE - 1), scalar2=float(l * NE), op0=ALU.min, op1=ALU.add), [r_eb], [r_eb])
                for kc in range(8):
                    P.op("dve", lambda e, kc=kc: e.tensor_scalar(out=widx1[:, :, kc], in0=eb[:], scalar1=float(D), scalar2=rowc[:, kc:kc + 1], op0=ALU.mult, op1=ALU.add), [r_eb, r_cR], [r_widx])
                for kc in range(16):
                    P.op("dve", lambda e, kc=kc: e.tensor_scalar(out=widx2[:, :, kc], in0=eb[:], scalar1=float(DFF), scalar2=rowc[:, kc:kc + 1], op0=ALU.mult, op1=ALU.add), [r_eb, r_cR], [r_widx])
                P.op("dve", lambda e: e.tensor_scalar(out=bidx1[:], in0=eb[:], scalar1=16.0, scalar2=rowc[:, 0:1], op0=ALU.mult, op1=ALU.add), [r_eb, r_cR], [r_widx])
                P.op("dve", lambda e: e.tensor_copy(out=bidx2[:], in_=eb[:]), [r_eb], [r_widx])
                zi = sbt(st, "R_zi", [128, NSLOT // 128], I32)
                r_zi = P.res("R_zi")
                P.op("dve", lambda e: e.memset(zi[:], 0), [], [r_zi])
                P.dma("sp", lambda e: e.dma_start(out=table.rearrange("(p f) o -> p (f o)", p=128), in_=zi[:]), [r_zi], [], r_zi)
                P.barrier()
                for i in range(NT):
                    for sl in (slot0, slot1):
                        P.dma("pool", lambda e, sl=sl, i=i: e.indirect_dma_start(
                            out=table[:, :], out_offset=bass.IndirectOffsetOnAxis(ap=sl[:, i:i + 1], axis=0),
                            in_=tokid[:, i:i + 1], in_offset=None), [r_slots, r_const], [], r_slots)
                P.barrier()
                P.release(r_xt + [r_cR, r_zi])

            with ExitStack() as _st:
                _pass(_st)
            def _pass(st, l=(l if "l" in dir() else 0), last=(last if "last" in dir() else False)):
                w1s = sbt(st, "M_w1", [128, 8, DFF], BF16)
                w2s = sbt(st, "M_w2", [128, 16, D], BF16)
                r_w1s, r_w2s = P.res("M_w1"), P.res("M_w2")
                tix = [sbt(st, f"M_tix{i}", [128, 4], I32) for i in range(2)]
                r_tix = [P.res(f"M_tix{i}") for i in range(2)]
                xg = [sbt(st, f"M_xg{i}", [128, 4, D], BF16) for i in range(2)]
                r_xg = [P.res(f"M_xg{i}") for i in range(2)]
                xT = [sbt(st, f"M_xT{i}", [128, 8, 512], BF16) for i in range(2)]
                r_xT = [P.res(f"M_xT{i}") for i in range(2)]
                hT = sbt(st, "M_hT", [128, 16, 512], BF16)
                r_hT = P.res("M_hT")
                b1r = [sbt(st, f"M_b1r{i}", [16, 128], F32) for i in range(2)]
                r_b1r = [P.res(f"M_b1r{i}") for i in range(2)]
                b1c = [sbt(st, f"M_b1c{i}", [128, 16], F32) for i in range(2)]
                r_b1c = [P.res(f"M_b1c{i}") for i in range(2)]
                b2b = [sbt(st, f"M_b2b{i}", [128, D], F32) for i in range(2)]
                r_b2b = [P.res(f"M_b2b{i}") for i in range(2)]
                ysb = [sbt(st, f"M_y{i}", [128, D], F32) for i in range(3)]
                r_ysb = [P.res(f"M_y{i}") for i in range(3)]
                psT = [pst(st, f"M_psT{i}", [128, 8, 128], BF16) for i in range(2)]
                r_psT = [P.res(f"M_psT{i}") for i in range(2)]
                psH = [pst(st, f"M_psH{i}", [128, 512], F32) for i in range(3)]
                r_psH = [P.res(f"M_psH{i}") for i in range(3)]
                psY = [pst(st, f"M_psY{i}", [128, 512], F32) for i in range(2)]
                r_psY = [P.res(f"M_psY{i}") for i in range(2)]
                psB_ = pst(st, "M_psB", [128, 512], F32)
                psB = psB_[:, 0:16]
                r_psB = P.res("M_psB")
                npt = 0
                nph = 0
                npy = 0
                ny = 0
                tview = table.rearrange("(b p j) o -> b p (j o)", p=128, j=4)
                yview = ybuf.rearrange("(b p j) d -> b j p d", p=128, j=4)
                for b in range(NBLK):
                    k = b % 2
                    P.dma("sp", lambda e, k=k, b=b: e.dma_start(out=tix[k][:], in_=tview[b]), [], [r_tix[k]], r_tix[k])
                    for j in range(4):
                        P.dma("pool", lambda e, k=k, j=j: e.indirect_dma_start(
                            out=xg[k][:, j, :], out_offset=None, in_=x1b[:, :],
                            in_offset=bass.IndirectOffsetOnAxis(ap=tix[k][:, j:j + 1], axis=0)), [r_tix[k]], [r_xg[k]], r_xg[k])
                    for kc in range(8):
                        P.dma("pool", lambda e, b=b, kc=kc: e.indirect_dma_start(
                            out=w1s[:, kc, :], out_offset=None, in_=w1_rows[:, :],
                            in_offset=bass.IndirectOffsetOnAxis(ap=widx1[:, b, kc:kc + 1], axis=0)), [r_widx], [r_w1s], r_w1s)
                    P.dma("pool", lambda e, k=k, b=b: e.indirect_dma_start(
                        out=b1r[k][:, :], out_offset=None, in_=b1_rows[:, :],
                        in_offset=bass.IndirectOffsetOnAxis(ap=bidx1[0:16, b:b + 1], axis=0)), [r_widx], [r_b1r[k]], r_b1r[k])
                    P.dma("pool", lambda e, k=k, b=b: e.indirect_dma_start(
                        out=b2b[k][:, :], out_offset=None, in_=b2_rows[:, :],
                        in_offset=bass.IndirectOffsetOnAxis(ap=bidx2[:, b:b + 1], axis=0)), [r_widx], [r_b2b[k]], r_b2b[k])
                    for j in range(4):
                        q = npt % 2
                        npt += 1
                        for kc in range(8):
                            P.op("pe", lambda e, q=q, k=k, j=j, kc=kc: e.transpose(
                                out=psT[q][:, kc, :], in_=xg[k][:, j, kc * 128:(kc + 1) * 128], identity=ident_bf[:]),
                                [r_xg[k], r_const], [r_psT[q]])
                        P.op("dve", lambda e, q=q, k=k, j=j: e.tensor_copy(out=xT[k][:, :, j * 128:(j + 1) * 128], in_=psT[q][:]),
                             [r_psT[q]], [r_xT[k]])
                    P.op("pe", lambda e, k=k: e.transpose(out=psB, in_=b1r[k][:, :], identity=ident_f[0:16, 0:16]),
                         [r_b1r[k], r_const], [r_psB])
                    P.op("dve", lambda e, k=k: e.tensor_copy(out=b1c[k][:], in_=psB), [r_psB], [r_b1c[k]])
                    for m in range(16):
                        q = nph % 3
                        nph += 1
                        for kc in range(8):
                            P.op("pe", lambda e, q=q, k=k, m=m, kc=kc: e.matmul(
                                psH[q][:], lhsT=w1s[:, kc, m * 128:(m + 1) * 128], rhs=xT[k][:, kc, :], start=(kc == 0), stop=(kc == 7)),
                                [r_w1s, r_xT[k]], [r_psH[q]])
                        P.op("act", lambda e, q=q, k=k, m=m: e.activation(
                            out=hT[:, m, :], in_=psH[q][:], func=AF.Gelu, bias=b1c[k][:, m:m + 1], scale=1.0),
                            [r_psH[q], r_b1c[k]], [r_hT])
                    for kc in range(16):
                        P.dma("pool", lambda e, b=b, kc=kc: e.indirect_dma_start(
                            out=w2s[:, kc, :], out_offset=None, in_=w2_rows[:, :],
                            in_offset=bass.IndirectOffsetOnAxis(ap=widx2[:, b, kc:kc + 1], axis=0)), [r_widx], [r_w2s], r_w2s)
                    for j in range(4):
                        yk = ny % 3
                        ny += 1
                        for h in range(2):
                            q = npy % 2
                            npy += 1
                            for kc in range(16):
                                P.op("pe", lambda e, q=q, j=j, h=h, kc=kc: e.matmul(
                                    psY[q][:], lhsT=hT[:, kc, j * 128:(j + 1) * 128], rhs=w2s[:, kc, h * 512:(h + 1) * 512],
                                    start=(kc == 0), stop=(kc == 15)), [r_w2s, r_hT], [r_psY[q]])
                            P.op("dve", lambda e, q=q, yk=yk, k=k, h=h: e.tensor_tensor(
                                out=ysb[yk][:, h * 512:(h + 1) * 512], in0=psY[q][:], in1=b2b[k][:, h * 512:(h + 1) * 512], op=ALU.add),
                                [r_psY[q], r_b2b[k]], [r_ysb[yk]])
                        P.dma("sp", lambda e, yk=yk, b=b, j=j: e.dma_start(out=yview[b, j], in_=ysb[yk][:]), [r_ysb[yk]], [], r_ysb[yk])
                P.barrier()
                P.release([r_w1s, r_w2s] + r_tix + r_xg + r_b1r + r_b2b + r_ysb)

            with ExitStack() as _st:
                _pass(_st)
            def _pass(st, l=(l if "l" in dir() else 0), last=(last if "last" in dir() else False)):
                g2 = sbt(st, "C_g", [128, D], F32)
                b2t = sbt(st, "C_b", [128, D], F32)
                r_cC = P.res("C_c")
                P.dma("sp", lambda e: e.dma_start(out=g2[:], in_=ln2_g[l][None, :].to_broadcast([128, D])), [], [r_cC], r_cC)
                P.dma("sp", lambda e: e.dma_start(out=b2t[:], in_=ln2_b[l][None, :].to_broadcast([128, D])), [], [r_cC], r_cC)
                NB = 3
                x1t = [sbt(st, f"C_x{i}", [128, D], F32) for i in range(NB)]
                r_x1t = [P.res(f"C_x{i}") for i in range(NB)]
                y0t = [sbt(st, f"C_y0{i}", [128, D], F32) for i in range(NB)]
                r_y0t = [P.res(f"C_y0{i}") for i in range(NB)]
                y1t = [sbt(st, f"C_y1{i}", [128, D], F32) for i in range(NB)]
                r_y1t = [P.res(f"C_y1{i}") for i in range(NB)]
                lsc = [ln_scratch(st, f"C_l{i}") for i in range(NB)]
                r_lsc = [P.res(f"C_ls{i}") for i in range(NB)]
                dst = y_out if last else xa
                for i in range(NT):
                    k = i % NB
                    P.dma("sp", lambda e, k=k, i=i: e.dma_start(out=x1t[k][:], in_=x1b[i * 128:(i + 1) * 128, :]), [], [r_x1t[k]], r_x1t[k])
                    P.dma("pool", lambda e, k=k, i=i: e.indirect_dma_start(
                        out=y0t[k][:, :], out_offset=None, in_=ybuf[:, :],
                        in_offset=bass.IndirectOffsetOnAxis(ap=slot0[:, i:i + 1], axis=0)), [r_slots], [r_y0t[k]], r_y0t[k])
                    P.dma("pool", lambda e, k=k, i=i: e.indirect_dma_start(
                        out=y1t[k][:, :], out_offset=None, in_=ybuf[:, :],
                        in_offset=bass.IndirectOffsetOnAxis(ap=slot1[:, i:i + 1], axis=0)), [r_slots], [r_y1t[k]], r_y1t[k])
                    P.op("act", lambda e, k=k: e.mul(out=x1t[k][:], in_=x1t[k][:], mul=ALPHA), [r_x1t[k]], [r_x1t[k]])
                    P.op("dve", lambda e, k=k, i=i: e.scalar_tensor_tensor(
                        out=x1t[k][:], in0=y0t[k][:], scalar=gates[:, i, 0:1], in1=x1t[k][:], op0=ALU.mult, op1=ALU.add),
                        [r_y0t[k], r_x1t[k], r_gates], [r_x1t[k]])
                    P.op("dve", lambda e, k=k, i=i: e.scalar_tensor_tensor(
                        out=x1t[k][:], in0=y1t[k][:], scalar=gates[:, i, 1:2], in1=x1t[k][:], op0=ALU.mult, op1=ALU.add),
                        [r_y1t[k], r_x1t[k], r_gates], [r_x1t[k]])
                    emit_ln(x1t[k][:], r_x1t[k], g2[:], b2t[:], lsc[k], r_lsc[k], cread=[r_cC])
                    P.dma("sp", lambda e, k=k, i=i: e.dma_start(out=dst[i * 128:(i + 1) * 128, :], in_=x1t[k][:]), [r_x1t[k]], [], r_x1t[k])
                P.barrier()
                P.release([r_cC] + r_x1t + r_y0t + r_y1t)
            with ExitStack() as _st:
                _pass(_st)
        cnt = P.emit()
    return nc, cnt


def _constants(T, NU):
    NT = T // 128
    NBLK = -(-(2 * T + NE * (BLK - 1)) // BLK)
    c = {}
    c["c_ident_bf"] = np.eye(128, dtype=np.float32).astype(ml_dtypes.bfloat16)
    c["c_ident_f"] = np.eye(128, dtype=np.float32)
    c["c_tri"] = np.triu(np.ones((128, 128), np.float32), 1).astype(ml_dtypes.bfloat16)
    c["c_ones"] = np.ones((128, 128), np.float32).astype(ml_dtypes.bfloat16)
    qc = np.arange(64)
    cs = np.clip(qc - 8, 0, 48)
    kc = np.arange(64)
    inw = (kc[None, :] >= cs[:, None]) & (kc[None, :] < cs[:, None] + 16)
    m = np.where(inw, 0.0, NEG).astype(np.float32)
    c["c_mask"] = np.concatenate([m, m], axis=0)
    invc = np.zeros((128, 4, 16), np.float32)
    L = 2048
    for g, w in enumerate((2, 4, 8, 16)):
        for i in range(8):
            lo, hi = max(i - w // 2, 0), min(i + w // 2, L)
            invc[:, g, i] = 1.0 / (hi - lo)
            p = L - 8 + i
            lo, hi = max(p - w // 2, 0), min(p + w // 2, L)
            invc[:, g, 8 + i] = 1.0 / (hi - lo)
    c["c_invc"] = invc
    c["c_tokid"] = (np.arange(NT)[None, :] * 128 + np.arange(128)[:, None]).astype(np.int32)
    c["c_rowc"] = (np.arange(16)[None, :] * 128 + np.arange(128)[:, None]).astype(np.float32)
    c["c_bstart"] = np.tile((np.arange(NBLK) * BLK).astype(np.float32)[None, :], (128, 1))
    return c


def _unit_tables(units, T):
    NU = len(units)
    kv = np.zeros((128, NU * 16), np.int32)
    oi = np.zeros((128, NU * 16), np.int32)
    for u, (t0, vlo, vhi) in enumerate(units):
        loc = np.arange(2048)
        tok = t0 + loc
        row = loc // 64
        valid = (row >= vlo) & (row < vhi)
        out = np.where(valid, tok, T + loc)
        kv[:, u * 16:(u + 1) * 16] = tok.reshape(16, 128).T
        oi[:, u * 16:(u + 1) * 16] = out.reshape(16, 128).T
    return kv, oi


_CACHE = {}


def kernel(x_prompt, x_sample, ln_in_g, ln_in_b, w_in, b_in, rpb, w_pool, b_pool, pool_scale,
           w_oa, w_op, w_out, b_out, ln1_g, ln1_b, w_router, b_router, w1, b1, w2, b2, ln2_g, ln2_b):
    T, NU = 12288, 7
    f = lambda a: np.ascontiguousarray(np.asarray(a, dtype=np.float32))
    x_prompt, x_sample = f(x_prompt), f(x_sample)
    shared = dict(ln_in_g=f(ln_in_g), ln_in_b=f(ln_in_b), w_in=f(w_in), b_in=f(b_in), w_pool=f(w_pool),
                  b_pool=f(b_pool), pool_scale=f(pool_scale), w_oa=f(w_oa), w_op=f(w_op), w_out=f(w_out), b_out=f(b_out),
                  ln1_g=f(ln1_g), ln1_b=f(ln1_b), w_router=f(w_router), b_router=f(b_router), w1=f(w1), b1=f(b1),
                  w2=f(w2), b2=f(b2), ln2_g=f(ln2_g), ln2_b=f(ln2_b))
    rp = f(rpb)
    qc = np.arange(64)[:, None]
    kcc = np.arange(64)[None, :]
    ti = np.clip(kcc - qc + 15, 0, 30)
    g = rp[:, :, :, ti]
    g = g.reshape(DEPTH, 4, 2, 15, 64, 64).transpose(0, 2, 4, 1, 3, 5)
    shared["rpbT"] = np.ascontiguousarray(g.reshape(DEPTH, 128, 4 * 15 * 64))
    shared.update(_constants(T, NU))
    in_maps = []
    for c in range(8):
        if c < 4:
            xc = np.concatenate([x_prompt[c], x_sample[2 * c], x_sample[2 * c + 1]], axis=0)
            units = [(0, 0, 28), (1536, 4, 28), (3072, 4, 28), (4608, 4, 28), (6144, 4, 32),
                     (8192, 0, 32), (10240, 0, 32)]
        else:
            s0 = 8 + 6 * (c - 4)
            xc = np.concatenate([x_sample[s0 + j] for j in range(6)], axis=0)
            units = [(2048 * j, 0, 32) for j in range(6)] + [(0, 0, 0)]
        kv, oi = _unit_tables(units, T)
        m = dict(shared)
        m["x_in"] = np.ascontiguousarray(xc)
        m["kvidx"] = kv
        m["outidx"] = oi
        in_maps.append(m)
    if "nc" not in _CACHE:
        _CACHE["nc"] = build_program(T, NU)[0]
    nc = _CACHE["nc"]
    res = run_bass_kernel_spmd(nc, in_maps, core_ids=list(range(8)))
    outs = [np.asarray(r["y_out"], dtype=np.float32) for r in res.results]
    y_prompt = np.stack([outs[c][0:8192] for c in range(4)], axis=0)
    y_sample = np.zeros((32, 2048, D), np.float32)
    for c in range(4):
        y_sample[2 * c] = outs[c][8192:10240]
        y_sample[2 * c + 1] = outs[c][10240:12288]
    for c in range(4, 8):
        s0 = 8 + 6 * (c - 4)
        for j in range(6):
            y_sample[s0 + j] = outs[c][2048 * j:2048 * (j + 1)]
    return (y_prompt, y_sample)
```

```python
from contextlib import ExitStack
import numpy as np
import ml_dtypes
import concourse.bass as bass
import concourse.mybir as mybir
from concourse.bass_utils import run_bass_kernel_spmd

F32 = mybir.dt.float32
BF16 = mybir.dt.bfloat16
I32 = mybir.dt.int32
AF = mybir.ActivationFunctionType
ALU = mybir.AluOpType
AX = mybir.AxisListType

D = 1024
DEPTH = 4
NE = 16
DFF = 2048
ALPHA = (2 * DEPTH) ** 0.25
EPS = 1e-5
BLK = 512
TRASH = 2048
NEG = -30000.0

ENGS = ("pe", "act", "dve", "pool", "sp")
EPOCH = 30000


class Res:
    __slots__ = ("name", "lw", "rd", "sem", "dcnt")

    def __init__(self, name):
        self.name = name
        self.lw = None
        self.rd = []
        self.sem = None
        self.dcnt = 0


class Op:
    __slots__ = ("eng", "fn", "deps", "marked", "seq", "dma", "dtok")

    def __init__(self, eng, fn, dma):
        self.eng = eng
        self.fn = fn
        self.deps = []
        self.marked = False
        self.seq = None
        self.dma = dma
        self.dtok = None


class Prog:
    def __init__(self, nc):
        self.nc = nc
        self.ops = []
        self.n_dma_sems = 0
        self.res_list = []
        self.last = {e: None for e in ENGS}
        self.sem_pool = []

    def res(self, name):
        r = Res(name)
        self.res_list.append(r)
        return r

    def release(self, rs):
        for r in rs:
            if r.sem is not None:
                self.sem_pool.append((r.sem, r.dcnt))
                r.sem = None

    def _add(self, eng, fn, reads, writes, dma_res=None):
        op = Op(eng, fn, dma_res is not None)
        deps = []
        for r in reads:
            if r.lw is not None:
                deps.append(r.lw)
        for w in writes:
            if w.lw is not None:
                deps.append(w.lw)
            deps.extend(w.rd)
        seen = set()
        for d in deps:
            if id(d) in seen:
                continue
            seen.add(id(d))
            if isinstance(d, Op):
                if d.eng == eng and eng == "pe" and not op.dma:
                    continue
                d.marked = True
            op.deps.append(d)
        if dma_res is not None:
            if dma_res.sem is None:
                if self.sem_pool:
                    dma_res.sem, dma_res.dcnt = self.sem_pool.pop()
                else:
                    dma_res.sem = self.n_dma_sems
                    dma_res.dcnt = 0
                    self.n_dma_sems += 1
            dma_res.dcnt += 16
            tok = ("D", dma_res.sem, dma_res.dcnt)
            op.dtok = tok
        else:
            tok = op
            if fn is not None:
                self.last[eng] = op
        for r in reads:
            if r.rd:
                p = r.rd[-1]
                if isinstance(tok, Op) and isinstance(p, Op) and p.eng == tok.eng:
                    r.rd[-1] = tok
                    continue
                if (not isinstance(tok, Op)) and (not isinstance(p, Op)) and p[1] == tok[1]:
                    r.rd[-1] = tok
                    continue
            r.rd.append(tok)
        for w in writes:
            w.lw = tok
            w.rd = []
        self.ops.append(op)
        return op

    def op(self, eng, fn, reads=(), writes=()):
        return self._add(eng, fn, reads, writes)

    def dma(self, eng, fn, reads, writes, sem_res):
        return self._add(eng, fn, reads, writes, dma_res=sem_res)

    def barrier(self):
        toks = []
        for e in ENGS:
            if self.last[e] is not None:
                toks.append(self.last[e])
        dt = {}
        for r in self.res_list:
            if r.sem is not None:
                dt[r.sem] = max(dt.get(r.sem, 0), r.dcnt)
        for s, c in self.sem_pool:
            dt[s] = max(dt.get(s, 0), c)
        for s, c in dt.items():
            toks.append(("D", s, c))
        for e in ENGS:
            op = Op(e, None, False)
            for t in toks:
                if isinstance(t, Op):
                    if t.eng == e:
                        continue
                    t.marked = True
                op.deps.append(t)
            self.ops.append(op)
        for r in self.res_list:
            r.lw = None
            r.rd = []

    def emit(self):
        nc = self.nc
        cnt = {e: 0 for e in ENGS}
        for op in self.ops:
            if op.marked and not op.dma:
                cnt[op.eng] += 1
                op.seq = cnt[op.eng]
        n_eng_sems = {e: (cnt[e] + EPOCH - 1) // EPOCH for e in ENGS}
        with ExitStack() as st:
            esems = {e: [st.enter_context(nc.semaphore(f"e_{e}_{i}")) for i in range(n_eng_sems[e])]
                     for e in ENGS}
            dsems = [st.enter_context(nc.semaphore(f"d_{i}")) for i in range(self.n_dma_sems)]
            block = st.enter_context(nc.Block())
            by_eng = {e: [op for op in self.ops if op.eng == e] for e in ENGS}

            def resolve(tok):
                if isinstance(tok, Op):
                    s = (tok.seq - 1) // EPOCH
                    return esems[tok.eng][s], tok.seq - s * EPOCH, ("E", tok.eng, s)
                return dsems[tok[1]], tok[2], ("D", tok[1])

            def run_engine(eng_name, eng):
                known = {}
                for op in by_eng[eng_name]:
                    for d in op.deps:
                        sem, val, key = resolve(d)
                        if known.get(key, 0) >= val:
                            continue
                        known[key] = val
                        eng.wait_ge(sem, val)
                    if op.fn is None:
                        continue
                    inst = op.fn(eng)
                    if op.dma:
                        inst.then_inc(dsems[op.dtok[1]], 16)
                    elif op.marked:
                        s = (op.seq - 1) // EPOCH
                        inst.then_inc(esems[op.eng][s], 1)

            @block.tensor
            def _(e):
                run_engine("pe", e)

            @block.scalar
            def _(e):
                run_engine("act", e)

            @block.vector
            def _(e):
                run_engine("dve", e)

            @block.gpsimd
            def _(e):
                run_engine("pool", e)

            @block.sync
            def _(e):
                run_engine("sp", e)
        return cnt


def build_program(T, NU, depth=DEPTH, debug=None):
    NT = T // 128
    NBLK = -(-(2 * T + NE * (BLK - 1)) // BLK)
    NSLOT = NBLK * BLK
    nc = bass.Bass("TRN2", target_bir_lowering=False)

    def din(name, shape, dt=F32):
        return nc.dram_tensor(name, list(shape), dt, kind="ExternalInput").ap()

    x_in = din("x_in", [T, D])
    ln_in_g = din("ln_in_g", [D]); ln_in_b = din("ln_in_b", [D])
    w_in = din("w_in", [DEPTH, D, 4096]); b_in = din("b_in", [DEPTH, 4096])
    rpbT = din("rpbT", [DEPTH, 128, 4 * 15 * 64])
    w_pool = din("w_pool", [DEPTH, 4, 128, 128]); b_pool = din("b_pool", [DEPTH, 4, 128])
    pool_scale = din("pool_scale", [DEPTH, 512])
    w_oa = din("w_oa", [DEPTH, 512, D]); w_op = din("w_op", [DEPTH, 512, D])
    w_out = din("w_out", [DEPTH, D, D]); b_out = din("b_out", [DEPTH, D])
    ln1_g = din("ln1_g", [DEPTH, D]); ln1_b = din("ln1_b", [DEPTH, D])
    w_router = din("w_router", [D, NE]); b_router = din("b_router", [NE])
    w1L = [din(f"w1_{i}", [NE * D, DFF]) for i in range(DEPTH)]; b1 = din("b1", [DEPTH, NE, DFF])
    w2L = [din(f"w2_{i}", [NE * DFF, D]) for i in range(DEPTH)]; b2 = din("b2", [DEPTH, NE, D])
    ln2_g = din("ln2_g", [DEPTH, D]); ln2_b = din("ln2_b", [DEPTH, D])
    kvidx_d = din("kvidx", [128, NU * 16], I32)
    outidx_d = din("outidx", [128, NU * 16], I32)
    c_ident_bf = din("c_ident_bf", [128, 128], BF16)
    c_ident_f = din("c_ident_f", [128, 128])
    c_tri = din("c_tri", [128, 128], BF16)
    c_ones = din("c_ones", [128, 128], BF16)
    c_mask = din("c_mask", [128, 64])
    c_invc = din("c_invc", [128, 4, 16])
    c_tokid = din("c_tokid", [128, NT], I32)
    c_rowc = din("c_rowc", [128, 16])
    c_bstart = din("c_bstart", [128, NBLK])
    y_out = nc.dram_tensor("y_out", [T, D], F32, kind="ExternalOutput").ap()

    sk = "ExternalOutput" if debug else "Internal"
    xa = nc.dram_tensor("xa", [T + TRASH, D], F32, kind=sk).ap()
    x1b = nc.dram_tensor("x1b", [T + TRASH, D], F32, kind=sk).ap()
    kvp = nc.dram_tensor("kvp", [NU, 128, 3, 8192], BF16, kind=sk).ap()
    ybuf = nc.dram_tensor("ybuf", [NSLOT, D], F32, kind=sk).ap()
    table = nc.dram_tensor("table", [NSLOT, 1], I32, kind=sk).ap()

    b1_rows = b1.rearrange("l e (c p) -> (l e c) p", p=128)
    b2_rows = b2.rearrange("l e d -> (l e) d")

    P = Prog(nc)
    top = ExitStack()
    _regs = {}

    def breg(e, val):
        if val not in _regs:
            _regs[val] = e.to_reg(val)
        return _regs[val]

    uniq = [0]

    def sbt(st, name, shape, dt):
        uniq[0] += 1
        return st.enter_context(nc.sbuf_tensor(f"{name}_{uniq[0]}", list(shape), dt))

    def pst(st, name, shape, dt):
        uniq[0] += 1
        return st.enter_context(nc.psum_tensor(f"{name}_{uniq[0]}", list(shape), dt))

    with top:
        ident_bf = sbt(top, "ident_bf", [128, 128], BF16)
        ident_f = sbt(top, "ident_f", [128, 128], F32)
        tri = sbt(top, "tri", [128, 128], BF16)
        ones = sbt(top, "ones", [128, 128], BF16)
        tokid = sbt(top, "tokid", [128, NT], I32)
        kvidx = sbt(top, "kvidx_sb", [128, NU * 16], I32)
        outidx = sbt(top, "outidx_sb", [128, NU * 16], I32)
        gates = sbt(top, "gates", [128, NT, 2], F32)
        slot0 = sbt(top, "slot0", [128, NT], I32)
        slot1 = sbt(top, "slot1", [128, NT], I32)
        mhalf = sbt(top, "mhalf", [128, 1], F32)
        widx1 = sbt(top, "widx1", [128, NBLK, 8], I32)
        widx2 = sbt(top, "widx2", [128, NBLK, 16], I32)
        bidx1 = sbt(top, "bidx1", [128, NBLK], I32)
        bidx2 = sbt(top, "bidx2", [128, NBLK], I32)
        r_const = P.res("const")
        r_gates = P.res("gates")
        r_slots = P.res("slots")
        r_widx = P.res("widx")
        for t, s in ((ident_bf, c_ident_bf), (ident_f, c_ident_f), (tri, c_tri), (ones, c_ones),
                     (tokid, c_tokid), (kvidx, kvidx_d), (outidx, outidx_d)):
            P.dma("sp", lambda e, t=t, s=s: e.dma_start(out=t[:], in_=s), [], [r_const], r_const)
        P.op("dve", lambda e: e.memset(mhalf[:], -0.5), [], [r_const])
        P.barrier()

        def emit_ln_multi(items, g_t, b_t, cread=()):
            def step(eng, mk, rd, wr):
                for it in items:
                    P.op(eng, mk(it), rd(it), wr(it))
            step("dve", lambda it: (lambda e: e.bn_stats(out=it[2][0][:, 0, :], in_=it[0][:, 0:512])), lambda it: [it[1]], lambda it: [it[3]])
            step("dve", lambda it: (lambda e: e.bn_stats(out=it[2][0][:, 1, :], in_=it[0][:, 512:1024])), lambda it: [it[1]], lambda it: [it[3]])
            step("dve", lambda it: (lambda e: e.bn_aggr(out=it[2][1][:], in_=it[2][0][:].rearrange("p a b -> p (a b)"))), lambda it: [it[3]], lambda it: [it[3]])
            step("dve", lambda it: (lambda e: e.tensor_scalar(out=it[2][2][:], in0=it[2][1][:, 1:2], scalar1=EPS, scalar2=None, op0=ALU.add)),
                 lambda it: [it[3]], lambda it: [it[3]])
            step("act", lambda it: (lambda e: e.activation(out=it[2][2][:], in_=it[2][2][:], func=AF.Ln)), lambda it: [it[3]], lambda it: [it[3]])
            step("act", lambda it: (lambda e: e.activation(out=it[2][2][:], in_=it[2][2][:], func=AF.Exp, scale=-0.5)), lambda it: [it[3]], lambda it: [it[3]])
            step("dve", lambda it: (lambda e: e.tensor_scalar(out=it[2][3][:], in0=it[2][1][:, 0:1], scalar1=it[2][2][:, 0:1], scalar2=-1.0,
                                                             op0=ALU.mult, op1=ALU.mult)), lambda it: [it[3]], lambda it: [it[3]])
            step("act", lambda it: (lambda e: e.activation(out=it[0], in_=it[0], func=AF.Identity, bias=it[2][3][:, 0:1], scale=it[2][2][:, 0:1])),
                 lambda it: [it[1], it[3]], lambda it: [it[1]])
            step("dve", lambda it: (lambda e: e.tensor_tensor(out=it[0], in0=it[0], in1=g_t, op=ALU.mult)), lambda it: [it[1]] + list(cread), lambda it: [it[1]])
            step("dve", lambda it: (lambda e: e.tensor_tensor(out=it[0], in0=it[0], in1=b_t, op=ALU.add)), lambda it: [it[1]] + list(cread), lambda it: [it[1]])

        def emit_ln(xt, r_x, g_t, b_t, scr, r_scr, cread=()):
            emit_ln_multi([(xt, r_x, scr, r_scr)], g_t, b_t, cread=cread)

        def ln_scratch(st, name):
            return (sbt(st, name + "_s6", [128, 2, 6], F32), sbt(st, name + "_mv", [128, 2], F32),
                    sbt(st, name + "_rs", [128, 1], F32), sbt(st, name + "_nm", [128, 1], F32))

        def _pass(st, l=(l if "l" in dir() else 0), last=(last if "last" in dir() else False)):
            g_t = sbt(st, "p0_g", [128, D], F32)
            b_t = sbt(st, "p0_b", [128, D], F32)
            r_gb = P.res("p0_gb")
            P.dma("sp", lambda e: e.dma_start(out=g_t[:], in_=ln_in_g[None, :].to_broadcast([128, D])), [], [r_gb], r_gb)
            P.dma("sp", lambda e: e.dma_start(out=b_t[:], in_=ln_in_b[None, :].to_broadcast([128, D])), [], [r_gb], r_gb)
            NB = 6
            xts = [sbt(st, f"p0_x{i}", [128, D], F32) for i in range(NB)]
            rxs = [P.res(f"p0_x{i}") for i in range(NB)]
            scrs = [ln_scratch(st, f"p0_l{i}") for i in range(NB)]
            rss = [P.res(f"p0_s{i}") for i in range(NB)]
            for i0 in range(0, NT, 3):
                grp = list(range(i0, min(i0 + 3, NT)))
                for i in grp:
                    k = i % NB
                    P.dma("sp", lambda e, xt=xts[k], i=i: e.dma_start(out=xt[:], in_=x_in[i * 128:(i + 1) * 128, :]), [], [rxs[k]], rxs[k])
                emit_ln_multi([(xts[i % NB][:], rxs[i % NB], scrs[i % NB], rss[i % NB]) for i in grp], g_t[:], b_t[:], cread=[r_gb])
                for i in grp:
                    k = i % NB
                    P.dma("sp", lambda e, xt=xts[k], i=i: e.dma_start(out=xa[i * 128:(i + 1) * 128, :], in_=xt[:]), [rxs[k]], [], rxs[k])
            P.barrier()
            P.release(rxs + [r_gb])

        with ExitStack() as _st:
            _pass(_st)
        for l in range(depth):
            last = (l == depth - 1)
            def _pass(st, l=(l if "l" in dir() else 0), last=(last if "last" in dir() else False)):
                wA = sbt(st, "wA", [128, 8, 1536], BF16)
                r_wA = P.res("wA")
                for kc in range(8):
                    P.dma("pool", lambda e, kc=kc: e.dma_start(out=wA[:, kc, :], in_=w_in[l, kc * 128:(kc + 1) * 128, 512:2048]),
                          [], [r_wA], r_wA)
                bcol = sbt(st, "A_bcol", [128, 32], F32)
                bv = sbt(st, "A_bv", [128, 512], F32)
                r_bA = P.res("A_b")
                P.dma("sp", lambda e: e.dma_start(out=bcol[:], in_=b_in[l].rearrange("(c p) -> p c", p=128),
                                                  allow_slow_non_contiguous=True), [], [r_bA], r_bA)
                P.dma("sp", lambda e: e.dma_start(out=bv[:], in_=b_in[l, 1024:1536][None, :].to_broadcast([128, 512])), [], [r_bA], r_bA)
                NB = 2
                xbf = [sbt(st, f"A_xbf{i}", [128, 4, D], BF16) for i in range(NB)]
                r_xbf = [P.res(f"A_xbf{i}") for i in range(NB)]
                xT = [sbt(st, f"A_xT{i}", [128, 8, 512], BF16) for i in range(NB)]
                r_xT = [P.res(f"A_xT{i}") for i in range(NB)]
                kTc = [sbt(st, f"A_kT{i}", [128, 4, 512], BF16) for i in range(NB)]
                r_kTc = [P.res(f"A_kT{i}") for i in range(NB)]
                pTc = [sbt(st, f"A_pT{i}", [128, 4, 512], BF16) for i in range(NB)]
                r_pTc = [P.res(f"A_pT{i}") for i in range(NB)]
                Vc = [sbt(st, f"A_V{i}", [128, 4, 512], BF16) for i in range(NB)]
                r_Vc = [P.res(f"A_V{i}") for i in range(NB)]
                psT = [pst(st, f"A_psT{i}", [128, 8, 128], BF16) for i in range(2)]
                r_psT = [P.res(f"A_psT{i}") for i in range(2)]
                psA = [pst(st, f"A_ps{i}", [128, 512], F32) for i in range(4)]
                r_psA = [P.res(f"A_ps{i}") for i in range(4)]
                nps = 0
                npt = 0
                for u in range(NU):
                    for c in range(4):
                        k = (u * 4 + c) % NB
                        for j in range(4):
                            col = u * 16 + c * 4 + j
                            P.dma("pool", lambda e, k=k, j=j, col=col: e.indirect_dma_start(
                                out=xbf[k][:, j, :], out_offset=None, in_=xa[:, :],
                                in_offset=bass.IndirectOffsetOnAxis(ap=kvidx[:, col:col + 1], axis=0)),
                                [r_const], [r_xbf[k]], r_xbf[k])
                        for j in range(4):
                            q = npt % 2
                            npt += 1
                            for kc in range(8):
                                P.op("pe", lambda e, q=q, k=k, j=j, kc=kc: e.transpose(
                                    out=psT[q][:, kc, :], in_=xbf[k][:, j, kc * 128:(kc + 1) * 128], identity=ident_bf[:]),
                                    [r_xbf[k], r_const], [r_psT[q]])
                            eng = "act" if j % 2 == 0 else "dve"
                            if eng == "act":
                                P.op("act", lambda e, q=q, k=k, j=j: e.copy(out=xT[k][:, :, j * 128:(j + 1) * 128], in_=psT[q][:]),
                                     [r_psT[q]], [r_xT[k]])
                            else:
                                P.op("dve", lambda e, q=q, k=k, j=j: e.tensor_copy(out=xT[k][:, :, j * 128:(j + 1) * 128], in_=psT[q][:]),
                                     [r_psT[q]], [r_xT[k]])
                        for m in range(4):
                            q = nps % 4
                            nps += 1
                            for kc in range(8):
                                P.op("pe", lambda e, q=q, k=k, m=m, kc=kc: e.matmul(
                                    psA[q][:], lhsT=wA[:, kc, m * 128:(m + 1) * 128], rhs=xT[k][:, kc, :],
                                    start=(kc == 0), stop=(kc == 7)), [r_wA, r_xT[k]], [r_psA[q]])
                            P.op("act", lambda e, q=q, k=k, m=m: e.activation(
                                out=kTc[k][:, m, :], in_=psA[q][:], func=AF.Identity, bias=bcol[:, 4 + m:5 + m], scale=1.0),
                                [r_psA[q], r_bA], [r_kTc[k]])
                        for m in range(4):
                            q = nps % 4
                            nps += 1
                            for kc in range(8):
                                P.op("pe", lambda e, q=q, k=k, m=m, kc=kc: e.matmul(
                                    psA[q][:], lhsT=wA[:, kc, 1024 + m * 128:1024 + (m + 1) * 128], rhs=xT[k][:, kc, :],
                                    start=(kc == 0), stop=(kc == 7)), [r_wA, r_xT[k]], [r_psA[q]])
                            P.op("act", lambda e, q=q, k=k, m=m: e.activation(
                                out=pTc[k][:, m, :], in_=psA[q][:], func=AF.Identity, bias=bcol[:, 12 + m:13 + m], scale=1.0),
                                [r_psA[q], r_bA], [r_pTc[k]])
                        for j in range(4):
                            q = nps % 4
                            nps += 1
                            for kc in range(8):
                                P.op("pe", lambda e, q=q, k=k, j=j, kc=kc: e.matmul(
                                    psA[q][:], lhsT=xT[k][:, kc, j * 128:(j + 1) * 128], rhs=wA[:, kc, 512:1024],
                                    start=(kc == 0), stop=(kc == 7)), [r_wA, r_xT[k]], [r_psA[q]])
                            P.op("dve", lambda e, q=q, k=k, j=j: e.tensor_tensor(
                                out=Vc[k][:, j, :], in0=psA[q][:], in1=bv[:], op=ALU.add),
                                [r_psA[q], r_bA], [r_Vc[k]])
                        P.dma("sp", lambda e, k=k, u=u, c=c: e.dma_start(
                            out=kvp[u, :, 0, :].rearrange("p (m t) -> p m t", m=4)[:, :, c * 512:(c + 1) * 512], in_=kTc[k][:]),
                            [r_kTc[k]], [], r_kTc[k])
                        P.dma("sp", lambda e, k=k, u=u, c=c: e.dma_start(
                            out=kvp[u, :, 2, :].rearrange("p (m t) -> p m t", m=4)[:, :, c * 512:(c + 1) * 512], in_=pTc[k][:]),
                            [r_pTc[k]], [], r_pTc[k])
                        P.dma("sp", lambda e, k=k, u=u, c=c: e.dma_start(
                            out=kvp[u, :, 1, c * 2048:(c + 1) * 2048], in_=Vc[k][:].rearrange("p j f -> p (j f)")),
                            [r_Vc[k]], [], r_Vc[k])
                P.barrier()
                P.release([r_wA, r_bA] + r_xbf + r_kTc + r_pTc + r_Vc)

            with ExitStack() as _st:
                _pass(_st)
            def _pass(st, l=(l if "l" in dir() else 0), last=(last if "last" in dir() else False)):
                CH = 256
                RPC = CH // 64
                TPC = CH // 128
                NCH = 2048 // CH
                EXT = CH + 16
                wB = sbt(st, "wB", [128, 8, 2560], BF16)
                woa = sbt(st, "woa", [128, 4, D], BF16)
                wop = sbt(st, "wop", [128, 4, D], BF16)
                wout = sbt(st, "wout", [128, 8, D], BF16)
                wpl = sbt(st, "wpl", [128, 4, 128], BF16)
                r_wB = P.res("wB")
                for kc in range(8):
                    P.dma("pool", lambda e, kc=kc: e.dma_start(out=wB[:, kc, 0:512], in_=w_in[l, kc * 128:(kc + 1) * 128, 0:512]), [], [r_wB], r_wB)
                    P.dma("pool", lambda e, kc=kc: e.dma_start(out=wB[:, kc, 512:2560], in_=w_in[l, kc * 128:(kc + 1) * 128, 2048:4096]), [], [r_wB], r_wB)
                    P.dma("pool", lambda e, kc=kc: e.dma_start(out=wout[:, kc, :], in_=w_out[l, kc * 128:(kc + 1) * 128, :]), [], [r_wB], r_wB)
                for kc in range(4):
                    P.dma("pool", lambda e, kc=kc: e.dma_start(out=woa[:, kc, :], in_=w_oa[l, kc * 128:(kc + 1) * 128, :]), [], [r_wB], r_wB)
                    P.dma("pool", lambda e, kc=kc: e.dma_start(out=wop[:, kc, :], in_=w_op[l, kc * 128:(kc + 1) * 128, :]), [], [r_wB], r_wB)
                    P.dma("pool", lambda e, kc=kc: e.dma_start(out=wpl[:, kc, :], in_=w_pool[l, kc, :, :]), [], [r_wB], r_wB)
                bcol = sbt(st, "B_bcol", [128, 32], F32)
                bq8 = sbt(st, "B_bq8", [128, 4], F32)
                psc = sbt(st, "B_psc", [128, 4], F32)
                bpc = sbt(st, "B_bpc", [128, 4], F32)
                bout_f = sbt(st, "B_boutf", [1, D], F32)
                bout = sbt(st, "B_bout", [1, D], BF16)
                g1 = sbt(st, "B_g1", [128, D], F32)
                b1t = sbt(st, "B_b1", [128, D], F32)
                invc = sbt(st, "B_invc", [128, 4, 16], F32)
                maskt = sbt(st, "B_mask", [128, 64], F32)
                Tb = sbt(st, "B_Tb", [128, 4, 15, 64], BF16)
                r_cB = P.res("B_c")
                P.dma("sp", lambda e: e.dma_start(out=bcol[:], in_=b_in[l].rearrange("(c p) -> p c", p=128),
                                                  allow_slow_non_contiguous=True), [], [r_cB], r_cB)
                P.dma("sp", lambda e: e.dma_start(out=psc[:], in_=pool_scale[l].rearrange("(c p) -> p c", p=128),
                                                  allow_slow_non_contiguous=True), [], [r_cB], r_cB)
                P.dma("sp", lambda e: e.dma_start(out=bpc[:], in_=b_pool[l].rearrange("c p -> p c"),
                                                  allow_slow_non_contiguous=True), [], [r_cB], r_cB)
                P.dma("sp", lambda e: e.dma_start(out=bout_f[:], in_=b_out[l][None, :]), [], [r_cB], r_cB)
                P.dma("sp", lambda e: e.dma_start(out=g1[:], in_=ln1_g[l][None, :].to_broadcast([128, D])), [], [r_cB], r_cB)
                P.dma("sp", lambda e: e.dma_start(out=b1t[:], in_=ln1_b[l][None, :].to_broadcast([128, D])), [], [r_cB], r_cB)
                P.dma("sp", lambda e: e.dma_start(out=invc[:], in_=c_invc), [], [r_cB], r_cB)
                P.dma("sp", lambda e: e.dma_start(out=maskt[:], in_=c_mask), [], [r_cB], r_cB)
                for hp in range(4):
                    P.dma("pool", lambda e, hp=hp: e.dma_start(out=Tb[:, hp, :, :].rearrange("p a b -> p (a b)"),
                                                               in_=rpbT[l, :, hp * 960:(hp + 1) * 960]), [], [r_cB], r_cB)
                P.op("dve", lambda e: e.tensor_tensor(
                    out=Tb[:].rearrange("p a b c -> p (a b) c"), in0=Tb[:].rearrange("p a b c -> p (a b) c"),
                    in1=maskt[:][:, None, :].to_broadcast([128, 60, 64]), op=ALU.add), [r_cB], [r_cB])
                P.op("dve", lambda e: e.tensor_scalar(out=bq8[:], in0=bcol[:, 0:4], scalar1=0.125, scalar2=None, op0=ALU.mult), [r_cB], [r_cB])
                P.op("dve", lambda e: e.tensor_tensor(out=bpc[:], in0=bpc[:], in1=psc[:], op=ALU.mult), [r_cB], [r_cB])
                P.op("dve", lambda e: e.tensor_copy(out=bout[:], in_=bout_f[:]), [r_cB], [r_cB])

                kT = sbt(st, "B_kT", [128, 4, 2048], BF16)
                Vt = sbt(st, "B_V", [128, 16, 512], BF16)
                r_kT, r_V = P.res("B_kT"), P.res("B_V")
                NXR = 4
                xres = [sbt(st, f"B_xres{i}", [128, D], F32) for i in range(NXR)]
                r_xres = [P.res(f"B_xres{i}") for i in range(NXR)]
                xbf = [sbt(st, f"B_xbf{i}", [128, D], BF16) for i in range(2)]
                r_xbf = [P.res(f"B_xbf{i}") for i in range(2)]

                def dbl(name, shape, dt):
                    return [sbt(st, f"{name}{i}", shape, dt) for i in range(2)], [P.res(f"{name}{i}") for i in range(2)]
                xT, r_xT = dbl("B_xT", [128, 8, CH], BF16)
                qT, r_qT = dbl("B_qT", [128, 4, CH], BF16)
                aT, r_aT = dbl("B_aT", [128, 4, CH], BF16)
                dT, r_dT = dbl("B_dT", [128, 4, CH], BF16)
                _tpmT = sbt(st, "B_pmT", [128, 4, CH], BF16)
                _rpmT = P.res("B_pmT")
                pmT, r_pmT = [_tpmT, _tpmT], [_rpmT, _rpmT]
                _m = sbt(st, "B_mixT", [128, 8, CH], BF16)
                _rm = P.res("B_mixT")
                mixT, r_mixT = [_m, _m], [_rm, _rm]
                _tpex = sbt(st, "B_pex", [128, 4, EXT], BF16)
                _rpex = P.res("B_pex")
                pex, r_pex = [_tpex, _tpex], [_rpex, _rpex]
                NA = 3
                Eb = [sbt(st, f"B_E{i}", [128, 512], BF16) for i in range(NA)]
                r_Eb = [P.res(f"B_E{i}") for i in range(NA)]
                rsum = [sbt(st, f"B_rs{i}", [128, 2], F32) for i in range(NA)]
                Pb = [sbt(st, f"B_P{i}", [128, 640], BF16) for i in range(NA)]
                r_Pb = [P.res(f"B_P{i}") for i in range(NA)]
                PT = [sbt(st, f"B_PT{i}", [128, 5, 128], BF16) for i in range(NA)]
                r_PT = [P.res(f"B_PT{i}") for i in range(NA)]
                pe_ = sbt(st, "B_pe", [128, EXT], F32)
                sA = sbt(st, "B_sA", [128, EXT], F32)
                sB_ = sbt(st, "B_sB", [128, EXT], F32)
                r_pl = P.res("B_pl")
                sg = [sbt(st, f"B_sg{i}", [128, CH], BF16) for i in range(2)]
                r_sg = [P.res(f"B_sg{i}") for i in range(2)]
                tm = [sbt(st, f"B_tm{i}", [128, CH], F32) for i in range(4)]
                r_tm = [P.res(f"B_tm{i}") for i in range(4)]
                lsc = [ln_scratch(st, f"B_l{i}") for i in range(NXR)]
                r_lsc = [P.res(f"B_ls{i}") for i in range(NXR)]
                psT = pst(st, "B_psT", [128, 8, 128], BF16)
                r_psT = P.res("B_psT")
                psP = [pst(st, f"B_psP{i}", [128, 512], F32) for i in range(3)]
                r_psP = [P.res(f"B_psP{i}") for i in range(3)]
                psS = [pst(st, f"B_psS{i}", [128, 512], F32) for i in range(2)]
                r_psS = [P.res(f"B_psS{i}") for i in range(2)]
                psPT = pst(st, "B_psPT", [128, 8, 128], BF16)
                r_psPT = P.res("B_psPT")
                psAT = pst(st, "B_psAT", [128, 512], F32)
                r_psAT = P.res("B_psAT")
                for i in range(NA):
                    P.op("dve", lambda e, i=i: e.memset(Pb[i][:], 0.0), [], [r_Pb[i]])
                npp = 0
                nat = 0
                nps = 0
                nxr = 0
                nxb = 0
                ncc = 0
                ntm = 0
                for u in range(NU):
                    P.dma("sp", lambda e, u=u: e.dma_start(out=kT[:].rearrange("p m t -> p (m t)"), in_=kvp[u, :, 0, :]), [], [r_kT], r_kT)
                    P.dma("sp", lambda e, u=u: e.dma_start(out=Vt[:].rearrange("p m t -> p (m t)"), in_=kvp[u, :, 1, :]), [], [r_V], r_V)
                    for c in range(NCH):
                        cc = ncc % 2
                        ncc += 1
                        for j in range(TPC):
                            col = u * 16 + c * TPC + j
                            kb = nxb % 2
                            nxb += 1
                            P.dma("pool", lambda e, kb=kb, col=col: e.indirect_dma_start(
                                out=xbf[kb][:, :], out_offset=None, in_=xa[:, :],
                                in_offset=bass.IndirectOffsetOnAxis(ap=kvidx[:, col:col + 1], axis=0)),
                                [r_const], [r_xbf[kb]], r_xbf[kb])
                            for kc in range(8):
                                P.op("pe", lambda e, kb=kb, kc=kc: e.transpose(
                                    out=psT[:, kc, :], in_=xbf[kb][:, kc * 128:(kc + 1) * 128], identity=ident_bf[:]),
                                    [r_xbf[kb], r_const], [r_psT])
                            P.op("dve", lambda e, j=j, cc=cc: e.tensor_copy(out=xT[cc][:, :, j * 128:(j + 1) * 128], in_=psT[:]),
                                 [r_psT], [r_xT[cc]])
                        xk = []
                        for j in range(TPC):
                            col = u * 16 + c * TPC + j
                            k = nxr % NXR
                            nxr += 1
                            xk.append(k)
                            P.dma("pool", lambda e, k=k, col=col: e.indirect_dma_start(
                                out=xres[k][:, :], out_offset=None, in_=xa[:, :],
                                in_offset=bass.IndirectOffsetOnAxis(ap=kvidx[:, col:col + 1], axis=0)),
                                [r_const], [r_xres[k]], r_xres[k])
                        lo_t = max(c * CH - 8, 0)
                        hi_t = min(c * CH + CH + 8, 2048)
                        eo = lo_t - (c * CH - 8)
                        nn = hi_t - lo_t
                        P.op("pool", lambda e, cc=cc: e.memset(pex[cc][:], 0.0), [], [r_pex[cc]])
                        P.dma("sp", lambda e, u=u, lo_t=lo_t, hi_t=hi_t, eo=eo, nn=nn, cc=cc: e.dma_start(
                            out=pex[cc][:, :, eo:eo + nn],
                            in_=kvp[u, :, 2, :].rearrange("p (m t) -> p m t", m=4)[:, :, lo_t:hi_t]), [], [r_pex[cc]], r_pex[cc])
                        for g in range(4):
                            wv = 2 ** (g + 1)
                            P.op("pool", lambda e, g=g, cc=cc: e.tensor_copy(out=pe_[:], in_=pex[cc][:, g, :]), [r_pex[cc]], [r_pl])
                            P.op("pool", lambda e: e.memset(sA[:], 0.0), [], [r_pl])
                            P.op("pool", lambda e: e.tensor_tensor(out=sA[:, 1:EXT], in0=pe_[:, 0:EXT - 1], in1=pe_[:, 1:EXT], op=ALU.add), [r_pl], [r_pl])
                            cur, oth = sA, sB_
                            for sh in (1, 2, 4)[:g]:
                                P.op("pool", lambda e, oth=oth: e.memset(oth[:], 0.0), [], [r_pl])
                                P.op("pool", lambda e, cur=cur, oth=oth, sh=sh: e.tensor_tensor(
                                    out=oth[:, sh:EXT - sh], in0=cur[:, 0:EXT - 2 * sh], in1=cur[:, 2 * sh:EXT], op=ALU.add), [r_pl], [r_pl])
                                cur, oth = oth, cur
                            P.op("pool", lambda e, cur=cur, oth=oth, wv=wv: e.tensor_scalar(
                                out=oth[:, 8:8 + CH], in0=cur[:, 8:8 + CH], scalar1=1.0 / wv, scalar2=0.0, op0=ALU.mult, op1=ALU.add), [r_pl], [r_pl])
                            if c == 0:
                                P.op("pool", lambda e, cur=cur, oth=oth, g=g: e.tensor_tensor(
                                    out=oth[:, 8:16], in0=cur[:, 8:16], in1=invc[:, g, 0:8], op=ALU.mult), [r_pl, r_cB], [r_pl])
                            if c == NCH - 1:
                                P.op("pool", lambda e, cur=cur, oth=oth, g=g: e.tensor_tensor(
                                    out=oth[:, CH:CH + 8], in0=cur[:, CH:CH + 8], in1=invc[:, g, 8:16], op=ALU.mult), [r_pl, r_cB], [r_pl])
                            P.op("pool", lambda e, oth=oth, g=g, cc=cc: e.tensor_tensor(
                                out=dT[cc][:, g, :], in0=oth[:, 8:8 + CH], in1=pe_[:, 8:8 + CH], op=ALU.subtract), [r_pl], [r_dT[cc]])
                        for m in range(4):
                            q = npp % 3
                            npp += 1
                            for kc in range(8):
                                P.op("pe", lambda e, q=q, m=m, kc=kc, cc=cc: e.matmul(
                                    psP[q][:, 0:CH], lhsT=wB[:, kc, m * 128:(m + 1) * 128], rhs=xT[cc][:, kc, :],
                                    start=(kc == 0), stop=(kc == 7)), [r_wB, r_xT[cc]], [r_psP[q]])
                            P.op("act", lambda e, q=q, m=m, cc=cc: e.activation(
                                out=qT[cc][:, m, :], in_=psP[q][:, 0:CH], func=AF.Identity, bias=bq8[:, m:m + 1], scale=0.125),
                                [r_psP[q], r_cB], [r_qT[cc]])
                        for hp in range(4):
                            for r8 in range(RPC):
                                ql = c * RPC + r8
                                ws = min(max(ql - 4, 0), 24)
                                dr0 = ws - ql + 7
                                k = nat % NA
                                nat += 1
                                ks = nps % 2
                                nps += 1
                                for hh in range(2):
                                    lo, hi = hh * 64, (hh + 1) * 64
                                    P.op("pe", lambda e, ks=ks, hp=hp, r8=r8, ws=ws, lo=lo, hi=hi, cc=cc: e.matmul(
                                        psS[ks][lo:hi, :], lhsT=qT[cc][lo:hi, hp, r8 * 64:(r8 + 1) * 64],
                                        rhs=kT[lo:hi, hp, ws * 64:ws * 64 + 512], start=True, stop=False),
                                        [r_qT[cc], r_kT], [r_psS[ks]])
                                P.op("pe", lambda e, ks=ks, hp=hp, dr0=dr0: e.matmul(
                                    psS[ks][:], lhsT=ident_bf[:], rhs=Tb[:, hp, dr0:dr0 + 8, :].rearrange("p a b -> p (a b)"),
                                    start=False, stop=True), [r_cB, r_const], [r_psS[ks]])
                                P.op("act", lambda e, k=k, ks=ks: e.activation(
                                    out=Eb[k][:], in_=psS[ks][:], func=AF.Exp, accum_out=rsum[k][:, 0:1]),
                                    [r_psS[ks]], [r_Eb[k]])
                                P.op("dve", lambda e, k=k: e.reciprocal(out=rsum[k][:, 1:2], in_=rsum[k][:, 0:1]),
                                     [r_Eb[k]], [r_Eb[k]])
                                P.op("dve", lambda e, k=k: e.tensor_scalar(
                                    out=Pb[k][:, 64:576], in0=Eb[k][:], scalar1=rsum[k][:, 1:2], scalar2=None, op0=ALU.mult),
                                    [r_Eb[k]], [r_Pb[k]])
                                if ws % 2 == 0:
                                    nch, off, vt0 = 4, 64, ws // 2
                                else:
                                    nch, off, vt0 = 5, 0, (ws - 1) // 2
                                for ch in range(nch):
                                    P.op("pe", lambda e, k=k, ch=ch, off=off: e.transpose(
                                        out=psPT[:, ch, :], in_=Pb[k][:, off + ch * 128:off + (ch + 1) * 128], identity=ident_bf[:]),
                                        [r_Pb[k], r_const], [r_psPT])
                                P.op("act", lambda e, k=k, nch=nch: e.copy(out=PT[k][:, 0:nch, :], in_=psPT[:, 0:nch, :]),
                                     [r_psPT], [r_PT[k]])
                                for hh in range(2):
                                    lo, hi = hh * 64, (hh + 1) * 64
                                    hcol = (2 * hp + hh) * 64
                                    for ch in range(nch):
                                        P.op("pe", lambda e, k=k, ch=ch, lo=lo, hi=hi, hcol=hcol, r8=r8, vt0=vt0, nch=nch: e.matmul(
                                            psAT[lo:hi, r8 * 64:(r8 + 1) * 64], lhsT=Vt[:, vt0 + ch, hcol:hcol + 64],
                                            rhs=PT[k][:, ch, lo:hi], start=(ch == 0), stop=(ch == nch - 1)),
                                            [r_V, r_PT[k]], [r_psAT])
                            P.op("dve", lambda e, hp=hp, cc=cc: e.tensor_copy(out=aT[cc][:, hp, :], in_=psAT[:, 0:CH]), [r_psAT], [r_aT[cc]])
                        for g in range(4):
                            q = npp % 3
                            npp += 1
                            P.op("pe", lambda e, q=q, g=g, cc=cc: e.matmul(psP[q][:, 0:CH], lhsT=wpl[:, g, :], rhs=dT[cc][:, g, :], start=True, stop=True),
                                 [r_wB, r_dT[cc]], [r_psP[q]])
                            P.op("act", lambda e, q=q, g=g, cc=cc: e.activation(
                                out=pmT[cc][:, g, :], in_=psP[q][:, 0:CH], func=AF.Identity, bias=bpc[:, g:g + 1], scale=psc[:, g:g + 1]),
                                [r_psP[q], r_cB], [r_pmT[cc]])
                        for m in range(8):
                            tk = []
                            for br in range(2):
                                q = npp % 3
                                npp += 1
                                wc = 512 + br * 1024 + m * 128
                                for kc in range(8):
                                    P.op("pe", lambda e, q=q, wc=wc, kc=kc, cc=cc: e.matmul(
                                        psP[q][:, 0:CH], lhsT=wB[:, kc, wc:wc + 128], rhs=xT[cc][:, kc, :], start=(kc == 0), stop=(kc == 7)),
                                        [r_wB, r_xT[cc]], [r_psP[q]])
                                bc = 16 + br * 8 + m
                                P.op("act", lambda e, q=q, br=br, bc=bc: e.activation(
                                    out=sg[br][:], in_=psP[q][:, 0:CH], func=AF.Sigmoid, bias=bcol[:, bc:bc + 1], scale=1.0),
                                    [r_psP[q], r_cB], [r_sg[br]])
                                q = npp % 3
                                npp += 1
                                wsrc, asrc, r_a = (woa, aT[cc], r_aT[cc]) if br == 0 else (wop, pmT[cc], r_pmT[cc])
                                for kc in range(4):
                                    P.op("pe", lambda e, q=q, wsrc=wsrc, asrc=asrc, m=m, kc=kc: e.matmul(
                                        psP[q][:, 0:CH], lhsT=wsrc[:, kc, m * 128:(m + 1) * 128], rhs=asrc[:, kc, :], start=(kc == 0), stop=(kc == 3)),
                                        [r_wB, r_a], [r_psP[q]])
                                t_ = ntm % 4
                                ntm += 1
                                tk.append(t_)
                                P.op("dve", lambda e, q=q, br=br, t_=t_: e.tensor_tensor(out=tm[t_][:], in0=psP[q][:, 0:CH], in1=sg[br][:], op=ALU.mult),
                                     [r_psP[q], r_sg[br]], [r_tm[t_]])
                            P.op("pool", lambda e, m=m, tk=tk, cc=cc: e.tensor_tensor(out=mixT[cc][:, m, :], in0=tm[tk[0]][:], in1=tm[tk[1]][:], op=ALU.add),
                                 [r_tm[tk[0]], r_tm[tk[1]]], [r_mixT[cc]])
                        for j in range(TPC):
                            k = xk[j]
                            for h in range(2):
                                q = npp % 3
                                npp += 1
                                for kc in range(8):
                                    P.op("pe", lambda e, q=q, j=j, h=h, kc=kc, cc=cc: e.matmul(
                                        psP[q][:], lhsT=mixT[cc][:, kc, j * 128:(j + 1) * 128], rhs=wout[:, kc, h * 512:(h + 1) * 512],
                                        start=(kc == 0), stop=False), [r_wB, r_mixT[cc]], [r_psP[q]])
                                P.op("pe", lambda e, q=q, h=h: e.matmul(
                                    psP[q][:], lhsT=ones[0:1, :], rhs=bout[0:1, h * 512:(h + 1) * 512], start=False, stop=True),
                                    [r_cB, r_const], [r_psP[q]])
                                P.op("dve", lambda e, q=q, k=k, h=h: e.scalar_tensor_tensor(
                                    out=xres[k][:, h * 512:(h + 1) * 512], in0=xres[k][:, h * 512:(h + 1) * 512], scalar=ALPHA,
                                    in1=psP[q][:], op0=ALU.mult, op1=ALU.add), [r_psP[q], r_xres[k]], [r_xres[k]])
                        emit_ln_multi([(xres[k][:], r_xres[k], lsc[k], r_lsc[k]) for k in xk], g1[:], b1t[:], cread=[r_cB])
                        for j in range(TPC):
                            col = u * 16 + c * TPC + j
                            k = xk[j]
                            P.dma("pool", lambda e, k=k, col=col: e.indirect_dma_start(
                                out=x1b[:, :], out_offset=bass.IndirectOffsetOnAxis(ap=outidx[:, col:col + 1], axis=0),
                                in_=xres[k][:, :], in_offset=None), [r_xres[k], r_const], [], r_xres[k])
                P.barrier()
                P.release([r_wB, r_cB, r_kT, r_V, r_pex[0]] + r_xbf + r_xres)

            with ExitStack() as _st:
                _pass(_st)
            def _pass(st, l=(l if "l" in dir() else 0), last=(last if "last" in dir() else False)):
                wr = sbt(st, "R_wr", [128, 8, NE], F32)
                brt = sbt(st, "R_br", [128, NE], F32)
                bst = sbt(st, "R_bst", [128, NBLK], F32)
                rowc = sbt(st, "R_rowc", [128, 16], F32)
                r_cR = P.res("R_c")
                P.dma("sp", lambda e: e.dma_start(out=wr[:], in_=w_router.rearrange("(k p) n -> p k n", p=128)), [], [r_cR], r_cR)
                P.dma("sp", lambda e: e.dma_start(out=brt[:], in_=b_router[None, :].to_broadcast([128, NE])), [], [r_cR], r_cR)
                P.dma("sp", lambda e: e.dma_start(out=bst[:], in_=c_bstart), [], [r_cR], r_cR)
                P.dma("sp", lambda e: e.dma_start(out=rowc[:], in_=c_rowc), [], [r_cR], r_cR)
                A0 = sbt(st, "R_A0", [128, NT, NE], F32)
                A1 = sbt(st, "R_A1", [128, NT, NE], F32)
                POS = sbt(st, "R_POS", [128, NT, NE], F32)
                r_A = P.res("R_A")
                base = sbt(st, "R_base", [128, NE], F32)
                r_base = P.res("R_base")
                P.op("dve", lambda e: e.memset(base[:], 0.0), [], [r_base])
                xt = [sbt(st, f"R_x{i}", [128, D], F32) for i in range(2)]
                r_xt = [P.res(f"R_x{i}") for i in range(2)]
                xT32 = [sbt(st, f"R_xT{i}", [128, 8, 128], F32) for i in range(2)]
                r_xT32 = [P.res(f"R_xT{i}") for i in range(2)]
                zt = [sbt(st, f"R_z{i}", [128, 16 * 8], F32) for i in range(2)]
                r_zt = [P.res(f"R_z{i}") for i in range(2)]
                sm = [sbt(st, f"R_sm{i}", [128, 32], F32) for i in range(2)]
                Abf = [sbt(st, f"R_Ab{i}", [128, NE], BF16) for i in range(2)]
                psX = [pst(st, f"R_psX{i}", [128, 8, 128], F32) for i in range(1)]
                r_psX = [P.res(f"R_psX{i}") for i in range(1)]
                psL_ = [pst(st, f"R_psL{i}", [128, 512], F32) for i in range(2)]
                psL = [t[:, 0:NE] for t in psL_]
                r_psL = [P.res(f"R_psL{i}") for i in range(2)]
                psC_ = [pst(st, f"R_psC{i}", [128, 512], F32) for i in range(2)]
                psC = [t[:, 0:2 * NE].rearrange("p (a b) -> p a b", a=2) for t in psC_]
                r_psC = [P.res(f"R_psC{i}") for i in range(2)]
                for i in range(NT):
                    k = i % 2
                    P.dma("sp", lambda e, k=k, i=i: e.dma_start(out=xt[k][:], in_=x1b[i * 128:(i + 1) * 128, :]), [], [r_xt[k]], r_xt[k])
                    for kc in range(8):
                        P.op("pe", lambda e, k=k, kc=kc: e.transpose(out=psX[0][:, kc, :], in_=xt[k][:, kc * 128:(kc + 1) * 128], identity=ident_f[:]),
                             [r_xt[k], r_const], [r_psX[0]])
                    P.op("act", lambda e, k=k: e.copy(out=xT32[k][:, 0:4, :], in_=psX[0][:, 0:4, :]), [r_psX[0]], [r_xT32[k]])
                    P.op("dve", lambda e, k=k: e.tensor_copy(out=xT32[k][:, 4:8, :], in_=psX[0][:, 4:8, :]), [r_psX[0]], [r_xT32[k]])
                    for kc in range(8):
                        P.op("pe", lambda e, k=k, kc=kc: e.matmul(psL[k], lhsT=xT32[k][:, kc, :], rhs=wr[:, kc, :], start=(kc == 0), stop=(kc == 7)),
                             [r_xT32[k], r_cR], [r_psL[k]])
                    z = zt[k]
                    Z = lambda a, z=z: z[:, a * 16:(a + 1) * 16]
                    Z4 = lambda a, z=z: z[:, a * 16:(a + 1) * 16].rearrange("p (g j) -> p g j", g=4)
                    s = sm[k]
                    rz = r_zt[k]
                    P.op("dve", lambda e, k=k, Z=Z: e.tensor_tensor(out=Z(0), in0=psL[k], in1=brt[:], op=ALU.add), [r_psL[k], r_cR], [rz])
                    P.op("dve", lambda e, Z=Z, s=s: e.tensor_reduce(out=s[:, 0:1], in_=Z(0), axis=AX.X, op=ALU.max, negate=True), [rz], [rz])
                    P.op("act", lambda e, Z=Z, s=s: e.activation(out=Z(1), in_=Z(0), func=AF.Exp, bias=s[:, 0:1], scale=1.0), [rz], [rz])
                    P.op("dve", lambda e, Z4=Z4, s=s: e.tensor_reduce(out=s[:, 4:8], in_=Z4(1), axis=AX.X, op=ALU.max), [rz], [rz])
                    P.op("dve", lambda e, Z4=Z4, s=s: e.tensor_tensor(out=Z4(2), in0=Z4(1), in1=s[:, 4:8][:, :, None].to_broadcast([128, 4, 4]), op=ALU.is_equal), [rz], [rz])
                    P.op("dve", lambda e, Z=Z: e.scalar_tensor_tensor(out=Z(3), in0=Z(2), scalar=-2.0, in1=Z(1), op0=ALU.mult, op1=ALU.add), [rz], [rz])
                    P.op("dve", lambda e, Z4=Z4, s=s: e.tensor_reduce(out=s[:, 8:12], in_=Z4(3), axis=AX.X, op=ALU.max), [rz], [rz])
                    P.op("dve", lambda e, Z4=Z4, s=s: e.tensor_tensor(out=Z4(4), in0=Z4(3), in1=s[:, 8:12][:, :, None].to_broadcast([128, 4, 4]), op=ALU.is_equal), [rz], [rz])
                    P.op("dve", lambda e, s=s: e.tensor_tensor(out=s[:, 12:16], in0=s[:, 4:8], in1=s[:, 8:12], op=ALU.add), [rz], [rz])
                    P.op("dve", lambda e, s=s: e.tensor_reduce(out=s[:, 1:2], in_=s[:, 12:16], axis=AX.X, op=ALU.max), [rz], [rz])
                    P.op("dve", lambda e, s=s: e.tensor_scalar(out=s[:, 16:20], in0=s[:, 12:16], scalar1=s[:, 1:2], scalar2=None, op0=ALU.is_equal), [rz], [rz])
                    P.op("dve", lambda e, Z4=Z4, s=s, i=i: e.tensor_tensor(out=A0[:, i, :].rearrange("p (g j) -> p g j", g=4), in0=Z4(2),
                                                                             in1=s[:, 16:20][:, :, None].to_broadcast([128, 4, 4]), op=ALU.mult), [rz], [r_A])
                    P.op("dve", lambda e, Z4=Z4, s=s, i=i: e.tensor_tensor(out=A1[:, i, :].rearrange("p (g j) -> p g j", g=4), in0=Z4(4),
                                                                             in1=s[:, 16:20][:, :, None].to_broadcast([128, 4, 4]), op=ALU.mult), [rz], [r_A])
                    P.op("dve", lambda e, s=s: e.tensor_tensor(out=s[:, 20:24], in0=s[:, 16:20], in1=s[:, 4:8], op=ALU.mult), [rz], [rz])
                    P.op("dve", lambda e, s=s: e.tensor_tensor(out=s[:, 24:28], in0=s[:, 16:20], in1=s[:, 8:12], op=ALU.mult), [rz], [rz])
                    P.op("dve", lambda e, s=s: e.tensor_reduce(out=s[:, 28:30], in_=s[:, 20:28].rearrange("p (a b) -> p a b", a=2), axis=AX.X, op=ALU.add), [rz], [rz])
                    P.op("dve", lambda e, s=s: e.tensor_reduce(out=s[:, 30:31], in_=s[:, 28:30], axis=AX.X, op=ALU.add), [rz], [rz])
                    P.op("dve", lambda e, s=s: e.reciprocal(out=s[:, 31:32], in_=s[:, 30:31]), [rz], [rz])
                    P.op("dve", lambda e, s=s, i=i: e.tensor_scalar(out=gates[:, i, :], in0=s[:, 28:30], scalar1=s[:, 31:32], scalar2=None, op0=ALU.mult), [rz], [r_gates])
                    P.op("dve", lambda e, k=k, i=i: e.tensor_tensor(out=Abf[k][:], in0=A0[:, i, :], in1=A1[:, i, :], op=ALU.add), [r_A], [rz])
                    P.op("pe", lambda e, k=k: e.matmul(psC[k][:, 0, :], lhsT=tri[:], rhs=Abf[k][:], start=True, stop=True), [rz, r_const], [r_psC[k]])
                    P.op("pe", lambda e, k=k: e.matmul(psC[k][:, 1, :], lhsT=ones[:], rhs=Abf[k][:], start=True, stop=True), [rz, r_const], [r_psC[k]])
                    P.op("dve", lambda e, k=k, i=i: e.tensor_tensor(out=POS[:, i, :], in0=psC[k][:, 0, :], in1=base[:], op=ALU.add), [r_psC[k], r_base], [r_A])
                    P.op("dve", lambda e, k=k: e.tensor_tensor(out=base[:], in0=psC[k][:, 1, :], in1=base[:], op=ALU.add), [r_psC[k], r_base], [r_base])
                ci = sbt(st, "R_ci", [128, NE], I32)
                pf = sbt(st, "R_pf", [128, 4, NE], F32)
                r_pf = P.res("R_pf")
                P.op("dve", lambda e: e.tensor_scalar(out=ci[:], in0=base[:], scalar1=float(BLK - 1), scalar2=None, op0=ALU.add), [r_base], [r_pf])
                P.op("dve", lambda e: e.tensor_scalar(out=ci[:], in0=ci[:], scalar1=9, scalar2=9, op0=ALU.arith_shift_right, op1=ALU.arith_shift_left), [r_pf], [r_pf])
                P.op("dve", lambda e: e.tensor_copy(out=pf[:, 0, :], in_=ci[:]), [r_pf], [r_pf])
                P.op("dve", lambda e: e.tensor_copy(out=pf[:, 1, :], in_=pf[:, 0, :]), [r_pf], [r_pf])
                for sh in (1, 2, 4, 8):
                    P.op("dve", lambda e: e.tensor_copy(out=pf[:, 2, :], in_=pf[:, 1, :]), [r_pf], [r_pf])
                    P.op("dve", lambda e, sh=sh: e.tensor_tensor(out=pf[:, 1, sh:NE], in0=pf[:, 2, sh:NE], in1=pf[:, 2, 0:NE - sh], op=ALU.add), [r_pf], [r_pf])
                P.op("dve", lambda e: e.tensor_tensor(out=pf[:, 3, :], in0=pf[:, 1, :], in1=pf[:, 0, :], op=ALU.subtract), [r_pf], [r_pf])
                P.op("dve", lambda e: e.tensor_tensor(out=POS[:], in0=POS[:], in1=pf[:, 3:4, :].to_broadcast([128, NT, NE]), op=ALU.add), [r_pf, r_A], [r_A])
                sf = sbt(st, "R_sf", [128, NT], F32)
                for Ak, sl in ((A0, slot0), (A1, slot1)):
                    P.op("dve", lambda e, Ak=Ak: e.tensor_tensor(out=Ak[:], in0=Ak[:], in1=POS[:], op=ALU.mult), [r_A], [r_A])
                    P.op("dve", lambda e, Ak=Ak: e.tensor_reduce(out=sf[:], in_=Ak[:], axis=AX.X, op=ALU.add), [r_A], [r_pf])
                    P.op("dve", lambda e, sl=sl: e.tensor_copy(out=sl[:], in_=sf[:]), [r_pf], [r_slots])
                eb = sbt(st, "R_eb", [128, NBLK], F32)
                tmpb = sbt(st, "R_tmpb", [128, NBLK], F32)
                r_eb = P.res("R_eb")
                P.op("dve", lambda e: e.memset(eb[:], 0.0), [], [r_eb])
                for ex in range(NE):
                    P.op("dve", lambda e, ex=ex: e.tensor_scalar(out=tmpb[:], in0=bst[:], scalar1=pf[:, 1, ex:ex + 1], scalar2=None, op0=ALU.is_ge), [r_pf, r_cR], [r_eb])
                    P.op("dve", lambda e: e.tensor_tensor(out=eb[:], in0=eb[:], in1=tmpb[:], op=ALU.add), [r_eb], [r_eb])
                P.op("dve", lambda e: e.tensor_scalar(out=eb[:], in0=eb[:], scalar1=float(NE - 1), scalar2=None, op0=ALU.min), [r_eb], [r_eb])
                chg = sbt(st, "R_chg", [128, NBLK], F32)
                ebo1 = sbt(st, "R_ebo1", [128, NBLK], F32)
                ebo2 = sbt(st, "R_ebo2", [128, NBLK], F32)
                P.op("dve", lambda e: e.memset(chg[:], 0.0), [], [r_eb])
                P.op("dve", lambda e: e.tensor_tensor(out=chg[:, 1:NBLK], in0=eb[:, 1:NBLK], in1=eb[:, 0:NBLK - 1], op=ALU.is_equal), [r_eb], [r_eb])
                P.op("dve", lambda e: e.tensor_scalar(out=chg[:], in0=chg[:], scalar1=float(2 ** 30), scalar2=None, op0=ALU.mult), [r_eb], [r_eb])
                P.op("dve", lambda e: e.scalar_tensor_tensor(out=ebo1[:], in0=eb[:], scalar=float(D), in1=chg[:], op0=ALU.mult, op1=ALU.add), [r_eb], [r_eb])
                P.op("dve", lambda e: e.scalar_tensor_tensor(out=ebo2[:], in0=eb[:], scalar=float(DFF), in1=chg[:], op0=ALU.mult, op1=ALU.add), [r_eb], [r_eb])
                for kc in range(8):
                    P.op("dve", lambda e, kc=kc: e.tensor_scalar(out=widx1[:, :, kc], in0=ebo1[:], scalar1=rowc[:, kc:kc + 1], scalar2=None, op0=ALU.add), [r_eb, r_cR], [r_widx])
                for kc in range(16):
                    P.op("dve", lambda e, kc=kc: e.tensor_scalar(out=widx2[:, :, kc], in0=ebo2[:], scalar1=rowc[:, kc:kc + 1], scalar2=None, op0=ALU.add), [r_eb, r_cR], [r_widx])
                ebl = sbt(st, "R_ebl", [128, NBLK], F32)
                P.op("dve", lambda e: e.tensor_scalar(out=ebl[:], in0=eb[:], scalar1=float(l * NE), scalar2=None, op0=ALU.add), [r_eb], [r_eb])
                P.op("dve", lambda e: e.tensor_scalar(out=bidx1[:], in0=ebl[:], scalar1=16.0, scalar2=rowc[:, 0:1], op0=ALU.mult, op1=ALU.add), [r_eb, r_cR], [r_widx])
                P.op("dve", lambda e: e.tensor_copy(out=bidx2[:], in_=ebl[:]), [r_eb], [r_widx])
                zi = sbt(st, "R_zi", [128, NSLOT // 128], I32)
                r_zi = P.res("R_zi")
                P.op("dve", lambda e: e.memset(zi[:], 0), [], [r_zi])
                P.dma("sp", lambda e: e.dma_start(out=table.rearrange("(p f) o -> p (f o)", p=128), in_=zi[:]), [r_zi], [], r_zi)
                P.barrier()
                for i in range(NT):
                    for sl in (slot0, slot1):
                        P.dma("pool", lambda e, sl=sl, i=i: e.indirect_dma_start(
                            out=table[:, :], out_offset=bass.IndirectOffsetOnAxis(ap=sl[:, i:i + 1], axis=0),
                            in_=tokid[:, i:i + 1], in_offset=None), [r_slots, r_const], [], r_slots)
                P.barrier()
                P.release(r_xt + [r_cR, r_zi])

            with ExitStack() as _st:
                _pass(_st)
            def _pass(st, l=(l if "l" in dir() else 0), last=(last if "last" in dir() else False)):
                w1s = sbt(st, "M_w1", [128, 8, DFF], BF16)
                w2s = sbt(st, "M_w2", [128, 16, D], BF16)
                r_w1s, r_w2s = P.res("M_w1"), P.res("M_w2")
                tix = [sbt(st, f"M_tix{i}", [128, 4], I32) for i in range(2)]
                r_tix = [P.res(f"M_tix{i}") for i in range(2)]
                xg = [sbt(st, f"M_xg{i}", [128, 4, D], BF16) for i in range(2)]
                r_xg = [P.res(f"M_xg{i}") for i in range(2)]
                xT = [sbt(st, f"M_xT{i}", [128, 8, 512], BF16) for i in range(2)]
                r_xT = [P.res(f"M_xT{i}") for i in range(2)]
                hT = sbt(st, "M_hT", [128, 16, 512], BF16)
                r_hT = P.res("M_hT")
                b1r = [sbt(st, f"M_b1r{i}", [16, 128], F32) for i in range(2)]
                r_b1r = [P.res(f"M_b1r{i}") for i in range(2)]
                b1c = [sbt(st, f"M_b1c{i}", [128, 16], F32) for i in range(2)]
                r_b1c = [P.res(f"M_b1c{i}") for i in range(2)]
                b2b = [sbt(st, f"M_b2b{i}", [128, D], F32) for i in range(2)]
                r_b2b = [P.res(f"M_b2b{i}") for i in range(2)]
                ysb = [sbt(st, f"M_y{i}", [128, D], F32) for i in range(3)]
                r_ysb = [P.res(f"M_y{i}") for i in range(3)]
                psT = [pst(st, f"M_psT{i}", [128, 8, 128], BF16) for i in range(2)]
                r_psT = [P.res(f"M_psT{i}") for i in range(2)]
                psH = [pst(st, f"M_psH{i}", [128, 512], F32) for i in range(3)]
                r_psH = [P.res(f"M_psH{i}") for i in range(3)]
                psY = [pst(st, f"M_psY{i}", [128, 512], F32) for i in range(2)]
                r_psY = [P.res(f"M_psY{i}") for i in range(2)]
                psB_ = pst(st, "M_psB", [128, 512], F32)
                psB = psB_[:, 0:16]
                r_psB = P.res("M_psB")
                npt = 0
                nph = 0
                npy = 0
                ny = 0
                tview = table.rearrange("(b p j) o -> b p (j o)", p=128, j=4)
                yview = ybuf.rearrange("(b p j) d -> b j p d", p=128, j=4)
                for b in range(NBLK):
                    k = b % 2
                    P.dma("sp", lambda e, k=k, b=b: e.dma_start(out=tix[k][:], in_=tview[b]), [], [r_tix[k]], r_tix[k])
                    for j in range(4):
                        P.dma("pool", lambda e, k=k, j=j: e.indirect_dma_start(
                            out=xg[k][:, j, :], out_offset=None, in_=x1b[:, :],
                            in_offset=bass.IndirectOffsetOnAxis(ap=tix[k][:, j:j + 1], axis=0)), [r_tix[k]], [r_xg[k]], r_xg[k])
                    for kc in range(8):
                        P.dma("pool", lambda e, b=b, kc=kc: e.indirect_dma_start(
                            out=w1s[:, kc, :], out_offset=None, in_=w1L[l][:, :],
                            in_offset=bass.IndirectOffsetOnAxis(ap=widx1[:, b, kc:kc + 1], axis=0),
                            bounds_check=breg(e, NE * D - 1), oob_is_err=False), [r_widx], [r_w1s], r_w1s)
                    P.dma("pool", lambda e, k=k, b=b: e.indirect_dma_start(
                        out=b1r[k][:, :], out_offset=None, in_=b1_rows[:, :],
                        in_offset=bass.IndirectOffsetOnAxis(ap=bidx1[0:16, b:b + 1], axis=0)), [r_widx], [r_b1r[k]], r_b1r[k])
                    P.dma("pool", lambda e, k=k, b=b: e.indirect_dma_start(
                        out=b2b[k][:, :], out_offset=None, in_=b2_rows[:, :],
                        in_offset=bass.IndirectOffsetOnAxis(ap=bidx2[:, b:b + 1], axis=0)), [r_widx], [r_b2b[k]], r_b2b[k])
                    for j in range(4):
                        q = npt % 2
                        npt += 1
                        for kc in range(8):
                            P.op("pe", lambda e, q=q, k=k, j=j, kc=kc: e.transpose(
                                out=psT[q][:, kc, :], in_=xg[k][:, j, kc * 128:(kc + 1) * 128], identity=ident_bf[:]),
                                [r_xg[k], r_const], [r_psT[q]])
                        P.op("dve", lambda e, q=q, k=k, j=j: e.tensor_copy(out=xT[k][:, :, j * 128:(j + 1) * 128], in_=psT[q][:]),
                             [r_psT[q]], [r_xT[k]])
                    P.op("pe", lambda e, k=k: e.transpose(out=psB, in_=b1r[k][:, :], identity=ident_f[0:16, 0:16]),
                         [r_b1r[k], r_const], [r_psB])
                    P.op("dve", lambda e, k=k: e.tensor_copy(out=b1c[k][:], in_=psB), [r_psB], [r_b1c[k]])
                    for m in range(16):
                        q = nph % 3
                        nph += 1
                        for kc in range(8):
                            P.op("pe", lambda e, q=q, k=k, m=m, kc=kc: e.matmul(
                                psH[q][:], lhsT=w1s[:, kc, m * 128:(m + 1) * 128], rhs=xT[k][:, kc, :], start=(kc == 0), stop=(kc == 7)),
                                [r_w1s, r_xT[k]], [r_psH[q]])
                        P.op("act", lambda e, q=q, k=k, m=m: e.activation(
                            out=hT[:, m, :], in_=psH[q][:], func=AF.Gelu, bias=b1c[k][:, m:m + 1], scale=1.0),
                            [r_psH[q], r_b1c[k]], [r_hT])
                    for kc in range(16):
                        P.dma("pool", lambda e, b=b, kc=kc: e.indirect_dma_start(
                            out=w2s[:, kc, :], out_offset=None, in_=w2L[l][:, :],
                            in_offset=bass.IndirectOffsetOnAxis(ap=widx2[:, b, kc:kc + 1], axis=0),
                            bounds_check=breg(e, NE * DFF - 1), oob_is_err=False), [r_widx], [r_w2s], r_w2s)
                    for j in range(4):
                        yk = ny % 3
                        ny += 1
                        for h in range(2):
                            q = npy % 2
                            npy += 1
                            for kc in range(16):
                                P.op("pe", lambda e, q=q, j=j, h=h, kc=kc: e.matmul(
                                    psY[q][:], lhsT=hT[:, kc, j * 128:(j + 1) * 128], rhs=w2s[:, kc, h * 512:(h + 1) * 512],
                                    start=(kc == 0), stop=(kc == 15)), [r_w2s, r_hT], [r_psY[q]])
                            P.op("dve", lambda e, q=q, yk=yk, k=k, h=h: e.tensor_tensor(
                                out=ysb[yk][:, h * 512:(h + 1) * 512], in0=psY[q][:], in1=b2b[k][:, h * 512:(h + 1) * 512], op=ALU.add),
                                [r_psY[q], r_b2b[k]], [r_ysb[yk]])
                        P.dma("sp", lambda e, yk=yk, b=b, j=j: e.dma_start(out=yview[b, j], in_=ysb[yk][:]), [r_ysb[yk]], [], r_ysb[yk])
                P.barrier()
                P.release([r_w1s, r_w2s] + r_tix + r_xg + r_b1r + r_b2b + r_ysb)

            with ExitStack() as _st:
                _pass(_st)
            def _pass(st, l=(l if "l" in dir() else 0), last=(last if "last" in dir() else False)):
                g2 = sbt(st, "C_g", [128, D], F32)
                b2t = sbt(st, "C_b", [128, D], F32)
                r_cC = P.res("C_c")
                P.dma("sp", lambda e: e.dma_start(out=g2[:], in_=ln2_g[l][None, :].to_broadcast([128, D])), [], [r_cC], r_cC)
                P.dma("sp", lambda e: e.dma_start(out=b2t[:], in_=ln2_b[l][None, :].to_broadcast([128, D])), [], [r_cC], r_cC)
                NB = 6
                x1t = [sbt(st, f"C_x{i}", [128, D], F32) for i in range(NB)]
                r_x1t = [P.res(f"C_x{i}") for i in range(NB)]
                y0t = [sbt(st, f"C_y0{i}", [128, D], F32) for i in range(NB)]
                r_y0t = [P.res(f"C_y0{i}") for i in range(NB)]
                y1t = [sbt(st, f"C_y1{i}", [128, D], F32) for i in range(NB)]
                r_y1t = [P.res(f"C_y1{i}") for i in range(NB)]
                lsc = [ln_scratch(st, f"C_l{i}") for i in range(NB)]
                r_lsc = [P.res(f"C_ls{i}") for i in range(NB)]
                dst = y_out if last else xa
                for i0 in range(0, NT, 3):
                    grp = list(range(i0, min(i0 + 3, NT)))
                    for i in grp:
                        k = i % NB
                        P.dma("sp", lambda e, k=k, i=i: e.dma_start(out=x1t[k][:], in_=x1b[i * 128:(i + 1) * 128, :]), [], [r_x1t[k]], r_x1t[k])
                        P.dma("pool", lambda e, k=k, i=i: e.indirect_dma_start(
                            out=y0t[k][:, :], out_offset=None, in_=ybuf[:, :],
                            in_offset=bass.IndirectOffsetOnAxis(ap=slot0[:, i:i + 1], axis=0)), [r_slots], [r_y0t[k]], r_y0t[k])
                        P.dma("pool", lambda e, k=k, i=i: e.indirect_dma_start(
                            out=y1t[k][:, :], out_offset=None, in_=ybuf[:, :],
                            in_offset=bass.IndirectOffsetOnAxis(ap=slot1[:, i:i + 1], axis=0)), [r_slots], [r_y1t[k]], r_y1t[k])
                    for i in grp:
                        k = i % NB
                        P.op("act", lambda e, k=k: e.mul(out=x1t[k][:], in_=x1t[k][:], mul=ALPHA), [r_x1t[k]], [r_x1t[k]])
                    for i in grp:
                        k = i % NB
                        P.op("dve", lambda e, k=k, i=i: e.scalar_tensor_tensor(
                            out=x1t[k][:], in0=y0t[k][:], scalar=gates[:, i, 0:1], in1=x1t[k][:], op0=ALU.mult, op1=ALU.add),
                            [r_y0t[k], r_x1t[k], r_gates], [r_x1t[k]])
                    for i in grp:
                        k = i % NB
                        P.op("dve", lambda e, k=k, i=i: e.scalar_tensor_tensor(
                            out=x1t[k][:], in0=y1t[k][:], scalar=gates[:, i, 1:2], in1=x1t[k][:], op0=ALU.mult, op1=ALU.add),
                            [r_y1t[k], r_x1t[k], r_gates], [r_x1t[k]])
                    emit_ln_multi([(x1t[i % NB][:], r_x1t[i % NB], lsc[i % NB], r_lsc[i % NB]) for i in grp], g2[:], b2t[:], cread=[r_cC])
                    for i in grp:
                        k = i % NB
                        P.dma("sp", lambda e, k=k, i=i: e.dma_start(out=dst[i * 128:(i + 1) * 128, :], in_=x1t[k][:]), [r_x1t[k]], [], r_x1t[k])
                P.barrier()
                P.release([r_cC] + r_x1t + r_y0t + r_y1t)
            with ExitStack() as _st:
                _pass(_st)
        cnt = P.emit()
    return nc, cnt


def _constants(T, NU):
    NT = T // 128
    NBLK = -(-(2 * T + NE * (BLK - 1)) // BLK)
    c = {}
    c["c_ident_bf"] = np.eye(128, dtype=np.float32).astype(ml_dtypes.bfloat16)
    c["c_ident_f"] = np.eye(128, dtype=np.float32)
    c["c_tri"] = np.triu(np.ones((128, 128), np.float32), 1).astype(ml_dtypes.bfloat16)
    c["c_ones"] = np.ones((128, 128), np.float32).astype(ml_dtypes.bfloat16)
    qc = np.arange(64)
    cs = np.clip(qc - 8, 0, 48)
    kc = np.arange(64)
    inw = (kc[None, :] >= cs[:, None]) & (kc[None, :] < cs[:, None] + 16)
    m = np.where(inw, 0.0, NEG).astype(np.float32)
    c["c_mask"] = np.concatenate([m, m], axis=0)
    invc = np.zeros((128, 4, 16), np.float32)
    L = 2048
    for g, w in enumerate((2, 4, 8, 16)):
        for i in range(8):
            lo, hi = max(i - w // 2, 0), min(i + w // 2, L)
            invc[:, g, i] = 1.0 / (hi - lo)
            p = L - 8 + i
            lo, hi = max(p - w // 2, 0), min(p + w // 2, L)
            invc[:, g, 8 + i] = 1.0 / (hi - lo)
    c["c_invc"] = invc
    c["c_tokid"] = (np.arange(NT)[None, :] * 128 + np.arange(128)[:, None]).astype(np.int32)
    c["c_rowc"] = (np.arange(16)[None, :] * 128 + np.arange(128)[:, None]).astype(np.float32)
    c["c_bstart"] = np.tile((np.arange(NBLK) * BLK).astype(np.float32)[None, :], (128, 1))
    return c


def _unit_tables(units, T):
    NU = len(units)
    kv = np.zeros((128, NU * 16), np.int32)
    oi = np.zeros((128, NU * 16), np.int32)
    for u, (t0, vlo, vhi) in enumerate(units):
        loc = np.arange(2048)
        tok = t0 + loc
        row = loc // 64
        valid = (row >= vlo) & (row < vhi)
        out = np.where(valid, tok, T + loc)
        kv[:, u * 16:(u + 1) * 16] = tok.reshape(16, 128).T
        oi[:, u * 16:(u + 1) * 16] = out.reshape(16, 128).T
    return kv, oi


_CACHE = {}


def kernel(x_prompt, x_sample, ln_in_g, ln_in_b, w_in, b_in, rpb, w_pool, b_pool, pool_scale,
           w_oa, w_op, w_out, b_out, ln1_g, ln1_b, w_router, b_router, w1, b1, w2, b2, ln2_g, ln2_b):
    T, NU = 12288, 7
    f = lambda a: np.ascontiguousarray(np.asarray(a, dtype=np.float32))
    x_prompt, x_sample = f(x_prompt), f(x_sample)
    shared = dict(ln_in_g=f(ln_in_g), ln_in_b=f(ln_in_b), w_in=f(w_in), b_in=f(b_in), w_pool=f(w_pool),
                  b_pool=f(b_pool), pool_scale=f(pool_scale), w_oa=f(w_oa), w_op=f(w_op), w_out=f(w_out), b_out=f(b_out),
                  ln1_g=f(ln1_g), ln1_b=f(ln1_b), w_router=f(w_router), b_router=f(b_router), b1=f(b1),
                  b2=f(b2), ln2_g=f(ln2_g), ln2_b=f(ln2_b))
    w1f, w2f = f(w1), f(w2)
    for i in range(DEPTH):
        shared[f"w1_{i}"] = w1f[i].reshape(NE * D, DFF)
        shared[f"w2_{i}"] = w2f[i].reshape(NE * DFF, D)
    rp = f(rpb)
    qc = np.arange(64)[:, None]
    kcc = np.arange(64)[None, :]
    ti = np.clip(kcc - qc + 15, 0, 30)
    g = rp[:, :, :, ti]
    g = g.reshape(DEPTH, 4, 2, 15, 64, 64).transpose(0, 2, 4, 1, 3, 5)
    shared["rpbT"] = np.ascontiguousarray(g.reshape(DEPTH, 128, 4 * 15 * 64))
    shared.update(_constants(T, NU))
    in_maps = []
    for c in range(8):
        if c < 4:
            xc = np.concatenate([x_prompt[c], x_sample[2 * c], x_sample[2 * c + 1]], axis=0)
            units = [(0, 0, 28), (1536, 4, 28), (3072, 4, 28), (4608, 4, 28), (6144, 4, 32),
                     (8192, 0, 32), (10240, 0, 32)]
        else:
            s0 = 8 + 6 * (c - 4)
            xc = np.concatenate([x_sample[s0 + j] for j in range(6)], axis=0)
            units = [(2048 * j, 0, 32) for j in range(6)] + [(0, 0, 0)]
        kv, oi = _unit_tables(units, T)
        m = dict(shared)
        m["x_in"] = np.ascontiguousarray(xc)
        m["kvidx"] = kv
        m["outidx"] = oi
        in_maps.append(m)
    if "nc" not in _CACHE:
        _CACHE["nc"] = build_program(T, NU)[0]
    nc = _CACHE["nc"]
    res = run_bass_kernel_spmd(nc, in_maps, core_ids=list(range(8)))
    outs = [np.asarray(r["y_out"], dtype=np.float32) for r in res.results]
    y_prompt = np.stack([outs[c][0:8192] for c in range(4)], axis=0)
    y_sample = np.zeros((32, 2048, D), np.float32)
    for c in range(4):
        y_sample[2 * c] = outs[c][8192:10240]
        y_sample[2 * c + 1] = outs[c][10240:12288]
    for c in range(4, 8):
        s0 = 8 + 6 * (c - 4)
        for j in range(6):
            y_sample[s0 + j] = outs[c][2048 * j:2048 * (j + 1)]
    return (y_prompt, y_sample)
```

```python
from contextlib import ExitStack
import numpy as np
import ml_dtypes
import concourse.bass as bass
import concourse.mybir as mybir
from concourse.bass_utils import run_bass_kernel_spmd

F32 = mybir.dt.float32
BF16 = mybir.dt.bfloat16
I32 = mybir.dt.int32
AF = mybir.ActivationFunctionType
ALU = mybir.AluOpType
AX = mybir.AxisListType

D = 1024
DEPTH = 4
NE = 16
DFF = 2048
ALPHA = (2 * DEPTH) ** 0.25
EPS = 1e-5
BLK = 512
TRASH = 2048
NEG = -30000.0

ENGS = ("pe", "act", "dve", "pool", "sp")
EPOCH = 30000


class Res:
    __slots__ = ("name", "lw", "rd", "sem", "dcnt")

    def __init__(self, name):
        self.name = name
        self.lw = None
        self.rd = []
        self.sem = None
        self.dcnt = 0


class Op:
    __slots__ = ("eng", "fn", "deps", "marked", "seq", "dma", "dtok")

    def __init__(self, eng, fn, dma):
        self.eng = eng
        self.fn = fn
        self.deps = []
        self.marked = False
        self.seq = None
        self.dma = dma
        self.dtok = None


class Prog:
    def __init__(self, nc):
        self.nc = nc
        self.ops = []
        self.n_dma_sems = 0
        self.res_list = []
        self.last = {e: None for e in ENGS}
        self.sem_pool = []

    def res(self, name):
        r = Res(name)
        self.res_list.append(r)
        return r

    def release(self, rs):
        for r in rs:
            if r.sem is not None:
                self.sem_pool.append((r.sem, r.dcnt))
                r.sem = None

    def _add(self, eng, fn, reads, writes, dma_res=None):
        op = Op(eng, fn, dma_res is not None)
        deps = []
        for r in reads:
            if r.lw is not None:
                deps.append(r.lw)
        for w in writes:
            if w.lw is not None:
                deps.append(w.lw)
            deps.extend(w.rd)
        seen = set()
        for d in deps:
            if id(d) in seen:
                continue
            seen.add(id(d))
            if isinstance(d, Op):
                if d.eng == eng and eng == "pe" and not op.dma:
                    continue
                d.marked = True
            op.deps.append(d)
        if dma_res is not None:
            if dma_res.sem is None:
                if self.sem_pool:
                    dma_res.sem, dma_res.dcnt = self.sem_pool.pop()
                else:
                    dma_res.sem = self.n_dma_sems
                    dma_res.dcnt = 0
                    self.n_dma_sems += 1
            dma_res.dcnt += 16
            tok = ("D", dma_res.sem, dma_res.dcnt)
            op.dtok = tok
        else:
            tok = op
            if fn is not None:
                self.last[eng] = op
        for r in reads:
            if r.rd:
                p = r.rd[-1]
                if isinstance(tok, Op) and isinstance(p, Op) and p.eng == tok.eng:
                    r.rd[-1] = tok
                    continue
                if (not isinstance(tok, Op)) and (not isinstance(p, Op)) and p[1] == tok[1]:
                    r.rd[-1] = tok
                    continue
            r.rd.append(tok)
        for w in writes:
            w.lw = tok
            w.rd = []
        self.ops.append(op)
        return op

    def op(self, eng, fn, reads=(), writes=()):
        return self._add(eng, fn, reads, writes)

    def dma(self, eng, fn, reads, writes, sem_res):
        return self._add(eng, fn, reads, writes, dma_res=sem_res)

    def barrier(self):
        toks = []
        for e in ENGS:
            if self.last[e] is not None:
                toks.append(self.last[e])
        dt = {}
        for r in self.res_list:
            if r.sem is not None:
                dt[r.sem] = max(dt.get(r.sem, 0), r.dcnt)
        for s, c in self.sem_pool:
            dt[s] = max(dt.get(s, 0), c)
        for s, c in dt.items():
            toks.append(("D", s, c))
        for e in ENGS:
            op = Op(e, None, False)
            for t in toks:
                if isinstance(t, Op):
                    if t.eng == e:
                        continue
                    t.marked = True
                op.deps.append(t)
            self.ops.append(op)
        for r in self.res_list:
            r.lw = None
            r.rd = []

    def emit(self):
        nc = self.nc
        cnt = {e: 0 for e in ENGS}
        for op in self.ops:
            if op.marked and not op.dma:
                cnt[op.eng] += 1
                op.seq = cnt[op.eng]
        n_eng_sems = {e: (cnt[e] + EPOCH - 1) // EPOCH for e in ENGS}
        with ExitStack() as st:
            esems = {e: [st.enter_context(nc.semaphore(f"e_{e}_{i}")) for i in range(n_eng_sems[e])]
                     for e in ENGS}
            dsems = [st.enter_context(nc.semaphore(f"d_{i}")) for i in range(self.n_dma_sems)]
            block = st.enter_context(nc.Block())
            by_eng = {e: [op for op in self.ops if op.eng == e] for e in ENGS}

            def resolve(tok):
                if isinstance(tok, Op):
                    s = (tok.seq - 1) // EPOCH
                    return esems[tok.eng][s], tok.seq - s * EPOCH, ("E", tok.eng, s)
                return dsems[tok[1]], tok[2], ("D", tok[1])

            def run_engine(eng_name, eng):
                known = {}
                for op in by_eng[eng_name]:
                    for d in op.deps:
                        sem, val, key = resolve(d)
                        if known.get(key, 0) >= val:
                            continue
                        known[key] = val
                        eng.wait_ge(sem, val)
                    if op.fn is None:
                        continue
                    inst = op.fn(eng)
                    if op.dma:
                        inst.then_inc(dsems[op.dtok[1]], 16)
                    elif op.marked:
                        s = (op.seq - 1) // EPOCH
                        inst.then_inc(esems[op.eng][s], 1)

            @block.tensor
            def _(e):
                run_engine("pe", e)

            @block.scalar
            def _(e):
                run_engine("act", e)

            @block.vector
            def _(e):
                run_engine("dve", e)

            @block.gpsimd
            def _(e):
                run_engine("pool", e)

            @block.sync
            def _(e):
                run_engine("sp", e)
        return cnt


def build_program(T, NU, depth=DEPTH, debug=None):
    NT = T // 128
    NBLK = -(-(2 * T + NE * (BLK - 1)) // BLK)
    NSLOT = NBLK * BLK
    nc = bass.Bass("TRN2", target_bir_lowering=False)

    def din(name, shape, dt=F32):
        return nc.dram_tensor(name, list(shape), dt, kind="ExternalInput").ap()

    x_in = din("x_in", [T, D])
    ln_in_g = din("ln_in_g", [D]); ln_in_b = din("ln_in_b", [D])
    w_in = din("w_in", [DEPTH, D, 4096]); b_in = din("b_in", [DEPTH, 4096])
    rpbT = din("rpbT", [DEPTH, 128, 4 * 15 * 64])
    w_pool = din("w_pool", [DEPTH, 4, 128, 128]); b_pool = din("b_pool", [DEPTH, 4, 128])
    pool_scale = din("pool_scale", [DEPTH, 512])
    w_oa = din("w_oa", [DEPTH, 512, D]); w_op = din("w_op", [DEPTH, 512, D])
    w_out = din("w_out", [DEPTH, D, D]); b_out = din("b_out", [DEPTH, D])
    ln1_g = din("ln1_g", [DEPTH, D]); ln1_b = din("ln1_b", [DEPTH, D])
    w_router = din("w_router", [D, NE]); b_router = din("b_router", [NE])
    w1L = [din(f"w1_{i}", [NE * D, DFF]) for i in range(DEPTH)]; b1 = din("b1", [DEPTH, NE, DFF])
    w2L = [din(f"w2_{i}", [NE * DFF, D]) for i in range(DEPTH)]; b2 = din("b2", [DEPTH, NE, D])
    ln2_g = din("ln2_g", [DEPTH, D]); ln2_b = din("ln2_b", [DEPTH, D])
    kvidx_d = din("kvidx", [128, NU * 16], I32)
    outidx_d = din("outidx", [128, NU * 16], I32)
    c_ident_bf = din("c_ident_bf", [128, 128], BF16)
    c_ident_f = din("c_ident_f", [128, 128])
    c_tri = din("c_tri", [128, 128], BF16)
    c_ones = din("c_ones", [128, 128], BF16)
    c_mask = din("c_mask", [128, 64])
    c_invc = din("c_invc", [128, 4, 16])
    c_tokid = din("c_tokid", [128, NT], I32)
    c_rowc = din("c_rowc", [128, 16])
    c_bstart = din("c_bstart", [128, NBLK])
    y_out = nc.dram_tensor("y_out", [T, D], F32, kind="ExternalOutput").ap()

    sk = "ExternalOutput" if debug else "Internal"
    xa = nc.dram_tensor("xa", [T + TRASH, D], F32, kind=sk).ap()
    x1b = nc.dram_tensor("x1b", [T + TRASH, D], F32, kind=sk).ap()
    kvp = nc.dram_tensor("kvp", [NU, 128, 3, 8192], BF16, kind=sk).ap()
    ybuf = nc.dram_tensor("ybuf", [NSLOT, D], F32, kind=sk).ap()
    table = nc.dram_tensor("table", [NSLOT, 1], I32, kind=sk).ap()

    b2_rows = b2.rearrange("l e d -> (l e) d")

    P = Prog(nc)
    top = ExitStack()
    _regs = {}

    def breg(e, val):
        if val not in _regs:
            _regs[val] = e.to_reg(val)
        return _regs[val]

    uniq = [0]

    def sbt(st, name, shape, dt):
        uniq[0] += 1
        return st.enter_context(nc.sbuf_tensor(f"{name}_{uniq[0]}", list(shape), dt))

    def pst(st, name, shape, dt):
        uniq[0] += 1
        return st.enter_context(nc.psum_tensor(f"{name}_{uniq[0]}", list(shape), dt))

    with top:
        ident_bf = sbt(top, "ident_bf", [128, 128], BF16)
        ident_f = sbt(top, "ident_f", [128, 128], F32)
        tri = sbt(top, "tri", [128, 128], BF16)
        ones = sbt(top, "ones", [128, 128], BF16)
        tokid = sbt(top, "tokid", [128, NT], I32)
        kvidx = sbt(top, "kvidx_sb", [128, NU * 16], I32)
        outidx = sbt(top, "outidx_sb", [128, NU * 16], I32)
        gates = sbt(top, "gates", [128, NT, 2], F32)
        slot0 = sbt(top, "slot0", [128, NT], I32)
        slot1 = sbt(top, "slot1", [128, NT], I32)
        mhalf = sbt(top, "mhalf", [128, 1], F32)
        widx1 = sbt(top, "widx1", [128, NBLK, 8], I32)
        widx2 = sbt(top, "widx2", [128, NBLK, 16], I32)
        bidx1 = sbt(top, "bidx1", [128, NBLK], I32)
        bidx2 = sbt(top, "bidx2", [128, NBLK], I32)
        r_const = P.res("const")
        r_gates = P.res("gates")
        r_slots = P.res("slots")
        r_widx = P.res("widx")
        for t, s in ((ident_bf, c_ident_bf), (ident_f, c_ident_f), (tri, c_tri), (ones, c_ones),
                     (tokid, c_tokid), (kvidx, kvidx_d), (outidx, outidx_d)):
            P.dma("sp", lambda e, t=t, s=s: e.dma_start(out=t[:], in_=s), [], [r_const], r_const)
        P.op("dve", lambda e: e.memset(mhalf[:], -0.5), [], [r_const])
        P.barrier()

        def emit_ln_multi(items, g_t, b_t, cread=()):
            def step(eng, mk, rd, wr):
                for it in items:
                    P.op(eng, mk(it), rd(it), wr(it))
            step("dve", lambda it: (lambda e: e.bn_stats(out=it[2][0][:, 0, :], in_=it[0][:, 0:512])), lambda it: [it[1]], lambda it: [it[3]])
            step("dve", lambda it: (lambda e: e.bn_stats(out=it[2][0][:, 1, :], in_=it[0][:, 512:1024])), lambda it: [it[1]], lambda it: [it[3]])
            step("dve", lambda it: (lambda e: e.bn_aggr(out=it[2][1][:], in_=it[2][0][:].rearrange("p a b -> p (a b)"))), lambda it: [it[3]], lambda it: [it[3]])
            step("dve", lambda it: (lambda e: e.tensor_scalar(out=it[2][2][:], in0=it[2][1][:, 1:2], scalar1=EPS, scalar2=None, op0=ALU.add)),
                 lambda it: [it[3]], lambda it: [it[3]])
            step("act", lambda it: (lambda e: e.activation(out=it[2][2][:], in_=it[2][2][:], func=AF.Ln)), lambda it: [it[3]], lambda it: [it[3]])
            step("act", lambda it: (lambda e: e.activation(out=it[2][2][:], in_=it[2][2][:], func=AF.Exp, scale=-0.5)), lambda it: [it[3]], lambda it: [it[3]])
            step("dve", lambda it: (lambda e: e.tensor_scalar(out=it[2][3][:], in0=it[2][1][:, 0:1], scalar1=it[2][2][:, 0:1], scalar2=-1.0,
                                                             op0=ALU.mult, op1=ALU.mult)), lambda it: [it[3]], lambda it: [it[3]])
            step("act", lambda it: (lambda e: e.activation(out=it[0], in_=it[0], func=AF.Identity, bias=it[2][3][:, 0:1], scale=it[2][2][:, 0:1])),
                 lambda it: [it[1], it[3]], lambda it: [it[1]])
            step("dve", lambda it: (lambda e: e.tensor_tensor(out=it[0], in0=it[0], in1=g_t, op=ALU.mult)), lambda it: [it[1]] + list(cread), lambda it: [it[1]])
            step("dve", lambda it: (lambda e: e.tensor_tensor(out=it[0], in0=it[0], in1=b_t, op=ALU.add)), lambda it: [it[1]] + list(cread), lambda it: [it[1]])

        def emit_ln(xt, r_x, g_t, b_t, scr, r_scr, cread=()):
            emit_ln_multi([(xt, r_x, scr, r_scr)], g_t, b_t, cread=cread)

        def ln_scratch(st, name):
            return (sbt(st, name + "_s6", [128, 2, 6], F32), sbt(st, name + "_mv", [128, 2], F32),
                    sbt(st, name + "_rs", [128, 1], F32), sbt(st, name + "_nm", [128, 1], F32))

        def _pass(st, l=(l if "l" in dir() else 0), last=(last if "last" in dir() else False)):
            g_t = sbt(st, "p0_g", [128, D], F32)
            b_t = sbt(st, "p0_b", [128, D], F32)
            r_gb = P.res("p0_gb")
            P.dma("sp", lambda e: e.dma_start(out=g_t[:], in_=ln_in_g[None, :].to_broadcast([128, D])), [], [r_gb], r_gb)
            P.dma("sp", lambda e: e.dma_start(out=b_t[:], in_=ln_in_b[None, :].to_broadcast([128, D])), [], [r_gb], r_gb)
            NB = 6
            xts = [sbt(st, f"p0_x{i}", [128, D], F32) for i in range(NB)]
            rxs = [P.res(f"p0_x{i}") for i in range(NB)]
            scrs = [ln_scratch(st, f"p0_l{i}") for i in range(NB)]
            rss = [P.res(f"p0_s{i}") for i in range(NB)]
            for i0 in range(0, NT, 3):
                grp = list(range(i0, min(i0 + 3, NT)))
                for i in grp:
                    k = i % NB
                    P.dma("sp", lambda e, xt=xts[k], i=i: e.dma_start(out=xt[:], in_=x_in[i * 128:(i + 1) * 128, :]), [], [rxs[k]], rxs[k])
                emit_ln_multi([(xts[i % NB][:], rxs[i % NB], scrs[i % NB], rss[i % NB]) for i in grp], g_t[:], b_t[:], cread=[r_gb])
                for i in grp:
                    k = i % NB
                    P.dma("sp", lambda e, xt=xts[k], i=i: e.dma_start(out=xa[i * 128:(i + 1) * 128, :], in_=xt[:]), [rxs[k]], [], rxs[k])
            P.barrier()
            P.release(rxs + [r_gb])

        with ExitStack() as _st:
            _pass(_st)
        for l in range(depth):
            last = (l == depth - 1)
            def _pass(st, l=(l if "l" in dir() else 0), last=(last if "last" in dir() else False)):
                wA = sbt(st, "wA", [128, 8, 1536], BF16)
                r_wA = P.res("wA")
                for kc in range(8):
                    P.dma("pool", lambda e, kc=kc: e.dma_start(out=wA[:, kc, :], in_=w_in[l, kc * 128:(kc + 1) * 128, 512:2048]),
                          [], [r_wA], r_wA)
                bcol = sbt(st, "A_bcol", [128, 32], F32)
                bv = sbt(st, "A_bv", [128, 512], F32)
                r_bA = P.res("A_b")
                P.dma("sp", lambda e: e.dma_start(out=bcol[:], in_=b_in[l].rearrange("(c p) -> p c", p=128),
                                                  allow_slow_non_contiguous=True), [], [r_bA], r_bA)
                P.dma("sp", lambda e: e.dma_start(out=bv[:], in_=b_in[l, 1024:1536][None, :].to_broadcast([128, 512])), [], [r_bA], r_bA)
                NB = 2
                xbf = [sbt(st, f"A_xbf{i}", [128, 4, D], BF16) for i in range(NB)]
                r_xbf = [P.res(f"A_xbf{i}") for i in range(NB)]
                xT = [sbt(st, f"A_xT{i}", [128, 8, 512], BF16) for i in range(NB)]
                r_xT = [P.res(f"A_xT{i}") for i in range(NB)]
                kTc = [sbt(st, f"A_kT{i}", [128, 4, 512], BF16) for i in range(NB)]
                r_kTc = [P.res(f"A_kT{i}") for i in range(NB)]
                pTc = [sbt(st, f"A_pT{i}", [128, 4, 512], BF16) for i in range(NB)]
                r_pTc = [P.res(f"A_pT{i}") for i in range(NB)]
                Vc = [sbt(st, f"A_V{i}", [128, 4, 512], BF16) for i in range(NB)]
                r_Vc = [P.res(f"A_V{i}") for i in range(NB)]
                psT = [pst(st, f"A_psT{i}", [128, 8, 128], BF16) for i in range(2)]
                r_psT = [P.res(f"A_psT{i}") for i in range(2)]
                psA = [pst(st, f"A_ps{i}", [128, 512], F32) for i in range(4)]
                r_psA = [P.res(f"A_ps{i}") for i in range(4)]
                nps = 0
                npt = 0
                for u in range(NU):
                    for c in range(4):
                        k = (u * 4 + c) % NB
                        for j in range(4):
                            col = u * 16 + c * 4 + j
                            P.dma("pool", lambda e, k=k, j=j, col=col: e.indirect_dma_start(
                                out=xbf[k][:, j, :], out_offset=None, in_=xa[:, :],
                                in_offset=bass.IndirectOffsetOnAxis(ap=kvidx[:, col:col + 1], axis=0)),
                                [r_const], [r_xbf[k]], r_xbf[k])
                        for j in range(4):
                            q = npt % 2
                            npt += 1
                            for kc in range(8):
                                P.op("pe", lambda e, q=q, k=k, j=j, kc=kc: e.transpose(
                                    out=psT[q][:, kc, :], in_=xbf[k][:, j, kc * 128:(kc + 1) * 128], identity=ident_bf[:]),
                                    [r_xbf[k], r_const], [r_psT[q]])
                            eng = "act" if j % 2 == 0 else "dve"
                            if eng == "act":
                                P.op("act", lambda e, q=q, k=k, j=j: e.copy(out=xT[k][:, :, j * 128:(j + 1) * 128], in_=psT[q][:]),
                                     [r_psT[q]], [r_xT[k]])
                            else:
                                P.op("dve", lambda e, q=q, k=k, j=j: e.tensor_copy(out=xT[k][:, :, j * 128:(j + 1) * 128], in_=psT[q][:]),
                                     [r_psT[q]], [r_xT[k]])
                        for m in range(4):
                            q = nps % 4
                            nps += 1
                            for kc in range(8):
                                P.op("pe", lambda e, q=q, k=k, m=m, kc=kc: e.matmul(
                                    psA[q][:], lhsT=wA[:, kc, m * 128:(m + 1) * 128], rhs=xT[k][:, kc, :],
                                    start=(kc == 0), stop=(kc == 7)), [r_wA, r_xT[k]], [r_psA[q]])
                            P.op("act", lambda e, q=q, k=k, m=m: e.activation(
                                out=kTc[k][:, m, :], in_=psA[q][:], func=AF.Identity, bias=bcol[:, 4 + m:5 + m], scale=1.0),
                                [r_psA[q], r_bA], [r_kTc[k]])
                        for m in range(4):
                            q = nps % 4
                            nps += 1
                            for kc in range(8):
                                P.op("pe", lambda e, q=q, k=k, m=m, kc=kc: e.matmul(
                                    psA[q][:], lhsT=wA[:, kc, 1024 + m * 128:1024 + (m + 1) * 128], rhs=xT[k][:, kc, :],
                                    start=(kc == 0), stop=(kc == 7)), [r_wA, r_xT[k]], [r_psA[q]])
                            P.op("act", lambda e, q=q, k=k, m=m: e.activation(
                                out=pTc[k][:, m, :], in_=psA[q][:], func=AF.Identity, bias=bcol[:, 12 + m:13 + m], scale=1.0),
                                [r_psA[q], r_bA], [r_pTc[k]])
                        for j in range(4):
                            q = nps % 4
                            nps += 1
                            for kc in range(8):
                                P.op("pe", lambda e, q=q, k=k, j=j, kc=kc: e.matmul(
                                    psA[q][:], lhsT=xT[k][:, kc, j * 128:(j + 1) * 128], rhs=wA[:, kc, 512:1024],
                                    start=(kc == 0), stop=(kc == 7)), [r_wA, r_xT[k]], [r_psA[q]])
                            P.op("dve", lambda e, q=q, k=k, j=j: e.tensor_tensor(
                                out=Vc[k][:, j, :], in0=psA[q][:], in1=bv[:], op=ALU.add),
                                [r_psA[q], r_bA], [r_Vc[k]])
                        P.dma("sp", lambda e, k=k, u=u, c=c: e.dma_start(
                            out=kvp[u, :, 0, :].rearrange("p (m t) -> p m t", m=4)[:, :, c * 512:(c + 1) * 512], in_=kTc[k][:]),
                            [r_kTc[k]], [], r_kTc[k])
                        P.dma("sp", lambda e, k=k, u=u, c=c: e.dma_start(
                            out=kvp[u, :, 2, :].rearrange("p (m t) -> p m t", m=4)[:, :, c * 512:(c + 1) * 512], in_=pTc[k][:]),
                            [r_pTc[k]], [], r_pTc[k])
                        P.dma("sp", lambda e, k=k, u=u, c=c: e.dma_start(
                            out=kvp[u, :, 1, c * 2048:(c + 1) * 2048], in_=Vc[k][:].rearrange("p j f -> p (j f)")),
                            [r_Vc[k]], [], r_Vc[k])
                P.barrier()
                P.release([r_wA, r_bA] + r_xbf + r_kTc + r_pTc + r_Vc)

            with ExitStack() as _st:
                _pass(_st)
            def _pass(st, l=(l if "l" in dir() else 0), last=(last if "last" in dir() else False)):
                CH = 256
                RPC = CH // 64
                TPC = CH // 128
                NCH = 2048 // CH
                EXT = CH + 16
                wB = sbt(st, "wB", [128, 8, 2560], BF16)
                woa = sbt(st, "woa", [128, 4, D], BF16)
                wop = sbt(st, "wop", [128, 4, D], BF16)
                wout = sbt(st, "wout", [128, 8, D], BF16)
                wpl = sbt(st, "wpl", [128, 4, 128], BF16)
                r_wB = P.res("wB")
                for kc in range(8):
                    P.dma("pool", lambda e, kc=kc: e.dma_start(out=wB[:, kc, 0:512], in_=w_in[l, kc * 128:(kc + 1) * 128, 0:512]), [], [r_wB], r_wB)
                    P.dma("pool", lambda e, kc=kc: e.dma_start(out=wB[:, kc, 512:2560], in_=w_in[l, kc * 128:(kc + 1) * 128, 2048:4096]), [], [r_wB], r_wB)
                    P.dma("pool", lambda e, kc=kc: e.dma_start(out=wout[:, kc, :], in_=w_out[l, kc * 128:(kc + 1) * 128, :]), [], [r_wB], r_wB)
                for kc in range(4):
                    P.dma("pool", lambda e, kc=kc: e.dma_start(out=woa[:, kc, :], in_=w_oa[l, kc * 128:(kc + 1) * 128, :]), [], [r_wB], r_wB)
                    P.dma("pool", lambda e, kc=kc: e.dma_start(out=wop[:, kc, :], in_=w_op[l, kc * 128:(kc + 1) * 128, :]), [], [r_wB], r_wB)
                    P.dma("pool", lambda e, kc=kc: e.dma_start(out=wpl[:, kc, :], in_=w_pool[l, kc, :, :]), [], [r_wB], r_wB)
                bcol = sbt(st, "B_bcol", [128, 32], F32)
                bq8 = sbt(st, "B_bq8", [128, 4], F32)
                psc = sbt(st, "B_psc", [128, 4], F32)
                bpc = sbt(st, "B_bpc", [128, 4], F32)
                bout_f = sbt(st, "B_boutf", [1, D], F32)
                bout = sbt(st, "B_bout", [1, D], BF16)
                g1 = sbt(st, "B_g1", [128, D], F32)
                b1t = sbt(st, "B_b1", [128, D], F32)
                invc = sbt(st, "B_invc", [128, 4, 16], F32)
                maskt = sbt(st, "B_mask", [128, 64], F32)
                Tb = sbt(st, "B_Tb", [128, 4, 15, 64], BF16)
                r_cB = P.res("B_c")
                P.dma("sp", lambda e: e.dma_start(out=bcol[:], in_=b_in[l].rearrange("(c p) -> p c", p=128),
                                                  allow_slow_non_contiguous=True), [], [r_cB], r_cB)
                P.dma("sp", lambda e: e.dma_start(out=psc[:], in_=pool_scale[l].rearrange("(c p) -> p c", p=128),
                                                  allow_slow_non_contiguous=True), [], [r_cB], r_cB)
                P.dma("sp", lambda e: e.dma_start(out=bpc[:], in_=b_pool[l].rearrange("c p -> p c"),
                                                  allow_slow_non_contiguous=True), [], [r_cB], r_cB)
                P.dma("sp", lambda e: e.dma_start(out=bout_f[:], in_=b_out[l][None, :]), [], [r_cB], r_cB)
                P.dma("sp", lambda e: e.dma_start(out=g1[:], in_=ln1_g[l][None, :].to_broadcast([128, D])), [], [r_cB], r_cB)
                P.dma("sp", lambda e: e.dma_start(out=b1t[:], in_=ln1_b[l][None, :].to_broadcast([128, D])), [], [r_cB], r_cB)
                P.dma("sp", lambda e: e.dma_start(out=invc[:], in_=c_invc), [], [r_cB], r_cB)
                P.dma("sp", lambda e: e.dma_start(out=maskt[:], in_=c_mask), [], [r_cB], r_cB)
                for hp in range(4):
                    P.dma("pool", lambda e, hp=hp: e.dma_start(out=Tb[:, hp, :, :].rearrange("p a b -> p (a b)"),
                                                               in_=rpbT[l, :, hp * 960:(hp + 1) * 960]), [], [r_cB], r_cB)
                P.op("dve", lambda e: e.tensor_tensor(
                    out=Tb[:].rearrange("p a b c -> p (a b) c"), in0=Tb[:].rearrange("p a b c -> p (a b) c"),
                    in1=maskt[:][:, None, :].to_broadcast([128, 60, 64]), op=ALU.add), [r_cB], [r_cB])
                P.op("dve", lambda e: e.tensor_scalar(out=bq8[:], in0=bcol[:, 0:4], scalar1=0.125, scalar2=None, op0=ALU.mult), [r_cB], [r_cB])
                P.op("dve", lambda e: e.tensor_tensor(out=bpc[:], in0=bpc[:], in1=psc[:], op=ALU.mult), [r_cB], [r_cB])
                P.op("dve", lambda e: e.tensor_copy(out=bout[:], in_=bout_f[:]), [r_cB], [r_cB])

                kT = sbt(st, "B_kT", [128, 4, 2048], BF16)
                Vt = sbt(st, "B_V", [128, 16, 512], BF16)
                r_kT, r_V = P.res("B_kT"), P.res("B_V")
                NXR = 4
                xres = [sbt(st, f"B_xres{i}", [128, D], F32) for i in range(NXR)]
                r_xres = [P.res(f"B_xres{i}") for i in range(NXR)]
                xbf = [sbt(st, f"B_xbf{i}", [128, D], BF16) for i in range(4)]
                r_xbf = [P.res(f"B_xbf{i}") for i in range(4)]

                def dbl(name, shape, dt):
                    t_, r_ = sbt(st, name, shape, dt), P.res(name)
                    return [t_, t_], [r_, r_]
                xT, r_xT = dbl("B_xT", [128, 8, CH], BF16)
                qT, r_qT = dbl("B_qT", [128, 4, CH], BF16)
                aT, r_aT = dbl("B_aT", [128, 4, CH], BF16)
                dT, r_dT = dbl("B_dT", [128, 4, CH], BF16)
                _tpmT = sbt(st, "B_pmT", [128, 4, CH], BF16)
                _rpmT = P.res("B_pmT")
                pmT, r_pmT = [_tpmT, _tpmT], [_rpmT, _rpmT]
                _m = sbt(st, "B_mixT", [128, 8, CH], BF16)
                _rm = P.res("B_mixT")
                mixT, r_mixT = [_m, _m], [_rm, _rm]
                _tpex = sbt(st, "B_pex", [128, 4, EXT], BF16)
                _rpex = P.res("B_pex")
                pex, r_pex = [_tpex, _tpex], [_rpex, _rpex]
                NA = 3
                Eb = [sbt(st, f"B_E{i}", [128, 512], BF16) for i in range(NA)]
                r_Eb = [P.res(f"B_E{i}") for i in range(NA)]
                rsum = [sbt(st, f"B_rs{i}", [128, 2], F32) for i in range(NA)]
                Pb = [sbt(st, f"B_P{i}", [128, 640], BF16) for i in range(NA)]
                r_Pb = [P.res(f"B_P{i}") for i in range(NA)]
                PT = [sbt(st, f"B_PT{i}", [128, 5, 128], BF16) for i in range(NA)]
                r_PT = [P.res(f"B_PT{i}") for i in range(NA)]
                pe_ = sbt(st, "B_pe", [128, EXT], F32)
                sA = sbt(st, "B_sA", [128, EXT], F32)
                sB_ = sbt(st, "B_sB", [128, EXT], F32)
                r_pl = P.res("B_pl")
                sg = [sbt(st, f"B_sg{i}", [128, CH], BF16) for i in range(2)]
                r_sg = [P.res(f"B_sg{i}") for i in range(2)]
                tm = [sbt(st, f"B_tm{i}", [128, CH], F32) for i in range(4)]
                r_tm = [P.res(f"B_tm{i}") for i in range(4)]
                lsc = [ln_scratch(st, f"B_l{i}") for i in range(NXR)]
                r_lsc = [P.res(f"B_ls{i}") for i in range(NXR)]
                psT = pst(st, "B_psT", [128, 8, 128], BF16)
                r_psT = P.res("B_psT")
                psP = [pst(st, f"B_psP{i}", [128, 512], F32) for i in range(3)]
                r_psP = [P.res(f"B_psP{i}") for i in range(3)]
                psS = [pst(st, f"B_psS{i}", [128, 512], F32) for i in range(2)]
                r_psS = [P.res(f"B_psS{i}") for i in range(2)]
                psPT = pst(st, "B_psPT", [128, 8, 128], BF16)
                r_psPT = P.res("B_psPT")
                psAT = pst(st, "B_psAT", [128, 512], F32)
                r_psAT = P.res("B_psAT")
                for i in range(NA):
                    P.op("dve", lambda e, i=i: e.memset(Pb[i][:], 0.0), [], [r_Pb[i]])
                npp = 0
                nat = 0
                nps = 0
                nxr = 0
                nxb = 0
                ncc = 0
                ntm = 0
                def gathers(gi):
                    for j in range(TPC):
                        col = gi * TPC + j
                        kb = col % 4
                        P.dma("pool", lambda e, kb=kb, col=col: e.indirect_dma_start(
                            out=xbf[kb][:, :], out_offset=None, in_=xa[:, :],
                            in_offset=bass.IndirectOffsetOnAxis(ap=kvidx[:, col:col + 1], axis=0)),
                            [r_const], [r_xbf[kb]], r_xbf[kb])
                    for j in range(TPC):
                        col = gi * TPC + j
                        k = col % NXR
                        P.dma("pool", lambda e, k=k, col=col: e.indirect_dma_start(
                            out=xres[k][:, :], out_offset=None, in_=xa[:, :],
                            in_offset=bass.IndirectOffsetOnAxis(ap=kvidx[:, col:col + 1], axis=0)),
                            [r_const], [r_xres[k]], r_xres[k])

                gathers(0)
                for u in range(NU):
                    P.dma("sp", lambda e, u=u: e.dma_start(out=kT[:].rearrange("p m t -> p (m t)"), in_=kvp[u, :, 0, :]), [], [r_kT], r_kT)
                    P.dma("sp", lambda e, u=u: e.dma_start(out=Vt[:].rearrange("p m t -> p (m t)"), in_=kvp[u, :, 1, :]), [], [r_V], r_V)
                    for c in range(NCH):
                        cc = ncc % 2
                        ncc += 1
                        gi = u * NCH + c
                        if gi + 1 < NU * NCH:
                            gathers(gi + 1)
                        for j in range(TPC):
                            col = u * 16 + c * TPC + j
                            kb = col % 4
                            for kc in range(8):
                                P.op("pe", lambda e, kb=kb, kc=kc: e.transpose(
                                    out=psT[:, kc, :], in_=xbf[kb][:, kc * 128:(kc + 1) * 128], identity=ident_bf[:]),
                                    [r_xbf[kb], r_const], [r_psT])
                            P.op("dve", lambda e, j=j, cc=cc: e.tensor_copy(out=xT[cc][:, :, j * 128:(j + 1) * 128], in_=psT[:]),
                                 [r_psT], [r_xT[cc]])
                        xk = [(u * 16 + c * TPC + j) % NXR for j in range(TPC)]
                        lo_t = max(c * CH - 8, 0)
                        hi_t = min(c * CH + CH + 8, 2048)
                        eo = lo_t - (c * CH - 8)
                        nn = hi_t - lo_t
                        P.op("pool", lambda e, cc=cc: e.memset(pex[cc][:], 0.0), [], [r_pex[cc]])
                        P.dma("sp", lambda e, u=u, lo_t=lo_t, hi_t=hi_t, eo=eo, nn=nn, cc=cc: e.dma_start(
                            out=pex[cc][:, :, eo:eo + nn],
                            in_=kvp[u, :, 2, :].rearrange("p (m t) -> p m t", m=4)[:, :, lo_t:hi_t]), [], [r_pex[cc]], r_pex[cc])
                        for g in range(4):
                            wv = 2 ** (g + 1)
                            P.op("pool", lambda e, g=g, cc=cc: e.tensor_copy(out=pe_[:], in_=pex[cc][:, g, :]), [r_pex[cc]], [r_pl])
                            P.op("pool", lambda e: e.memset(sA[:], 0.0), [], [r_pl])
                            P.op("pool", lambda e: e.tensor_tensor(out=sA[:, 1:EXT], in0=pe_[:, 0:EXT - 1], in1=pe_[:, 1:EXT], op=ALU.add), [r_pl], [r_pl])
                            cur, oth = sA, sB_
                            for sh in (1, 2, 4)[:g]:
                                P.op("pool", lambda e, oth=oth: e.memset(oth[:], 0.0), [], [r_pl])
                                P.op("pool", lambda e, cur=cur, oth=oth, sh=sh: e.tensor_tensor(
                                    out=oth[:, sh:EXT - sh], in0=cur[:, 0:EXT - 2 * sh], in1=cur[:, 2 * sh:EXT], op=ALU.add), [r_pl], [r_pl])
                                cur, oth = oth, cur
                            P.op("pool", lambda e, cur=cur, oth=oth, wv=wv: e.tensor_scalar(
                                out=oth[:, 8:8 + CH], in0=cur[:, 8:8 + CH], scalar1=1.0 / wv, scalar2=0.0, op0=ALU.mult, op1=ALU.add), [r_pl], [r_pl])
                            if c == 0:
                                P.op("pool", lambda e, cur=cur, oth=oth, g=g: e.tensor_tensor(
                                    out=oth[:, 8:16], in0=cur[:, 8:16], in1=invc[:, g, 0:8], op=ALU.mult), [r_pl, r_cB], [r_pl])
                            if c == NCH - 1:
                                P.op("pool", lambda e, cur=cur, oth=oth, g=g: e.tensor_tensor(
                                    out=oth[:, CH:CH + 8], in0=cur[:, CH:CH + 8], in1=invc[:, g, 8:16], op=ALU.mult), [r_pl, r_cB], [r_pl])
                            P.op("pool", lambda e, oth=oth, g=g, cc=cc: e.tensor_tensor(
                                out=dT[cc][:, g, :], in0=oth[:, 8:8 + CH], in1=pe_[:, 8:8 + CH], op=ALU.subtract), [r_pl], [r_dT[cc]])
                        for m in range(4):
                            q = npp % 3
                            npp += 1
                            for kc in range(8):
                                P.op("pe", lambda e, q=q, m=m, kc=kc, cc=cc: e.matmul(
                                    psP[q][:, 0:CH], lhsT=wB[:, kc, m * 128:(m + 1) * 128], rhs=xT[cc][:, kc, :],
                                    start=(kc == 0), stop=(kc == 7)), [r_wB, r_xT[cc]], [r_psP[q]])
                            P.op("act", lambda e, q=q, m=m, cc=cc: e.activation(
                                out=qT[cc][:, m, :], in_=psP[q][:, 0:CH], func=AF.Identity, bias=bq8[:, m:m + 1], scale=0.125),
                                [r_psP[q], r_cB], [r_qT[cc]])
                        items = [(hp, r8) for hp in range(4) for r8 in range(RPC)]

                        def att_geom(r8):
                            ql = c * RPC + r8
                            ws = min(max(ql - 4, 0), 24)
                            dr0 = ws - ql + 7
                            if ws % 2 == 0:
                                return ws, dr0, 4, 64, ws // 2
                            return ws, dr0, 5, 0, (ws - 1) // 2

                        def S1(t):
                            hp, r8 = items[t]
                            ws, dr0, nch, off, vt0 = att_geom(r8)
                            k = (nat + t) % NA
                            ks = (nat + t) % 2
                            for hh in range(2):
                                lo, hi = hh * 64, (hh + 1) * 64
                                P.op("pe", lambda e, ks=ks, hp=hp, r8=r8, ws=ws, lo=lo, hi=hi, cc=cc: e.matmul(
                                    psS[ks][lo:hi, :], lhsT=qT[cc][lo:hi, hp, r8 * 64:(r8 + 1) * 64],
                                    rhs=kT[lo:hi, hp, ws * 64:ws * 64 + 512], start=True, stop=False),
                                    [r_qT[cc], r_kT], [r_psS[ks]])
                            P.op("pe", lambda e, ks=ks, hp=hp, dr0=dr0: e.matmul(
                                psS[ks][:], lhsT=ident_bf[:], rhs=Tb[:, hp, dr0:dr0 + 8, :].rearrange("p a b -> p (a b)"),
                                start=False, stop=True), [r_cB, r_const], [r_psS[ks]])
                            P.op("act", lambda e, k=k, ks=ks: e.activation(
                                out=Eb[k][:], in_=psS[ks][:], func=AF.Exp, accum_out=rsum[k][:, 0:1]),
                                [r_psS[ks]], [r_Eb[k]])
                            P.op("dve", lambda e, k=k: e.reciprocal(out=rsum[k][:, 1:2], in_=rsum[k][:, 0:1]),
                                 [r_Eb[k]], [r_Eb[k]])
                            P.op("dve", lambda e, k=k: e.tensor_scalar(
                                out=Pb[k][:, 64:576], in0=Eb[k][:], scalar1=rsum[k][:, 1:2], scalar2=None, op0=ALU.mult),
                                [r_Eb[k]], [r_Pb[k]])

                        def S2(t):
                            hp, r8 = items[t]
                            ws, dr0, nch, off, vt0 = att_geom(r8)
                            k = (nat + t) % NA
                            for ch in range(nch):
                                P.op("pe", lambda e, k=k, ch=ch, off=off: e.transpose(
                                    out=psPT[:, ch, :], in_=Pb[k][:, off + ch * 128:off + (ch + 1) * 128], identity=ident_bf[:]),
                                    [r_Pb[k], r_const], [r_psPT])
                            P.op("act", lambda e, k=k, nch=nch: e.copy(out=PT[k][:, 0:nch, :], in_=psPT[:, 0:nch, :]),
                                 [r_psPT], [r_PT[k]])

                        def S3(t):
                            hp, r8 = items[t]
                            ws, dr0, nch, off, vt0 = att_geom(r8)
                            k = (nat + t) % NA
                            for hh in range(2):
                                lo, hi = hh * 64, (hh + 1) * 64
                                hcol = (2 * hp + hh) * 64
                                for ch in range(nch):
                                    P.op("pe", lambda e, k=k, ch=ch, lo=lo, hi=hi, hcol=hcol, r8=r8, vt0=vt0, nch=nch: e.matmul(
                                        psAT[lo:hi, r8 * 64:(r8 + 1) * 64], lhsT=Vt[:, vt0 + ch, hcol:hcol + 64],
                                        rhs=PT[k][:, ch, lo:hi], start=(ch == 0), stop=(ch == nch - 1)),
                                        [r_V, r_PT[k]], [r_psAT])
                            if r8 == RPC - 1:
                                P.op("dve", lambda e, hp=hp, cc=cc: e.tensor_copy(out=aT[cc][:, hp, :], in_=psAT[:, 0:CH]), [r_psAT], [r_aT[cc]])

                        NI = len(items)
                        for t in range(NI + 2):
                            if t < NI:
                                S1(t)
                            if 0 <= t - 1 < NI:
                                S2(t - 1)
                            if 0 <= t - 2 < NI:
                                S3(t - 2)
                        nat += NI
                        for g in range(4):
                            q = npp % 3
                            npp += 1
                            P.op("pe", lambda e, q=q, g=g, cc=cc: e.matmul(psP[q][:, 0:CH], lhsT=wpl[:, g, :], rhs=dT[cc][:, g, :], start=True, stop=True),
                                 [r_wB, r_dT[cc]], [r_psP[q]])
                            P.op("act", lambda e, q=q, g=g, cc=cc: e.activation(
                                out=pmT[cc][:, g, :], in_=psP[q][:, 0:CH], func=AF.Identity, bias=bpc[:, g:g + 1], scale=psc[:, g:g + 1]),
                                [r_psP[q], r_cB], [r_pmT[cc]])
                        for m in range(8):
                            tk = []
                            for br in range(2):
                                q = npp % 3
                                npp += 1
                                wc = 512 + br * 1024 + m * 128
                                for kc in range(8):
                                    P.op("pe", lambda e, q=q, wc=wc, kc=kc, cc=cc: e.matmul(
                                        psP[q][:, 0:CH], lhsT=wB[:, kc, wc:wc + 128], rhs=xT[cc][:, kc, :], start=(kc == 0), stop=(kc == 7)),
                                        [r_wB, r_xT[cc]], [r_psP[q]])
                                bc = 16 + br * 8 + m
                                P.op("act", lambda e, q=q, br=br, bc=bc: e.activation(
                                    out=sg[br][:], in_=psP[q][:, 0:CH], func=AF.Sigmoid, bias=bcol[:, bc:bc + 1], scale=1.0),
                                    [r_psP[q], r_cB], [r_sg[br]])
                                q = npp % 3
                                npp += 1
                                wsrc, asrc, r_a = (woa, aT[cc], r_aT[cc]) if br == 0 else (wop, pmT[cc], r_pmT[cc])
                                for kc in range(4):
                                    P.op("pe", lambda e, q=q, wsrc=wsrc, asrc=asrc, m=m, kc=kc: e.matmul(
                                        psP[q][:, 0:CH], lhsT=wsrc[:, kc, m * 128:(m + 1) * 128], rhs=asrc[:, kc, :], start=(kc == 0), stop=(kc == 3)),
                                        [r_wB, r_a], [r_psP[q]])
                                t_ = ntm % 4
                                ntm += 1
                                tk.append(t_)
                                P.op("dve", lambda e, q=q, br=br, t_=t_: e.tensor_tensor(out=tm[t_][:], in0=psP[q][:, 0:CH], in1=sg[br][:], op=ALU.mult),
                                     [r_psP[q], r_sg[br]], [r_tm[t_]])
                            P.op("pool", lambda e, m=m, tk=tk, cc=cc: e.tensor_tensor(out=mixT[cc][:, m, :], in0=tm[tk[0]][:], in1=tm[tk[1]][:], op=ALU.add),
                                 [r_tm[tk[0]], r_tm[tk[1]]], [r_mixT[cc]])
                        for j in range(TPC):
                            k = xk[j]
                            for h in range(2):
                                q = npp % 3
                                npp += 1
                                for kc in range(8):
                                    P.op("pe", lambda e, q=q, j=j, h=h, kc=kc, cc=cc: e.matmul(
                                        psP[q][:], lhsT=mixT[cc][:, kc, j * 128:(j + 1) * 128], rhs=wout[:, kc, h * 512:(h + 1) * 512],
                                        start=(kc == 0), stop=False), [r_wB, r_mixT[cc]], [r_psP[q]])
                                P.op("pe", lambda e, q=q, h=h: e.matmul(
                                    psP[q][:], lhsT=ones[0:1, :], rhs=bout[0:1, h * 512:(h + 1) * 512], start=False, stop=True),
                                    [r_cB, r_const], [r_psP[q]])
                                P.op("dve", lambda e, q=q, k=k, h=h: e.scalar_tensor_tensor(
                                    out=xres[k][:, h * 512:(h + 1) * 512], in0=xres[k][:, h * 512:(h + 1) * 512], scalar=ALPHA,
                                    in1=psP[q][:], op0=ALU.mult, op1=ALU.add), [r_psP[q], r_xres[k]], [r_xres[k]])
                        emit_ln_multi([(xres[k][:], r_xres[k], lsc[k], r_lsc[k]) for k in xk], g1[:], b1t[:], cread=[r_cB])
                        for j in range(TPC):
                            col = u * 16 + c * TPC + j
                            k = xk[j]
                            P.dma("pool", lambda e, k=k, col=col: e.indirect_dma_start(
                                out=x1b[:, :], out_offset=bass.IndirectOffsetOnAxis(ap=outidx[:, col:col + 1], axis=0),
                                in_=xres[k][:, :], in_offset=None), [r_xres[k], r_const], [], r_xres[k])
                P.barrier()
                P.release([r_wB, r_cB, r_kT, r_V, r_pex[0]] + r_xbf + r_xres)

            with ExitStack() as _st:
                _pass(_st)
            def _pass(st, l=(l if "l" in dir() else 0), last=(last if "last" in dir() else False)):
                wr = sbt(st, "R_wr", [128, 8, NE], F32)
                brt = sbt(st, "R_br", [128, NE], F32)
                bst = sbt(st, "R_bst", [128, NBLK], F32)
                rowc = sbt(st, "R_rowc", [128, 16], F32)
                r_cR = P.res("R_c")
                P.dma("sp", lambda e: e.dma_start(out=wr[:], in_=w_router.rearrange("(k p) n -> p k n", p=128)), [], [r_cR], r_cR)
                P.dma("sp", lambda e: e.dma_start(out=brt[:], in_=b_router[None, :].to_broadcast([128, NE])), [], [r_cR], r_cR)
                P.dma("sp", lambda e: e.dma_start(out=bst[:], in_=c_bstart), [], [r_cR], r_cR)
                P.dma("sp", lambda e: e.dma_start(out=rowc[:], in_=c_rowc), [], [r_cR], r_cR)
                A0 = sbt(st, "R_A0", [128, NT, NE], F32)
                A1 = sbt(st, "R_A1", [128, NT, NE], F32)
                POS = sbt(st, "R_POS", [128, NT, NE], F32)
                r_A = P.res("R_A")
                base = sbt(st, "R_base", [128, NE], F32)
                r_base = P.res("R_base")
                P.op("dve", lambda e: e.memset(base[:], 0.0), [], [r_base])
                xt = [sbt(st, f"R_x{i}", [128, D], F32) for i in range(2)]
                r_xt = [P.res(f"R_x{i}") for i in range(2)]
                xT32 = [sbt(st, f"R_xT{i}", [128, 8, 128], F32) for i in range(2)]
                r_xT32 = [P.res(f"R_xT{i}") for i in range(2)]
                zt = [sbt(st, f"R_z{i}", [128, 16 * 8], F32) for i in range(2)]
                r_zt = [P.res(f"R_z{i}") for i in range(2)]
                sm = [sbt(st, f"R_sm{i}", [128, 32], F32) for i in range(2)]
                Abf = [sbt(st, f"R_Ab{i}", [128, NE], BF16) for i in range(2)]
                psX = [pst(st, f"R_psX{i}", [128, 8, 128], F32) for i in range(1)]
                r_psX = [P.res(f"R_psX{i}") for i in range(1)]
                psL_ = [pst(st, f"R_psL{i}", [128, 512], F32) for i in range(2)]
                psL = [t[:, 0:NE] for t in psL_]
                r_psL = [P.res(f"R_psL{i}") for i in range(2)]
                psC_ = [pst(st, f"R_psC{i}", [128, 512], F32) for i in range(2)]
                psC = [t[:, 0:2 * NE].rearrange("p (a b) -> p a b", a=2) for t in psC_]
                r_psC = [P.res(f"R_psC{i}") for i in range(2)]
                for i in range(NT):
                    k = i % 2
                    P.dma("sp", lambda e, k=k, i=i: e.dma_start(out=xt[k][:], in_=x1b[i * 128:(i + 1) * 128, :]), [], [r_xt[k]], r_xt[k])
                    for kc in range(8):
                        P.op("pe", lambda e, k=k, kc=kc: e.transpose(out=psX[0][:, kc, :], in_=xt[k][:, kc * 128:(kc + 1) * 128], identity=ident_f[:]),
                             [r_xt[k], r_const], [r_psX[0]])
                    P.op("act", lambda e, k=k: e.copy(out=xT32[k][:, 0:4, :], in_=psX[0][:, 0:4, :]), [r_psX[0]], [r_xT32[k]])
                    P.op("dve", lambda e, k=k: e.tensor_copy(out=xT32[k][:, 4:8, :], in_=psX[0][:, 4:8, :]), [r_psX[0]], [r_xT32[k]])
                    for kc in range(8):
                        P.op("pe", lambda e, k=k, kc=kc: e.matmul(psL[k], lhsT=xT32[k][:, kc, :], rhs=wr[:, kc, :], start=(kc == 0), stop=(kc == 7)),
                             [r_xT32[k], r_cR], [r_psL[k]])
                    z = zt[k]
                    Z = lambda a, z=z: z[:, a * 16:(a + 1) * 16]
                    Z4 = lambda a, z=z: z[:, a * 16:(a + 1) * 16].rearrange("p (g j) -> p g j", g=4)
                    s = sm[k]
                    rz = r_zt[k]
                    P.op("dve", lambda e, k=k, Z=Z: e.tensor_tensor(out=Z(0), in0=psL[k], in1=brt[:], op=ALU.add), [r_psL[k], r_cR], [rz])
                    P.op("dve", lambda e, Z=Z, s=s: e.tensor_reduce(out=s[:, 0:1], in_=Z(0), axis=AX.X, op=ALU.max, negate=True), [rz], [rz])
                    P.op("act", lambda e, Z=Z, s=s: e.activation(out=Z(1), in_=Z(0), func=AF.Exp, bias=s[:, 0:1], scale=1.0), [rz], [rz])
                    P.op("dve", lambda e, Z4=Z4, s=s: e.tensor_reduce(out=s[:, 4:8], in_=Z4(1), axis=AX.X, op=ALU.max), [rz], [rz])
                    P.op("dve", lambda e, Z4=Z4, s=s: e.tensor_tensor(out=Z4(2), in0=Z4(1), in1=s[:, 4:8][:, :, None].to_broadcast([128, 4, 4]), op=ALU.is_equal), [rz], [rz])
                    P.op("dve", lambda e, Z=Z: e.scalar_tensor_tensor(out=Z(3), in0=Z(2), scalar=-2.0, in1=Z(1), op0=ALU.mult, op1=ALU.add), [rz], [rz])
                    P.op("dve", lambda e, Z4=Z4, s=s: e.tensor_reduce(out=s[:, 8:12], in_=Z4(3), axis=AX.X, op=ALU.max), [rz], [rz])
                    P.op("dve", lambda e, Z4=Z4, s=s: e.tensor_tensor(out=Z4(4), in0=Z4(3), in1=s[:, 8:12][:, :, None].to_broadcast([128, 4, 4]), op=ALU.is_equal), [rz], [rz])
                    P.op("dve", lambda e, s=s: e.tensor_tensor(out=s[:, 12:16], in0=s[:, 4:8], in1=s[:, 8:12], op=ALU.add), [rz], [rz])
                    P.op("dve", lambda e, s=s: e.tensor_reduce(out=s[:, 1:2], in_=s[:, 12:16], axis=AX.X, op=ALU.max), [rz], [rz])
                    P.op("dve", lambda e, s=s: e.tensor_scalar(out=s[:, 16:20], in0=s[:, 12:16], scalar1=s[:, 1:2], scalar2=None, op0=ALU.is_equal), [rz], [rz])
                    P.op("dve", lambda e, Z4=Z4, s=s, i=i: e.tensor_tensor(out=A0[:, i, :].rearrange("p (g j) -> p g j", g=4), in0=Z4(2),
                                                                             in1=s[:, 16:20][:, :, None].to_broadcast([128, 4, 4]), op=ALU.mult), [rz], [r_A])
                    P.op("dve", lambda e, Z4=Z4, s=s, i=i: e.tensor_tensor(out=A1[:, i, :].rearrange("p (g j) -> p g j", g=4), in0=Z4(4),
                                                                             in1=s[:, 16:20][:, :, None].to_broadcast([128, 4, 4]), op=ALU.mult), [rz], [r_A])
                    P.op("dve", lambda e, s=s: e.tensor_tensor(out=s[:, 20:24], in0=s[:, 16:20], in1=s[:, 4:8], op=ALU.mult), [rz], [rz])
                    P.op("dve", lambda e, s=s: e.tensor_tensor(out=s[:, 24:28], in0=s[:, 16:20], in1=s[:, 8:12], op=ALU.mult), [rz], [rz])
                    P.op("dve", lambda e, s=s: e.tensor_reduce(out=s[:, 28:30], in_=s[:, 20:28].rearrange("p (a b) -> p a b", a=2), axis=AX.X, op=ALU.add), [rz], [rz])
                    P.op("dve", lambda e, s=s: e.tensor_reduce(out=s[:, 30:31], in_=s[:, 28:30], axis=AX.X, op=ALU.add), [rz], [rz])
                    P.op("dve", lambda e, s=s: e.reciprocal(out=s[:, 31:32], in_=s[:, 30:31]), [rz], [rz])
                    P.op("dve", lambda e, s=s, i=i: e.tensor_scalar(out=gates[:, i, :], in0=s[:, 28:30], scalar1=s[:, 31:32], scalar2=None, op0=ALU.mult), [rz], [r_gates])
                    P.op("dve", lambda e, k=k, i=i: e.tensor_tensor(out=Abf[k][:], in0=A0[:, i, :], in1=A1[:, i, :], op=ALU.add), [r_A], [rz])
                    P.op("pe", lambda e, k=k: e.matmul(psC[k][:, 0, :], lhsT=tri[:], rhs=Abf[k][:], start=True, stop=True), [rz, r_const], [r_psC[k]])
                    P.op("pe", lambda e, k=k: e.matmul(psC[k][:, 1, :], lhsT=ones[:], rhs=Abf[k][:], start=True, stop=True), [rz, r_const], [r_psC[k]])
                    P.op("dve", lambda e, k=k, i=i: e.tensor_tensor(out=POS[:, i, :], in0=psC[k][:, 0, :], in1=base[:], op=ALU.add), [r_psC[k], r_base], [r_A])
                    P.op("dve", lambda e, k=k: e.tensor_tensor(out=base[:], in0=psC[k][:, 1, :], in1=base[:], op=ALU.add), [r_psC[k], r_base], [r_base])
                ci = sbt(st, "R_ci", [128, NE], I32)
                pf = sbt(st, "R_pf", [128, 4, NE], F32)
                r_pf = P.res("R_pf")
                P.op("dve", lambda e: e.tensor_scalar(out=ci[:], in0=base[:], scalar1=float(BLK - 1), scalar2=None, op0=ALU.add), [r_base], [r_pf])
                P.op("dve", lambda e: e.tensor_scalar(out=ci[:], in0=ci[:], scalar1=9, scalar2=9, op0=ALU.arith_shift_right, op1=ALU.arith_shift_left), [r_pf], [r_pf])
                P.op("dve", lambda e: e.tensor_copy(out=pf[:, 0, :], in_=ci[:]), [r_pf], [r_pf])
                P.op("dve", lambda e: e.tensor_copy(out=pf[:, 1, :], in_=pf[:, 0, :]), [r_pf], [r_pf])
                for sh in (1, 2, 4, 8):
                    P.op("dve", lambda e: e.tensor_copy(out=pf[:, 2, :], in_=pf[:, 1, :]), [r_pf], [r_pf])
                    P.op("dve", lambda e, sh=sh: e.tensor_tensor(out=pf[:, 1, sh:NE], in0=pf[:, 2, sh:NE], in1=pf[:, 2, 0:NE - sh], op=ALU.add), [r_pf], [r_pf])
                P.op("dve", lambda e: e.tensor_tensor(out=pf[:, 3, :], in0=pf[:, 1, :], in1=pf[:, 0, :], op=ALU.subtract), [r_pf], [r_pf])
                P.op("dve", lambda e: e.tensor_tensor(out=POS[:], in0=POS[:], in1=pf[:, 3:4, :].to_broadcast([128, NT, NE]), op=ALU.add), [r_pf, r_A], [r_A])
                sf = sbt(st, "R_sf", [128, NT], F32)
                for Ak, sl in ((A0, slot0), (A1, slot1)):
                    P.op("dve", lambda e, Ak=Ak: e.tensor_tensor(out=Ak[:], in0=Ak[:], in1=POS[:], op=ALU.mult), [r_A], [r_A])
                    P.op("dve", lambda e, Ak=Ak: e.tensor_reduce(out=sf[:], in_=Ak[:], axis=AX.X, op=ALU.add), [r_A], [r_pf])
                    P.op("dve", lambda e, sl=sl: e.tensor_copy(out=sl[:], in_=sf[:]), [r_pf], [r_slots])
                eb = sbt(st, "R_eb", [128, NBLK], F32)
                tmpb = sbt(st, "R_tmpb", [128, NBLK], F32)
                r_eb = P.res("R_eb")
                P.op("dve", lambda e: e.memset(eb[:], 0.0), [], [r_eb])
                for ex in range(NE):
                    P.op("dve", lambda e, ex=ex: e.tensor_scalar(out=tmpb[:], in0=bst[:], scalar1=pf[:, 1, ex:ex + 1], scalar2=None, op0=ALU.is_ge), [r_pf, r_cR], [r_eb])
                    P.op("dve", lambda e: e.tensor_tensor(out=eb[:], in0=eb[:], in1=tmpb[:], op=ALU.add), [r_eb], [r_eb])
                P.op("dve", lambda e: e.tensor_scalar(out=eb[:], in0=eb[:], scalar1=float(NE - 1), scalar2=None, op0=ALU.min), [r_eb], [r_eb])
                chg = sbt(st, "R_chg", [128, NBLK], F32)
                ebo1 = sbt(st, "R_ebo1", [128, NBLK], F32)
                ebo2 = sbt(st, "R_ebo2", [128, NBLK], F32)
                P.op("dve", lambda e: e.memset(chg[:], 0.0), [], [r_eb])
                P.op("dve", lambda e: e.tensor_tensor(out=chg[:, 1:NBLK], in0=eb[:, 1:NBLK], in1=eb[:, 0:NBLK - 1], op=ALU.is_equal), [r_eb], [r_eb])
                P.op("dve", lambda e: e.tensor_scalar(out=chg[:], in0=chg[:], scalar1=float(2 ** 30), scalar2=None, op0=ALU.mult), [r_eb], [r_eb])
                P.op("dve", lambda e: e.scalar_tensor_tensor(out=ebo1[:], in0=eb[:], scalar=float(D), in1=chg[:], op0=ALU.mult, op1=ALU.add), [r_eb], [r_eb])
                P.op("dve", lambda e: e.scalar_tensor_tensor(out=ebo2[:], in0=eb[:], scalar=float(DFF), in1=chg[:], op0=ALU.mult, op1=ALU.add), [r_eb], [r_eb])
                for kc in range(8):
                    P.op("dve", lambda e, kc=kc: e.tensor_scalar(out=widx1[:, :, kc], in0=ebo1[:], scalar1=rowc[:, kc:kc + 1], scalar2=None, op0=ALU.add), [r_eb, r_cR], [r_widx])
                for kc in range(16):
                    P.op("dve", lambda e, kc=kc: e.tensor_scalar(out=widx2[:, :, kc], in0=ebo2[:], scalar1=rowc[:, kc:kc + 1], scalar2=None, op0=ALU.add), [r_eb, r_cR], [r_widx])
                ebl = sbt(st, "R_ebl", [128, NBLK], F32)
                P.op("dve", lambda e: e.tensor_scalar(out=ebl[:], in0=eb[:], scalar1=float(l * NE), scalar2=None, op0=ALU.add), [r_eb], [r_eb])
                P.op("dve", lambda e: e.tensor_scalar(out=bidx1[:], in0=ebl[:], scalar1=16.0, scalar2=rowc[:, 0:1], op0=ALU.mult, op1=ALU.add), [r_eb, r_cR], [r_widx])
                P.op("dve", lambda e: e.tensor_copy(out=bidx2[:], in_=ebl[:]), [r_eb], [r_widx])
                zi = sbt(st, "R_zi", [128, NSLOT // 128], I32)
                r_zi = P.res("R_zi")
                P.op("dve", lambda e: e.memset(zi[:], 0), [], [r_zi])
                P.dma("sp", lambda e: e.dma_start(out=table.rearrange("(p f) o -> p (f o)", p=128), in_=zi[:]), [r_zi], [], r_zi)
                P.barrier()
                for i in range(NT):
                    for sl in (slot0, slot1):
                        P.dma("pool", lambda e, sl=sl, i=i: e.indirect_dma_start(
                            out=table[:, :], out_offset=bass.IndirectOffsetOnAxis(ap=sl[:, i:i + 1], axis=0),
                            in_=tokid[:, i:i + 1], in_offset=None), [r_slots, r_const], [], r_slots)
                P.barrier()
                P.release(r_xt + [r_cR, r_zi])

            with ExitStack() as _st:
                _pass(_st)
            if debug == "noM":
                continue
            def _pass(st, l=l, last=last):
                w1s = sbt(st, "M_w1", [128, 8, DFF], BF16)
                w2s = sbt(st, "M_w2", [128, 16, D], BF16)
                r_w1s, r_w2s = P.res("M_w1"), P.res("M_w2")
                tix = [sbt(st, f"M_tix{i}", [128, 4], I32) for i in range(2)]
                r_tix = [P.res(f"M_tix{i}") for i in range(2)]
                xg = [sbt(st, f"M_xg{i}", [128, 4, D], BF16) for i in range(2)]
                r_xg = [P.res(f"M_xg{i}") for i in range(2)]
                xT = [sbt(st, f"M_xT{i}", [128, 8, 512], BF16) for i in range(2)]
                r_xT = [P.res(f"M_xT{i}") for i in range(2)]
                hT = sbt(st, "M_hT", [128, 16, 512], BF16)
                r_hT = P.res("M_hT")
                b1r = [sbt(st, f"M_b1r{i}", [16, 128], F32) for i in range(2)]
                r_b1r = [P.res(f"M_b1r{i}") for i in range(2)]
                b1c = [sbt(st, f"M_b1c{i}", [128, 16], F32) for i in range(2)]
                r_b1c = [P.res(f"M_b1c{i}") for i in range(2)]
                b2b = [sbt(st, f"M_b2b{i}", [128, D], F32) for i in range(2)]
                r_b2b = [P.res(f"M_b2b{i}") for i in range(2)]
                ysb = [sbt(st, f"M_y{i}", [128, D], F32) for i in range(4)]
                r_ysb = [P.res(f"M_y{i}") for i in range(4)]
                psT = [pst(st, f"M_psT{i}", [128, 8, 128], BF16) for i in range(2)]
                r_psT = [P.res(f"M_psT{i}") for i in range(2)]
                psH = [pst(st, f"M_psH{i}", [128, 512], F32) for i in range(3)]
                r_psH = [P.res(f"M_psH{i}") for i in range(3)]
                psY = [pst(st, f"M_psY{i}", [128, 512], F32) for i in range(2)]
                r_psY = [P.res(f"M_psY{i}") for i in range(2)]
                psB_ = pst(st, "M_psB", [128, 512], F32)
                psB = psB_[:, 0:16]
                r_psB = P.res("M_psB")
                cnt = dict(npt=0, nph=0, npy=0, ny=0)
                tview = table.rearrange("(b p j) o -> b p (j o)", p=128, j=4)
                yview = ybuf.rearrange("(b p j) d -> b j p d", p=128, j=4)
                b1_rows = b1.rearrange("l e (c p) -> (l e c) p", p=128)

                def tokens(b):
                    k = b % 2
                    P.dma("sp", lambda e, k=k, b=b: e.dma_start(out=tix[k][:], in_=tview[b]), [], [r_tix[k]], r_tix[k])
                    for j in range(4):
                        P.dma("pool", lambda e, k=k, j=j: e.indirect_dma_start(
                            out=xg[k][:, j, :], out_offset=None, in_=x1b[:, :],
                            in_offset=bass.IndirectOffsetOnAxis(ap=tix[k][:, j:j + 1], axis=0)), [r_tix[k]], [r_xg[k]], r_xg[k])
                    P.dma("pool", lambda e, k=k, b=b: e.indirect_dma_start(
                        out=b1r[k][:, :], out_offset=None, in_=b1_rows[:, :],
                        in_offset=bass.IndirectOffsetOnAxis(ap=bidx1[0:16, b:b + 1], axis=0)), [r_widx], [r_b1r[k]], r_b1r[k])
                    P.dma("pool", lambda e, k=k, b=b: e.indirect_dma_start(
                        out=b2b[k][:, :], out_offset=None, in_=b2_rows[:, :],
                        in_offset=bass.IndirectOffsetOnAxis(ap=bidx2[:, b:b + 1], axis=0)), [r_widx], [r_b2b[k]], r_b2b[k])

                def transposes(b):
                    k = b % 2
                    for j in range(4):
                        q = cnt["npt"] % 2
                        cnt["npt"] += 1
                        for kc in range(8):
                            P.op("pe", lambda e, q=q, k=k, j=j, kc=kc: e.transpose(
                                out=psT[q][:, kc, :], in_=xg[k][:, j, kc * 128:(kc + 1) * 128], identity=ident_bf[:]),
                                [r_xg[k], r_const], [r_psT[q]])
                        P.op("dve", lambda e, q=q, k=k, j=j: e.tensor_copy(out=xT[k][:, :, j * 128:(j + 1) * 128], in_=psT[q][:]),
                             [r_psT[q]], [r_xT[k]])
                    P.op("pe", lambda e, k=k: e.transpose(out=psB, in_=b1r[k][:, :], identity=ident_f[0:16, 0:16]),
                         [r_b1r[k], r_const], [r_psB])
                    P.op("dve", lambda e, k=k: e.tensor_copy(out=b1c[k][:], in_=psB), [r_psB], [r_b1c[k]])

                def w1_load(b):
                    for kc in range(8):
                        P.dma("pool", lambda e, b=b, kc=kc: e.indirect_dma_start(
                            out=w1s[:, kc, :], out_offset=None, in_=w1L[l][:, :],
                            in_offset=bass.IndirectOffsetOnAxis(ap=widx1[:, b, kc:kc + 1], axis=0),
                            bounds_check=breg(e, NE * D - 1), oob_is_err=False), [r_widx], [r_w1s], r_w1s)

                def w2_load(b):
                    for kc in range(16):
                        P.dma("pool", lambda e, b=b, kc=kc: e.indirect_dma_start(
                            out=w2s[:, kc, :], out_offset=None, in_=w2L[l][:, :],
                            in_offset=bass.IndirectOffsetOnAxis(ap=widx2[:, b, kc:kc + 1], axis=0),
                            bounds_check=breg(e, NE * DFF - 1), oob_is_err=False), [r_widx], [r_w2s], r_w2s)

                tokens(0)
                w1_load(0)
                w2_load(0)
                transposes(0)
                for b in range(NBLK):
                    k = b % 2
                    if b + 1 < NBLK:
                        tokens(b + 1)
                    for m in range(16):
                        q = cnt["nph"] % 3
                        cnt["nph"] += 1
                        for kc in range(8):
                            P.op("pe", lambda e, q=q, k=k, m=m, kc=kc: e.matmul(
                                psH[q][:], lhsT=w1s[:, kc, m * 128:(m + 1) * 128], rhs=xT[k][:, kc, :],
                                start=(kc == 0), stop=(kc == 7)), [r_w1s, r_xT[k]], [r_psH[q]])
                        P.op("act", lambda e, q=q, k=k, m=m: e.activation(
                            out=hT[:, m, :], in_=psH[q][:], func=AF.Gelu, bias=b1c[k][:, m:m + 1], scale=1.0),
                            [r_psH[q], r_b1c[k]], [r_hT])
                    if b + 1 < NBLK:
                        w1_load(b + 1)
                        transposes(b + 1)
                    for j in range(4):
                        yk = cnt["ny"] % 4
                        cnt["ny"] += 1
                        for h in range(2):
                            q = cnt["npy"] % 2
                            cnt["npy"] += 1
                            for kc in range(16):
                                P.op("pe", lambda e, q=q, j=j, h=h, kc=kc: e.matmul(
                                    psY[q][:], lhsT=hT[:, kc, j * 128:(j + 1) * 128], rhs=w2s[:, kc, h * 512:(h + 1) * 512],
                                    start=(kc == 0), stop=(kc == 15)), [r_w2s, r_hT], [r_psY[q]])
                            P.op("dve", lambda e, q=q, yk=yk, k=k, h=h: e.tensor_tensor(
                                out=ysb[yk][:, h * 512:(h + 1) * 512], in0=psY[q][:], in1=b2b[k][:, h * 512:(h + 1) * 512], op=ALU.add),
                                [r_psY[q], r_b2b[k]], [r_ysb[yk]])
                        P.dma("sp", lambda e, yk=yk, b=b, j=j: e.dma_start(out=yview[b, j], in_=ysb[yk][:]), [r_ysb[yk]], [], r_ysb[yk])
                    if b + 1 < NBLK:
                        w2_load(b + 1)
                P.barrier()
                P.release([r_w1s, r_w2s] + r_tix + r_xg + r_b1r + r_b2b + r_ysb)
            with ExitStack() as _st:
                _pass(_st)

            def _pass(st, l=(l if "l" in dir() else 0), last=(last if "last" in dir() else False)):
                g2 = sbt(st, "C_g", [128, D], F32)
                b2t = sbt(st, "C_b", [128, D], F32)
                r_cC = P.res("C_c")
                P.dma("sp", lambda e: e.dma_start(out=g2[:], in_=ln2_g[l][None, :].to_broadcast([128, D])), [], [r_cC], r_cC)
                P.dma("sp", lambda e: e.dma_start(out=b2t[:], in_=ln2_b[l][None, :].to_broadcast([128, D])), [], [r_cC], r_cC)
                NB = 6
                x1t = [sbt(st, f"C_x{i}", [128, D], F32) for i in range(NB)]
                r_x1t = [P.res(f"C_x{i}") for i in range(NB)]
                y0t = [sbt(st, f"C_y0{i}", [128, D], F32) for i in range(NB)]
                r_y0t = [P.res(f"C_y0{i}") for i in range(NB)]
                y1t = [sbt(st, f"C_y1{i}", [128, D], F32) for i in range(NB)]
                r_y1t = [P.res(f"C_y1{i}") for i in range(NB)]
                lsc = [ln_scratch(st, f"C_l{i}") for i in range(NB)]
                r_lsc = [P.res(f"C_ls{i}") for i in range(NB)]
                dst = y_out if last else xa
                for i0 in range(0, NT, 3):
                    grp = list(range(i0, min(i0 + 3, NT)))
                    for i in grp:
                        k = i % NB
                        P.dma("sp", lambda e, k=k, i=i: e.dma_start(out=x1t[k][:], in_=x1b[i * 128:(i + 1) * 128, :]), [], [r_x1t[k]], r_x1t[k])
                        P.dma("pool", lambda e, k=k, i=i: e.indirect_dma_start(
                            out=y0t[k][:, :], out_offset=None, in_=ybuf[:, :],
                            in_offset=bass.IndirectOffsetOnAxis(ap=slot0[:, i:i + 1], axis=0)), [r_slots], [r_y0t[k]], r_y0t[k])
                        P.dma("pool", lambda e, k=k, i=i: e.indirect_dma_start(
                            out=y1t[k][:, :], out_offset=None, in_=ybuf[:, :],
                            in_offset=bass.IndirectOffsetOnAxis(ap=slot1[:, i:i + 1], axis=0)), [r_slots], [r_y1t[k]], r_y1t[k])
                    for i in grp:
                        k = i % NB
                        P.op("act", lambda e, k=k: e.mul(out=x1t[k][:], in_=x1t[k][:], mul=ALPHA), [r_x1t[k]], [r_x1t[k]])
                    for i in grp:
                        k = i % NB
                        P.op("dve", lambda e, k=k, i=i: e.scalar_tensor_tensor(
                            out=x1t[k][:], in0=y0t[k][:], scalar=gates[:, i, 0:1], in1=x1t[k][:], op0=ALU.mult, op1=ALU.add),
                            [r_y0t[k], r_x1t[k], r_gates], [r_x1t[k]])
                    for i in grp:
                        k = i % NB
                        P.op("dve", lambda e, k=k, i=i: e.scalar_tensor_tensor(
                            out=x1t[k][:], in0=y1t[k][:], scalar=gates[:, i, 1:2], in1=x1t[k][:], op0=ALU.mult, op1=ALU.add),
                            [r_y1t[k], r_x1t[k], r_gates], [r_x1t[k]])
                    emit_ln_multi([(x1t[i % NB][:], r_x1t[i % NB], lsc[i % NB], r_lsc[i % NB]) for i in grp], g2[:], b2t[:], cread=[r_cC])
                    for i in grp:
                        k = i % NB
                        P.dma("sp", lambda e, k=k, i=i: e.dma_start(out=dst[i * 128:(i + 1) * 128, :], in_=x1t[k][:]), [r_x1t[k]], [], r_x1t[k])
                P.barrier()
                P.release([r_cC] + r_x1t + r_y0t + r_y1t)
            with ExitStack() as _st:
                _pass(_st)
        cnt = P.emit()
    return nc, cnt


def _constants(T, NU):
    NT = T // 128
    NBLK = -(-(2 * T + NE * (BLK - 1)) // BLK)
    c = {}
    c["c_ident_bf"] = np.eye(128, dtype=np.float32).astype(ml_dtypes.bfloat16)
    c["c_ident_f"] = np.eye(128, dtype=np.float32)
    c["c_tri"] = np.triu(np.ones((128, 128), np.float32), 1).astype(ml_dtypes.bfloat16)
    c["c_ones"] = np.ones((128, 128), np.float32).astype(ml_dtypes.bfloat16)
    qc = np.arange(64)
    cs = np.clip(qc - 8, 0, 48)
    kc = np.arange(64)
    inw = (kc[None, :] >= cs[:, None]) & (kc[None, :] < cs[:, None] + 16)
    m = np.where(inw, 0.0, NEG).astype(np.float32)
    c["c_mask"] = np.concatenate([m, m], axis=0)
    invc = np.zeros((128, 4, 16), np.float32)
    L = 2048
    for g, w in enumerate((2, 4, 8, 16)):
        for i in range(8):
            lo, hi = max(i - w // 2, 0), min(i + w // 2, L)
            invc[:, g, i] = 1.0 / (hi - lo)
            p = L - 8 + i
            lo, hi = max(p - w // 2, 0), min(p + w // 2, L)
            invc[:, g, 8 + i] = 1.0 / (hi - lo)
    c["c_invc"] = invc
    c["c_tokid"] = (np.arange(NT)[None, :] * 128 + np.arange(128)[:, None]).astype(np.int32)
    c["c_rowc"] = (np.arange(16)[None, :] * 128 + np.arange(128)[:, None]).astype(np.float32)
    c["c_bstart"] = np.tile((np.arange(NBLK) * BLK).astype(np.float32)[None, :], (128, 1))
    return c


def _unit_tables(units, T):
    NU = len(units)
    kv = np.zeros((128, NU * 16), np.int32)
    oi = np.zeros((128, NU * 16), np.int32)
    for u, (t0, vlo, vhi) in enumerate(units):
        loc = np.arange(2048)
        tok = t0 + loc
        row = loc // 64
        valid = (row >= vlo) & (row < vhi)
        out = np.where(valid, tok, T + loc)
        kv[:, u * 16:(u + 1) * 16] = tok.reshape(16, 128).T
        oi[:, u * 16:(u + 1) * 16] = out.reshape(16, 128).T
    return kv, oi


_CACHE = {}


def kernel(x_prompt, x_sample, ln_in_g, ln_in_b, w_in, b_in, rpb, w_pool, b_pool, pool_scale,
           w_oa, w_op, w_out, b_out, ln1_g, ln1_b, w_router, b_router, w1, b1, w2, b2, ln2_g, ln2_b):
    T, NU = 12288, 7
    f = lambda a: np.ascontiguousarray(np.asarray(a, dtype=np.float32))
    x_prompt, x_sample = f(x_prompt), f(x_sample)
    shared = dict(ln_in_g=f(ln_in_g), ln_in_b=f(ln_in_b), w_in=f(w_in), b_in=f(b_in), w_pool=f(w_pool),
                  b_pool=f(b_pool), pool_scale=f(pool_scale), w_oa=f(w_oa), w_op=f(w_op), w_out=f(w_out), b_out=f(b_out),
                  ln1_g=f(ln1_g), ln1_b=f(ln1_b), w_router=f(w_router), b_router=f(b_router), b1=f(b1),
                  b2=f(b2), ln2_g=f(ln2_g), ln2_b=f(ln2_b))
    w1f, w2f = f(w1), f(w2)
    for i in range(DEPTH):
        shared[f"w1_{i}"] = w1f[i].reshape(NE * D, DFF)
        shared[f"w2_{i}"] = w2f[i].reshape(NE * DFF, D)
    rp = f(rpb)
    qc = np.arange(64)[:, None]
    kcc = np.arange(64)[None, :]
    ti = np.clip(kcc - qc + 15, 0, 30)
    g = rp[:, :, :, ti]
    g = g.reshape(DEPTH, 4, 2, 15, 64, 64).transpose(0, 2, 4, 1, 3, 5)
    shared["rpbT"] = np.ascontiguousarray(g.reshape(DEPTH, 128, 4 * 15 * 64))
    shared.update(_constants(T, NU))
    in_maps = []
    for c in range(8):
        if c < 4:
            xc = np.concatenate([x_prompt[c], x_sample[2 * c], x_sample[2 * c + 1]], axis=0)
            units = [(0, 0, 28), (1536, 4, 28), (3072, 4, 28), (4608, 4, 28), (6144, 4, 32),
                     (8192, 0, 32), (10240, 0, 32)]
        else:
            s0 = 8 + 6 * (c - 4)
            xc = np.concatenate([x_sample[s0 + j] for j in range(6)], axis=0)
            units = [(2048 * j, 0, 32) for j in range(6)] + [(0, 0, 0)]
        kv, oi = _unit_tables(units, T)
        m = dict(shared)
        m["x_in"] = np.ascontiguousarray(xc)
        m["kvidx"] = kv
        m["outidx"] = oi
        in_maps.append(m)
    if "nc" not in _CACHE:
        _CACHE["nc"] = build_program(T, NU)[0]
    nc = _CACHE["nc"]
    res = run_bass_kernel_spmd(nc, in_maps, core_ids=list(range(8)))
    outs = [np.asarray(r["y_out"], dtype=np.float32) for r in res.results]
    y_prompt = np.stack([outs[c][0:8192] for c in range(4)], axis=0)
    y_sample = np.zeros((32, 2048, D), np.float32)
    for c in range(4):
        y_sample[2 * c] = outs[c][8192:10240]
        y_sample[2 * c + 1] = outs[c][10240:12288]
    for c in range(4, 8):
        s0 = 8 + 6 * (c - 4)
        for j in range(6):
            y_sample[s0 + j] = outs[c][2048 * j:2048 * (j + 1)]
    return (y_prompt, y_sample)
```

```python
from contextlib import ExitStack
import numpy as np
import ml_dtypes
import concourse.bass as bass
import concourse.mybir as mybir
from concourse.bass_utils import run_bass_kernel_spmd

F32 = mybir.dt.float32
BF16 = mybir.dt.bfloat16
I32 = mybir.dt.int32
AF = mybir.ActivationFunctionType
ALU = mybir.AluOpType
AX = mybir.AxisListType

D = 1024
DEPTH = 4
NE = 16
DFF = 2048
ALPHA = (2 * DEPTH) ** 0.25
EPS = 1e-5
BLK = 512
TRASH = 2048
NEG = -30000.0

ENGS = ("pe", "act", "dve", "pool", "sp")
EPOCH = 30000


class Res:
    __slots__ = ("name", "lw", "rd", "sem", "dcnt")

    def __init__(self, name):
        self.name = name
        self.lw = None
        self.rd = []
        self.sem = None
        self.dcnt = 0


class Op:
    __slots__ = ("eng", "fn", "deps", "marked", "seq", "dma", "dtok")

    def __init__(self, eng, fn, dma):
        self.eng = eng
        self.fn = fn
        self.deps = []
        self.marked = False
        self.seq = None
        self.dma = dma
        self.dtok = None


class Prog:
    def __init__(self, nc):
        self.nc = nc
        self.ops = []
        self.n_dma_sems = 0
        self.res_list = []
        self.last = {e: None for e in ENGS}
        self.sem_pool = []

    def res(self, name):
        r = Res(name)
        self.res_list.append(r)
        return r

    def release(self, rs):
        for r in rs:
            if r.sem is not None:
                self.sem_pool.append((r.sem, r.dcnt))
                r.sem = None

    def _add(self, eng, fn, reads, writes, dma_res=None):
        op = Op(eng, fn, dma_res is not None)
        deps = []
        for r in reads:
            if r.lw is not None:
                deps.append(r.lw)
        for w in writes:
            if w.lw is not None:
                deps.append(w.lw)
            deps.extend(w.rd)
        seen = set()
        for d in deps:
            if id(d) in seen:
                continue
            seen.add(id(d))
            if isinstance(d, Op):
                if d.eng == eng and eng == "pe" and not op.dma:
                    continue
                d.marked = True
            op.deps.append(d)
        if dma_res is not None:
            if dma_res.sem is None:
                if self.sem_pool:
                    dma_res.sem, dma_res.dcnt = self.sem_pool.pop()
                else:
                    dma_res.sem = self.n_dma_sems
                    dma_res.dcnt = 0
                    self.n_dma_sems += 1
            dma_res.dcnt += 16
            tok = ("D", dma_res.sem, dma_res.dcnt)
            op.dtok = tok
        else:
            tok = op
            if fn is not None:
                self.last[eng] = op
        for r in reads:
            if r.rd:
                p = r.rd[-1]
                if isinstance(tok, Op) and isinstance(p, Op) and p.eng == tok.eng:
                    r.rd[-1] = tok
                    continue
                if (not isinstance(tok, Op)) and (not isinstance(p, Op)) and p[1] == tok[1]:
                    r.rd[-1] = tok
                    continue
            r.rd.append(tok)
        for w in writes:
            w.lw = tok
            w.rd = []
        self.ops.append(op)
        return op

    def op(self, eng, fn, reads=(), writes=()):
        return self._add(eng, fn, reads, writes)

    def dma(self, eng, fn, reads, writes, sem_res):
        return self._add(eng, fn, reads, writes, dma_res=sem_res)

    def barrier(self):
        toks = []
        for e in ENGS:
            if self.last[e] is not None:
                toks.append(self.last[e])
        dt = {}
        for r in self.res_list:
            if r.sem is not None:
                dt[r.sem] = max(dt.get(r.sem, 0), r.dcnt)
        for s, c in self.sem_pool:
            dt[s] = max(dt.get(s, 0), c)
        for s, c in dt.items():
            toks.append(("D", s, c))
        for e in ENGS:
            op = Op(e, None, False)
            for t in toks:
                if isinstance(t, Op):
                    if t.eng == e:
                        continue
                    t.marked = True
                op.deps.append(t)
            self.ops.append(op)
        for r in self.res_list:
            r.lw = None
            r.rd = []

    def emit(self):
        nc = self.nc
        cnt = {e: 0 for e in ENGS}
        for op in self.ops:
            if op.marked and not op.dma:
                cnt[op.eng] += 1
                op.seq = cnt[op.eng]
        n_eng_sems = {e: (cnt[e] + EPOCH - 1) // EPOCH for e in ENGS}
        with ExitStack() as st:
            esems = {e: [st.enter_context(nc.semaphore(f"e_{e}_{i}")) for i in range(n_eng_sems[e])]
                     for e in ENGS}
            dsems = [st.enter_context(nc.semaphore(f"d_{i}")) for i in range(self.n_dma_sems)]
            block = st.enter_context(nc.Block())
            by_eng = {e: [op for op in self.ops if op.eng == e] for e in ENGS}

            def resolve(tok):
                if isinstance(tok, Op):
                    s = (tok.seq - 1) // EPOCH
                    return esems[tok.eng][s], tok.seq - s * EPOCH, ("E", tok.eng, s)
                return dsems[tok[1]], tok[2], ("D", tok[1])

            def run_engine(eng_name, eng):
                known = {}
                for op in by_eng[eng_name]:
                    for d in op.deps:
                        sem, val, key = resolve(d)
                        if known.get(key, 0) >= val:
                            continue
                        known[key] = val
                        eng.wait_ge(sem, val)
                    if op.fn is None:
                        continue
                    inst = op.fn(eng)
                    if op.dma:
                        inst.then_inc(dsems[op.dtok[1]], 16)
                    elif op.marked:
                        s = (op.seq - 1) // EPOCH
                        inst.then_inc(esems[op.eng][s], 1)

            @block.tensor
            def _(e):
                run_engine("pe", e)

            @block.scalar
            def _(e):
                run_engine("act", e)

            @block.vector
            def _(e):
                run_engine("dve", e)

            @block.gpsimd
            def _(e):
                run_engine("pool", e)

            @block.sync
            def _(e):
                run_engine("sp", e)
        return cnt


def build_program(T, NU, depth=DEPTH, debug=None):
    NT = T // 128
    NBLK = -(-(2 * T + NE * (BLK - 1)) // BLK)
    NSLOT = NBLK * BLK
    nc = bass.Bass("TRN2", target_bir_lowering=False)

    def din(name, shape, dt=F32):
        return nc.dram_tensor(name, list(shape), dt, kind="ExternalInput").ap()

    x_in = din("x_in", [T, D])
    ln_in_g = din("ln_in_g", [D]); ln_in_b = din("ln_in_b", [D])
    w_in = din("w_in", [DEPTH, D, 4096]); b_in = din("b_in", [DEPTH, 4096])
    rpbT = din("rpbT", [DEPTH, 128, 4 * 15 * 64])
    w_pool = din("w_pool", [DEPTH, 4, 128, 128]); b_pool = din("b_pool", [DEPTH, 4, 128])
    pool_scale = din("pool_scale", [DEPTH, 512])
    w_oa = din("w_oa", [DEPTH, 512, D]); w_op = din("w_op", [DEPTH, 512, D])
    w_out = din("w_out", [DEPTH, D, D]); b_out = din("b_out", [DEPTH, D])
    ln1_g = din("ln1_g", [DEPTH, D]); ln1_b = din("ln1_b", [DEPTH, D])
    w_router = din("w_router", [D, NE]); b_router = din("b_router", [NE])
    w1L = [din(f"w1_{i}", [NE * D, DFF]) for i in range(DEPTH)]; b1 = din("b1", [DEPTH, NE, DFF])
    w2L = [din(f"w2_{i}", [NE * DFF, D]) for i in range(DEPTH)]; b2 = din("b2", [DEPTH, NE, D])
    ln2_g = din("ln2_g", [DEPTH, D]); ln2_b = din("ln2_b", [DEPTH, D])
    kvidx_d = din("kvidx", [128, NU * 16], I32)
    outidx_d = din("outidx", [128, NU * 16], I32)
    c_ident_bf = din("c_ident_bf", [128, 128], BF16)
    c_ident_f = din("c_ident_f", [128, 128])
    c_tri = din("c_tri", [128, 128], BF16)
    c_ones = din("c_ones", [128, 128], BF16)
    c_mask = din("c_mask", [128, 64])
    c_invc = din("c_invc", [128, 4, 16])
    c_tokid = din("c_tokid", [128, NT], I32)
    c_rowc = din("c_rowc", [128, 16])
    c_bstart = din("c_bstart", [128, NBLK])
    y_out = nc.dram_tensor("y_out", [T, D], F32, kind="ExternalOutput").ap()

    sk = "ExternalOutput" if debug else "Internal"
    xa = nc.dram_tensor("xa", [T + TRASH, D], F32, kind=sk).ap()
    x1b = nc.dram_tensor("x1b", [T + TRASH, D], F32, kind=sk).ap()
    kvp = nc.dram_tensor("kvp", [NU, 128, 3, 8192], BF16, kind=sk).ap()
    ybuf = nc.dram_tensor("ybuf", [NSLOT, D], F32, kind=sk).ap()
    table = nc.dram_tensor("table", [NSLOT, 1], I32, kind=sk).ap()

    b2_rows = b2.rearrange("l e d -> (l e) d")

    P = Prog(nc)
    top = ExitStack()
    _regs = {}

    def breg(e, val):
        if val not in _regs:
            _regs[val] = e.to_reg(val)
        return _regs[val]

    uniq = [0]

    def sbt(st, name, shape, dt):
        uniq[0] += 1
        return st.enter_context(nc.sbuf_tensor(f"{name}_{uniq[0]}", list(shape), dt))

    def pst(st, name, shape, dt):
        uniq[0] += 1
        return st.enter_context(nc.psum_tensor(f"{name}_{uniq[0]}", list(shape), dt))

    with top:
        ident_bf = sbt(top, "ident_bf", [128, 128], BF16)
        ident_f = sbt(top, "ident_f", [128, 128], F32)
        tri = sbt(top, "tri", [128, 128], BF16)
        ones = sbt(top, "ones", [128, 128], BF16)
        tokid = sbt(top, "tokid", [128, NT], I32)
        kvidx = sbt(top, "kvidx_sb", [128, NU * 16], I32)
        outidx = sbt(top, "outidx_sb", [128, NU * 16], I32)
        gates = sbt(top, "gates", [128, NT, 2], F32)
        slot0 = sbt(top, "slot0", [128, NT], I32)
        slot1 = sbt(top, "slot1", [128, NT], I32)
        mhalf = sbt(top, "mhalf", [128, 1], F32)
        widx1 = sbt(top, "widx1", [128, NBLK, 8], I32)
        widx2 = sbt(top, "widx2", [128, NBLK, 16], I32)
        bidx1 = sbt(top, "bidx1", [128, NBLK], I32)
        bidx2 = sbt(top, "bidx2", [128, NBLK], I32)
        r_const = P.res("const")
        r_gates = P.res("gates")
        r_slots = P.res("slots")
        r_widx = P.res("widx")
        for t, s in ((ident_bf, c_ident_bf), (ident_f, c_ident_f), (tri, c_tri), (ones, c_ones),
                     (tokid, c_tokid), (kvidx, kvidx_d), (outidx, outidx_d)):
            P.dma("sp", lambda e, t=t, s=s: e.dma_start(out=t[:], in_=s), [], [r_const], r_const)
        P.op("dve", lambda e: e.memset(mhalf[:], -0.5), [], [r_const])
        P.barrier()

        def emit_ln_multi(items, g_t, b_t, cread=()):
            def step(eng, mk, rd, wr):
                for it in items:
                    P.op(eng, mk(it), rd(it), wr(it))
            step("dve", lambda it: (lambda e: e.bn_stats(out=it[2][0][:, 0, :], in_=it[0][:, 0:512])), lambda it: [it[1]], lambda it: [it[3]])
            step("dve", lambda it: (lambda e: e.bn_stats(out=it[2][0][:, 1, :], in_=it[0][:, 512:1024])), lambda it: [it[1]], lambda it: [it[3]])
            step("dve", lambda it: (lambda e: e.bn_aggr(out=it[2][1][:], in_=it[2][0][:].rearrange("p a b -> p (a b)"))), lambda it: [it[3]], lambda it: [it[3]])
            step("dve", lambda it: (lambda e: e.tensor_scalar(out=it[2][2][:], in0=it[2][1][:, 1:2], scalar1=EPS, scalar2=None, op0=ALU.add)),
                 lambda it: [it[3]], lambda it: [it[3]])
            step("act", lambda it: (lambda e: e.activation(out=it[2][2][:], in_=it[2][2][:], func=AF.Ln)), lambda it: [it[3]], lambda it: [it[3]])
            step("act", lambda it: (lambda e: e.activation(out=it[2][2][:], in_=it[2][2][:], func=AF.Exp, scale=-0.5)), lambda it: [it[3]], lambda it: [it[3]])
            step("dve", lambda it: (lambda e: e.tensor_scalar(out=it[2][3][:], in0=it[2][1][:, 0:1], scalar1=it[2][2][:, 0:1], scalar2=-1.0,
                                                             op0=ALU.mult, op1=ALU.mult)), lambda it: [it[3]], lambda it: [it[3]])
            step("act", lambda it: (lambda e: e.activation(out=it[0], in_=it[0], func=AF.Identity, bias=it[2][3][:, 0:1], scale=it[2][2][:, 0:1])),
                 lambda it: [it[1], it[3]], lambda it: [it[1]])
            step("dve", lambda it: (lambda e: e.tensor_tensor(out=it[0], in0=it[0], in1=g_t, op=ALU.mult)), lambda it: [it[1]] + list(cread), lambda it: [it[1]])
            step("dve", lambda it: (lambda e: e.tensor_tensor(out=it[0], in0=it[0], in1=b_t, op=ALU.add)), lambda it: [it[1]] + list(cread), lambda it: [it[1]])

        def emit_ln(xt, r_x, g_t, b_t, scr, r_scr, cread=()):
            emit_ln_multi([(xt, r_x, scr, r_scr)], g_t, b_t, cread=cread)

        def ln_scratch(st, name):
            return (sbt(st, name + "_s6", [128, 2, 6], F32), sbt(st, name + "_mv", [128, 2], F32),
                    sbt(st, name + "_rs", [128, 1], F32), sbt(st, name + "_nm", [128, 1], F32))

        def _pass(st, l=(l if "l" in dir() else 0), last=(last if "last" in dir() else False)):
            g_t = sbt(st, "p0_g", [128, D], F32)
            b_t = sbt(st, "p0_b", [128, D], F32)
            r_gb = P.res("p0_gb")
            P.dma("sp", lambda e: e.dma_start(out=g_t[:], in_=ln_in_g[None, :].to_broadcast([128, D])), [], [r_gb], r_gb)
            P.dma("sp", lambda e: e.dma_start(out=b_t[:], in_=ln_in_b[None, :].to_broadcast([128, D])), [], [r_gb], r_gb)
            NB = 6
            xts = [sbt(st, f"p0_x{i}", [128, D], F32) for i in range(NB)]
            rxs = [P.res(f"p0_x{i}") for i in range(NB)]
            scrs = [ln_scratch(st, f"p0_l{i}") for i in range(NB)]
            rss = [P.res(f"p0_s{i}") for i in range(NB)]
            for i0 in range(0, NT, 3):
                grp = list(range(i0, min(i0 + 3, NT)))
                for i in grp:
                    k = i % NB
                    P.dma("sp", lambda e, xt=xts[k], i=i: e.dma_start(out=xt[:], in_=x_in[i * 128:(i + 1) * 128, :]), [], [rxs[k]], rxs[k])
                emit_ln_multi([(xts[i % NB][:], rxs[i % NB], scrs[i % NB], rss[i % NB]) for i in grp], g_t[:], b_t[:], cread=[r_gb])
                for i in grp:
                    k = i % NB
                    P.dma("sp", lambda e, xt=xts[k], i=i: e.dma_start(out=xa[i * 128:(i + 1) * 128, :], in_=xt[:]), [rxs[k]], [], rxs[k])
            P.barrier()
            P.release(rxs + [r_gb])

        with ExitStack() as _st:
            _pass(_st)
        for l in range(depth):
            last = (l == depth - 1)
            def _pass(st, l=(l if "l" in dir() else 0), last=(last if "last" in dir() else False)):
                wA = sbt(st, "wA", [128, 8, 1536], BF16)
                r_wA = P.res("wA")
                for kc in range(8):
                    P.dma("pool", lambda e, kc=kc: e.dma_start(out=wA[:, kc, :], in_=w_in[l, kc * 128:(kc + 1) * 128, 512:2048]),
                          [], [r_wA], r_wA)
                bcol = sbt(st, "A_bcol", [128, 32], F32)
                bv = sbt(st, "A_bv", [128, 512], F32)
                r_bA = P.res("A_b")
                P.dma("sp", lambda e: e.dma_start(out=bcol[:], in_=b_in[l].rearrange("(c p) -> p c", p=128),
                                                  allow_slow_non_contiguous=True), [], [r_bA], r_bA)
                P.dma("sp", lambda e: e.dma_start(out=bv[:], in_=b_in[l, 1024:1536][None, :].to_broadcast([128, 512])), [], [r_bA], r_bA)
                NB = 2
                xbf = [sbt(st, f"A_xbf{i}", [128, 4, D], BF16) for i in range(NB)]
                r_xbf = [P.res(f"A_xbf{i}") for i in range(NB)]
                xT = [sbt(st, f"A_xT{i}", [128, 8, 512], BF16) for i in range(NB)]
                r_xT = [P.res(f"A_xT{i}") for i in range(NB)]
                kTc = [sbt(st, f"A_kT{i}", [128, 4, 512], BF16) for i in range(NB)]
                r_kTc = [P.res(f"A_kT{i}") for i in range(NB)]
                pTc = [sbt(st, f"A_pT{i}", [128, 4, 512], BF16) for i in range(NB)]
                r_pTc = [P.res(f"A_pT{i}") for i in range(NB)]
                Vc = [sbt(st, f"A_V{i}", [128, 4, 512], BF16) for i in range(NB)]
                r_Vc = [P.res(f"A_V{i}") for i in range(NB)]
                psT = [pst(st, f"A_psT{i}", [128, 8, 128], BF16) for i in range(2)]
                r_psT = [P.res(f"A_psT{i}") for i in range(2)]
                psA = [pst(st, f"A_ps{i}", [128, 512], F32) for i in range(4)]
                r_psA = [P.res(f"A_ps{i}") for i in range(4)]
                nps = 0
                npt = 0
                for u in range(NU):
                    for c in range(4):
                        k = (u * 4 + c) % NB
                        for j in range(4):
                            col = u * 16 + c * 4 + j
                            P.dma("pool", lambda e, k=k, j=j, col=col: e.indirect_dma_start(
                                out=xbf[k][:, j, :], out_offset=None, in_=xa[:, :],
                                in_offset=bass.IndirectOffsetOnAxis(ap=kvidx[:, col:col + 1], axis=0)),
                                [r_const], [r_xbf[k]], r_xbf[k])
                        for j in range(4):
                            q = npt % 2
                            npt += 1
                            for kc in range(8):
                                P.op("pe", lambda e, q=q, k=k, j=j, kc=kc: e.transpose(
                                    out=psT[q][:, kc, :], in_=xbf[k][:, j, kc * 128:(kc + 1) * 128], identity=ident_bf[:]),
                                    [r_xbf[k], r_const], [r_psT[q]])
                            eng = "act" if j % 2 == 0 else "dve"
                            if eng == "act":
                                P.op("act", lambda e, q=q, k=k, j=j: e.copy(out=xT[k][:, :, j * 128:(j + 1) * 128], in_=psT[q][:]),
                                     [r_psT[q]], [r_xT[k]])
                            else:
                                P.op("dve", lambda e, q=q, k=k, j=j: e.tensor_copy(out=xT[k][:, :, j * 128:(j + 1) * 128], in_=psT[q][:]),
                                     [r_psT[q]], [r_xT[k]])
                        for m in range(4):
                            q = nps % 4
                            nps += 1
                            for kc in range(8):
                                P.op("pe", lambda e, q=q, k=k, m=m, kc=kc: e.matmul(
                                    psA[q][:], lhsT=wA[:, kc, m * 128:(m + 1) * 128], rhs=xT[k][:, kc, :],
                                    start=(kc == 0), stop=(kc == 7)), [r_wA, r_xT[k]], [r_psA[q]])
                            P.op("act", lambda e, q=q, k=k, m=m: e.activation(
                                out=kTc[k][:, m, :], in_=psA[q][:], func=AF.Identity, bias=bcol[:, 4 + m:5 + m], scale=1.0),
                                [r_psA[q], r_bA], [r_kTc[k]])
                        for m in range(4):
                            q = nps % 4
                            nps += 1
                            for kc in range(8):
                                P.op("pe", lambda e, q=q, k=k, m=m, kc=kc: e.matmul(
                                    psA[q][:], lhsT=wA[:, kc, 1024 + m * 128:1024 + (m + 1) * 128], rhs=xT[k][:, kc, :],
                                    start=(kc == 0), stop=(kc == 7)), [r_wA, r_xT[k]], [r_psA[q]])
                            P.op("act", lambda e, q=q, k=k, m=m: e.activation(
                                out=pTc[k][:, m, :], in_=psA[q][:], func=AF.Identity, bias=bcol[:, 12 + m:13 + m], scale=1.0),
                                [r_psA[q], r_bA], [r_pTc[k]])
                        for j in range(4):
                            q = nps % 4
                            nps += 1
                            for kc in range(8):
                                P.op("pe", lambda e, q=q, k=k, j=j, kc=kc: e.matmul(
                                    psA[q][:], lhsT=xT[k][:, kc, j * 128:(j + 1) * 128], rhs=wA[:, kc, 512:1024],
                                    start=(kc == 0), stop=(kc == 7)), [r_wA, r_xT[k]], [r_psA[q]])
                            P.op("dve", lambda e, q=q, k=k, j=j: e.tensor_tensor(
                                out=Vc[k][:, j, :], in0=psA[q][:], in1=bv[:], op=ALU.add),
                                [r_psA[q], r_bA], [r_Vc[k]])
                        P.dma("sp", lambda e, k=k, u=u, c=c: e.dma_start(
                            out=kvp[u, :, 0, :].rearrange("p (m t) -> p m t", m=4)[:, :, c * 512:(c + 1) * 512], in_=kTc[k][:]),
                            [r_kTc[k]], [], r_kTc[k])
                        P.dma("sp", lambda e, k=k, u=u, c=c: e.dma_start(
                            out=kvp[u, :, 2, :].rearrange("p (m t) -> p m t", m=4)[:, :, c * 512:(c + 1) * 512], in_=pTc[k][:]),
                            [r_pTc[k]], [], r_pTc[k])
                        P.dma("sp", lambda e, k=k, u=u, c=c: e.dma_start(
                            out=kvp[u, :, 1, c * 2048:(c + 1) * 2048], in_=Vc[k][:].rearrange("p j f -> p (j f)")),
                            [r_Vc[k]], [], r_Vc[k])
                P.barrier()
                P.release([r_wA, r_bA] + r_xbf + r_kTc + r_pTc + r_Vc)

            with ExitStack() as _st:
                _pass(_st)
            def _pass(st, l=(l if "l" in dir() else 0), last=(last if "last" in dir() else False)):
                CH = 256
                RPC = CH // 64
                TPC = CH // 128
                NCH = 2048 // CH
                EXT = CH + 16
                wB = sbt(st, "wB", [128, 8, 2560], BF16)
                woa = sbt(st, "woa", [128, 4, D], BF16)
                wop = sbt(st, "wop", [128, 4, D], BF16)
                wout = sbt(st, "wout", [128, 8, D], BF16)
                wpl = sbt(st, "wpl", [128, 4, 128], BF16)
                r_wB = P.res("wB")
                for kc in range(8):
                    P.dma("pool", lambda e, kc=kc: e.dma_start(out=wB[:, kc, 0:512], in_=w_in[l, kc * 128:(kc + 1) * 128, 0:512]), [], [r_wB], r_wB)
                    P.dma("pool", lambda e, kc=kc: e.dma_start(out=wB[:, kc, 512:2560], in_=w_in[l, kc * 128:(kc + 1) * 128, 2048:4096]), [], [r_wB], r_wB)
                    P.dma("pool", lambda e, kc=kc: e.dma_start(out=wout[:, kc, :], in_=w_out[l, kc * 128:(kc + 1) * 128, :]), [], [r_wB], r_wB)
                for kc in range(4):
                    P.dma("pool", lambda e, kc=kc: e.dma_start(out=woa[:, kc, :], in_=w_oa[l, kc * 128:(kc + 1) * 128, :]), [], [r_wB], r_wB)
                    P.dma("pool", lambda e, kc=kc: e.dma_start(out=wop[:, kc, :], in_=w_op[l, kc * 128:(kc + 1) * 128, :]), [], [r_wB], r_wB)
                    P.dma("pool", lambda e, kc=kc: e.dma_start(out=wpl[:, kc, :], in_=w_pool[l, kc, :, :]), [], [r_wB], r_wB)
                bcol = sbt(st, "B_bcol", [128, 32], F32)
                bq8 = sbt(st, "B_bq8", [128, 4], F32)
                psc = sbt(st, "B_psc", [128, 4], F32)
                bpc = sbt(st, "B_bpc", [128, 4], F32)
                bout_f = sbt(st, "B_boutf", [1, D], F32)
                bout = sbt(st, "B_bout", [1, D], BF16)
                g1 = sbt(st, "B_g1", [128, D], F32)
                b1t = sbt(st, "B_b1", [128, D], F32)
                invc = sbt(st, "B_invc", [128, 4, 16], F32)
                maskt = sbt(st, "B_mask", [128, 64], F32)
                Tb = sbt(st, "B_Tb", [128, 4, 15, 64], BF16)
                r_cB = P.res("B_c")
                P.dma("sp", lambda e: e.dma_start(out=bcol[:], in_=b_in[l].rearrange("(c p) -> p c", p=128),
                                                  allow_slow_non_contiguous=True), [], [r_cB], r_cB)
                P.dma("sp", lambda e: e.dma_start(out=psc[:], in_=pool_scale[l].rearrange("(c p) -> p c", p=128),
                                                  allow_slow_non_contiguous=True), [], [r_cB], r_cB)
                P.dma("sp", lambda e: e.dma_start(out=bpc[:], in_=b_pool[l].rearrange("c p -> p c"),
                                                  allow_slow_non_contiguous=True), [], [r_cB], r_cB)
                P.dma("sp", lambda e: e.dma_start(out=bout_f[:], in_=b_out[l][None, :]), [], [r_cB], r_cB)
                P.dma("sp", lambda e: e.dma_start(out=g1[:], in_=ln1_g[l][None, :].to_broadcast([128, D])), [], [r_cB], r_cB)
                P.dma("sp", lambda e: e.dma_start(out=b1t[:], in_=ln1_b[l][None, :].to_broadcast([128, D])), [], [r_cB], r_cB)
                P.dma("sp", lambda e: e.dma_start(out=invc[:], in_=c_invc), [], [r_cB], r_cB)
                P.dma("sp", lambda e: e.dma_start(out=maskt[:], in_=c_mask), [], [r_cB], r_cB)
                for hp in range(4):
                    P.dma("pool", lambda e, hp=hp: e.dma_start(out=Tb[:, hp, :, :].rearrange("p a b -> p (a b)"),
                                                               in_=rpbT[l, :, hp * 960:(hp + 1) * 960]), [], [r_cB], r_cB)
                P.op("dve", lambda e: e.tensor_tensor(
                    out=Tb[:].rearrange("p a b c -> p (a b) c"), in0=Tb[:].rearrange("p a b c -> p (a b) c"),
                    in1=maskt[:][:, None, :].to_broadcast([128, 60, 64]), op=ALU.add), [r_cB], [r_cB])
                P.op("dve", lambda e: e.tensor_scalar(out=bq8[:], in0=bcol[:, 0:4], scalar1=0.125, scalar2=None, op0=ALU.mult), [r_cB], [r_cB])
                P.op("dve", lambda e: e.tensor_tensor(out=bpc[:], in0=bpc[:], in1=psc[:], op=ALU.mult), [r_cB], [r_cB])
                P.op("dve", lambda e: e.tensor_copy(out=bout[:], in_=bout_f[:]), [r_cB], [r_cB])

                kT = sbt(st, "B_kT", [128, 4, 2048], BF16)
                Vt = sbt(st, "B_V", [128, 16, 512], BF16)
                r_kT, r_V = P.res("B_kT"), P.res("B_V")
                NXR = 4
                xres = [sbt(st, f"B_xres{i}", [128, D], F32) for i in range(NXR)]
                r_xres = [P.res(f"B_xres{i}") for i in range(NXR)]
                xbf = [sbt(st, f"B_xbf{i}", [128, D], BF16) for i in range(4)]
                r_xbf = [P.res(f"B_xbf{i}") for i in range(4)]

                def dbl(name, shape, dt):
                    t_, r_ = sbt(st, name, shape, dt), P.res(name)
                    return [t_, t_], [r_, r_]
                xT, r_xT = dbl("B_xT", [128, 8, CH], BF16)
                qT, r_qT = dbl("B_qT", [128, 4, CH], BF16)
                aT, r_aT = dbl("B_aT", [128, 4, CH], BF16)
                dT, r_dT = dbl("B_dT", [128, 4, CH], BF16)
                _tpmT = sbt(st, "B_pmT", [128, 4, CH], BF16)
                _rpmT = P.res("B_pmT")
                pmT, r_pmT = [_tpmT, _tpmT], [_rpmT, _rpmT]
                _m = sbt(st, "B_mixT", [128, 8, CH], BF16)
                _rm = P.res("B_mixT")
                mixT, r_mixT = [_m, _m], [_rm, _rm]
                _tpex = sbt(st, "B_pex", [128, 4, EXT], BF16)
                _rpex = P.res("B_pex")
                pex, r_pex = [_tpex, _tpex], [_rpex, _rpex]
                NA = 3
                Eb = [sbt(st, f"B_E{i}", [128, 512], BF16) for i in range(NA)]
                r_Eb = [P.res(f"B_E{i}") for i in range(NA)]
                rsum = [sbt(st, f"B_rs{i}", [128, 2], F32) for i in range(NA)]
                Pb = [sbt(st, f"B_P{i}", [128, 640], BF16) for i in range(NA)]
                r_Pb = [P.res(f"B_P{i}") for i in range(NA)]
                PT = [sbt(st, f"B_PT{i}", [128, 5, 128], BF16) for i in range(NA)]
                r_PT = [P.res(f"B_PT{i}") for i in range(NA)]
                pe_ = sbt(st, "B_pe", [128, EXT], F32)
                sA = sbt(st, "B_sA", [128, EXT], F32)
                sB_ = sbt(st, "B_sB", [128, EXT], F32)
                r_pl = P.res("B_pl")
                sg = [sbt(st, f"B_sg{i}", [128, CH], BF16) for i in range(2)]
                r_sg = [P.res(f"B_sg{i}") for i in range(2)]
                tm = [sbt(st, f"B_tm{i}", [128, CH], F32) for i in range(4)]
                r_tm = [P.res(f"B_tm{i}") for i in range(4)]
                lsc = [ln_scratch(st, f"B_l{i}") for i in range(NXR)]
                r_lsc = [P.res(f"B_ls{i}") for i in range(NXR)]
                psT = pst(st, "B_psT", [128, 8, 128], BF16)
                r_psT = P.res("B_psT")
                psP = [pst(st, f"B_psP{i}", [128, 512], F32) for i in range(3)]
                r_psP = [P.res(f"B_psP{i}") for i in range(3)]
                psS = [pst(st, f"B_psS{i}", [128, 512], F32) for i in range(2)]
                r_psS = [P.res(f"B_psS{i}") for i in range(2)]
                psPT = pst(st, "B_psPT", [128, 8, 128], BF16)
                r_psPT = P.res("B_psPT")
                psAT = pst(st, "B_psAT", [128, 512], F32)
                r_psAT = P.res("B_psAT")
                for i in range(NA):
                    P.op("dve", lambda e, i=i: e.memset(Pb[i][:], 0.0), [], [r_Pb[i]])
                npp = 0
                nat = 0
                nps = 0
                nxr = 0
                nxb = 0
                ncc = 0
                ntm = 0
                def gathers(gi):
                    for j in range(TPC):
                        col = gi * TPC + j
                        kb = col % 4
                        P.dma("pool", lambda e, kb=kb, col=col: e.indirect_dma_start(
                            out=xbf[kb][:, :], out_offset=None, in_=xa[:, :],
                            in_offset=bass.IndirectOffsetOnAxis(ap=kvidx[:, col:col + 1], axis=0)),
                            [r_const], [r_xbf[kb]], r_xbf[kb])
                    for j in range(TPC):
                        col = gi * TPC + j
                        k = col % NXR
                        P.dma("pool", lambda e, k=k, col=col: e.indirect_dma_start(
                            out=xres[k][:, :], out_offset=None, in_=xa[:, :],
                            in_offset=bass.IndirectOffsetOnAxis(ap=kvidx[:, col:col + 1], axis=0)),
                            [r_const], [r_xres[k]], r_xres[k])

                gathers(0)
                for u in range(NU):
                    P.dma("sp", lambda e, u=u: e.dma_start(out=kT[:].rearrange("p m t -> p (m t)"), in_=kvp[u, :, 0, :]), [], [r_kT], r_kT)
                    P.dma("sp", lambda e, u=u: e.dma_start(out=Vt[:].rearrange("p m t -> p (m t)"), in_=kvp[u, :, 1, :]), [], [r_V], r_V)
                    for c in range(NCH):
                        cc = ncc % 2
                        ncc += 1
                        gi = u * NCH + c
                        if gi + 1 < NU * NCH:
                            gathers(gi + 1)
                        for j in range(TPC):
                            col = u * 16 + c * TPC + j
                            kb = col % 4
                            for kc in range(8):
                                P.op("pe", lambda e, kb=kb, kc=kc: e.transpose(
                                    out=psT[:, kc, :], in_=xbf[kb][:, kc * 128:(kc + 1) * 128], identity=ident_bf[:]),
                                    [r_xbf[kb], r_const], [r_psT])
                            P.op("dve", lambda e, j=j, cc=cc: e.tensor_copy(out=xT[cc][:, :, j * 128:(j + 1) * 128], in_=psT[:]),
                                 [r_psT], [r_xT[cc]])
                        xk = [(u * 16 + c * TPC + j) % NXR for j in range(TPC)]
                        lo_t = max(c * CH - 8, 0)
                        hi_t = min(c * CH + CH + 8, 2048)
                        eo = lo_t - (c * CH - 8)
                        nn = hi_t - lo_t
                        P.op("pool", lambda e, cc=cc: e.memset(pex[cc][:], 0.0), [], [r_pex[cc]])
                        P.dma("sp", lambda e, u=u, lo_t=lo_t, hi_t=hi_t, eo=eo, nn=nn, cc=cc: e.dma_start(
                            out=pex[cc][:, :, eo:eo + nn],
                            in_=kvp[u, :, 2, :].rearrange("p (m t) -> p m t", m=4)[:, :, lo_t:hi_t]), [], [r_pex[cc]], r_pex[cc])
                        for g in range(4):
                            wv = 2 ** (g + 1)
                            P.op("pool", lambda e, g=g, cc=cc: e.tensor_copy(out=pe_[:], in_=pex[cc][:, g, :]), [r_pex[cc]], [r_pl])
                            P.op("pool", lambda e: e.memset(sA[:], 0.0), [], [r_pl])
                            P.op("pool", lambda e: e.tensor_tensor(out=sA[:, 1:EXT], in0=pe_[:, 0:EXT - 1], in1=pe_[:, 1:EXT], op=ALU.add), [r_pl], [r_pl])
                            cur, oth = sA, sB_
                            for sh in (1, 2, 4)[:g]:
                                P.op("pool", lambda e, oth=oth: e.memset(oth[:], 0.0), [], [r_pl])
                                P.op("pool", lambda e, cur=cur, oth=oth, sh=sh: e.tensor_tensor(
                                    out=oth[:, sh:EXT - sh], in0=cur[:, 0:EXT - 2 * sh], in1=cur[:, 2 * sh:EXT], op=ALU.add), [r_pl], [r_pl])
                                cur, oth = oth, cur
                            P.op("pool", lambda e, cur=cur, oth=oth, wv=wv: e.tensor_scalar(
                                out=oth[:, 8:8 + CH], in0=cur[:, 8:8 + CH], scalar1=1.0 / wv, scalar2=0.0, op0=ALU.mult, op1=ALU.add), [r_pl], [r_pl])
                            if c == 0:
                                P.op("pool", lambda e, cur=cur, oth=oth, g=g: e.tensor_tensor(
                                    out=oth[:, 8:16], in0=cur[:, 8:16], in1=invc[:, g, 0:8], op=ALU.mult), [r_pl, r_cB], [r_pl])
                            if c == NCH - 1:
                                P.op("pool", lambda e, cur=cur, oth=oth, g=g: e.tensor_tensor(
                                    out=oth[:, CH:CH + 8], in0=cur[:, CH:CH + 8], in1=invc[:, g, 8:16], op=ALU.mult), [r_pl, r_cB], [r_pl])
                            P.op("pool", lambda e, oth=oth, g=g, cc=cc: e.tensor_tensor(
                                out=dT[cc][:, g, :], in0=oth[:, 8:8 + CH], in1=pe_[:, 8:8 + CH], op=ALU.subtract), [r_pl], [r_dT[cc]])
                        for m in range(4):
                            q = npp % 3
                            npp += 1
                            for kc in range(8):
                                P.op("pe", lambda e, q=q, m=m, kc=kc, cc=cc: e.matmul(
                                    psP[q][:, 0:CH], lhsT=wB[:, kc, m * 128:(m + 1) * 128], rhs=xT[cc][:, kc, :],
                                    start=(kc == 0), stop=(kc == 7)), [r_wB, r_xT[cc]], [r_psP[q]])
                            P.op("act", lambda e, q=q, m=m, cc=cc: e.activation(
                                out=qT[cc][:, m, :], in_=psP[q][:, 0:CH], func=AF.Identity, bias=bq8[:, m:m + 1], scale=0.125),
                                [r_psP[q], r_cB], [r_qT[cc]])
                        items = [(hp, r8) for hp in range(4) for r8 in range(RPC)]

                        def att_geom(r8):
                            ql = c * RPC + r8
                            ws = min(max(ql - 4, 0), 24)
                            dr0 = ws - ql + 7
                            if ws % 2 == 0:
                                return ws, dr0, 4, 64, ws // 2
                            return ws, dr0, 5, 0, (ws - 1) // 2

                        def S1(t):
                            hp, r8 = items[t]
                            ws, dr0, nch, off, vt0 = att_geom(r8)
                            k = (nat + t) % NA
                            ks = (nat + t) % 2
                            for hh in range(2):
                                lo, hi = hh * 64, (hh + 1) * 64
                                P.op("pe", lambda e, ks=ks, hp=hp, r8=r8, ws=ws, lo=lo, hi=hi, cc=cc: e.matmul(
                                    psS[ks][lo:hi, :], lhsT=qT[cc][lo:hi, hp, r8 * 64:(r8 + 1) * 64],
                                    rhs=kT[lo:hi, hp, ws * 64:ws * 64 + 512], start=True, stop=False),
                                    [r_qT[cc], r_kT], [r_psS[ks]])
                            P.op("pe", lambda e, ks=ks, hp=hp, dr0=dr0: e.matmul(
                                psS[ks][:], lhsT=ident_bf[:], rhs=Tb[:, hp, dr0:dr0 + 8, :].rearrange("p a b -> p (a b)"),
                                start=False, stop=True), [r_cB, r_const], [r_psS[ks]])
                            P.op("act", lambda e, k=k, ks=ks: e.activation(
                                out=Eb[k][:], in_=psS[ks][:], func=AF.Exp, accum_out=rsum[k][:, 0:1]),
                                [r_psS[ks]], [r_Eb[k]])
                            P.op("dve", lambda e, k=k: e.reciprocal(out=rsum[k][:, 1:2], in_=rsum[k][:, 0:1]),
                                 [r_Eb[k]], [r_Eb[k]])
                            P.op("dve", lambda e, k=k: e.tensor_scalar(
                                out=Pb[k][:, 64:576], in0=Eb[k][:], scalar1=rsum[k][:, 1:2], scalar2=None, op0=ALU.mult),
                                [r_Eb[k]], [r_Pb[k]])

                        def S2(t):
                            hp, r8 = items[t]
                            ws, dr0, nch, off, vt0 = att_geom(r8)
                            k = (nat + t) % NA
                            for ch in range(nch):
                                P.op("pe", lambda e, k=k, ch=ch, off=off: e.transpose(
                                    out=psPT[:, ch, :], in_=Pb[k][:, off + ch * 128:off + (ch + 1) * 128], identity=ident_bf[:]),
                                    [r_Pb[k], r_const], [r_psPT])
                            P.op("act", lambda e, k=k, nch=nch: e.copy(out=PT[k][:, 0:nch, :], in_=psPT[:, 0:nch, :]),
                                 [r_psPT], [r_PT[k]])

                        def S3(t):
                            hp, r8 = items[t]
                            ws, dr0, nch, off, vt0 = att_geom(r8)
                            k = (nat + t) % NA
                            for hh in range(2):
                                lo, hi = hh * 64, (hh + 1) * 64
                                hcol = (2 * hp + hh) * 64
                                for ch in range(nch):
                                    P.op("pe", lambda e, k=k, ch=ch, lo=lo, hi=hi, hcol=hcol, r8=r8, vt0=vt0, nch=nch: e.matmul(
                                        psAT[lo:hi, r8 * 64:(r8 + 1) * 64], lhsT=Vt[:, vt0 + ch, hcol:hcol + 64],
                                        rhs=PT[k][:, ch, lo:hi], start=(ch == 0), stop=(ch == nch - 1)),
                                        [r_V, r_PT[k]], [r_psAT])
                            if r8 == RPC - 1:
                                P.op("dve", lambda e, hp=hp, cc=cc: e.tensor_copy(out=aT[cc][:, hp, :], in_=psAT[:, 0:CH]), [r_psAT], [r_aT[cc]])

                        NI = len(items)
                        for t in range(NI + 2):
                            if t < NI:
                                S1(t)
                            if 0 <= t - 1 < NI:
                                S2(t - 1)
                            if 0 <= t - 2 < NI:
                                S3(t - 2)
                        nat += NI
                        for g in range(4):
                            q = npp % 3
                            npp += 1
                            P.op("pe", lambda e, q=q, g=g, cc=cc: e.matmul(psP[q][:, 0:CH], lhsT=wpl[:, g, :], rhs=dT[cc][:, g, :], start=True, stop=True),
                                 [r_wB, r_dT[cc]], [r_psP[q]])
                            P.op("act", lambda e, q=q, g=g, cc=cc: e.activation(
                                out=pmT[cc][:, g, :], in_=psP[q][:, 0:CH], func=AF.Identity, bias=bpc[:, g:g + 1], scale=psc[:, g:g + 1]),
                                [r_psP[q], r_cB], [r_pmT[cc]])
                        for m in range(8):
                            tk = []
                            for br in range(2):
                                q = npp % 3
                                npp += 1
                                wc = 512 + br * 1024 + m * 128
                                for kc in range(8):
                                    P.op("pe", lambda e, q=q, wc=wc, kc=kc, cc=cc: e.matmul(
                                        psP[q][:, 0:CH], lhsT=wB[:, kc, wc:wc + 128], rhs=xT[cc][:, kc, :], start=(kc == 0), stop=(kc == 7)),
                                        [r_wB, r_xT[cc]], [r_psP[q]])
                                bc = 16 + br * 8 + m
                                P.op("act", lambda e, q=q, br=br, bc=bc: e.activation(
                                    out=sg[br][:], in_=psP[q][:, 0:CH], func=AF.Sigmoid, bias=bcol[:, bc:bc + 1], scale=1.0),
                                    [r_psP[q], r_cB], [r_sg[br]])
                                q = npp % 3
                                npp += 1
                                wsrc, asrc, r_a = (woa, aT[cc], r_aT[cc]) if br == 0 else (wop, pmT[cc], r_pmT[cc])
                                for kc in range(4):
                                    P.op("pe", lambda e, q=q, wsrc=wsrc, asrc=asrc, m=m, kc=kc: e.matmul(
                                        psP[q][:, 0:CH], lhsT=wsrc[:, kc, m * 128:(m + 1) * 128], rhs=asrc[:, kc, :], start=(kc == 0), stop=(kc == 3)),
                                        [r_wB, r_a], [r_psP[q]])
                                t_ = ntm % 4
                                ntm += 1
                                tk.append(t_)
                                P.op("dve", lambda e, q=q, br=br, t_=t_: e.tensor_tensor(out=tm[t_][:], in0=psP[q][:, 0:CH], in1=sg[br][:], op=ALU.mult),
                                     [r_psP[q], r_sg[br]], [r_tm[t_]])
                            P.op("pool", lambda e, m=m, tk=tk, cc=cc: e.tensor_tensor(out=mixT[cc][:, m, :], in0=tm[tk[0]][:], in1=tm[tk[1]][:], op=ALU.add),
                                 [r_tm[tk[0]], r_tm[tk[1]]], [r_mixT[cc]])
                        for j in range(TPC):
                            k = xk[j]
                            for h in range(2):
                                q = npp % 3
                                npp += 1
                                for kc in range(8):
                                    P.op("pe", lambda e, q=q, j=j, h=h, kc=kc, cc=cc: e.matmul(
                                        psP[q][:], lhsT=mixT[cc][:, kc, j * 128:(j + 1) * 128], rhs=wout[:, kc, h * 512:(h + 1) * 512],
                                        start=(kc == 0), stop=False), [r_wB, r_mixT[cc]], [r_psP[q]])
                                P.op("pe", lambda e, q=q, h=h: e.matmul(
                                    psP[q][:], lhsT=ones[0:1, :], rhs=bout[0:1, h * 512:(h + 1) * 512], start=False, stop=True),
                                    [r_cB, r_const], [r_psP[q]])
                                P.op("dve", lambda e, q=q, k=k, h=h: e.scalar_tensor_tensor(
                                    out=xres[k][:, h * 512:(h + 1) * 512], in0=xres[k][:, h * 512:(h + 1) * 512], scalar=ALPHA,
                                    in1=psP[q][:], op0=ALU.mult, op1=ALU.add), [r_psP[q], r_xres[k]], [r_xres[k]])
                        emit_ln_multi([(xres[k][:], r_xres[k], lsc[k], r_lsc[k]) for k in xk], g1[:], b1t[:], cread=[r_cB])
                        for j in range(TPC):
                            col = u * 16 + c * TPC + j
                            k = xk[j]
                            P.dma("pool", lambda e, k=k, col=col: e.indirect_dma_start(
                                out=x1b[:, :], out_offset=bass.IndirectOffsetOnAxis(ap=outidx[:, col:col + 1], axis=0),
                                in_=xres[k][:, :], in_offset=None), [r_xres[k], r_const], [], r_xres[k])
                P.barrier()
                P.release([r_wB, r_cB, r_kT, r_V, r_pex[0]] + r_xbf + r_xres)

            with ExitStack() as _st:
                _pass(_st)
            def _pass(st, l=(l if "l" in dir() else 0), last=(last if "last" in dir() else False)):
                wr = sbt(st, "R_wr", [128, 8, NE], F32)
                brt = sbt(st, "R_br", [128, NE], F32)
                bst = sbt(st, "R_bst", [128, NBLK], F32)
                rowc = sbt(st, "R_rowc", [128, 16], F32)
                r_cR = P.res("R_c")
                P.dma("sp", lambda e: e.dma_start(out=wr[:], in_=w_router.rearrange("(k p) n -> p k n", p=128)), [], [r_cR], r_cR)
                P.dma("sp", lambda e: e.dma_start(out=brt[:], in_=b_router[None, :].to_broadcast([128, NE])), [], [r_cR], r_cR)
                P.dma("sp", lambda e: e.dma_start(out=bst[:], in_=c_bstart), [], [r_cR], r_cR)
                P.dma("sp", lambda e: e.dma_start(out=rowc[:], in_=c_rowc), [], [r_cR], r_cR)
                A0 = sbt(st, "R_A0", [128, NT, NE], F32)
                A1 = sbt(st, "R_A1", [128, NT, NE], F32)
                POS = sbt(st, "R_POS", [128, NT, NE], F32)
                r_A = P.res("R_A")
                base = sbt(st, "R_base", [128, NE], F32)
                r_base = P.res("R_base")
                P.op("dve", lambda e: e.memset(base[:], 0.0), [], [r_base])
                xt = [sbt(st, f"R_x{i}", [128, D], F32) for i in range(2)]
                r_xt = [P.res(f"R_x{i}") for i in range(2)]
                xT32 = [sbt(st, f"R_xT{i}", [128, 8, 128], F32) for i in range(2)]
                r_xT32 = [P.res(f"R_xT{i}") for i in range(2)]
                zt = [sbt(st, f"R_z{i}", [128, 16 * 8], F32) for i in range(2)]
                r_zt = [P.res(f"R_z{i}") for i in range(2)]
                sm = [sbt(st, f"R_sm{i}", [128, 32], F32) for i in range(2)]
                Abf = [sbt(st, f"R_Ab{i}", [128, NE], BF16) for i in range(2)]
                psX = [pst(st, f"R_psX{i}", [128, 8, 128], F32) for i in range(1)]
                r_psX = [P.res(f"R_psX{i}") for i in range(1)]
                psL_ = [pst(st, f"R_psL{i}", [128, 512], F32) for i in range(2)]
                psL = [t[:, 0:NE] for t in psL_]
                r_psL = [P.res(f"R_psL{i}") for i in range(2)]
                psC_ = [pst(st, f"R_psC{i}", [128, 512], F32) for i in range(2)]
                psC = [t[:, 0:2 * NE].rearrange("p (a b) -> p a b", a=2) for t in psC_]
                r_psC = [P.res(f"R_psC{i}") for i in range(2)]
                for i in range(NT):
                    k = i % 2
                    P.dma("sp", lambda e, k=k, i=i: e.dma_start(out=xt[k][:], in_=x1b[i * 128:(i + 1) * 128, :]), [], [r_xt[k]], r_xt[k])
                    for kc in range(8):
                        P.op("pe", lambda e, k=k, kc=kc: e.transpose(out=psX[0][:, kc, :], in_=xt[k][:, kc * 128:(kc + 1) * 128], identity=ident_f[:]),
                             [r_xt[k], r_const], [r_psX[0]])
                    P.op("act", lambda e, k=k: e.copy(out=xT32[k][:, 0:4, :], in_=psX[0][:, 0:4, :]), [r_psX[0]], [r_xT32[k]])
                    P.op("dve", lambda e, k=k: e.tensor_copy(out=xT32[k][:, 4:8, :], in_=psX[0][:, 4:8, :]), [r_psX[0]], [r_xT32[k]])
                    for kc in range(8):
                        P.op("pe", lambda e, k=k, kc=kc: e.matmul(psL[k], lhsT=xT32[k][:, kc, :], rhs=wr[:, kc, :], start=(kc == 0), stop=(kc == 7)),
                             [r_xT32[k], r_cR], [r_psL[k]])
                    z = zt[k]
                    Z = lambda a, z=z: z[:, a * 16:(a + 1) * 16]
                    Z4 = lambda a, z=z: z[:, a * 16:(a + 1) * 16].rearrange("p (g j) -> p g j", g=4)
                    s = sm[k]
                    rz = r_zt[k]
                    P.op("dve", lambda e, k=k, Z=Z: e.tensor_tensor(out=Z(0), in0=psL[k], in1=brt[:], op=ALU.add), [r_psL[k], r_cR], [rz])
                    P.op("dve", lambda e, Z=Z, s=s: e.tensor_reduce(out=s[:, 0:1], in_=Z(0), axis=AX.X, op=ALU.max, negate=True), [rz], [rz])
                    P.op("act", lambda e, Z=Z, s=s: e.activation(out=Z(1), in_=Z(0), func=AF.Exp, bias=s[:, 0:1], scale=1.0), [rz], [rz])
                    P.op("dve", lambda e, Z4=Z4, s=s: e.tensor_reduce(out=s[:, 4:8], in_=Z4(1), axis=AX.X, op=ALU.max), [rz], [rz])
                    P.op("dve", lambda e, Z4=Z4, s=s: e.tensor_tensor(out=Z4(2), in0=Z4(1), in1=s[:, 4:8][:, :, None].to_broadcast([128, 4, 4]), op=ALU.is_equal), [rz], [rz])
                    P.op("dve", lambda e, Z=Z: e.scalar_tensor_tensor(out=Z(3), in0=Z(2), scalar=-2.0, in1=Z(1), op0=ALU.mult, op1=ALU.add), [rz], [rz])
                    P.op("dve", lambda e, Z4=Z4, s=s: e.tensor_reduce(out=s[:, 8:12], in_=Z4(3), axis=AX.X, op=ALU.max), [rz], [rz])
                    P.op("dve", lambda e, Z4=Z4, s=s: e.tensor_tensor(out=Z4(4), in0=Z4(3), in1=s[:, 8:12][:, :, None].to_broadcast([128, 4, 4]), op=ALU.is_equal), [rz], [rz])
                    P.op("dve", lambda e, s=s: e.tensor_tensor(out=s[:, 12:16], in0=s[:, 4:8], in1=s[:, 8:12], op=ALU.add), [rz], [rz])
                    P.op("dve", lambda e, s=s: e.tensor_reduce(out=s[:, 1:2], in_=s[:, 12:16], axis=AX.X, op=ALU.max), [rz], [rz])
                    P.op("dve", lambda e, s=s: e.tensor_scalar(out=s[:, 16:20], in0=s[:, 12:16], scalar1=s[:, 1:2], scalar2=None, op0=ALU.is_equal), [rz], [rz])
                    P.op("dve", lambda e, Z4=Z4, s=s, i=i: e.tensor_tensor(out=A0[:, i, :].rearrange("p (g j) -> p g j", g=4), in0=Z4(2),
                                                                             in1=s[:, 16:20][:, :, None].to_broadcast([128, 4, 4]), op=ALU.mult), [rz], [r_A])
                    P.op("dve", lambda e, Z4=Z4, s=s, i=i: e.tensor_tensor(out=A1[:, i, :].rearrange("p (g j) -> p g j", g=4), in0=Z4(4),
                                                                             in1=s[:, 16:20][:, :, None].to_broadcast([128, 4, 4]), op=ALU.mult), [rz], [r_A])
                    P.op("dve", lambda e, s=s: e.tensor_tensor(out=s[:, 20:24], in0=s[:, 16:20], in1=s[:, 4:8], op=ALU.mult), [rz], [rz])
                    P.op("dve", lambda e, s=s: e.tensor_tensor(out=s[:, 24:28], in0=s[:, 16:20], in1=s[:, 8:12], op=ALU.mult), [rz], [rz])
                    P.op("dve", lambda e, s=s: e.tensor_reduce(out=s[:, 28:30], in_=s[:, 20:28].rearrange("p (a b) -> p a b", a=2), axis=AX.X, op=ALU.add), [rz], [rz])
                    P.op("dve", lambda e, s=s: e.tensor_reduce(out=s[:, 30:31], in_=s[:, 28:30], axis=AX.X, op=ALU.add), [rz], [rz])
                    P.op("dve", lambda e, s=s: e.reciprocal(out=s[:, 31:32], in_=s[:, 30:31]), [rz], [rz])
                    P.op("dve", lambda e, s=s, i=i: e.tensor_scalar(out=gates[:, i, :], in0=s[:, 28:30], scalar1=s[:, 31:32], scalar2=None, op0=ALU.mult), [rz], [r_gates])
                    P.op("dve", lambda e, k=k, i=i: e.tensor_tensor(out=Abf[k][:], in0=A0[:, i, :], in1=A1[:, i, :], op=ALU.add), [r_A], [rz])
                    P.op("pe", lambda e, k=k: e.matmul(psC[k][:, 0, :], lhsT=tri[:], rhs=Abf[k][:], start=True, stop=True), [rz, r_const], [r_psC[k]])
                    P.op("pe", lambda e, k=k: e.matmul(psC[k][:, 1, :], lhsT=ones[:], rhs=Abf[k][:], start=True, stop=True), [rz, r_const], [r_psC[k]])
                    P.op("dve", lambda e, k=k, i=i: e.tensor_tensor(out=POS[:, i, :], in0=psC[k][:, 0, :], in1=base[:], op=ALU.add), [r_psC[k], r_base], [r_A])
                    P.op("dve", lambda e, k=k: e.tensor_tensor(out=base[:], in0=psC[k][:, 1, :], in1=base[:], op=ALU.add), [r_psC[k], r_base], [r_base])
                ci = sbt(st, "R_ci", [128, NE], I32)
                pf = sbt(st, "R_pf", [128, 4, NE], F32)
                r_pf = P.res("R_pf")
                P.op("dve", lambda e: e.tensor_scalar(out=ci[:], in0=base[:], scalar1=float(BLK - 1), scalar2=None, op0=ALU.add), [r_base], [r_pf])
                P.op("dve", lambda e: e.tensor_scalar(out=ci[:], in0=ci[:], scalar1=9, scalar2=9, op0=ALU.arith_shift_right, op1=ALU.arith_shift_left), [r_pf], [r_pf])
                P.op("dve", lambda e: e.tensor_copy(out=pf[:, 0, :], in_=ci[:]), [r_pf], [r_pf])
                P.op("dve", lambda e: e.tensor_copy(out=pf[:, 1, :], in_=pf[:, 0, :]), [r_pf], [r_pf])
                for sh in (1, 2, 4, 8):
                    P.op("dve", lambda e: e.tensor_copy(out=pf[:, 2, :], in_=pf[:, 1, :]), [r_pf], [r_pf])
                    P.op("dve", lambda e, sh=sh: e.tensor_tensor(out=pf[:, 1, sh:NE], in0=pf[:, 2, sh:NE], in1=pf[:, 2, 0:NE - sh], op=ALU.add), [r_pf], [r_pf])
                P.op("dve", lambda e: e.tensor_tensor(out=pf[:, 3, :], in0=pf[:, 1, :], in1=pf[:, 0, :], op=ALU.subtract), [r_pf], [r_pf])
                P.op("dve", lambda e: e.tensor_tensor(out=POS[:], in0=POS[:], in1=pf[:, 3:4, :].to_broadcast([128, NT, NE]), op=ALU.add), [r_pf, r_A], [r_A])
                sf = sbt(st, "R_sf", [128, NT], F32)
                for Ak, sl in ((A0, slot0), (A1, slot1)):
                    P.op("dve", lambda e, Ak=Ak: e.tensor_tensor(out=Ak[:], in0=Ak[:], in1=POS[:], op=ALU.mult), [r_A], [r_A])
                    P.op("dve", lambda e, Ak=Ak: e.tensor_reduce(out=sf[:], in_=Ak[:], axis=AX.X, op=ALU.add), [r_A], [r_pf])
                    P.op("dve", lambda e, sl=sl: e.tensor_copy(out=sl[:], in_=sf[:]), [r_pf], [r_slots])
                eb = sbt(st, "R_eb", [128, NBLK], F32)
                tmpb = sbt(st, "R_tmpb", [128, NBLK], F32)
                r_eb = P.res("R_eb")
                P.op("dve", lambda e: e.memset(eb[:], 0.0), [], [r_eb])
                for ex in range(NE):
                    P.op("dve", lambda e, ex=ex: e.tensor_scalar(out=tmpb[:], in0=bst[:], scalar1=pf[:, 1, ex:ex + 1], scalar2=None, op0=ALU.is_ge), [r_pf, r_cR], [r_eb])
                    P.op("dve", lambda e: e.tensor_tensor(out=eb[:], in0=eb[:], in1=tmpb[:], op=ALU.add), [r_eb], [r_eb])
                P.op("dve", lambda e: e.tensor_scalar(out=eb[:], in0=eb[:], scalar1=float(NE - 1), scalar2=None, op0=ALU.min), [r_eb], [r_eb])
                chg = sbt(st, "R_chg", [128, NBLK], F32)
                ebo1 = sbt(st, "R_ebo1", [128, NBLK], F32)
                ebo2 = sbt(st, "R_ebo2", [128, NBLK], F32)
                P.op("dve", lambda e: e.memset(chg[:], 0.0), [], [r_eb])
                P.op("dve", lambda e: e.tensor_tensor(out=chg[:, 1:NBLK], in0=eb[:, 1:NBLK], in1=eb[:, 0:NBLK - 1], op=ALU.is_equal), [r_eb], [r_eb])
                P.op("dve", lambda e: e.tensor_scalar(out=chg[:], in0=chg[:], scalar1=float(2 ** 30), scalar2=None, op0=ALU.mult), [r_eb], [r_eb])
                P.op("dve", lambda e: e.scalar_tensor_tensor(out=ebo1[:], in0=eb[:], scalar=float(D), in1=chg[:], op0=ALU.mult, op1=ALU.add), [r_eb], [r_eb])
                P.op("dve", lambda e: e.scalar_tensor_tensor(out=ebo2[:], in0=eb[:], scalar=float(DFF), in1=chg[:], op0=ALU.mult, op1=ALU.add), [r_eb], [r_eb])
                for kc in range(8):
                    P.op("dve", lambda e, kc=kc: e.tensor_scalar(out=widx1[:, :, kc], in0=ebo1[:], scalar1=rowc[:, kc:kc + 1], scalar2=None, op0=ALU.add), [r_eb, r_cR], [r_widx])
                for kc in range(16):
                    P.op("dve", lambda e, kc=kc: e.tensor_scalar(out=widx2[:, :, kc], in0=ebo2[:], scalar1=rowc[:, kc:kc + 1], scalar2=None, op0=ALU.add), [r_eb, r_cR], [r_widx])
                ebl = sbt(st, "R_ebl", [128, NBLK], F32)
                P.op("dve", lambda e: e.tensor_scalar(out=ebl[:], in0=eb[:], scalar1=float(l * NE), scalar2=None, op0=ALU.add), [r_eb], [r_eb])
                P.op("dve", lambda e: e.tensor_scalar(out=bidx1[:], in0=ebl[:], scalar1=16.0, scalar2=rowc[:, 0:1], op0=ALU.mult, op1=ALU.add), [r_eb, r_cR], [r_widx])
                P.op("dve", lambda e: e.tensor_copy(out=bidx2[:], in_=ebl[:]), [r_eb], [r_widx])
                zi = sbt(st, "R_zi", [128, NSLOT // 128], I32)
                r_zi = P.res("R_zi")
                P.op("dve", lambda e: e.memset(zi[:], 0), [], [r_zi])
                P.dma("sp", lambda e: e.dma_start(out=table.rearrange("(p f) o -> p (f o)", p=128), in_=zi[:]), [r_zi], [], r_zi)
                P.barrier()
                for i in range(NT):
                    for sl in (slot0, slot1):
                        P.dma("pool", lambda e, sl=sl, i=i: e.indirect_dma_start(
                            out=table[:, :], out_offset=bass.IndirectOffsetOnAxis(ap=sl[:, i:i + 1], axis=0),
                            in_=tokid[:, i:i + 1], in_offset=None), [r_slots, r_const], [], r_slots)
                P.barrier()
                P.release(r_xt + [r_cR, r_zi])

            with ExitStack() as _st:
                _pass(_st)
            if debug == "noM":
                continue
            def _pass(st, l=l, last=last):
                w1s = sbt(st, "M_w1", [128, 8, DFF], BF16)
                w2s = sbt(st, "M_w2", [128, 16, D], BF16)
                r_w1s, r_w2s = P.res("M_w1"), P.res("M_w2")
                tix = [sbt(st, f"M_tix{i}", [128, 4], I32) for i in range(2)]
                r_tix = [P.res(f"M_tix{i}") for i in range(2)]
                xg = [sbt(st, f"M_xg{i}", [128, 4, D], BF16) for i in range(2)]
                r_xg = [P.res(f"M_xg{i}") for i in range(2)]
                xT = [sbt(st, f"M_xT{i}", [128, 8, 512], BF16) for i in range(2)]
                r_xT = [P.res(f"M_xT{i}") for i in range(2)]
                hT = sbt(st, "M_hT", [128, 16, 512], BF16)
                r_hT = P.res("M_hT")
                b1r = [sbt(st, f"M_b1r{i}", [16, 128], F32) for i in range(2)]
                r_b1r = [P.res(f"M_b1r{i}") for i in range(2)]
                b1c = [sbt(st, f"M_b1c{i}", [128, 16], F32) for i in range(2)]
                r_b1c = [P.res(f"M_b1c{i}") for i in range(2)]
                b2b = [sbt(st, f"M_b2b{i}", [128, D], F32) for i in range(2)]
                r_b2b = [P.res(f"M_b2b{i}") for i in range(2)]
                ysb = [sbt(st, f"M_y{i}", [128, D], F32) for i in range(4)]
                r_ysb = [P.res(f"M_y{i}") for i in range(4)]
                psT = [pst(st, f"M_psT{i}", [128, 8, 128], BF16) for i in range(2)]
                r_psT = [P.res(f"M_psT{i}") for i in range(2)]
                psH = [pst(st, f"M_psH{i}", [128, 512], F32) for i in range(3)]
                r_psH = [P.res(f"M_psH{i}") for i in range(3)]
                psY = [pst(st, f"M_psY{i}", [128, 512], F32) for i in range(2)]
                r_psY = [P.res(f"M_psY{i}") for i in range(2)]
                psB_ = pst(st, "M_psB", [128, 512], F32)
                psB = psB_[:, 0:16]
                r_psB = P.res("M_psB")
                cnt = dict(npt=0, nph=0, npy=0, ny=0)
                tview = table.rearrange("(b p j) o -> b p (j o)", p=128, j=4)
                yview = ybuf.rearrange("(b p j) d -> b j p d", p=128, j=4)
                b1_rows = b1.rearrange("l e (c p) -> (l e c) p", p=128)

                def tokens(b):
                    k = b % 2
                    P.dma("sp", lambda e, k=k, b=b: e.dma_start(out=tix[k][:], in_=tview[b]), [], [r_tix[k]], r_tix[k])
                    for j in range(4):
                        P.dma("pool", lambda e, k=k, j=j: e.indirect_dma_start(
                            out=xg[k][:, j, :], out_offset=None, in_=x1b[:, :],
                            in_offset=bass.IndirectOffsetOnAxis(ap=tix[k][:, j:j + 1], axis=0)), [r_tix[k]], [r_xg[k]], r_xg[k])
                    P.dma("pool", lambda e, k=k, b=b: e.indirect_dma_start(
                        out=b1r[k][:, :], out_offset=None, in_=b1_rows[:, :],
                        in_offset=bass.IndirectOffsetOnAxis(ap=bidx1[0:16, b:b + 1], axis=0)), [r_widx], [r_b1r[k]], r_b1r[k])

                def transposes(b):
                    k = b % 2
                    for j in range(4):
                        q = cnt["npt"] % 2
                        cnt["npt"] += 1
                        for kc in range(8):
                            P.op("pe", lambda e, q=q, k=k, j=j, kc=kc: e.transpose(
                                out=psT[q][:, kc, :], in_=xg[k][:, j, kc * 128:(kc + 1) * 128], identity=ident_bf[:]),
                                [r_xg[k], r_const], [r_psT[q]])
                        P.op("dve", lambda e, q=q, k=k, j=j: e.tensor_copy(out=xT[k][:, :, j * 128:(j + 1) * 128], in_=psT[q][:]),
                             [r_psT[q]], [r_xT[k]])
                    P.op("pe", lambda e, k=k: e.transpose(out=psB, in_=b1r[k][:, :], identity=ident_f[0:16, 0:16]),
                         [r_b1r[k], r_const], [r_psB])
                    P.op("dve", lambda e, k=k: e.tensor_copy(out=b1c[k][:], in_=psB), [r_psB], [r_b1c[k]])

                def w1_load(b):
                    for kc in range(8):
                        P.dma("pool", lambda e, b=b, kc=kc: e.indirect_dma_start(
                            out=w1s[:, kc, :], out_offset=None, in_=w1L[l][:, :],
                            in_offset=bass.IndirectOffsetOnAxis(ap=widx1[:, b, kc:kc + 1], axis=0),
                            bounds_check=breg(e, NE * D - 1), oob_is_err=False), [r_widx], [r_w1s], r_w1s)

                def w2_load(b):
                    k = b % 2
                    P.dma("pool", lambda e, k=k, b=b: e.indirect_dma_start(
                        out=b2b[k][:, :], out_offset=None, in_=b2_rows[:, :],
                        in_offset=bass.IndirectOffsetOnAxis(ap=bidx2[:, b:b + 1], axis=0)), [r_widx], [r_b2b[k]], r_b2b[k])
                    for kc in range(16):
                        P.dma("pool", lambda e, b=b, kc=kc: e.indirect_dma_start(
                            out=w2s[:, kc, :], out_offset=None, in_=w2L[l][:, :],
                            in_offset=bass.IndirectOffsetOnAxis(ap=widx2[:, b, kc:kc + 1], axis=0),
                            bounds_check=breg(e, NE * DFF - 1), oob_is_err=False), [r_widx], [r_w2s], r_w2s)

                tokens(0)
                tokens(1)
                w1_load(0)
                w2_load(0)
                transposes(0)
                for b in range(NBLK):
                    k = b % 2
                    if b + 2 < NBLK:
                        tokens(b + 2)
                    for m in range(16):
                        q = cnt["nph"] % 3
                        cnt["nph"] += 1
                        for kc in range(8):
                            P.op("pe", lambda e, q=q, k=k, m=m, kc=kc: e.matmul(
                                psH[q][:], lhsT=w1s[:, kc, m * 128:(m + 1) * 128], rhs=xT[k][:, kc, :],
                                start=(kc == 0), stop=(kc == 7)), [r_w1s, r_xT[k]], [r_psH[q]])
                        P.op("act", lambda e, q=q, k=k, m=m: e.activation(
                            out=hT[:, m, :], in_=psH[q][:], func=AF.Gelu, bias=b1c[k][:, m:m + 1], scale=1.0),
                            [r_psH[q], r_b1c[k]], [r_hT])
                    if b + 1 < NBLK:
                        w1_load(b + 1)
                        transposes(b + 1)
                    for j in range(4):
                        yk = cnt["ny"] % 4
                        cnt["ny"] += 1
                        for h in range(2):
                            q = cnt["npy"] % 2
                            cnt["npy"] += 1
                            for kc in range(16):
                                P.op("pe", lambda e, q=q, j=j, h=h, kc=kc: e.matmul(
                                    psY[q][:], lhsT=hT[:, kc, j * 128:(j + 1) * 128], rhs=w2s[:, kc, h * 512:(h + 1) * 512],
                                    start=(kc == 0), stop=(kc == 15)), [r_w2s, r_hT], [r_psY[q]])
                            P.op("dve", lambda e, q=q, yk=yk, k=k, h=h: e.tensor_tensor(
                                out=ysb[yk][:, h * 512:(h + 1) * 512], in0=psY[q][:], in1=b2b[k][:, h * 512:(h + 1) * 512], op=ALU.add),
                                [r_psY[q], r_b2b[k]], [r_ysb[yk]])
                        P.dma("sp", lambda e, yk=yk, b=b, j=j: e.dma_start(out=yview[b, j], in_=ysb[yk][:]), [r_ysb[yk]], [], r_ysb[yk])
                    if b + 1 < NBLK:
                        w2_load(b + 1)
                P.barrier()
                P.release([r_w1s, r_w2s] + r_tix + r_xg + r_b1r + r_b2b + r_ysb)
            with ExitStack() as _st:
                _pass(_st)

            def _pass(st, l=(l if "l" in dir() else 0), last=(last if "last" in dir() else False)):
                g2 = sbt(st, "C_g", [128, D], F32)
                b2t = sbt(st, "C_b", [128, D], F32)
                r_cC = P.res("C_c")
                P.dma("sp", lambda e: e.dma_start(out=g2[:], in_=ln2_g[l][None, :].to_broadcast([128, D])), [], [r_cC], r_cC)
                P.dma("sp", lambda e: e.dma_start(out=b2t[:], in_=ln2_b[l][None, :].to_broadcast([128, D])), [], [r_cC], r_cC)
                NB = 6
                x1t = [sbt(st, f"C_x{i}", [128, D], F32) for i in range(NB)]
                r_x1t = [P.res(f"C_x{i}") for i in range(NB)]
                y0t = [sbt(st, f"C_y0{i}", [128, D], F32) for i in range(NB)]
                r_y0t = [P.res(f"C_y0{i}") for i in range(NB)]
                y1t = [sbt(st, f"C_y1{i}", [128, D], F32) for i in range(NB)]
                r_y1t = [P.res(f"C_y1{i}") for i in range(NB)]
                lsc = [ln_scratch(st, f"C_l{i}") for i in range(NB)]
                r_lsc = [P.res(f"C_ls{i}") for i in range(NB)]
                dst = y_out if last else xa
                for i0 in range(0, NT, 3):
                    grp = list(range(i0, min(i0 + 3, NT)))
                    for i in grp:
                        k = i % NB
                        P.dma("sp", lambda e, k=k, i=i: e.dma_start(out=x1t[k][:], in_=x1b[i * 128:(i + 1) * 128, :]), [], [r_x1t[k]], r_x1t[k])
                        P.dma("pool", lambda e, k=k, i=i: e.indirect_dma_start(
                            out=y0t[k][:, :], out_offset=None, in_=ybuf[:, :],
                            in_offset=bass.IndirectOffsetOnAxis(ap=slot0[:, i:i + 1], axis=0)), [r_slots], [r_y0t[k]], r_y0t[k])
                        P.dma("pool", lambda e, k=k, i=i: e.indirect_dma_start(
                            out=y1t[k][:, :], out_offset=None, in_=ybuf[:, :],
                            in_offset=bass.IndirectOffsetOnAxis(ap=slot1[:, i:i + 1], axis=0)), [r_slots], [r_y1t[k]], r_y1t[k])
                    for i in grp:
                        k = i % NB
                        P.op("act", lambda e, k=k: e.mul(out=x1t[k][:], in_=x1t[k][:], mul=ALPHA), [r_x1t[k]], [r_x1t[k]])
                    for i in grp:
                        k = i % NB
                        P.op("dve", lambda e, k=k, i=i: e.scalar_tensor_tensor(
                            out=x1t[k][:], in0=y0t[k][:], scalar=gates[:, i, 0:1], in1=x1t[k][:], op0=ALU.mult, op1=ALU.add),
                            [r_y0t[k], r_x1t[k], r_gates], [r_x1t[k]])
                    for i in grp:
                        k = i % NB
                        P.op("dve", lambda e, k=k, i=i: e.scalar_tensor_tensor(
                            out=x1t[k][:], in0=y1t[k][:], scalar=gates[:, i, 1:2], in1=x1t[k][:], op0=ALU.mult, op1=ALU.add),
                            [r_y1t[k], r_x1t[k], r_gates], [r_x1t[k]])
                    emit_ln_multi([(x1t[i % NB][:], r_x1t[i % NB], lsc[i % NB], r_lsc[i % NB]) for i in grp], g2[:], b2t[:], cread=[r_cC])
                    for i in grp:
                        k = i % NB
                        P.dma("sp", lambda e, k=k, i=i: e.dma_start(out=dst[i * 128:(i + 1) * 128, :], in_=x1t[k][:]), [r_x1t[k]], [], r_x1t[k])
                P.barrier()
                P.release([r_cC] + r_x1t + r_y0t + r_y1t)
            with ExitStack() as _st:
                _pass(_st)
        cnt = P.emit()
    return nc, cnt


def _constants(T, NU):
    NT = T // 128
    NBLK = -(-(2 * T + NE * (BLK - 1)) // BLK)
    c = {}
    c["c_ident_bf"] = np.eye(128, dtype=np.float32).astype(ml_dtypes.bfloat16)
    c["c_ident_f"] = np.eye(128, dtype=np.float32)
    c["c_tri"] = np.triu(np.ones((128, 128), np.float32), 1).astype(ml_dtypes.bfloat16)
    c["c_ones"] = np.ones((128, 128), np.float32).astype(ml_dtypes.bfloat16)
    qc = np.arange(64)
    cs = np.clip(qc - 8, 0, 48)
    kc = np.arange(64)
    inw = (kc[None, :] >= cs[:, None]) & (kc[None, :] < cs[:, None] + 16)
    m = np.where(inw, 0.0, NEG).astype(np.float32)
    c["c_mask"] = np.concatenate([m, m], axis=0)
    invc = np.zeros((128, 4, 16), np.float32)
    L = 2048
    for g, w in enumerate((2, 4, 8, 16)):
        for i in range(8):
            lo, hi = max(i - w // 2, 0), min(i + w // 2, L)
            invc[:, g, i] = 1.0 / (hi - lo)
            p = L - 8 + i
            lo, hi = max(p - w // 2, 0), min(p + w // 2, L)
            invc[:, g, 8 + i] = 1.0 / (hi - lo)
    c["c_invc"] = invc
    c["c_tokid"] = (np.arange(NT)[None, :] * 128 + np.arange(128)[:, None]).astype(np.int32)
    c["c_rowc"] = (np.arange(16)[None, :] * 128 + np.arange(128)[:, None]).astype(np.float32)
    c["c_bstart"] = np.tile((np.arange(NBLK) * BLK).astype(np.float32)[None, :], (128, 1))
    return c


def _unit_tables(units, T):
    NU = len(units)
    kv = np.zeros((128, NU * 16), np.int32)
    oi = np.zeros((128, NU * 16), np.int32)
    for u, (t0, vlo, vhi) in enumerate(units):
        loc = np.arange(2048)
        tok = t0 + loc
        row = loc // 64
        valid = (row >= vlo) & (row < vhi)
        out = np.where(valid, tok, T + loc)
        kv[:, u * 16:(u + 1) * 16] = tok.reshape(16, 128).T
        oi[:, u * 16:(u + 1) * 16] = out.reshape(16, 128).T
    return kv, oi


_CACHE = {}


def kernel(x_prompt, x_sample, ln_in_g, ln_in_b, w_in, b_in, rpb, w_pool, b_pool, pool_scale,
           w_oa, w_op, w_out, b_out, ln1_g, ln1_b, w_router, b_router, w1, b1, w2, b2, ln2_g, ln2_b):
    T, NU = 12288, 7
    f = lambda a: np.ascontiguousarray(np.asarray(a, dtype=np.float32))
    x_prompt, x_sample = f(x_prompt), f(x_sample)
    shared = dict(ln_in_g=f(ln_in_g), ln_in_b=f(ln_in_b), w_in=f(w_in), b_in=f(b_in), w_pool=f(w_pool),
                  b_pool=f(b_pool), pool_scale=f(pool_scale), w_oa=f(w_oa), w_op=f(w_op), w_out=f(w_out), b_out=f(b_out),
                  ln1_g=f(ln1_g), ln1_b=f(ln1_b), w_router=f(w_router), b_router=f(b_router), b1=f(b1),
                  b2=f(b2), ln2_g=f(ln2_g), ln2_b=f(ln2_b))
    w1f, w2f = f(w1), f(w2)
    for i in range(DEPTH):
        shared[f"w1_{i}"] = w1f[i].reshape(NE * D, DFF)
        shared[f"w2_{i}"] = w2f[i].reshape(NE * DFF, D)
    rp = f(rpb)
    qc = np.arange(64)[:, None]
    kcc = np.arange(64)[None, :]
    ti = np.clip(kcc - qc + 15, 0, 30)
    g = rp[:, :, :, ti]
    g = g.reshape(DEPTH, 4, 2, 15, 64, 64).transpose(0, 2, 4, 1, 3, 5)
    shared["rpbT"] = np.ascontiguousarray(g.reshape(DEPTH, 128, 4 * 15 * 64))
    shared.update(_constants(T, NU))
    in_maps = []
    for c in range(8):
        if c < 4:
            xc = np.concatenate([x_prompt[c], x_sample[2 * c], x_sample[2 * c + 1]], axis=0)
            units = [(0, 0, 28), (1536, 4, 28), (3072, 4, 28), (4608, 4, 28), (6144, 4, 32),
                     (8192, 0, 32), (10240, 0, 32)]
        else:
            s0 = 8 + 6 * (c - 4)
            xc = np.concatenate([x_sample[s0 + j] for j in range(6)], axis=0)
            units = [(2048 * j, 0, 32) for j in range(6)] + [(0, 0, 0)]
        kv, oi = _unit_tables(units, T)
        m = dict(shared)
        m["x_in"] = np.ascontiguousarray(xc)
        m["kvidx"] = kv
        m["outidx"] = oi
        in_maps.append(m)
    if "nc" not in _CACHE:
        _CACHE["nc"] = build_program(T, NU)[0]
    nc = _CACHE["nc"]
    res = run_bass_kernel_spmd(nc, in_maps, core_ids=list(range(8)))
    outs = [np.asarray(r["y_out"], dtype=np.float32) for r in res.results]
    y_prompt = np.stack([outs[c][0:8192] for c in range(4)], axis=0)
    y_sample = np.zeros((32, 2048, D), np.float32)
    for c in range(4):
        y_sample[2 * c] = outs[c][8192:10240]
        y_sample[2 * c + 1] = outs[c][10240:12288]
    for c in range(4, 8):
        s0 = 8 + 6 * (c - 4)
        for j in range(6):
            y_sample[s0 + j] = outs[c][2048 * j:2048 * (j + 1)]
    return (y_prompt, y_sample)
```

```python
from contextlib import ExitStack
import numpy as np
import ml_dtypes
import concourse.bass as bass
import concourse.mybir as mybir
from concourse.bass_utils import run_bass_kernel_spmd

F32 = mybir.dt.float32
BF16 = mybir.dt.bfloat16
I32 = mybir.dt.int32
AF = mybir.ActivationFunctionType
ALU = mybir.AluOpType
AX = mybir.AxisListType

D = 1024
DEPTH = 4
NE = 16
DFF = 2048
ALPHA = (2 * DEPTH) ** 0.25
EPS = 1e-5
BLK = 512
TRASH = 2048
NEG = -30000.0

ENGS = ("pe", "act", "dve", "pool", "sp")
EPOCH = 30000


class Res:
    __slots__ = ("name", "lw", "rd", "sem", "dcnt")

    def __init__(self, name):
        self.name = name
        self.lw = None
        self.rd = []
        self.sem = None
        self.dcnt = 0


class Op:
    __slots__ = ("eng", "fn", "deps", "marked", "seq", "dma", "dtok")

    def __init__(self, eng, fn, dma):
        self.eng = eng
        self.fn = fn
        self.deps = []
        self.marked = False
        self.seq = None
        self.dma = dma
        self.dtok = None


class Prog:
    def __init__(self, nc):
        self.nc = nc
        self.ops = []
        self.n_dma_sems = 0
        self.res_list = []
        self.last = {e: None for e in ENGS}
        self.sem_pool = []

    def res(self, name):
        r = Res(name)
        self.res_list.append(r)
        return r

    def release(self, rs):
        for r in rs:
            if r.sem is not None:
                self.sem_pool.append((r.sem, r.dcnt))
                r.sem = None

    def _add(self, eng, fn, reads, writes, dma_res=None):
        op = Op(eng, fn, dma_res is not None)
        deps = []
        for r in reads:
            if r.lw is not None:
                deps.append(r.lw)
        for w in writes:
            if w.lw is not None:
                deps.append(w.lw)
            deps.extend(w.rd)
        seen = set()
        for d in deps:
            if id(d) in seen:
                continue
            seen.add(id(d))
            if isinstance(d, Op):
                if d.eng == eng and eng == "pe" and not op.dma:
                    continue
                d.marked = True
            op.deps.append(d)
        if dma_res is not None:
            if dma_res.sem is None:
                if self.sem_pool:
                    dma_res.sem, dma_res.dcnt = self.sem_pool.pop()
                else:
                    dma_res.sem = self.n_dma_sems
                    dma_res.dcnt = 0
                    self.n_dma_sems += 1
            dma_res.dcnt += 16
            tok = ("D", dma_res.sem, dma_res.dcnt)
            op.dtok = tok
        else:
            tok = op
            if fn is not None:
                self.last[eng] = op
        for r in reads:
            if r.rd:
                p = r.rd[-1]
                if isinstance(tok, Op) and isinstance(p, Op) and p.eng == tok.eng:
                    r.rd[-1] = tok
                    continue
                if (not isinstance(tok, Op)) and (not isinstance(p, Op)) and p[1] == tok[1]:
                    r.rd[-1] = tok
                    continue
            r.rd.append(tok)
        for w in writes:
            w.lw = tok
            w.rd = []
        self.ops.append(op)
        return op

    def op(self, eng, fn, reads=(), writes=()):
        return self._add(eng, fn, reads, writes)

    def dma(self, eng, fn, reads, writes, sem_res):
        return self._add(eng, fn, reads, writes, dma_res=sem_res)

    def barrier(self):
        toks = []
        for e in ENGS:
            if self.last[e] is not None:
                toks.append(self.last[e])
        dt = {}
        for r in self.res_list:
            if r.sem is not None:
                dt[r.sem] = max(dt.get(r.sem, 0), r.dcnt)
        for s, c in self.sem_pool:
            dt[s] = max(dt.get(s, 0), c)
        for s, c in dt.items():
            toks.append(("D", s, c))
        for e in ENGS:
            op = Op(e, None, False)
            for t in toks:
                if isinstance(t, Op):
                    if t.eng == e:
                        continue
                    t.marked = True
                op.deps.append(t)
            self.ops.append(op)
        for r in self.res_list:
            r.lw = None
            r.rd = []

    def emit(self):
        nc = self.nc
        cnt = {e: 0 for e in ENGS}
        for op in self.ops:
            if op.marked and not op.dma:
                cnt[op.eng] += 1
                op.seq = cnt[op.eng]
        n_eng_sems = {e: (cnt[e] + EPOCH - 1) // EPOCH for e in ENGS}
        with ExitStack() as st:
            esems = {e: [st.enter_context(nc.semaphore(f"e_{e}_{i}")) for i in range(n_eng_sems[e])]
                     for e in ENGS}
            dsems = [st.enter_context(nc.semaphore(f"d_{i}")) for i in range(self.n_dma_sems)]
            block = st.enter_context(nc.Block())
            by_eng = {e: [op for op in self.ops if op.eng == e] for e in ENGS}

            def resolve(tok):
                if isinstance(tok, Op):
                    s = (tok.seq - 1) // EPOCH
                    return esems[tok.eng][s], tok.seq - s * EPOCH, ("E", tok.eng, s)
                return dsems[tok[1]], tok[2], ("D", tok[1])

            def run_engine(eng_name, eng):
                known = {}
                for op in by_eng[eng_name]:
                    for d in op.deps:
                        sem, val, key = resolve(d)
                        if known.get(key, 0) >= val:
                            continue
                        known[key] = val
                        eng.wait_ge(sem, val)
                    if op.fn is None:
                        continue
                    inst = op.fn(eng)
                    if op.dma:
                        inst.then_inc(dsems[op.dtok[1]], 16)
                    elif op.marked:
                        s = (op.seq - 1) // EPOCH
                        inst.then_inc(esems[op.eng][s], 1)

            @block.tensor
            def _(e):
                run_engine("pe", e)

            @block.scalar
            def _(e):
                run_engine("act", e)

            @block.vector
            def _(e):
                run_engine("dve", e)

            @block.gpsimd
            def _(e):
                run_engine("pool", e)

            @block.sync
            def _(e):
                run_engine("sp", e)
        return cnt


def build_program(T, NU, depth=DEPTH, debug=None):
    NT = T // 128
    NBLK = -(-(2 * T + NE * (BLK - 1)) // BLK)
    NSLOT = NBLK * BLK
    nc = bass.Bass("TRN2", target_bir_lowering=False)

    def din(name, shape, dt=F32):
        return nc.dram_tensor(name, list(shape), dt, kind="ExternalInput").ap()

    x_in = din("x_in", [T, D])
    ln_in_g = din("ln_in_g", [D]); ln_in_b = din("ln_in_b", [D])
    w_in = din("w_in", [DEPTH, D, 4096]); b_in = din("b_in", [DEPTH, 4096])
    rpbT = din("rpbT", [DEPTH, 128, 4 * 15 * 64])
    w_pool = din("w_pool", [DEPTH, 4, 128, 128]); b_pool = din("b_pool", [DEPTH, 4, 128])
    pool_scale = din("pool_scale", [DEPTH, 512])
    w_oa = din("w_oa", [DEPTH, 512, D]); w_op = din("w_op", [DEPTH, 512, D])
    w_out = din("w_out", [DEPTH, D, D]); b_out = din("b_out", [DEPTH, D])
    ln1_g = din("ln1_g", [DEPTH, D]); ln1_b = din("ln1_b", [DEPTH, D])
    w_router = din("w_router", [D, NE]); b_router = din("b_router", [NE])
    w1L = [din(f"w1_{i}", [NE * D, DFF]) for i in range(DEPTH)]; b1 = din("b1", [DEPTH, NE, DFF])
    w2L = [din(f"w2_{i}", [NE * DFF, D]) for i in range(DEPTH)]; b2 = din("b2", [DEPTH, NE, D])
    ln2_g = din("ln2_g", [DEPTH, D]); ln2_b = din("ln2_b", [DEPTH, D])
    kvidx_d = din("kvidx", [128, NU * 16], I32)
    outidx_d = din("outidx", [128, NU * 16], I32)
    c_ident_bf = din("c_ident_bf", [128, 128], BF16)
    c_ident_f = din("c_ident_f", [128, 128])
    c_tri = din("c_tri", [128, 128], BF16)
    c_ones = din("c_ones", [128, 128], BF16)
    c_mask = din("c_mask", [128, 64])
    c_invc = din("c_invc", [128, 4, 16])
    c_tokid = din("c_tokid", [128, NT], I32)
    c_rowc = din("c_rowc", [128, 16])
    c_bstart = din("c_bstart", [128, NBLK])
    y_out = nc.dram_tensor("y_out", [T, D], F32, kind="ExternalOutput").ap()

    sk = "ExternalOutput" if debug else "Internal"
    xa = nc.dram_tensor("xa", [T + TRASH, D], F32, kind=sk).ap()
    x1b = nc.dram_tensor("x1b", [T + TRASH, D], F32, kind=sk).ap()
    kvp = nc.dram_tensor("kvp", [NU, 128, 3, 8192], BF16, kind=sk).ap()
    ybuf = nc.dram_tensor("ybuf", [NSLOT, D], F32, kind=sk).ap()
    table = nc.dram_tensor("table", [NSLOT, 1], I32, kind=sk).ap()

    b2_rows = b2.rearrange("l e d -> (l e) d")

    P = Prog(nc)
    top = ExitStack()
    _regs = {}

    def breg(e, val):
        if val not in _regs:
            _regs[val] = e.to_reg(val)
        return _regs[val]

    uniq = [0]

    def sbt(st, name, shape, dt):
        uniq[0] += 1
        return st.enter_context(nc.sbuf_tensor(f"{name}_{uniq[0]}", list(shape), dt))

    def pst(st, name, shape, dt):
        uniq[0] += 1
        return st.enter_context(nc.psum_tensor(f"{name}_{uniq[0]}", list(shape), dt))

    with top:
        ident_bf = sbt(top, "ident_bf", [128, 128], BF16)
        ident_f = sbt(top, "ident_f", [128, 128], F32)
        tri = sbt(top, "tri", [128, 128], BF16)
        ones = sbt(top, "ones", [128, 128], BF16)
        tokid = sbt(top, "tokid", [128, NT], I32)
        kvidx = sbt(top, "kvidx_sb", [128, NU * 16], I32)
        outidx = sbt(top, "outidx_sb", [128, NU * 16], I32)
        gates = sbt(top, "gates", [128, NT, 2], F32)
        slot0 = sbt(top, "slot0", [128, NT], I32)
        slot1 = sbt(top, "slot1", [128, NT], I32)
        mhalf = sbt(top, "mhalf", [128, 1], F32)
        widx1 = sbt(top, "widx1", [128, NBLK, 8], I32)
        widx2 = sbt(top, "widx2", [128, NBLK, 2], I32)
        bidx1 = sbt(top, "bidx1", [128, NBLK], I32)
        bidx2 = sbt(top, "bidx2", [128, NBLK], I32)
        r_const = P.res("const")
        r_gates = P.res("gates")
        r_slots = P.res("slots")
        r_widx = P.res("widx")
        for t, s in ((ident_bf, c_ident_bf), (ident_f, c_ident_f), (tri, c_tri), (ones, c_ones),
                     (tokid, c_tokid), (kvidx, kvidx_d), (outidx, outidx_d)):
            P.dma("sp", lambda e, t=t, s=s: e.dma_start(out=t[:], in_=s), [], [r_const], r_const)
        P.op("dve", lambda e: e.memset(mhalf[:], -0.5), [], [r_const])
        P.barrier()

        def emit_ln_multi(items, g_t, b_t, cread=()):
            def step(eng, mk, rd, wr):
                for it in items:
                    P.op(eng, mk(it), rd(it), wr(it))
            step("dve", lambda it: (lambda e: e.bn_stats(out=it[2][0][:, 0, :], in_=it[0][:, 0:512])), lambda it: [it[1]], lambda it: [it[3]])
            step("dve", lambda it: (lambda e: e.bn_stats(out=it[2][0][:, 1, :], in_=it[0][:, 512:1024])), lambda it: [it[1]], lambda it: [it[3]])
            step("dve", lambda it: (lambda e: e.bn_aggr(out=it[2][1][:], in_=it[2][0][:].rearrange("p a b -> p (a b)"))), lambda it: [it[3]], lambda it: [it[3]])
            step("dve", lambda it: (lambda e: e.tensor_scalar(out=it[2][2][:], in0=it[2][1][:, 1:2], scalar1=EPS, scalar2=None, op0=ALU.add)),
                 lambda it: [it[3]], lambda it: [it[3]])
            step("act", lambda it: (lambda e: e.activation(out=it[2][2][:], in_=it[2][2][:], func=AF.Ln)), lambda it: [it[3]], lambda it: [it[3]])
            step("act", lambda it: (lambda e: e.activation(out=it[2][2][:], in_=it[2][2][:], func=AF.Exp, scale=-0.5)), lambda it: [it[3]], lambda it: [it[3]])
            step("dve", lambda it: (lambda e: e.tensor_scalar(out=it[2][3][:], in0=it[2][1][:, 0:1], scalar1=it[2][2][:, 0:1], scalar2=-1.0,
                                                             op0=ALU.mult, op1=ALU.mult)), lambda it: [it[3]], lambda it: [it[3]])
            step("act", lambda it: (lambda e: e.activation(out=it[0], in_=it[0], func=AF.Identity, bias=it[2][3][:, 0:1], scale=it[2][2][:, 0:1])),
                 lambda it: [it[1], it[3]], lambda it: [it[1]])
            step("dve", lambda it: (lambda e: e.tensor_tensor(out=it[0], in0=it[0], in1=g_t, op=ALU.mult)), lambda it: [it[1]] + list(cread), lambda it: [it[1]])
            step("dve", lambda it: (lambda e: e.tensor_tensor(out=it[0], in0=it[0], in1=b_t, op=ALU.add)), lambda it: [it[1]] + list(cread), lambda it: [it[1]])

        def emit_ln(xt, r_x, g_t, b_t, scr, r_scr, cread=()):
            emit_ln_multi([(xt, r_x, scr, r_scr)], g_t, b_t, cread=cread)

        def ln_scratch(st, name):
            return (sbt(st, name + "_s6", [128, 2, 6], F32), sbt(st, name + "_mv", [128, 2], F32),
                    sbt(st, name + "_rs", [128, 1], F32), sbt(st, name + "_nm", [128, 1], F32))

        def _pass(st, l=(l if "l" in dir() else 0), last=(last if "last" in dir() else False)):
            g_t = sbt(st, "p0_g", [128, D], F32)
            b_t = sbt(st, "p0_b", [128, D], F32)
            r_gb = P.res("p0_gb")
            P.dma("sp", lambda e: e.dma_start(out=g_t[:], in_=ln_in_g[None, :].to_broadcast([128, D])), [], [r_gb], r_gb)
            P.dma("sp", lambda e: e.dma_start(out=b_t[:], in_=ln_in_b[None, :].to_broadcast([128, D])), [], [r_gb], r_gb)
            NB = 6
            xts = [sbt(st, f"p0_x{i}", [128, D], F32) for i in range(NB)]
            rxs = [P.res(f"p0_x{i}") for i in range(NB)]
            scrs = [ln_scratch(st, f"p0_l{i}") for i in range(NB)]
            rss = [P.res(f"p0_s{i}") for i in range(NB)]
            for i0 in range(0, NT, 3):
                grp = list(range(i0, min(i0 + 3, NT)))
                for i in grp:
                    k = i % NB
                    P.dma("sp", lambda e, xt=xts[k], i=i: e.dma_start(out=xt[:], in_=x_in[i * 128:(i + 1) * 128, :]), [], [rxs[k]], rxs[k])
                emit_ln_multi([(xts[i % NB][:], rxs[i % NB], scrs[i % NB], rss[i % NB]) for i in grp], g_t[:], b_t[:], cread=[r_gb])
                for i in grp:
                    k = i % NB
                    P.dma("sp", lambda e, xt=xts[k], i=i: e.dma_start(out=xa[i * 128:(i + 1) * 128, :], in_=xt[:]), [rxs[k]], [], rxs[k])
            P.barrier()
            P.release(rxs + [r_gb])

        with ExitStack() as _st:
            _pass(_st)
        for l in range(depth):
            last = (l == depth - 1)
            def _pass(st, l=(l if "l" in dir() else 0), last=(last if "last" in dir() else False)):
                wA = sbt(st, "wA", [128, 8, 1536], BF16)
                r_wA = P.res("wA")
                for kc in range(8):
                    P.dma("pool", lambda e, kc=kc: e.dma_start(out=wA[:, kc, :], in_=w_in[l, kc * 128:(kc + 1) * 128, 512:2048]),
                          [], [r_wA], r_wA)
                bcol = sbt(st, "A_bcol", [128, 32], F32)
                bv = sbt(st, "A_bv", [128, 512], F32)
                r_bA = P.res("A_b")
                P.dma("sp", lambda e: e.dma_start(out=bcol[:], in_=b_in[l].rearrange("(c p) -> p c", p=128),
                                                  allow_slow_non_contiguous=True), [], [r_bA], r_bA)
                P.dma("sp", lambda e: e.dma_start(out=bv[:], in_=b_in[l, 1024:1536][None, :].to_broadcast([128, 512])), [], [r_bA], r_bA)
                NB = 2
                xbf = [sbt(st, f"A_xbf{i}", [128, 4, D], BF16) for i in range(NB)]
                r_xbf = [P.res(f"A_xbf{i}") for i in range(NB)]
                xT = [sbt(st, f"A_xT{i}", [128, 8, 512], BF16) for i in range(NB)]
                r_xT = [P.res(f"A_xT{i}") for i in range(NB)]
                kTc = [sbt(st, f"A_kT{i}", [128, 4, 512], BF16) for i in range(NB)]
                r_kTc = [P.res(f"A_kT{i}") for i in range(NB)]
                pTc = [sbt(st, f"A_pT{i}", [128, 4, 512], BF16) for i in range(NB)]
                r_pTc = [P.res(f"A_pT{i}") for i in range(NB)]
                Vc = [sbt(st, f"A_V{i}", [128, 4, 512], BF16) for i in range(NB)]
                r_Vc = [P.res(f"A_V{i}") for i in range(NB)]
                psT = [pst(st, f"A_psT{i}", [128, 8, 128], BF16) for i in range(2)]
                r_psT = [P.res(f"A_psT{i}") for i in range(2)]
                psA = [pst(st, f"A_ps{i}", [128, 512], F32) for i in range(4)]
                r_psA = [P.res(f"A_ps{i}") for i in range(4)]
                nps = 0
                npt = 0
                for u in range(NU):
                    for c in range(4):
                        k = (u * 4 + c) % NB
                        for j in range(4):
                            col = u * 16 + c * 4 + j
                            P.dma("pool", lambda e, k=k, j=j, col=col: e.indirect_dma_start(
                                out=xbf[k][:, j, :], out_offset=None, in_=xa[:, :],
                                in_offset=bass.IndirectOffsetOnAxis(ap=kvidx[:, col:col + 1], axis=0)),
                                [r_const], [r_xbf[k]], r_xbf[k])
                        for j in range(4):
                            q = npt % 2
                            npt += 1
                            for kc in range(8):
                                P.op("pe", lambda e, q=q, k=k, j=j, kc=kc: e.transpose(
                                    out=psT[q][:, kc, :], in_=xbf[k][:, j, kc * 128:(kc + 1) * 128], identity=ident_bf[:]),
                                    [r_xbf[k], r_const], [r_psT[q]])
                            eng = "act" if j % 2 == 0 else "dve"
                            if eng == "act":
                                P.op("act", lambda e, q=q, k=k, j=j: e.copy(out=xT[k][:, :, j * 128:(j + 1) * 128], in_=psT[q][:]),
                                     [r_psT[q]], [r_xT[k]])
                            else:
                                P.op("dve", lambda e, q=q, k=k, j=j: e.tensor_copy(out=xT[k][:, :, j * 128:(j + 1) * 128], in_=psT[q][:]),
                                     [r_psT[q]], [r_xT[k]])
                        for m in range(4):
                            q = nps % 4
                            nps += 1
                            for kc in range(8):
                                P.op("pe", lambda e, q=q, k=k, m=m, kc=kc: e.matmul(
                                    psA[q][:], lhsT=wA[:, kc, m * 128:(m + 1) * 128], rhs=xT[k][:, kc, :],
                                    start=(kc == 0), stop=(kc == 7)), [r_wA, r_xT[k]], [r_psA[q]])
                            P.op("act", lambda e, q=q, k=k, m=m: e.activation(
                                out=kTc[k][:, m, :], in_=psA[q][:], func=AF.Identity, bias=bcol[:, 4 + m:5 + m], scale=1.0),
                                [r_psA[q], r_bA], [r_kTc[k]])
                        for m in range(4):
                            q = nps % 4
                            nps += 1
                            for kc in range(8):
                                P.op("pe", lambda e, q=q, k=k, m=m, kc=kc: e.matmul(
                                    psA[q][:], lhsT=wA[:, kc, 1024 + m * 128:1024 + (m + 1) * 128], rhs=xT[k][:, kc, :],
                                    start=(kc == 0), stop=(kc == 7)), [r_wA, r_xT[k]], [r_psA[q]])
                            P.op("act", lambda e, q=q, k=k, m=m: e.activation(
                                out=pTc[k][:, m, :], in_=psA[q][:], func=AF.Identity, bias=bcol[:, 12 + m:13 + m], scale=1.0),
                                [r_psA[q], r_bA], [r_pTc[k]])
                        for j in range(4):
                            q = nps % 4
                            nps += 1
                            for kc in range(8):
                                P.op("pe", lambda e, q=q, k=k, j=j, kc=kc: e.matmul(
                                    psA[q][:], lhsT=xT[k][:, kc, j * 128:(j + 1) * 128], rhs=wA[:, kc, 512:1024],
                                    start=(kc == 0), stop=(kc == 7)), [r_wA, r_xT[k]], [r_psA[q]])
                            P.op("dve", lambda e, q=q, k=k, j=j: e.tensor_tensor(
                                out=Vc[k][:, j, :], in0=psA[q][:], in1=bv[:], op=ALU.add),
                                [r_psA[q], r_bA], [r_Vc[k]])
                        P.dma("sp", lambda e, k=k, u=u, c=c: e.dma_start(
                            out=kvp[u, :, 0, :].rearrange("p (m t) -> p m t", m=4)[:, :, c * 512:(c + 1) * 512], in_=kTc[k][:]),
                            [r_kTc[k]], [], r_kTc[k])
                        P.dma("sp", lambda e, k=k, u=u, c=c: e.dma_start(
                            out=kvp[u, :, 2, :].rearrange("p (m t) -> p m t", m=4)[:, :, c * 512:(c + 1) * 512], in_=pTc[k][:]),
                            [r_pTc[k]], [], r_pTc[k])
                        P.dma("sp", lambda e, k=k, u=u, c=c: e.dma_start(
                            out=kvp[u, :, 1, c * 2048:(c + 1) * 2048], in_=Vc[k][:].rearrange("p j f -> p (j f)")),
                            [r_Vc[k]], [], r_Vc[k])
                P.barrier()
                P.release([r_wA, r_bA] + r_xbf + r_kTc + r_pTc + r_Vc)

            with ExitStack() as _st:
                _pass(_st)
            def _pass(st, l=(l if "l" in dir() else 0), last=(last if "last" in dir() else False)):
                CH = 256
                RPC = CH // 64
                TPC = CH // 128
                NCH = 2048 // CH
                EXT = CH + 16
                wB = sbt(st, "wB", [128, 8, 2560], BF16)
                woa = sbt(st, "woa", [128, 4, D], BF16)
                wop = sbt(st, "wop", [128, 4, D], BF16)
                wout = sbt(st, "wout", [128, 8, D], BF16)
                wpl = sbt(st, "wpl", [128, 4, 128], BF16)
                r_wB = P.res("wB")
                for kc in range(8):
                    P.dma("pool", lambda e, kc=kc: e.dma_start(out=wB[:, kc, 0:512], in_=w_in[l, kc * 128:(kc + 1) * 128, 0:512]), [], [r_wB], r_wB)
                    P.dma("pool", lambda e, kc=kc: e.dma_start(out=wB[:, kc, 512:2560], in_=w_in[l, kc * 128:(kc + 1) * 128, 2048:4096]), [], [r_wB], r_wB)
                    P.dma("pool", lambda e, kc=kc: e.dma_start(out=wout[:, kc, :], in_=w_out[l, kc * 128:(kc + 1) * 128, :]), [], [r_wB], r_wB)
                for kc in range(4):
                    P.dma("pool", lambda e, kc=kc: e.dma_start(out=woa[:, kc, :], in_=w_oa[l, kc * 128:(kc + 1) * 128, :]), [], [r_wB], r_wB)
                    P.dma("pool", lambda e, kc=kc: e.dma_start(out=wop[:, kc, :], in_=w_op[l, kc * 128:(kc + 1) * 128, :]), [], [r_wB], r_wB)
                    P.dma("pool", lambda e, kc=kc: e.dma_start(out=wpl[:, kc, :], in_=w_pool[l, kc, :, :]), [], [r_wB], r_wB)
                bcol = sbt(st, "B_bcol", [128, 32], F32)
                bq8 = sbt(st, "B_bq8", [128, 4], F32)
                psc = sbt(st, "B_psc", [128, 4], F32)
                bpc = sbt(st, "B_bpc", [128, 4], F32)
                bout_f = sbt(st, "B_boutf", [1, D], F32)
                bout = sbt(st, "B_bout", [1, D], BF16)
                g1 = sbt(st, "B_g1", [128, D], F32)
                b1t = sbt(st, "B_b1", [128, D], F32)
                invc = sbt(st, "B_invc", [128, 4, 16], F32)
                maskt = sbt(st, "B_mask", [128, 64], F32)
                Tb = sbt(st, "B_Tb", [128, 4, 15, 64], BF16)
                r_cB = P.res("B_c")
                P.dma("sp", lambda e: e.dma_start(out=bcol[:], in_=b_in[l].rearrange("(c p) -> p c", p=128),
                                                  allow_slow_non_contiguous=True), [], [r_cB], r_cB)
                P.dma("sp", lambda e: e.dma_start(out=psc[:], in_=pool_scale[l].rearrange("(c p) -> p c", p=128),
                                                  allow_slow_non_contiguous=True), [], [r_cB], r_cB)
                P.dma("sp", lambda e: e.dma_start(out=bpc[:], in_=b_pool[l].rearrange("c p -> p c"),
                                                  allow_slow_non_contiguous=True), [], [r_cB], r_cB)
                P.dma("sp", lambda e: e.dma_start(out=bout_f[:], in_=b_out[l][None, :]), [], [r_cB], r_cB)
                P.dma("sp", lambda e: e.dma_start(out=g1[:], in_=ln1_g[l][None, :].to_broadcast([128, D])), [], [r_cB], r_cB)
                P.dma("sp", lambda e: e.dma_start(out=b1t[:], in_=ln1_b[l][None, :].to_broadcast([128, D])), [], [r_cB], r_cB)
                P.dma("sp", lambda e: e.dma_start(out=invc[:], in_=c_invc), [], [r_cB], r_cB)
                P.dma("sp", lambda e: e.dma_start(out=maskt[:], in_=c_mask), [], [r_cB], r_cB)
                for hp in range(4):
                    P.dma("pool", lambda e, hp=hp: e.dma_start(out=Tb[:, hp, :, :].rearrange("p a b -> p (a b)"),
                                                               in_=rpbT[l, :, hp * 960:(hp + 1) * 960]), [], [r_cB], r_cB)
                P.op("dve", lambda e: e.tensor_tensor(
                    out=Tb[:].rearrange("p a b c -> p (a b) c"), in0=Tb[:].rearrange("p a b c -> p (a b) c"),
                    in1=maskt[:][:, None, :].to_broadcast([128, 60, 64]), op=ALU.add), [r_cB], [r_cB])
                P.op("dve", lambda e: e.tensor_scalar(out=bq8[:], in0=bcol[:, 0:4], scalar1=0.125, scalar2=None, op0=ALU.mult), [r_cB], [r_cB])
                P.op("dve", lambda e: e.tensor_tensor(out=bpc[:], in0=bpc[:], in1=psc[:], op=ALU.mult), [r_cB], [r_cB])
                P.op("dve", lambda e: e.tensor_copy(out=bout[:], in_=bout_f[:]), [r_cB], [r_cB])

                kT = sbt(st, "B_kT", [128, 4, 2048], BF16)
                Vt = sbt(st, "B_V", [128, 16, 512], BF16)
                r_kT, r_V = P.res("B_kT"), P.res("B_V")
                NXR = 4
                xres = [sbt(st, f"B_xres{i}", [128, D], F32) for i in range(NXR)]
                r_xres = [P.res(f"B_xres{i}") for i in range(NXR)]
                xbf = [sbt(st, f"B_xbf{i}", [128, D], BF16) for i in range(4)]
                r_xbf = [P.res(f"B_xbf{i}") for i in range(4)]

                def dbl(name, shape, dt):
                    t_, r_ = sbt(st, name, shape, dt), P.res(name)
                    return [t_, t_], [r_, r_]
                xT, r_xT = dbl("B_xT", [128, 8, CH], BF16)
                qT, r_qT = dbl("B_qT", [128, 4, CH], BF16)
                aT, r_aT = dbl("B_aT", [128, 4, CH], BF16)
                dT, r_dT = dbl("B_dT", [128, 4, CH], BF16)
                _tpmT = sbt(st, "B_pmT", [128, 4, CH], BF16)
                _rpmT = P.res("B_pmT")
                pmT, r_pmT = [_tpmT, _tpmT], [_rpmT, _rpmT]
                _m = sbt(st, "B_mixT", [128, 8, CH], BF16)
                _rm = P.res("B_mixT")
                mixT, r_mixT = [_m, _m], [_rm, _rm]
                _tpex = sbt(st, "B_pex", [128, 4, EXT], BF16)
                _rpex = P.res("B_pex")
                pex, r_pex = [_tpex, _tpex], [_rpex, _rpex]
                NA = 3
                Eb = [sbt(st, f"B_E{i}", [128, 512], BF16) for i in range(NA)]
                r_Eb = [P.res(f"B_E{i}") for i in range(NA)]
                rsum = [sbt(st, f"B_rs{i}", [128, 2], F32) for i in range(NA)]
                Pb = [sbt(st, f"B_P{i}", [128, 640], BF16) for i in range(NA)]
                r_Pb = [P.res(f"B_P{i}") for i in range(NA)]
                PT = [sbt(st, f"B_PT{i}", [128, 5, 128], BF16) for i in range(NA)]
                r_PT = [P.res(f"B_PT{i}") for i in range(NA)]
                pe_ = sbt(st, "B_pe", [128, EXT], F32)
                sA = sbt(st, "B_sA", [128, EXT], F32)
                sB_ = sbt(st, "B_sB", [128, EXT], F32)
                r_pl = P.res("B_pl")
                sg = [sbt(st, f"B_sg{i}", [128, CH], BF16) for i in range(2)]
                r_sg = [P.res(f"B_sg{i}") for i in range(2)]
                tm = [sbt(st, f"B_tm{i}", [128, CH], F32) for i in range(4)]
                r_tm = [P.res(f"B_tm{i}") for i in range(4)]
                lsc = [ln_scratch(st, f"B_l{i}") for i in range(NXR)]
                r_lsc = [P.res(f"B_ls{i}") for i in range(NXR)]
                psT = pst(st, "B_psT", [128, 8, 128], BF16)
                r_psT = P.res("B_psT")
                psP = [pst(st, f"B_psP{i}", [128, 512], F32) for i in range(3)]
                r_psP = [P.res(f"B_psP{i}") for i in range(3)]
                psS = [pst(st, f"B_psS{i}", [128, 512], F32) for i in range(2)]
                r_psS = [P.res(f"B_psS{i}") for i in range(2)]
                psPT = pst(st, "B_psPT", [128, 8, 128], BF16)
                r_psPT = P.res("B_psPT")
                psAT = pst(st, "B_psAT", [128, 512], F32)
                r_psAT = P.res("B_psAT")
                for i in range(NA):
                    P.op("dve", lambda e, i=i: e.memset(Pb[i][:], 0.0), [], [r_Pb[i]])
                npp = 0
                nat = 0
                nps = 0
                nxr = 0
                nxb = 0
                ncc = 0
                ntm = 0
                def gathers(gi):
                    for j in range(TPC):
                        col = gi * TPC + j
                        kb = col % 4
                        P.dma("pool", lambda e, kb=kb, col=col: e.indirect_dma_start(
                            out=xbf[kb][:, :], out_offset=None, in_=xa[:, :],
                            in_offset=bass.IndirectOffsetOnAxis(ap=kvidx[:, col:col + 1], axis=0)),
                            [r_const], [r_xbf[kb]], r_xbf[kb])
                    for j in range(TPC):
                        col = gi * TPC + j
                        k = col % NXR
                        P.dma("pool", lambda e, k=k, col=col: e.indirect_dma_start(
                            out=xres[k][:, :], out_offset=None, in_=xa[:, :],
                            in_offset=bass.IndirectOffsetOnAxis(ap=kvidx[:, col:col + 1], axis=0)),
                            [r_const], [r_xres[k]], r_xres[k])

                gathers(0)
                for u in range(NU):
                    P.dma("sp", lambda e, u=u: e.dma_start(out=kT[:].rearrange("p m t -> p (m t)"), in_=kvp[u, :, 0, :]), [], [r_kT], r_kT)
                    P.dma("sp", lambda e, u=u: e.dma_start(out=Vt[:].rearrange("p m t -> p (m t)"), in_=kvp[u, :, 1, :]), [], [r_V], r_V)
                    for c in range(NCH):
                        cc = ncc % 2
                        ncc += 1
                        gi = u * NCH + c
                        if gi + 1 < NU * NCH:
                            gathers(gi + 1)
                        for j in range(TPC):
                            col = u * 16 + c * TPC + j
                            kb = col % 4
                            for kc in range(8):
                                P.op("pe", lambda e, kb=kb, kc=kc: e.transpose(
                                    out=psT[:, kc, :], in_=xbf[kb][:, kc * 128:(kc + 1) * 128], identity=ident_bf[:]),
                                    [r_xbf[kb], r_const], [r_psT])
                            P.op("dve", lambda e, j=j, cc=cc: e.tensor_copy(out=xT[cc][:, :, j * 128:(j + 1) * 128], in_=psT[:]),
                                 [r_psT], [r_xT[cc]])
                        xk = [(u * 16 + c * TPC + j) % NXR for j in range(TPC)]
                        lo_t = max(c * CH - 8, 0)
                        hi_t = min(c * CH + CH + 8, 2048)
                        eo = lo_t - (c * CH - 8)
                        nn = hi_t - lo_t
                        P.op("pool", lambda e, cc=cc: e.memset(pex[cc][:], 0.0), [], [r_pex[cc]])
                        P.dma("sp", lambda e, u=u, lo_t=lo_t, hi_t=hi_t, eo=eo, nn=nn, cc=cc: e.dma_start(
                            out=pex[cc][:, :, eo:eo + nn],
                            in_=kvp[u, :, 2, :].rearrange("p (m t) -> p m t", m=4)[:, :, lo_t:hi_t]), [], [r_pex[cc]], r_pex[cc])
                        for g in range(4):
                            wv = 2 ** (g + 1)
                            P.op("pool", lambda e, g=g, cc=cc: e.tensor_copy(out=pe_[:], in_=pex[cc][:, g, :]), [r_pex[cc]], [r_pl])
                            P.op("pool", lambda e: e.memset(sA[:], 0.0), [], [r_pl])
                            P.op("pool", lambda e: e.tensor_tensor(out=sA[:, 1:EXT], in0=pe_[:, 0:EXT - 1], in1=pe_[:, 1:EXT], op=ALU.add), [r_pl], [r_pl])
                            cur, oth = sA, sB_
                            for sh in (1, 2, 4)[:g]:
                                P.op("pool", lambda e, oth=oth: e.memset(oth[:], 0.0), [], [r_pl])
                                P.op("pool", lambda e, cur=cur, oth=oth, sh=sh: e.tensor_tensor(
                                    out=oth[:, sh:EXT - sh], in0=cur[:, 0:EXT - 2 * sh], in1=cur[:, 2 * sh:EXT], op=ALU.add), [r_pl], [r_pl])
                                cur, oth = oth, cur
                            P.op("pool", lambda e, cur=cur, oth=oth, wv=wv: e.tensor_scalar(
                                out=oth[:, 8:8 + CH], in0=cur[:, 8:8 + CH], scalar1=1.0 / wv, scalar2=0.0, op0=ALU.mult, op1=ALU.add), [r_pl], [r_pl])
                            if c == 0:
                                P.op("pool", lambda e, cur=cur, oth=oth, g=g: e.tensor_tensor(
                                    out=oth[:, 8:16], in0=cur[:, 8:16], in1=invc[:, g, 0:8], op=ALU.mult), [r_pl, r_cB], [r_pl])
                            if c == NCH - 1:
                                P.op("pool", lambda e, cur=cur, oth=oth, g=g: e.tensor_tensor(
                                    out=oth[:, CH:CH + 8], in0=cur[:, CH:CH + 8], in1=invc[:, g, 8:16], op=ALU.mult), [r_pl, r_cB], [r_pl])
                            P.op("pool", lambda e, oth=oth, g=g, cc=cc: e.tensor_tensor(
                                out=dT[cc][:, g, :], in0=oth[:, 8:8 + CH], in1=pe_[:, 8:8 + CH], op=ALU.subtract), [r_pl], [r_dT[cc]])
                        for m in range(4):
                            q = npp % 3
                            npp += 1
                            for kc in range(8):
                                P.op("pe", lambda e, q=q, m=m, kc=kc, cc=cc: e.matmul(
                                    psP[q][:, 0:CH], lhsT=wB[:, kc, m * 128:(m + 1) * 128], rhs=xT[cc][:, kc, :],
                                    start=(kc == 0), stop=(kc == 7)), [r_wB, r_xT[cc]], [r_psP[q]])
                            P.op("act", lambda e, q=q, m=m, cc=cc: e.activation(
                                out=qT[cc][:, m, :], in_=psP[q][:, 0:CH], func=AF.Identity, bias=bq8[:, m:m + 1], scale=0.125),
                                [r_psP[q], r_cB], [r_qT[cc]])
                        items = [(hp, r8) for hp in range(4) for r8 in range(RPC)]

                        def att_geom(r8):
                            ql = c * RPC + r8
                            ws = min(max(ql - 4, 0), 24)
                            dr0 = ws - ql + 7
                            if ws % 2 == 0:
                                return ws, dr0, 4, 64, ws // 2
                            return ws, dr0, 5, 0, (ws - 1) // 2

                        def S1(t):
                            hp, r8 = items[t]
                            ws, dr0, nch, off, vt0 = att_geom(r8)
                            k = (nat + t) % NA
                            ks = (nat + t) % 2
                            for hh in range(2):
                                lo, hi = hh * 64, (hh + 1) * 64
                                P.op("pe", lambda e, ks=ks, hp=hp, r8=r8, ws=ws, lo=lo, hi=hi, cc=cc: e.matmul(
                                    psS[ks][lo:hi, :], lhsT=qT[cc][lo:hi, hp, r8 * 64:(r8 + 1) * 64],
                                    rhs=kT[lo:hi, hp, ws * 64:ws * 64 + 512], start=True, stop=False),
                                    [r_qT[cc], r_kT], [r_psS[ks]])
                            P.op("pe", lambda e, ks=ks, hp=hp, dr0=dr0: e.matmul(
                                psS[ks][:], lhsT=ident_bf[:], rhs=Tb[:, hp, dr0:dr0 + 8, :].rearrange("p a b -> p (a b)"),
                                start=False, stop=True), [r_cB, r_const], [r_psS[ks]])
                            P.op("act", lambda e, k=k, ks=ks: e.activation(
                                out=Eb[k][:], in_=psS[ks][:], func=AF.Exp, accum_out=rsum[k][:, 0:1]),
                                [r_psS[ks]], [r_Eb[k]])
                            P.op("dve", lambda e, k=k: e.reciprocal(out=rsum[k][:, 1:2], in_=rsum[k][:, 0:1]),
                                 [r_Eb[k]], [r_Eb[k]])
                            P.op("dve", lambda e, k=k: e.tensor_scalar(
                                out=Pb[k][:, 64:576], in0=Eb[k][:], scalar1=rsum[k][:, 1:2], scalar2=None, op0=ALU.mult),
                                [r_Eb[k]], [r_Pb[k]])

                        def S2(t):
                            hp, r8 = items[t]
                            ws, dr0, nch, off, vt0 = att_geom(r8)
                            k = (nat + t) % NA
                            for ch in range(nch):
                                P.op("pe", lambda e, k=k, ch=ch, off=off: e.transpose(
                                    out=psPT[:, ch, :], in_=Pb[k][:, off + ch * 128:off + (ch + 1) * 128], identity=ident_bf[:]),
                                    [r_Pb[k], r_const], [r_psPT])
                            P.op("act", lambda e, k=k, nch=nch: e.copy(out=PT[k][:, 0:nch, :], in_=psPT[:, 0:nch, :]),
                                 [r_psPT], [r_PT[k]])

                        def S3(t):
                            hp, r8 = items[t]
                            ws, dr0, nch, off, vt0 = att_geom(r8)
                            k = (nat + t) % NA
                            for hh in range(2):
                                lo, hi = hh * 64, (hh + 1) * 64
                                hcol = (2 * hp + hh) * 64
                                for ch in range(nch):
                                    P.op("pe", lambda e, k=k, ch=ch, lo=lo, hi=hi, hcol=hcol, r8=r8, vt0=vt0, nch=nch: e.matmul(
                                        psAT[lo:hi, r8 * 64:(r8 + 1) * 64], lhsT=Vt[:, vt0 + ch, hcol:hcol + 64],
                                        rhs=PT[k][:, ch, lo:hi], start=(ch == 0), stop=(ch == nch - 1)),
                                        [r_V, r_PT[k]], [r_psAT])
                            if r8 == RPC - 1:
                                P.op("dve", lambda e, hp=hp, cc=cc: e.tensor_copy(out=aT[cc][:, hp, :], in_=psAT[:, 0:CH]), [r_psAT], [r_aT[cc]])

                        NI = len(items)
                        for t in range(NI + 2):
                            if t < NI:
                                S1(t)
                            if 0 <= t - 1 < NI:
                                S2(t - 1)
                            if 0 <= t - 2 < NI:
                                S3(t - 2)
                        nat += NI
                        for g in range(4):
                            q = npp % 3
                            npp += 1
                            P.op("pe", lambda e, q=q, g=g, cc=cc: e.matmul(psP[q][:, 0:CH], lhsT=wpl[:, g, :], rhs=dT[cc][:, g, :], start=True, stop=True),
                                 [r_wB, r_dT[cc]], [r_psP[q]])
                            P.op("act", lambda e, q=q, g=g, cc=cc: e.activation(
                                out=pmT[cc][:, g, :], in_=psP[q][:, 0:CH], func=AF.Identity, bias=bpc[:, g:g + 1], scale=psc[:, g:g + 1]),
                                [r_psP[q], r_cB], [r_pmT[cc]])
                        for m in range(8):
                            tk = []
                            for br in range(2):
                                q = npp % 3
                                npp += 1
                                wc = 512 + br * 1024 + m * 128
                                for kc in range(8):
                                    P.op("pe", lambda e, q=q, wc=wc, kc=kc, cc=cc: e.matmul(
                                        psP[q][:, 0:CH], lhsT=wB[:, kc, wc:wc + 128], rhs=xT[cc][:, kc, :], start=(kc == 0), stop=(kc == 7)),
                                        [r_wB, r_xT[cc]], [r_psP[q]])
                                bc = 16 + br * 8 + m
                                P.op("act", lambda e, q=q, br=br, bc=bc: e.activation(
                                    out=sg[br][:], in_=psP[q][:, 0:CH], func=AF.Sigmoid, bias=bcol[:, bc:bc + 1], scale=1.0),
                                    [r_psP[q], r_cB], [r_sg[br]])
                                q = npp % 3
                                npp += 1
                                wsrc, asrc, r_a = (woa, aT[cc], r_aT[cc]) if br == 0 else (wop, pmT[cc], r_pmT[cc])
                                for kc in range(4):
                                    P.op("pe", lambda e, q=q, wsrc=wsrc, asrc=asrc, m=m, kc=kc: e.matmul(
                                        psP[q][:, 0:CH], lhsT=wsrc[:, kc, m * 128:(m + 1) * 128], rhs=asrc[:, kc, :], start=(kc == 0), stop=(kc == 3)),
                                        [r_wB, r_a], [r_psP[q]])
                                t_ = ntm % 4
                                ntm += 1
                                tk.append(t_)
                                P.op("dve", lambda e, q=q, br=br, t_=t_: e.tensor_tensor(out=tm[t_][:], in0=psP[q][:, 0:CH], in1=sg[br][:], op=ALU.mult),
                                     [r_psP[q], r_sg[br]], [r_tm[t_]])
                            P.op("pool", lambda e, m=m, tk=tk, cc=cc: e.tensor_tensor(out=mixT[cc][:, m, :], in0=tm[tk[0]][:], in1=tm[tk[1]][:], op=ALU.add),
                                 [r_tm[tk[0]], r_tm[tk[1]]], [r_mixT[cc]])
                        for j in range(TPC):
                            k = xk[j]
                            for h in range(2):
                                q = npp % 3
                                npp += 1
                                for kc in range(8):
                                    P.op("pe", lambda e, q=q, j=j, h=h, kc=kc, cc=cc: e.matmul(
                                        psP[q][:], lhsT=mixT[cc][:, kc, j * 128:(j + 1) * 128], rhs=wout[:, kc, h * 512:(h + 1) * 512],
                                        start=(kc == 0), stop=False), [r_wB, r_mixT[cc]], [r_psP[q]])
                                P.op("pe", lambda e, q=q, h=h: e.matmul(
                                    psP[q][:], lhsT=ones[0:1, :], rhs=bout[0:1, h * 512:(h + 1) * 512], start=False, stop=True),
                                    [r_cB, r_const], [r_psP[q]])
                                P.op("dve", lambda e, q=q, k=k, h=h: e.scalar_tensor_tensor(
                                    out=xres[k][:, h * 512:(h + 1) * 512], in0=xres[k][:, h * 512:(h + 1) * 512], scalar=ALPHA,
                                    in1=psP[q][:], op0=ALU.mult, op1=ALU.add), [r_psP[q], r_xres[k]], [r_xres[k]])
                        emit_ln_multi([(xres[k][:], r_xres[k], lsc[k], r_lsc[k]) for k in xk], g1[:], b1t[:], cread=[r_cB])
                        for j in range(TPC):
                            col = u * 16 + c * TPC + j
                            k = xk[j]
                            P.dma("pool", lambda e, k=k, col=col: e.indirect_dma_start(
                                out=x1b[:, :], out_offset=bass.IndirectOffsetOnAxis(ap=outidx[:, col:col + 1], axis=0),
                                in_=xres[k][:, :], in_offset=None), [r_xres[k], r_const], [], r_xres[k])
                P.barrier()
                P.release([r_wB, r_cB, r_kT, r_V, r_pex[0]] + r_xbf + r_xres)

            with ExitStack() as _st:
                _pass(_st)
            def _pass(st, l=(l if "l" in dir() else 0), last=(last if "last" in dir() else False)):
                wr = sbt(st, "R_wr", [128, 8, NE], F32)
                brt = sbt(st, "R_br", [128, NE], F32)
                bst = sbt(st, "R_bst", [128, NBLK], F32)
                rowc = sbt(st, "R_rowc", [128, 16], F32)
                r_cR = P.res("R_c")
                P.dma("sp", lambda e: e.dma_start(out=wr[:], in_=w_router.rearrange("(k p) n -> p k n", p=128)), [], [r_cR], r_cR)
                P.dma("sp", lambda e: e.dma_start(out=brt[:], in_=b_router[None, :].to_broadcast([128, NE])), [], [r_cR], r_cR)
                P.dma("sp", lambda e: e.dma_start(out=bst[:], in_=c_bstart), [], [r_cR], r_cR)
                P.dma("sp", lambda e: e.dma_start(out=rowc[:], in_=c_rowc), [], [r_cR], r_cR)
                A0 = sbt(st, "R_A0", [128, NT, NE], F32)
                A1 = sbt(st, "R_A1", [128, NT, NE], F32)
                POS = sbt(st, "R_POS", [128, NT, NE], F32)
                r_A = P.res("R_A")
                base = sbt(st, "R_base", [128, NE], F32)
                r_base = P.res("R_base")
                P.op("dve", lambda e: e.memset(base[:], 0.0), [], [r_base])
                xt = [sbt(st, f"R_x{i}", [128, D], F32) for i in range(2)]
                r_xt = [P.res(f"R_x{i}") for i in range(2)]
                xT32 = [sbt(st, f"R_xT{i}", [128, 8, 128], F32) for i in range(2)]
                r_xT32 = [P.res(f"R_xT{i}") for i in range(2)]
                zt = [sbt(st, f"R_z{i}", [128, 16 * 8], F32) for i in range(2)]
                r_zt = [P.res(f"R_z{i}") for i in range(2)]
                sm = [sbt(st, f"R_sm{i}", [128, 32], F32) for i in range(2)]
                Abf = [sbt(st, f"R_Ab{i}", [128, NE], BF16) for i in range(2)]
                psX = [pst(st, f"R_psX{i}", [128, 8, 128], F32) for i in range(1)]
                r_psX = [P.res(f"R_psX{i}") for i in range(1)]
                psL_ = [pst(st, f"R_psL{i}", [128, 512], F32) for i in range(2)]
                psL = [t[:, 0:NE] for t in psL_]
                r_psL = [P.res(f"R_psL{i}") for i in range(2)]
                psC_ = [pst(st, f"R_psC{i}", [128, 512], F32) for i in range(2)]
                psC = [t[:, 0:2 * NE].rearrange("p (a b) -> p a b", a=2) for t in psC_]
                r_psC = [P.res(f"R_psC{i}") for i in range(2)]
                for i in range(NT):
                    k = i % 2
                    P.dma("sp", lambda e, k=k, i=i: e.dma_start(out=xt[k][:], in_=x1b[i * 128:(i + 1) * 128, :]), [], [r_xt[k]], r_xt[k])
                    for kc in range(8):
                        P.op("pe", lambda e, k=k, kc=kc: e.transpose(out=psX[0][:, kc, :], in_=xt[k][:, kc * 128:(kc + 1) * 128], identity=ident_f[:]),
                             [r_xt[k], r_const], [r_psX[0]])
                    P.op("act", lambda e, k=k: e.copy(out=xT32[k][:, 0:4, :], in_=psX[0][:, 0:4, :]), [r_psX[0]], [r_xT32[k]])
                    P.op("dve", lambda e, k=k: e.tensor_copy(out=xT32[k][:, 4:8, :], in_=psX[0][:, 4:8, :]), [r_psX[0]], [r_xT32[k]])
                    for kc in range(8):
                        P.op("pe", lambda e, k=k, kc=kc: e.matmul(psL[k], lhsT=xT32[k][:, kc, :], rhs=wr[:, kc, :], start=(kc == 0), stop=(kc == 7)),
                             [r_xT32[k], r_cR], [r_psL[k]])
                    z = zt[k]
                    Z = lambda a, z=z: z[:, a * 16:(a + 1) * 16]
                    Z4 = lambda a, z=z: z[:, a * 16:(a + 1) * 16].rearrange("p (g j) -> p g j", g=4)
                    s = sm[k]
                    rz = r_zt[k]
                    P.op("dve", lambda e, k=k, Z=Z: e.tensor_tensor(out=Z(0), in0=psL[k], in1=brt[:], op=ALU.add), [r_psL[k], r_cR], [rz])
                    P.op("dve", lambda e, Z=Z, s=s: e.tensor_reduce(out=s[:, 0:1], in_=Z(0), axis=AX.X, op=ALU.max, negate=True), [rz], [rz])
                    P.op("act", lambda e, Z=Z, s=s: e.activation(out=Z(1), in_=Z(0), func=AF.Exp, bias=s[:, 0:1], scale=1.0), [rz], [rz])
                    P.op("dve", lambda e, Z4=Z4, s=s: e.tensor_reduce(out=s[:, 4:8], in_=Z4(1), axis=AX.X, op=ALU.max), [rz], [rz])
                    P.op("dve", lambda e, Z4=Z4, s=s: e.tensor_tensor(out=Z4(2), in0=Z4(1), in1=s[:, 4:8][:, :, None].to_broadcast([128, 4, 4]), op=ALU.is_equal), [rz], [rz])
                    P.op("dve", lambda e, Z=Z: e.scalar_tensor_tensor(out=Z(3), in0=Z(2), scalar=-2.0, in1=Z(1), op0=ALU.mult, op1=ALU.add), [rz], [rz])
                    P.op("dve", lambda e, Z4=Z4, s=s: e.tensor_reduce(out=s[:, 8:12], in_=Z4(3), axis=AX.X, op=ALU.max), [rz], [rz])
                    P.op("dve", lambda e, Z4=Z4, s=s: e.tensor_tensor(out=Z4(4), in0=Z4(3), in1=s[:, 8:12][:, :, None].to_broadcast([128, 4, 4]), op=ALU.is_equal), [rz], [rz])
                    P.op("dve", lambda e, s=s: e.tensor_tensor(out=s[:, 12:16], in0=s[:, 4:8], in1=s[:, 8:12], op=ALU.add), [rz], [rz])
                    P.op("dve", lambda e, s=s: e.tensor_reduce(out=s[:, 1:2], in_=s[:, 12:16], axis=AX.X, op=ALU.max), [rz], [rz])
                    P.op("dve", lambda e, s=s: e.tensor_scalar(out=s[:, 16:20], in0=s[:, 12:16], scalar1=s[:, 1:2], scalar2=None, op0=ALU.is_equal), [rz], [rz])
                    P.op("dve", lambda e, Z4=Z4, s=s, i=i: e.tensor_tensor(out=A0[:, i, :].rearrange("p (g j) -> p g j", g=4), in0=Z4(2),
                                                                             in1=s[:, 16:20][:, :, None].to_broadcast([128, 4, 4]), op=ALU.mult), [rz], [r_A])
                    P.op("dve", lambda e, Z4=Z4, s=s, i=i: e.tensor_tensor(out=A1[:, i, :].rearrange("p (g j) -> p g j", g=4), in0=Z4(4),
                                                                             in1=s[:, 16:20][:, :, None].to_broadcast([128, 4, 4]), op=ALU.mult), [rz], [r_A])
                    P.op("dve", lambda e, s=s: e.tensor_tensor(out=s[:, 20:24], in0=s[:, 16:20], in1=s[:, 4:8], op=ALU.mult), [rz], [rz])
                    P.op("dve", lambda e, s=s: e.tensor_tensor(out=s[:, 24:28], in0=s[:, 16:20], in1=s[:, 8:12], op=ALU.mult), [rz], [rz])
                    P.op("dve", lambda e, s=s: e.tensor_reduce(out=s[:, 28:30], in_=s[:, 20:28].rearrange("p (a b) -> p a b", a=2), axis=AX.X, op=ALU.add), [rz], [rz])
                    P.op("dve", lambda e, s=s: e.tensor_reduce(out=s[:, 30:31], in_=s[:, 28:30], axis=AX.X, op=ALU.add), [rz], [rz])
                    P.op("dve", lambda e, s=s: e.reciprocal(out=s[:, 31:32], in_=s[:, 30:31]), [rz], [rz])
                    P.op("dve", lambda e, s=s, i=i: e.tensor_scalar(out=gates[:, i, :], in0=s[:, 28:30], scalar1=s[:, 31:32], scalar2=None, op0=ALU.mult), [rz], [r_gates])
                    P.op("dve", lambda e, k=k, i=i: e.tensor_tensor(out=Abf[k][:], in0=A0[:, i, :], in1=A1[:, i, :], op=ALU.add), [r_A], [rz])
                    P.op("pe", lambda e, k=k: e.matmul(psC[k][:, 0, :], lhsT=tri[:], rhs=Abf[k][:], start=True, stop=True), [rz, r_const], [r_psC[k]])
                    P.op("pe", lambda e, k=k: e.matmul(psC[k][:, 1, :], lhsT=ones[:], rhs=Abf[k][:], start=True, stop=True), [rz, r_const], [r_psC[k]])
                    P.op("dve", lambda e, k=k, i=i: e.tensor_tensor(out=POS[:, i, :], in0=psC[k][:, 0, :], in1=base[:], op=ALU.add), [r_psC[k], r_base], [r_A])
                    P.op("dve", lambda e, k=k: e.tensor_tensor(out=base[:], in0=psC[k][:, 1, :], in1=base[:], op=ALU.add), [r_psC[k], r_base], [r_base])
                ci = sbt(st, "R_ci", [128, NE], I32)
                pf = sbt(st, "R_pf", [128, 4, NE], F32)
                r_pf = P.res("R_pf")
                P.op("dve", lambda e: e.tensor_scalar(out=ci[:], in0=base[:], scalar1=float(BLK - 1), scalar2=None, op0=ALU.add), [r_base], [r_pf])
                P.op("dve", lambda e: e.tensor_scalar(out=ci[:], in0=ci[:], scalar1=9, scalar2=9, op0=ALU.arith_shift_right, op1=ALU.arith_shift_left), [r_pf], [r_pf])
                P.op("dve", lambda e: e.tensor_copy(out=pf[:, 0, :], in_=ci[:]), [r_pf], [r_pf])
                P.op("dve", lambda e: e.tensor_copy(out=pf[:, 1, :], in_=pf[:, 0, :]), [r_pf], [r_pf])
                for sh in (1, 2, 4, 8):
                    P.op("dve", lambda e: e.tensor_copy(out=pf[:, 2, :], in_=pf[:, 1, :]), [r_pf], [r_pf])
                    P.op("dve", lambda e, sh=sh: e.tensor_tensor(out=pf[:, 1, sh:NE], in0=pf[:, 2, sh:NE], in1=pf[:, 2, 0:NE - sh], op=ALU.add), [r_pf], [r_pf])
                P.op("dve", lambda e: e.tensor_tensor(out=pf[:, 3, :], in0=pf[:, 1, :], in1=pf[:, 0, :], op=ALU.subtract), [r_pf], [r_pf])
                P.op("dve", lambda e: e.tensor_tensor(out=POS[:], in0=POS[:], in1=pf[:, 3:4, :].to_broadcast([128, NT, NE]), op=ALU.add), [r_pf, r_A], [r_A])
                sf = sbt(st, "R_sf", [128, NT], F32)
                for Ak, sl in ((A0, slot0), (A1, slot1)):
                    P.op("dve", lambda e, Ak=Ak: e.tensor_tensor(out=Ak[:], in0=Ak[:], in1=POS[:], op=ALU.mult), [r_A], [r_A])
                    P.op("dve", lambda e, Ak=Ak: e.tensor_reduce(out=sf[:], in_=Ak[:], axis=AX.X, op=ALU.add), [r_A], [r_pf])
                    P.op("dve", lambda e, sl=sl: e.tensor_copy(out=sl[:], in_=sf[:]), [r_pf], [r_slots])
                eb = sbt(st, "R_eb", [128, NBLK], F32)
                tmpb = sbt(st, "R_tmpb", [128, NBLK], F32)
                r_eb = P.res("R_eb")
                P.op("dve", lambda e: e.memset(eb[:], 0.0), [], [r_eb])
                for ex in range(NE):
                    P.op("dve", lambda e, ex=ex: e.tensor_scalar(out=tmpb[:], in0=bst[:], scalar1=pf[:, 1, ex:ex + 1], scalar2=None, op0=ALU.is_ge), [r_pf, r_cR], [r_eb])
                    P.op("dve", lambda e: e.tensor_tensor(out=eb[:], in0=eb[:], in1=tmpb[:], op=ALU.add), [r_eb], [r_eb])
                P.op("dve", lambda e: e.tensor_scalar(out=eb[:], in0=eb[:], scalar1=float(NE - 1), scalar2=None, op0=ALU.min), [r_eb], [r_eb])
                chg = sbt(st, "R_chg", [128, NBLK], F32)
                ebo1 = sbt(st, "R_ebo1", [128, NBLK], F32)
                ebo2 = sbt(st, "R_ebo2", [128, NBLK], F32)
                P.op("dve", lambda e: e.memset(chg[:], 0.0), [], [r_eb])
                P.op("dve", lambda e: e.tensor_tensor(out=chg[:, 1:NBLK], in0=eb[:, 1:NBLK], in1=eb[:, 0:NBLK - 1], op=ALU.is_equal), [r_eb], [r_eb])
                P.op("dve", lambda e: e.tensor_scalar(out=chg[:], in0=chg[:], scalar1=float(2 ** 30), scalar2=None, op0=ALU.mult), [r_eb], [r_eb])
                P.op("dve", lambda e: e.scalar_tensor_tensor(out=ebo1[:], in0=eb[:], scalar=float(D), in1=chg[:], op0=ALU.mult, op1=ALU.add), [r_eb], [r_eb])
                P.op("dve", lambda e: e.scalar_tensor_tensor(out=ebo2[:], in0=eb[:], scalar=256.0, in1=chg[:], op0=ALU.mult, op1=ALU.add), [r_eb], [r_eb])
                for kc in range(8):
                    P.op("dve", lambda e, kc=kc: e.tensor_scalar(out=widx1[:, :, kc], in0=ebo1[:], scalar1=rowc[:, kc:kc + 1], scalar2=None, op0=ALU.add), [r_eb, r_cR], [r_widx])
                p2 = sbt(st, "R_p2", [128, 2], F32)
                P.op("dve", lambda e: e.tensor_scalar(out=p2[:, 0:1], in0=rowc[:, 0:1], scalar1=2.0, scalar2=None, op0=ALU.mult), [r_cR], [r_eb])
                P.op("dve", lambda e: e.tensor_scalar(out=p2[:, 1:2], in0=rowc[:, 0:1], scalar1=2.0, scalar2=1.0, op0=ALU.mult, op1=ALU.add), [r_cR], [r_eb])
                for h in range(2):
                    P.op("dve", lambda e, h=h: e.tensor_scalar(out=widx2[:, :, h], in0=ebo2[:], scalar1=p2[:, h:h + 1], scalar2=None, op0=ALU.add), [r_eb, r_cR], [r_widx])
                ebl = sbt(st, "R_ebl", [128, NBLK], F32)
                P.op("dve", lambda e: e.tensor_scalar(out=ebl[:], in0=eb[:], scalar1=float(l * NE), scalar2=None, op0=ALU.add), [r_eb], [r_eb])
                P.op("dve", lambda e: e.tensor_scalar(out=bidx1[:], in0=ebl[:], scalar1=128.0, scalar2=rowc[:, 0:1], op0=ALU.mult, op1=ALU.add), [r_eb, r_cR], [r_widx])
                P.op("dve", lambda e: e.tensor_copy(out=bidx2[:], in_=ebl[:]), [r_eb], [r_widx])
                zi = sbt(st, "R_zi", [128, NSLOT // 128], I32)
                r_zi = P.res("R_zi")
                P.op("dve", lambda e: e.memset(zi[:], 0), [], [r_zi])
                P.dma("sp", lambda e: e.dma_start(out=table.rearrange("(p f) o -> p (f o)", p=128), in_=zi[:]), [r_zi], [], r_zi)
                P.barrier()
                for i in range(NT):
                    for sl in (slot0, slot1):
                        P.dma("pool", lambda e, sl=sl, i=i: e.indirect_dma_start(
                            out=table[:, :], out_offset=bass.IndirectOffsetOnAxis(ap=sl[:, i:i + 1], axis=0),
                            in_=tokid[:, i:i + 1], in_offset=None), [r_slots, r_const], [], r_slots)
                P.barrier()
                P.release(r_xt + [r_cR, r_zi])

            with ExitStack() as _st:
                _pass(_st)
            if debug == "noM":
                continue
            def _pass(st, l=l, last=last):
                w1s = sbt(st, "M_w1", [128, 8, DFF], BF16)
                w2s = sbt(st, "M_w2", [128, 16, D], BF16)
                stg2 = sbt(st, "M_stg2", [128, 2, 8 * D], F32)
                r_stg2 = P.res("M_stg2")
                w2v = w2L[l].rearrange("(r k) d -> r (k d)", k=8)
                b1v = b1.rearrange("l e (q m) -> (l e q) m", m=16)
                r_w1s, r_w2s = P.res("M_w1"), P.res("M_w2")
                tix = [sbt(st, f"M_tix{i}", [128, 4], I32) for i in range(2)]
                r_tix = [P.res(f"M_tix{i}") for i in range(2)]
                xg = [sbt(st, f"M_xg{i}", [128, 4, D], BF16) for i in range(2)]
                r_xg = [P.res(f"M_xg{i}") for i in range(2)]
                xT = [sbt(st, f"M_xT{i}", [128, 8, 512], BF16) for i in range(2)]
                r_xT = [P.res(f"M_xT{i}") for i in range(2)]
                hT = sbt(st, "M_hT", [128, 16, 512], BF16)
                r_hT = P.res("M_hT")
                b1c = [sbt(st, f"M_b1c{i}", [128, 16], F32) for i in range(2)]
                r_b1c = [P.res(f"M_b1c{i}") for i in range(2)]
                b2b = [sbt(st, f"M_b2b{i}", [128, D], F32) for i in range(2)]
                r_b2b = [P.res(f"M_b2b{i}") for i in range(2)]
                ysb = [sbt(st, f"M_y{i}", [128, D], F32) for i in range(2)]
                r_ysb = [P.res(f"M_y{i}") for i in range(2)]
                psT = [pst(st, f"M_psT{i}", [128, 8, 128], BF16) for i in range(2)]
                r_psT = [P.res(f"M_psT{i}") for i in range(2)]
                psH = [pst(st, f"M_psH{i}", [128, 512], F32) for i in range(3)]
                r_psH = [P.res(f"M_psH{i}") for i in range(3)]
                psY = [pst(st, f"M_psY{i}", [128, 512], F32) for i in range(3)]
                r_psY = [P.res(f"M_psY{i}") for i in range(3)]
                cnt = dict(npt=0, nph=0, npy=0, ny=0)
                tview = table.rearrange("(b p j) o -> b p (j o)", p=128, j=4)
                yview = ybuf.rearrange("(b p j) d -> b j p d", p=128, j=4)

                def tokens(b):
                    k = b % 2
                    P.dma("sp", lambda e, k=k, b=b: e.dma_start(out=tix[k][:], in_=tview[b]), [], [r_tix[k]], r_tix[k])
                    for j in range(4):
                        P.dma("pool", lambda e, k=k, j=j: e.indirect_dma_start(
                            out=xg[k][:, j, :], out_offset=None, in_=x1b[:, :],
                            in_offset=bass.IndirectOffsetOnAxis(ap=tix[k][:, j:j + 1], axis=0)), [r_tix[k]], [r_xg[k]], r_xg[k])

                def b1_load(b):
                    k = b % 2
                    P.dma("pool", lambda e, k=k, b=b: e.indirect_dma_start(
                        out=b1c[k][:, :], out_offset=None, in_=b1v[:, :],
                        in_offset=bass.IndirectOffsetOnAxis(ap=bidx1[:, b:b + 1], axis=0)), [r_widx], [r_b1c[k]], r_b1c[k])

                def transposes(b):
                    k = b % 2
                    for j in range(4):
                        q = cnt["npt"] % 2
                        cnt["npt"] += 1
                        for kc in range(8):
                            P.op("pe", lambda e, q=q, k=k, j=j, kc=kc: e.transpose(
                                out=psT[q][:, kc, :], in_=xg[k][:, j, kc * 128:(kc + 1) * 128], identity=ident_bf[:]),
                                [r_xg[k], r_const], [r_psT[q]])
                        P.op("dve", lambda e, q=q, k=k, j=j: e.tensor_copy(out=xT[k][:, :, j * 128:(j + 1) * 128], in_=psT[q][:]),
                             [r_psT[q]], [r_xT[k]])

                def w1_load(b):
                    for kc in range(8):
                        P.dma("pool", lambda e, b=b, kc=kc: e.indirect_dma_start(
                            out=w1s[:, kc, :], out_offset=None, in_=w1L[l][:, :],
                            in_offset=bass.IndirectOffsetOnAxis(ap=widx1[:, b, kc:kc + 1], axis=0),
                            bounds_check=breg(e, NE * D - 1), oob_is_err=False), [r_widx], [r_w1s], r_w1s)

                def w2_load(b):
                    k = b % 2
                    P.dma("pool", lambda e, k=k, b=b: e.indirect_dma_start(
                        out=b2b[k][:, :], out_offset=None, in_=b2_rows[:, :],
                        in_offset=bass.IndirectOffsetOnAxis(ap=bidx2[:, b:b + 1], axis=0)), [r_widx], [r_b2b[k]], r_b2b[k])
                    for h in range(2):
                        P.dma("pool", lambda e, b=b, h=h: e.indirect_dma_start(
                            out=stg2[:, h, :], out_offset=None, in_=w2v[:, :],
                            in_offset=bass.IndirectOffsetOnAxis(ap=widx2[:, b, h:h + 1], axis=0),
                            bounds_check=breg(e, NE * 256 - 1), oob_is_err=False), [r_widx], [r_stg2], r_stg2)
                    for kk in range(8):
                        h, k8 = kk // 4, (kk % 4) * 2
                        eng = "dve"
                        if eng == "dve":
                            P.op("dve", lambda e, h=h, k8=k8: e.tensor_copy(
                                out=w2s[:, 8 * h + k8:8 * h + k8 + 2, :].rearrange("p a d -> p (a d)"), in_=stg2[:, h, k8 * D:(k8 + 2) * D]),
                                [r_stg2], [r_w2s])
                        else:
                            P.op("act", lambda e, h=h, k8=k8: e.copy(
                                out=w2s[:, 8 * h + k8:8 * h + k8 + 2, :].rearrange("p a d -> p (a d)"), in_=stg2[:, h, k8 * D:(k8 + 2) * D]),
                                [r_stg2], [r_w2s])

                tokens(0)
                tokens(1)
                b1_load(0)
                b1_load(1)
                w1_load(0)
                w2_load(0)
                transposes(0)
                for b in range(NBLK):
                    k = b % 2
                    if b + 2 < NBLK:
                        tokens(b + 2)
                    for m in range(16):
                        q = cnt["nph"] % 3
                        cnt["nph"] += 1
                        for kc in range(8):
                            P.op("pe", lambda e, q=q, k=k, m=m, kc=kc: e.matmul(
                                psH[q][:], lhsT=w1s[:, kc, :].rearrange("p (q m) -> p m q", m=16)[:, m, :], rhs=xT[k][:, kc, :],
                                start=(kc == 0), stop=(kc == 7)), [r_w1s, r_xT[k]], [r_psH[q]])
                        P.op("act", lambda e, q=q, k=k, m=m: e.activation(
                            out=hT[:, m, :], in_=psH[q][:], func=AF.Gelu, bias=b1c[k][:, m:m + 1], scale=1.0),
                            [r_psH[q], r_b1c[k]], [r_hT])
                    if b + 1 < NBLK:
                        w1_load(b + 1)
                        if b + 2 < NBLK:
                            b1_load(b + 2)
                        transposes(b + 1)
                    for j in range(4):
                        yk = cnt["ny"] % 2
                        cnt["ny"] += 1
                        for h in range(2):
                            q = cnt["npy"] % 3
                            cnt["npy"] += 1
                            for kc in range(16):
                                P.op("pe", lambda e, q=q, j=j, h=h, kc=kc: e.matmul(
                                    psY[q][:], lhsT=hT[:, kc, j * 128:(j + 1) * 128], rhs=w2s[:, kc, h * 512:(h + 1) * 512],
                                    start=(kc == 0), stop=(kc == 15)), [r_w2s, r_hT], [r_psY[q]])
                            P.op("dve", lambda e, q=q, yk=yk, k=k, h=h: e.tensor_tensor(
                                out=ysb[yk][:, h * 512:(h + 1) * 512], in0=psY[q][:], in1=b2b[k][:, h * 512:(h + 1) * 512], op=ALU.add),
                                [r_psY[q], r_b2b[k]], [r_ysb[yk]])
                        P.dma("sp", lambda e, yk=yk, b=b, j=j: e.dma_start(out=yview[b, j], in_=ysb[yk][:]), [r_ysb[yk]], [], r_ysb[yk])
                    if b + 1 < NBLK:
                        w2_load(b + 1)
                P.barrier()
                P.release([r_w1s, r_w2s, r_stg2] + r_tix + r_xg + r_b1c + r_b2b + r_ysb)
            with ExitStack() as _st:
                _pass(_st)

            def _pass(st, l=(l if "l" in dir() else 0), last=(last if "last" in dir() else False)):
                g2 = sbt(st, "C_g", [128, D], F32)
                b2t = sbt(st, "C_b", [128, D], F32)
                r_cC = P.res("C_c")
                P.dma("sp", lambda e: e.dma_start(out=g2[:], in_=ln2_g[l][None, :].to_broadcast([128, D])), [], [r_cC], r_cC)
                P.dma("sp", lambda e: e.dma_start(out=b2t[:], in_=ln2_b[l][None, :].to_broadcast([128, D])), [], [r_cC], r_cC)
                NB = 6
                x1t = [sbt(st, f"C_x{i}", [128, D], F32) for i in range(NB)]
                r_x1t = [P.res(f"C_x{i}") for i in range(NB)]
                y0t = [sbt(st, f"C_y0{i}", [128, D], F32) for i in range(NB)]
                r_y0t = [P.res(f"C_y0{i}") for i in range(NB)]
                y1t = [sbt(st, f"C_y1{i}", [128, D], F32) for i in range(NB)]
                r_y1t = [P.res(f"C_y1{i}") for i in range(NB)]
                lsc = [ln_scratch(st, f"C_l{i}") for i in range(NB)]
                r_lsc = [P.res(f"C_ls{i}") for i in range(NB)]
                dst = y_out if last else xa
                for i0 in range(0, NT, 3):
                    grp = list(range(i0, min(i0 + 3, NT)))
                    for i in grp:
                        k = i % NB
                        P.dma("sp", lambda e, k=k, i=i: e.dma_start(out=x1t[k][:], in_=x1b[i * 128:(i + 1) * 128, :]), [], [r_x1t[k]], r_x1t[k])
                        P.dma("pool", lambda e, k=k, i=i: e.indirect_dma_start(
                            out=y0t[k][:, :], out_offset=None, in_=ybuf[:, :],
                            in_offset=bass.IndirectOffsetOnAxis(ap=slot0[:, i:i + 1], axis=0)), [r_slots], [r_y0t[k]], r_y0t[k])
                        P.dma("pool", lambda e, k=k, i=i: e.indirect_dma_start(
                            out=y1t[k][:, :], out_offset=None, in_=ybuf[:, :],
                            in_offset=bass.IndirectOffsetOnAxis(ap=slot1[:, i:i + 1], axis=0)), [r_slots], [r_y1t[k]], r_y1t[k])
                    for i in grp:
                        k = i % NB
                        P.op("act", lambda e, k=k: e.mul(out=x1t[k][:], in_=x1t[k][:], mul=ALPHA), [r_x1t[k]], [r_x1t[k]])
                    for i in grp:
                        k = i % NB
                        P.op("dve", lambda e, k=k, i=i: e.scalar_tensor_tensor(
                            out=x1t[k][:], in0=y0t[k][:], scalar=gates[:, i, 0:1], in1=x1t[k][:], op0=ALU.mult, op1=ALU.add),
                            [r_y0t[k], r_x1t[k], r_gates], [r_x1t[k]])
                    for i in grp:
                        k = i % NB
                        P.op("dve", lambda e, k=k, i=i: e.scalar_tensor_tensor(
                            out=x1t[k][:], in0=y1t[k][:], scalar=gates[:, i, 1:2], in1=x1t[k][:], op0=ALU.mult, op1=ALU.add),
                            [r_y1t[k], r_x1t[k], r_gates], [r_x1t[k]])
                    emit_ln_multi([(x1t[i % NB][:], r_x1t[i % NB], lsc[i % NB], r_lsc[i % NB]) for i in grp], g2[:], b2t[:], cread=[r_cC])
                    for i in grp:
                        k = i % NB
                        P.dma("sp", lambda e, k=k, i=i: e.dma_start(out=dst[i * 128:(i + 1) * 128, :], in_=x1t[k][:]), [r_x1t[k]], [], r_x1t[k])
                P.barrier()
                P.release([r_cC] + r_x1t + r_y0t + r_y1t)
            with ExitStack() as _st:
                _pass(_st)
        cnt = P.emit()
    return nc, cnt


def _constants(T, NU):
    NT = T // 128
    NBLK = -(-(2 * T + NE * (BLK - 1)) // BLK)
    c = {}
    c["c_ident_bf"] = np.eye(128, dtype=np.float32).astype(ml_dtypes.bfloat16)
    c["c_ident_f"] = np.eye(128, dtype=np.float32)
    c["c_tri"] = np.triu(np.ones((128, 128), np.float32), 1).astype(ml_dtypes.bfloat16)
    c["c_ones"] = np.ones((128, 128), np.float32).astype(ml_dtypes.bfloat16)
    qc = np.arange(64)
    cs = np.clip(qc - 8, 0, 48)
    kc = np.arange(64)
    inw = (kc[None, :] >= cs[:, None]) & (kc[None, :] < cs[:, None] + 16)
    m = np.where(inw, 0.0, NEG).astype(np.float32)
    c["c_mask"] = np.concatenate([m, m], axis=0)
    invc = np.zeros((128, 4, 16), np.float32)
    L = 2048
    for g, w in enumerate((2, 4, 8, 16)):
        for i in range(8):
            lo, hi = max(i - w // 2, 0), min(i + w // 2, L)
            invc[:, g, i] = 1.0 / (hi - lo)
            p = L - 8 + i
            lo, hi = max(p - w // 2, 0), min(p + w // 2, L)
            invc[:, g, 8 + i] = 1.0 / (hi - lo)
    c["c_invc"] = invc
    c["c_tokid"] = (np.arange(NT)[None, :] * 128 + np.arange(128)[:, None]).astype(np.int32)
    c["c_rowc"] = (np.arange(16)[None, :] * 128 + np.arange(128)[:, None]).astype(np.float32)
    c["c_bstart"] = np.tile((np.arange(NBLK) * BLK).astype(np.float32)[None, :], (128, 1))
    return c


def _unit_tables(units, T):
    NU = len(units)
    kv = np.zeros((128, NU * 16), np.int32)
    oi = np.zeros((128, NU * 16), np.int32)
    for u, (t0, vlo, vhi) in enumerate(units):
        loc = np.arange(2048)
        tok = t0 + loc
        row = loc // 64
        valid = (row >= vlo) & (row < vhi)
        out = np.where(valid, tok, T + loc)
        kv[:, u * 16:(u + 1) * 16] = tok.reshape(16, 128).T
        oi[:, u * 16:(u + 1) * 16] = out.reshape(16, 128).T
    return kv, oi


_CACHE = {}


def kernel(x_prompt, x_sample, ln_in_g, ln_in_b, w_in, b_in, rpb, w_pool, b_pool, pool_scale,
           w_oa, w_op, w_out, b_out, ln1_g, ln1_b, w_router, b_router, w1, b1, w2, b2, ln2_g, ln2_b):
    T, NU = 12288, 7
    f = lambda a: np.ascontiguousarray(np.asarray(a, dtype=np.float32))
    x_prompt, x_sample = f(x_prompt), f(x_sample)
    shared = dict(ln_in_g=f(ln_in_g), ln_in_b=f(ln_in_b), w_in=f(w_in), b_in=f(b_in), w_pool=f(w_pool),
                  b_pool=f(b_pool), pool_scale=f(pool_scale), w_oa=f(w_oa), w_op=f(w_op), w_out=f(w_out), b_out=f(b_out),
                  ln1_g=f(ln1_g), ln1_b=f(ln1_b), w_router=f(w_router), b_router=f(b_router), b1=f(b1),
                  b2=f(b2), ln2_g=f(ln2_g), ln2_b=f(ln2_b))
    w1f, w2f = f(w1), f(w2)
    for i in range(DEPTH):
        shared[f"w1_{i}"] = w1f[i].reshape(NE * D, DFF)
        shared[f"w2_{i}"] = w2f[i].reshape(NE * DFF, D)
    rp = f(rpb)
    qc = np.arange(64)[:, None]
    kcc = np.arange(64)[None, :]
    ti = np.clip(kcc - qc + 15, 0, 30)
    g = rp[:, :, :, ti]
    g = g.reshape(DEPTH, 4, 2, 15, 64, 64).transpose(0, 2, 4, 1, 3, 5)
    shared["rpbT"] = np.ascontiguousarray(g.reshape(DEPTH, 128, 4 * 15 * 64))
    shared.update(_constants(T, NU))
    in_maps = []
    for c in range(8):
        if c < 4:
            xc = np.concatenate([x_prompt[c], x_sample[2 * c], x_sample[2 * c + 1]], axis=0)
            units = [(0, 0, 28), (1536, 4, 28), (3072, 4, 28), (4608, 4, 28), (6144, 4, 32),
                     (8192, 0, 32), (10240, 0, 32)]
        else:
            s0 = 8 + 6 * (c - 4)
            xc = np.concatenate([x_sample[s0 + j] for j in range(6)], axis=0)
            units = [(2048 * j, 0, 32) for j in range(6)] + [(0, 0, 0)]
        kv, oi = _unit_tables(units, T)
        m = dict(shared)
        m["x_in"] = np.ascontiguousarray(xc)
        m["kvidx"] = kv
        m["outidx"] = oi
        in_maps.append(m)
    if "nc" not in _CACHE:
        _CACHE["nc"] = build_program(T, NU)[0]
    nc = _CACHE["nc"]
    res = run_bass_kernel_spmd(nc, in_maps, core_ids=list(range(8)))
    outs = [np.asarray(r["y_out"], dtype=np.float32) for r in res.results]
    y_prompt = np.stack([outs[c][0:8192] for c in range(4)], axis=0)
    y_sample = np.zeros((32, 2048, D), np.float32)
    for c in range(4):
        y_sample[2 * c] = outs[c][8192:10240]
        y_sample[2 * c + 1] = outs[c][10240:12288]
    for c in range(4, 8):
        s0 = 8 + 6 * (c - 4)
        for j in range(6):
            y_sample[s0 + j] = outs[c][2048 * j:2048 * (j + 1)]
    return (y_prompt, y_sample)
```
